# Optimizing a Trainium2 kernel written in Bass

```python
import jax
import jax.numpy as jnp
from jax import lax
import numpy as np

D_MODEL = 1024
BATCH = 4
SEQ = 8192
DEPTH = 2

CHUNK = 64
Q_BLOCK = 128
EPS = 1e-6
ROPE_THETA = 10000.0

MLA_HEADS = 8
MLA_Q_RANK = 384
MLA_KV_RANK = 256
MLA_NOPE = 64
MLA_ROPE = 32
MLA_V = 64

CONV_WIDTH = 512
CONV_K = 3

RET_HEADS = 4
RET_DK = 128
RET_DV = 256

N_GROUPS = 4
EXPERTS_PER_GROUP = 8
N_EXPERTS = N_GROUPS * EXPERTS_PER_GROUP
EXPERT_FF = 512
TOP_K = 2

N_BRANCHES = 3
IN_SIZES = (MLA_Q_RANK, MLA_KV_RANK, MLA_ROPE, CONV_WIDTH, CONV_WIDTH, CONV_WIDTH, RET_HEADS * RET_DK, RET_HEADS * RET_DK, RET_HEADS * RET_DV, RET_HEADS * RET_DV, N_BRANCHES * D_MODEL)
IN_TOTAL = sum(IN_SIZES)

kernel_name = 'hybrid_mla_shortconv_retention_hmoe'


def _rmsnorm(x, g):
    xf = x.astype(jnp.float32)
    y = xf * lax.rsqrt(jnp.mean(xf * xf, axis=-1, keepdims=True) + EPS)
    return (y * g.astype(jnp.float32)).astype(x.dtype)


def _rope_tables(positions, dim):
    inv = ROPE_THETA ** (-jnp.arange(0, dim, 2, dtype=jnp.float32) / dim)
    ang = positions.astype(jnp.float32)[..., None] * inv
    return jnp.cos(ang)[:, :, None, :], jnp.sin(ang)[:, :, None, :]


def _apply_rope(x, cos, sin):
    xf = x.astype(jnp.float32)
    x1, x2 = jnp.split(xf, 2, axis=-1)
    return jnp.concatenate([x1 * cos - x2 * sin, x1 * sin + x2 * cos], axis=-1).astype(x.dtype)


def _split_cols(a, sizes):
    outs = []
    start = 0
    for size in sizes:
        outs.append(a[..., start:start + size])
        start += size
    return outs


def _mla_branch(q_lat, kv_lat, k_rope, q_norm_g, kv_norm_g, w_uq, w_ukv, w_o, cos, sin):
    b, s, _ = q_lat.shape
    dq = MLA_NOPE + MLA_ROPE
    q = (_rmsnorm(q_lat, q_norm_g) @ w_uq).reshape(b, s, MLA_HEADS, dq)
    q = jnp.concatenate([q[..., :MLA_NOPE], _apply_rope(q[..., MLA_NOPE:], cos, sin)], axis=-1)
    kv = (_rmsnorm(kv_lat, kv_norm_g) @ w_ukv).reshape(b, s, MLA_HEADS, MLA_NOPE + MLA_V)
    k_nope, v = kv[..., :MLA_NOPE], kv[..., MLA_NOPE:]
    k_pe = _apply_rope(k_rope[:, :, None, :], cos, sin)
    k = jnp.concatenate([k_nope, jnp.broadcast_to(k_pe, (b, s, MLA_HEADS, MLA_ROPE))], axis=-1)
    scale = dq ** -0.5
    n_blocks = s // Q_BLOCK
    q_blocks = q.reshape(b, n_blocks, Q_BLOCK, MLA_HEADS, dq).swapaxes(0, 1)
    key_chunk = jnp.arange(s) // CHUNK

    def attend(args):
        q_blk, blk = args
        q_chunk = (blk * Q_BLOCK + jnp.arange(Q_BLOCK)) // CHUNK
        sc = jnp.einsum('bqhd,bkhd->bhqk', q_blk, k, preferred_element_type=jnp.float32) * scale
        sc = jnp.where(key_chunk[None, :] <= q_chunk[:, None], sc, -jnp.inf)
        p = jax.nn.softmax(sc, axis=-1)
        return jnp.einsum('bhqk,bkhd->bqhd', p.astype(v.dtype), v)

    o = lax.map(attend, (q_blocks, jnp.arange(n_blocks)))
    o = o.swapaxes(0, 1).reshape(b, s, MLA_HEADS * MLA_V)
    return o @ w_o


def _shortconv_branch(b_gate, c_gate, xv, conv_w, w_o):
    u = c_gate * xv
    y = lax.conv_general_dilated(u, conv_w[:, None, :], window_strides=(1,), padding=((CONV_K - 1, 0),), dimension_numbers=('NWC', 'WIO', 'NWC'), feature_group_count=CONV_WIDTH)
    return (b_gate * y) @ w_o


def _retention_branch(q, k, v, g, w_o, cos, sin):
    b, s, _ = q.shape
    n = s // CHUNK
    q = _apply_rope(q.reshape(b, s, RET_HEADS, RET_DK), cos, sin).astype(jnp.float32)
    k = _apply_rope(k.reshape(b, s, RET_HEADS, RET_DK), cos, sin).astype(jnp.float32) * (RET_DK ** -0.5)
    v = v.reshape(b, s, RET_HEADS, RET_DV).astype(jnp.float32)
    log_gamma = jnp.log(1.0 - 2.0 ** (-5.0 - jnp.arange(RET_HEADS, dtype=jnp.float32)))
    pos = jnp.arange(CHUNK, dtype=jnp.float32)
    intra_decay = jnp.exp(log_gamma[:, None, None] * jnp.abs(pos[:, None] - pos[None, :]))[None]
    q_decay = jnp.exp(log_gamma[:, None] * (pos + 1.0))[None, :, :, None]
    k_decay = jnp.exp(log_gamma[:, None] * (CHUNK - 1.0 - pos))[None, :, :, None]
    chunk_decay = jnp.exp(log_gamma * CHUNK)[None, :, None, None]

    def to_chunks(a):
        return a.reshape(b, n, CHUNK, RET_HEADS, a.shape[-1]).transpose(1, 0, 3, 2, 4)

    def step(state, inp):
        qc, kc, vc = inp
        sc = jnp.einsum('bhid,bhjd->bhij', qc, kc) * intra_decay
        o = jnp.einsum('bhij,bhjv->bhiv', sc, vc) + jnp.einsum('bhid,bhdv->bhiv', qc, state) * q_decay
        state = state * chunk_decay + jnp.einsum('bhjd,bhjv->bhdv', kc * k_decay, vc)
        return state, o

    state0 = jnp.zeros((b, RET_HEADS, RET_DK, RET_DV), jnp.float32)
    _, o = lax.scan(step, state0, (to_chunks(q), to_chunks(k), to_chunks(v)))
    o = o.transpose(1, 0, 3, 2, 4).reshape(b, s, RET_HEADS, RET_DV)
    o = o * lax.rsqrt(jnp.mean(o * o, axis=-1, keepdims=True) + EPS)
    o = o.reshape(b, s, RET_HEADS * RET_DV).astype(g.dtype)
    return (jax.nn.silu(g) * o) @ w_o


def _hier_moe(h, w_rg, b_rg, w_re, b_re, w_gate, w_up, w_down):
    b, s, d = h.shape
    t = h.reshape(b * s, d)
    n_tok = t.shape[0]
    g_prob = jax.nn.softmax((t @ w_rg).astype(jnp.float32) + b_rg.astype(jnp.float32), axis=-1)
    g_val, g_idx = lax.top_k(g_prob, 1)
    e_logits = ((t @ w_re).astype(jnp.float32) + b_re.astype(jnp.float32)).reshape(n_tok, N_GROUPS, EXPERTS_PER_GROUP)
    e_logits = jnp.take_along_axis(e_logits, g_idx[:, :, None], axis=1)[:, 0]
    e_val, e_idx = lax.top_k(jax.nn.softmax(e_logits, axis=-1), TOP_K)
    e_val = e_val / jnp.sum(e_val, axis=-1, keepdims=True)
    weights = g_val * e_val
    expert_id = g_idx * EXPERTS_PER_GROUP + e_idx
    combine = jnp.einsum('tk,tke->et', weights, jax.nn.one_hot(expert_id, N_EXPERTS, dtype=jnp.float32))

    def expert(acc, inp):
        wg, wu, wd, gate = inp
        hid = jax.nn.silu(t @ wg) * (t @ wu)
        return acc + gate[:, None] * (hid @ wd).astype(jnp.float32), None

    out, _ = lax.scan(expert, jnp.zeros((n_tok, d), jnp.float32), (w_gate, w_up, w_down, combine))
    return out.reshape(b, s, d).astype(h.dtype)


def _normal(key, shape, scale):
    return jax.random.normal(key, shape, jnp.float32) * scale


def setup_inputs(seed: int = 0) -> dict:
    key = jax.random.key(seed)
    ks = jax.random.split(key, 32)
    D = D_MODEL
    x = _normal(ks[0], (BATCH, SEQ, D), 1.0)
    c = _normal(ks[1], (BATCH, D), 1.0)
    positions = jax.random.randint(ks[2], (BATCH, 1), 0, 4096, dtype=jnp.int32) + jnp.arange(SEQ, dtype=jnp.int32)[None, :]
    return {
        'x': x,
        'c': c,
        'positions': positions,
        'w_ada': _normal(ks[3], (DEPTH, D, 6 * D), 0.5 * D ** -0.5),
        'b_ada': _normal(ks[4], (DEPTH, 6 * D), 0.02),
        'norm_mix_g': 1.0 + _normal(ks[5], (DEPTH, D), 0.05),
        'norm_ffn_g': 1.0 + _normal(ks[6], (DEPTH, D), 0.05),
        'w_in': _normal(ks[7], (DEPTH, D, IN_TOTAL), D ** -0.5),
        'mla_q_norm_g': 1.0 + _normal(ks[8], (DEPTH, MLA_Q_RANK), 0.05),
        'mla_kv_norm_g': 1.0 + _normal(ks[9], (DEPTH, MLA_KV_RANK), 0.05),
        'w_uq': _normal(ks[10], (DEPTH, MLA_Q_RANK, MLA_HEADS * (MLA_NOPE + MLA_ROPE)), MLA_Q_RANK ** -0.5),
        'w_ukv': _normal(ks[11], (DEPTH, MLA_KV_RANK, MLA_HEADS * (MLA_NOPE + MLA_V)), MLA_KV_RANK ** -0.5),
        'w_o_mla': _normal(ks[12], (DEPTH, MLA_HEADS * MLA_V, D), (MLA_HEADS * MLA_V) ** -0.5),
        'conv_w': _normal(ks[13], (DEPTH, CONV_K, CONV_WIDTH), CONV_K ** -0.5),
        'w_o_conv': _normal(ks[14], (DEPTH, CONV_WIDTH, D), CONV_WIDTH ** -0.5),
        'w_o_ret': _normal(ks[15], (DEPTH, RET_HEADS * RET_DV, D), (RET_HEADS * RET_DV) ** -0.5),
        'w_mix_out': _normal(ks[16], (DEPTH, D, D), D ** -0.5),
        'w_route_group': _normal(ks[17], (DEPTH, D, N_GROUPS), D ** -0.5),
        'b_route_group': _normal(ks[18], (DEPTH, N_GROUPS), 0.01),
        'w_route_expert': _normal(ks[19], (DEPTH, D, N_EXPERTS), D ** -0.5),
        'b_route_expert': _normal(ks[20], (DEPTH, N_EXPERTS), 0.01),
        'w_exp_gate': _normal(ks[21], (DEPTH, N_EXPERTS, D, EXPERT_FF), D ** -0.5),
        'w_exp_up': _normal(ks[22], (DEPTH, N_EXPERTS, D, EXPERT_FF), D ** -0.5),
        'w_exp_down': _normal(ks[23], (DEPTH, N_EXPERTS, EXPERT_FF, D), EXPERT_FF ** -0.5),
        'final_g': 1.0 + _normal(ks[24], (D,), 0.05),
    }


def reference(x, c, positions, w_ada, b_ada, norm_mix_g, norm_ffn_g, w_in, mla_q_norm_g, mla_kv_norm_g, w_uq, w_ukv, w_o_mla, conv_w, w_o_conv, w_o_ret, w_mix_out, w_route_group, b_route_group, w_route_expert, b_route_expert, w_exp_gate, w_exp_up, w_exp_down, final_g):
    b, s, d = x.shape
    mla_cos, mla_sin = _rope_tables(positions, MLA_ROPE)
    ret_cos, ret_sin = _rope_tables(positions, RET_DK)
    c_act = jax.nn.silu(c)
    for l in range(DEPTH):
        mod = c_act @ w_ada[l] + b_ada[l]
        sh_mix, sc_mix, gt_mix, sh_ffn, sc_ffn, gt_ffn = [m[:, None, :] for m in jnp.split(mod, 6, axis=-1)]
        h = _rmsnorm(x, norm_mix_g[l]) * (1.0 + sc_mix) + sh_mix
        q_lat, kv_lat, k_rope, cb, cc, cx, rq, rk, rv, rg, gl = _split_cols(h @ w_in[l], IN_SIZES)
        y_mla = _mla_branch(q_lat, kv_lat, k_rope, mla_q_norm_g[l], mla_kv_norm_g[l], w_uq[l], w_ukv[l], w_o_mla[l], mla_cos, mla_sin)
        y_conv = _shortconv_branch(cb, cc, cx, conv_w[l], w_o_conv[l])
        y_ret = _retention_branch(rq, rk, rv, rg, w_o_ret[l], ret_cos, ret_sin)
        gates = jax.nn.sigmoid(gl).reshape(b, s, N_BRANCHES, d)
        merged = gates[:, :, 0] * y_mla + gates[:, :, 1] * y_conv + gates[:, :, 2] * y_ret
        x = x + gt_mix * (merged @ w_mix_out[l])
        h = _rmsnorm(x, norm_ffn_g[l]) * (1.0 + sc_ffn) + sh_ffn
        x = x + gt_ffn * _hier_moe(h, w_route_group[l], b_route_group[l], w_route_expert[l], b_route_expert[l], w_exp_gate[l], w_exp_up[l], w_exp_down[l])
    return _rmsnorm(x, final_g)
```

```python
import math
from contextlib import ExitStack, contextmanager
import numpy as np
import ml_dtypes
import concourse.bass as bass
import concourse.mybir as mybir
from concourse.bass_utils import run_bass_kernel_spmd

F32 = mybir.dt.float32; BF16 = mybir.dt.bfloat16; I32 = mybir.dt.int32
ALU = mybir.AluOpType; AF = mybir.ActivationFunctionType

D = 1024; S = 8192; TT = 512; NT = S // TT
EPS = 1e-6
TWO_PI = 2.0 * math.pi
CW1 = 6.28125
CW2 = TWO_PI - CW1
PI_LO = 3.1415925
MLA_SCALE = 96.0 ** -0.5
RET_KS = 128.0 ** -0.5


class Buf:
    def __init__(self, name, ap=None, dram=False):
        self.name = name; self.ap = ap; self.dram = dram
        self.writers = {}; self.readers = {}
        self.dsem = None; self.dval = 0; self.dkey = None; self.psum = False

    def __getitem__(self, idx):
        return self.ap[idx]


class Rot:
    def __init__(self, bufs):
        self.bufs = bufs; self.i = 0

    def next(self):
        b = self.bufs[self.i % len(self.bufs)]; self.i += 1
        return b


class K:
    def __init__(self, nc, root):
        self.nc = nc; self.root = root; self.stacks = [root]
        self.eng = {'pe': nc.tensor, 'dve': nc.vector, 'act': nc.scalar, 'pool': nc.gpsimd, 'sp': nc.sync}
        self.sem = {}; self.cnt = {}
        for e in self.eng:
            self.sem[e] = root.enter_context(nc.semaphore('s_' + e)); self.cnt[e] = 0
        self.waited = {e: {} for e in self.eng}
        self.dma_latest = {}
        self.uid = 0
        self.free_dsems = []
        self.scope_bufs = [[]]

    def sb(self, name, shape, dt):
        self.uid += 1
        t = self.stacks[-1].enter_context(self.nc.sbuf_tensor('%s_%d' % (name, self.uid), shape, dt))
        b = Buf(name, t)
        self.scope_bufs[-1].append(b)
        return b

    def ps(self, name, shape, dt=F32):
        t = self.root.enter_context(self.nc.psum_tensor(name, shape, dt))
        b = Buf(name, t); b.psum = True
        return b

    def dram(self, name, shape, dt, kind="Internal"):
        t = self.nc.dram_tensor(name, shape, dt, kind=kind).ap()
        return Buf(name, t, dram=True)

    @contextmanager
    def scope(self):
        st = ExitStack()
        self.stacks.append(st)
        self.scope_bufs.append([])
        try:
            yield
        finally:
            self.barrier()
            for b in self.scope_bufs.pop():
                if b.dsem is not None:
                    self.free_dsems.append((b.dsem, b.dval, b.dkey))
                    b.dsem = None
            self.stacks.pop()
            st.close()

    def _wait(self, e, sem, val, key):
        if self.waited[e].get(key, 0) >= val:
            return
        self.waited[e][key] = val
        self.eng[e].wait_ge(sem, val)

    def _deps(self, e, reads, writes, partial):
        for b in reads:
            for key, (sem, val) in b.writers.items():
                if e == 'pe' and key == 'pe':
                    continue
                self._wait(e, sem, val, key)
            if b.psum:
                for key, (sem, val) in b.readers.items():
                    if key != e:
                        self._wait(e, sem, val, key)
        for b in writes:
            if not partial:
                for key, (sem, val) in b.writers.items():
                    if e == 'pe' and key == 'pe':
                        continue
                    self._wait(e, sem, val, key)
            for key, (sem, val) in b.readers.items():
                if key == e:
                    continue
                self._wait(e, sem, val, key)

    def _mark(self, sem, val, key, reads, writes, partial):
        for b in reads:
            b.readers[key] = (sem, val)
        for b in writes:
            if partial:
                b.writers[key] = (sem, val)
            else:
                b.writers = {key: (sem, val)}

    def op(self, e, fn, reads=(), writes=(), partial=False):
        self._deps(e, reads, writes, partial)
        ins = fn(self.eng[e])
        self.cnt[e] += 1
        ins.then_inc(self.sem[e], 1)
        self._mark(self.sem[e], self.cnt[e], e, reads, writes, partial)
        return ins

    def dma(self, q, out_ap, in_ap, reads=(), writes=(), owner=None, partial=False, **kw):
        if owner is None:
            owner = [b for b in list(writes) + list(reads) if not b.dram][0]
        if owner.dsem is None:
            if self.free_dsems:
                owner.dsem, owner.dval, owner.dkey = self.free_dsems.pop()
            else:
                self.uid += 1
                owner.dkey = 'dsem%d' % self.uid
                owner.dsem = self.root.enter_context(self.nc.semaphore(owner.dkey))
        self._deps(q, reads, writes, partial)
        if owner.dval > 0:
            self._wait(q, owner.dsem, owner.dval, owner.dkey)
        ins = self.eng[q].dma_start(out=out_ap, in_=in_ap, **kw)
        owner.dval += 16
        ins.then_inc(owner.dsem, 16)
        self._mark(owner.dsem, owner.dval, owner.dkey, reads, writes, partial)
        self.dma_latest[owner.dkey] = (owner.dsem, owner.dval)
        return ins

    def idma(self, out_ap, in_ap, idx_ap, scatter, reads, writes, owner, partial=True):
        q = 'pool'
        if owner.dsem is None:
            if self.free_dsems:
                owner.dsem, owner.dval, owner.dkey = self.free_dsems.pop()
            else:
                self.uid += 1
                owner.dkey = 'dsem%d' % self.uid
                owner.dsem = self.root.enter_context(self.nc.semaphore(owner.dkey))
        self._deps(q, reads, writes, partial)
        if owner.dval > 0:
            self._wait(q, owner.dsem, owner.dval, owner.dkey)
        off = bass.IndirectOffsetOnAxis(ap=idx_ap, axis=0)
        if scatter:
            ins = self.nc.gpsimd.indirect_dma_start(out=out_ap, out_offset=off, in_=in_ap, in_offset=None)
        else:
            ins = self.nc.gpsimd.indirect_dma_start(out=out_ap, out_offset=None, in_=in_ap, in_offset=off)
        owner.dval += 16
        ins.then_inc(owner.dsem, 16)
        self._mark(owner.dsem, owner.dval, owner.dkey, reads, writes, partial)
        self.dma_latest[owner.dkey] = (owner.dsem, owner.dval)
        return ins

    def barrier(self):
        for e in self.eng:
            for e2 in self.eng:
                if e2 != e and self.cnt[e2] > 0:
                    self._wait(e, self.sem[e2], self.cnt[e2], e2)
            for key, (sem, val) in self.dma_latest.items():
                self._wait(e, sem, val, key)

    def mm(self, out, lhsT, rhs, start, stop, reads, writes):
        return self.op('pe', lambda e: e.matmul(out, lhsT, rhs, start=start, stop=stop),
                       reads=reads, writes=writes, partial=not start)

    def tr(self, out, in_, ident, reads, writes, partial=True):
        return self.op('pe', lambda e: e.transpose(out, in_, ident), reads=reads, writes=writes, partial=partial)

    def act(self, out, in_, func, reads, writes, bias=None, scale=None, accum_out=None, partial=False, eng='act'):
        kw = {}
        if bias is not None: kw['bias'] = bias
        if scale is not None: kw['scale'] = scale
        if accum_out is not None: kw['accum_out'] = accum_out
        return self.op(eng, lambda e: e.activation(out=out, in_=in_, func=func, **kw),
                       reads=reads, writes=writes, partial=partial)

    def tt(self, out, in0, in1, op, reads, writes, partial=False, eng='dve'):
        return self.op(eng, lambda e: e.tensor_tensor(out=out, in0=in0, in1=in1, op=op),
                       reads=reads, writes=writes, partial=partial)

    def ts(self, out, in0, s1, op0, reads, writes, s2=None, op1=None, partial=False, eng='dve'):
        if op1 is None:
            return self.op(eng, lambda e: e.tensor_scalar(out=out, in0=in0, scalar1=s1, scalar2=None, op0=op0),
                           reads=reads, writes=writes, partial=partial)
        return self.op(eng, lambda e: e.tensor_scalar(out=out, in0=in0, scalar1=s1, scalar2=s2, op0=op0, op1=op1),
                       reads=reads, writes=writes, partial=partial)

    def stt(self, out, in0, scalar, in1, op0, op1, reads, writes, partial=False):
        return self.op('dve', lambda e: e.scalar_tensor_tensor(out=out, in0=in0, scalar=scalar, in1=in1, op0=op0, op1=op1),
                       reads=reads, writes=writes, partial=partial)

    def copy(self, out, in_, reads, writes, partial=False, eng='dve'):
        return self.op(eng, lambda e: e.tensor_copy(out=out, in_=in_), reads=reads, writes=writes, partial=partial)

    def recip(self, out, in_, reads, writes, partial=False):
        return self.op('dve', lambda e: e.reciprocal(out=out, in_=in_), reads=reads, writes=writes, partial=partial)

    def memset(self, ap, val, writes, eng='dve', partial=False, reads=()):
        return self.op(eng, lambda e: e.memset(ap, val), reads=reads, writes=writes, partial=partial)


def load_cast(k, dst, dst_ap_fn, src_ap_fn, ncols, step=2048):
    c = 0
    while c < ncols:
        n = min(step, ncols - c)
        k.dma('pool', dst_ap_fn(c, n), src_ap_fn(c, n), writes=[dst], partial=True)
        c += n


class Ctx:
    pass


def setup_common(k, A):
    X = Ctx()
    X.banks = [k.ps('bank%d' % i, [128, 512], F32) for i in range(8)]
    for b in X.banks:
        b.bf = b.ap.bitcast(BF16)
    X.ones_bf = k.sb('ones_bf', [128, 128], BF16)
    k.memset(X.ones_bf[:], 1.0, [X.ones_bf])
    X.id_f = k.sb('id_f', [128, 128], F32)
    k.dma('sp', X.id_f[:], A['ident'][:, :], writes=[X.id_f])
    X.id_bf = k.sb('id_bf', [128, 128], BF16)
    k.copy(X.id_bf[:], X.id_f[:], [X.id_f], [X.id_bf])
    return X


def rope_tables(k, W, pos_ap, invcol, cosb, sinb):
    k.dma('sp', W.posi[:], pos_ap.to_broadcast([128, TT]), writes=[W.posi])
    k.copy(W.ang[:], W.posi[:], [W.posi], [W.ang])
    k.ts(W.ang[:], W.ang[:], invcol, ALU.mult, [W.ang], [W.ang])
    k.ts(W.kq[:], W.ang[:], 1.0 / TWO_PI, ALU.mult, [W.ang], [W.kq])
    k.copy(W.kf[:], W.kq[:], [W.kq], [W.kf])
    k.stt(W.r1[:], W.kf[:], -CW1, W.ang[:], ALU.mult, ALU.add, [W.kf, W.ang], [W.r1])
    k.stt(W.r1[:], W.kf[:], -CW2, W.r1[:], ALU.mult, ALU.add, [W.kf, W.r1], [W.r1])
    k.ts(W.r1[:], W.r1[:], PI_LO, ALU.min, [W.r1], [W.r1], s2=-PI_LO, op1=ALU.max)
    k.act(sinb[:], W.r1[:], AF.Sin, [W.r1], [sinb])
    k.stt(W.kf[:], W.r1[:], -1.0, W.r1[:], ALU.mult, ALU.max, [W.r1], [W.kf])
    k.act(cosb[:], W.kf[:], AF.Sin, [W.kf], [cosb], bias=W.halfpi[:, 0:1], scale=-1.0)


def rope_work(k):
    W = Ctx()
    W.posi = k.sb('posi', [128, TT], I32)
    W.ang = k.sb('ang', [128, TT], F32)
    W.kq = k.sb('kq', [128, TT], I32)
    W.kf = k.sb('kf', [128, TT], F32)
    W.r1 = k.sb('r1', [128, TT], F32)
    W.halfpi = k.sb('halfpi', [128, 1], F32)
    k.memset(W.halfpi[:], math.pi / 2.0, [W.halfpi])
    return W


def emit_mod(k, X, A, modT):
    with k.scope():
        cc = k.sb('cc', [128, 8], F32)
        k.dma('sp', cc[:], A['cc8'][:, :], writes=[cc])
        cact = k.sb('cact', [128, 8], F32)
        k.act(cact[:], cc[:], AF.Silu, [cc], [cact])
        bada = k.sb('bada', [128, 48], F32)
        k.dma('sp', bada[:], A['b_ada8'][:, :], writes=[bada])
        wv = A['w_ada'].rearrange("(kc p) n -> p kc n", p=128)
        wb = [k.sb('wada%d' % i, [128, 8, 768], F32) for i in range(2)]
        pm = X.banks[0]
        for blk in range(8):
            w = wb[blk % 2]
            for kc in range(8):
                k.dma('sp', w[:, kc, :], wv[:, kc, blk * 768:(blk + 1) * 768], writes=[w], partial=(kc > 0))
            for j in range(6):
                jj = blk * 6 + j
                for kc in range(8):
                    k.mm(pm[:, jj:jj + 1], w[:, kc, j * 128:(j + 1) * 128], cact[:, kc:kc + 1],
                         kc == 0, kc == 7, [w, cact], [pm])
        k.tt(modT[:], pm[:, 0:48], bada[:], ALU.add, [pm, bada], [modT])


def emit_mixer(k, X, A, modT, V_sb, write_ht, phases=('A', 'B', 'S2')):
    if True:
        banks = X.banks
        rot = Rot(banks)
        gmix = k.sb('gmix', [128, 8], F32)
        k.dma('sp', gmix[:], A['gmix8'], writes=[gmix])
        gsc = k.sb('gsc', [128, 8], F32)
        k.stt(gsc[:], modT[:, 8:16], 1.0, gmix[:], ALU.add, ALU.mult, [modT, gmix], [gsc])
        invf = k.sb('invf', [128, 2], F32)
        k.dma('sp', invf[:], A['invf'], writes=[invf])
        prot_f = k.sb('prot_f', [128, 2, 128], F32)
        k.dma('sp', prot_f[:], A['prot'], writes=[prot_f])
        prot = k.sb('prot', [128, 2, 128], BF16)
        k.copy(prot[:], prot_f[:], [prot_f], [prot])
        HT = A['HT_buf']; ZT = A['ZT_buf']; QTd = A['QTd_buf']; KTd = A['KTd_buf']
        HTv = A['HT'].rearrange("(kc p) c -> p kc c", p=128)
        xTv = A['xT'].rearrange("(kc p) c -> p kc c", p=128)

        if 'A' in phases:
          with k.scope():
            NA = 1504
            w_a = k.sb('w_a', [128, 8, NA], BF16)
            wav = A['w_a'].rearrange("(kc p) n -> p kc n", p=128)
            for kc in range(8):
                load_cast(k, w_a, lambda c, n: w_a[:, kc, c:c + n], lambda c, n: wav[:, kc, c:c + n], NA, step=752)
            st_f = k.sb('st_f', [128, 3, 384], F32)
            qg = k.sb('qg', [128, 3], F32); kvg = k.sb('kvg', [128, 2], F32)
            k.dma('sp', qg[:], A['qg3'], writes=[qg]); k.dma('sp', kvg[:], A['kvg2'], writes=[kvg])
            wuq = k.sb('wuq', [128, 3, 384], BF16)
            k.dma('sp', st_f[:], A['wuq'].rearrange("(rc p) n -> p rc n", p=128), writes=[st_f])
            for rc in range(3):
                k.ts(wuq[:, rc, :], st_f[:, rc, :], qg[:, rc:rc + 1], ALU.mult, [st_f, qg], [wuq], partial=True)
            wuk = k.sb('wuk', [128, 2, 256], BF16); wuv = k.sb('wuv', [128, 2, 256], BF16)
            st2 = k.sb('st2', [128, 2, 256], F32); st3 = k.sb('st3', [128, 2, 256], F32)
            k.dma('sp', st2[:], A['wuk'].rearrange("(rc p) n -> p rc n", p=128), writes=[st2])
            k.dma('sp', st3[:], A['wuv'].rearrange("(rc p) n -> p rc n", p=128), writes=[st3])
            for rc in range(2):
                k.ts(wuk[:, rc, :], st2[:, rc, :], kvg[:, rc:rc + 1], ALU.mult, [st2, kvg], [wuk], partial=True)
                k.ts(wuv[:, rc, :], st3[:, rc, :], kvg[:, rc:rc + 1], ALU.mult, [st3, kvg], [wuv], partial=True)
            cw = k.sb('cw', [128, 2, 3], F32)
            k.dma('sp', cw[:], A['convw'], writes=[cw])
            xts = [k.sb('xt%d' % i, [128, 8, TT], F32) for i in range(2)]
            sq = k.sb('sq', [128, 8, TT], BF16)
            hT = k.sb('hT', [128, 8, TT], BF16)
            rtmp = k.sb('rtmp', [128, TT], F32)
            rstd = k.sb('rstd', [128, TT], F32)
            hx = [k.sb('hx%d' % i, [128, TT], F32) for i in range(2)]
            lat_f = k.sb('lat_f', [128, 5, TT], F32)
            sql = k.sb('sql', [128, 5, TT], BF16)
            rq_bc = k.sb('rq_bc', [128, TT], F32); rkv_bc = k.sb('rkv_bc', [128, TT], F32)
            qn = k.sb('qn', [128, 3, TT], BF16); kvn = k.sb('kvn', [128, 2, TT], BF16)
            QTs = [k.sb('QTs%d' % i, [128, TT], BF16) for i in range(4)]
            KTs = [k.sb('KTs%d' % i, [128, TT], BF16) for i in range(4)]
            qr_f = k.sb('qr_f', [128, TT], F32)
            kr_f = k.sb('kr_f', [128, TT], F32); kr_b = k.sb('kr_b', [128, TT], BF16)
            kpe = k.sb('kpe', [128, TT], BF16)
            t1 = k.sb('t1', [128, TT], F32); t2 = k.sb('t2', [128, TT], F32)
            cosm = k.sb('cosm', [128, TT], F32); sinm = k.sb('sinm', [128, TT], F32)
            W = rope_work(k)
            u = [k.sb('u%d' % i, [128, TT + 2], F32) for i in range(2)]
            cxs = k.sb('cxs', [128, TT], F32); yv = k.sb('yv', [128, TT], F32)
            zc = [k.sb('zc%d' % i, [128, TT], BF16) for i in range(2)]
            for ch in range(2):
                k.memset(u[ch][:], 0.0, [u[ch]])

            def load_x(t):
                xt = xts[t % 2]
                for kc in range(8):
                    k.dma('sp', xt[:, kc, :], xTv[:, kc, t * TT:(t + 1) * TT], writes=[xt], partial=(kc > 0))

            load_x(0)
            for t in range(NT):
                cs = slice(t * TT, (t + 1) * TT)
                if t + 1 < NT:
                    load_x(t + 1)
                xt = xts[t % 2]
                for kc in range(8):
                    k.act(sq[:, kc, :], xt[:, kc, :], AF.Square, [xt], [sq], partial=(kc > 0))
                pss = rot.next()
                for kc in range(8):
                    k.mm(pss[:, :], X.ones_bf[:, :], sq[:, kc, :], kc == 0, kc == 7, [X.ones_bf, sq], [pss])
                k.act(rtmp[:], pss[:, :], AF.Sqrt, [pss], [rtmp], bias=EPS, scale=1.0 / D)
                k.recip(rstd[:], rtmp[:], [rtmp], [rstd])
                for kc in range(8):
                    hb = hx[kc % 2]
                    k.stt(hb[:], xt[:, kc, :], gsc[:, kc:kc + 1], rstd[:], ALU.mult, ALU.mult, [xt, gsc, rstd], [hb])
                    k.act(hT[:, kc, :], hb[:], AF.Identity, [hb, modT], [hT], bias=modT[:, kc:kc + 1], partial=(kc > 0))
                if write_ht:
                    k.dma('pool', HTv[:, :, cs], hT[:], reads=[hT], writes=[HT], partial=True)
                rope_tables(k, W, A['pos'][0:1, cs], invf[:, 1:2], cosm, sinm)
                for j in range(5):
                    ps = rot.next()
                    for kc in range(8):
                        k.mm(ps[:, :], w_a[:, kc, j * 128:(j + 1) * 128], hT[:, kc, :], kc == 0, kc == 7, [w_a, hT], [ps])
                    k.act(lat_f[:, j, :], ps[:, :], AF.Copy, [ps], [lat_f], partial=(j > 0))
                    k.act(sql[:, j, :], ps[:, :], AF.Square, [ps], [sql], partial=(j > 0))
                for (j0, j1, n, dst) in ((0, 3, 384.0, rq_bc), (3, 5, 256.0, rkv_bc)):
                    ps = rot.next()
                    for j in range(j0, j1):
                        k.mm(ps[:, :], X.ones_bf[:, :], sql[:, j, :], j == j0, j == j1 - 1, [X.ones_bf, sql], [ps])
                    k.act(rtmp[:], ps[:, :], AF.Sqrt, [ps], [rtmp], bias=EPS, scale=1.0 / n)
                    k.recip(dst[:], rtmp[:], [rtmp], [dst])
                for j in range(3):
                    k.tt(qn[:, j, :], lat_f[:, j, :], rq_bc[:], ALU.mult, [lat_f, rq_bc], [qn], partial=(j > 0))
                for j in range(2):
                    k.tt(kvn[:, j, :], lat_f[:, 3 + j, :], rkv_bc[:], ALU.mult, [lat_f, rkv_bc], [kvn], partial=(j > 0))
                ps = rot.next()
                for kc in range(8):
                    k.mm(ps[0:96, :], w_a[:, kc, 640:736], hT[:, kc, :], kc == 0, kc == 7, [w_a, hT], [ps])
                k.act(kr_f[64:96, :], ps[64:96, :], AF.Copy, [ps], [kr_f])
                k.act(kr_b[0:96, :], ps[0:96, :], AF.Copy, [ps], [kr_b])
                ps2 = rot.next()
                k.mm(ps2[0:96, :], prot[0:96, 1, 0:96], kr_b[0:96, :], True, True, [prot, kr_b], [ps2])
                k.tt(t1[64:96, :], kr_f[64:96, :], cosm[64:96, :], ALU.mult, [kr_f, cosm], [t1])
                k.tt(t2[64:96, :], ps2[64:96, :], sinm[64:96, :], ALU.mult, [ps2, sinm], [t2])
                k.tt(kpe[64:96, :], t1[64:96, :], t2[64:96, :], ALU.add, [t1, t2], [kpe])
                for h in range(4):
                    k.dma('pool', A['KTd'][h, 64:96, cs], kpe[64:96, :], reads=[kpe], writes=[KTd], partial=True)
                for h in range(4):
                    ps = rot.next()
                    for rc in range(3):
                        k.mm(ps[0:96, :], wuq[:, rc, h * 96:(h + 1) * 96], qn[:, rc, :], rc == 0, rc == 2, [wuq, qn], [ps])
                    Q = QTs[h]
                    k.act(Q[0:96, :], ps[0:96, :], AF.Copy, [ps], [Q], scale=MLA_SCALE)
                    k.act(qr_f[64:96, :], ps[64:96, :], AF.Copy, [ps], [qr_f], scale=MLA_SCALE)
                    ps2 = rot.next()
                    k.mm(ps2[0:96, :], prot[0:96, 1, 0:96], Q[0:96, :], True, True, [prot, Q], [ps2])
                    k.tt(t1[64:96, :], qr_f[64:96, :], cosm[64:96, :], ALU.mult, [qr_f, cosm], [t1])
                    k.tt(t2[64:96, :], ps2[64:96, :], sinm[64:96, :], ALU.mult, [ps2, sinm], [t2])
                    k.tt(Q[64:96, :], t1[64:96, :], t2[64:96, :], ALU.add, [t1, t2], [Q])
                    k.dma('pool', A['QTd'][h, :, cs], Q[0:96, :], reads=[Q], writes=[QTd], partial=True)
                for h in range(4):
                    ps = rot.next()
                    for rc in range(2):
                        k.mm(ps[0:64, :], wuk[:, rc, h * 64:(h + 1) * 64], kvn[:, rc, :], rc == 0, rc == 1, [wuk, kvn], [ps])
                    Kt = KTs[h]
                    k.act(Kt[0:64, :], ps[0:64, :], AF.Copy, [ps], [Kt])
                    k.dma('pool', A['KTd'][h, 0:64, cs], Kt[0:64, :], reads=[Kt], writes=[KTd], partial=True)
                for blk in range(4):
                    ps = rot.next()
                    for rc in range(2):
                        k.mm(ps[:, 0:256], kvn[:, rc, blk * 128:(blk + 1) * 128], wuv[:, rc, :], rc == 0, rc == 1, [kvn, wuv], [ps])
                    k.act(V_sb[:, t * 4 + blk, :, 0:64], ps[:, 0:256].rearrange("p (h v) -> p h v", h=4), AF.Copy,
                          [ps], [V_sb], partial=True)
                for ch in range(2):
                    pb = rot.next(); pc = rot.next(); px = rot.next()
                    for (pp, base) in ((pb, 736), (pc, 992), (px, 1248)):
                        for kc in range(8):
                            k.mm(pp[:, :], w_a[:, kc, base + ch * 128: base + (ch + 1) * 128], hT[:, kc, :],
                                 kc == 0, kc == 7, [w_a, hT], [pp])
                    k.act(cxs[:], px[:, :], AF.Copy, [px], [cxs])
                    U = u[ch]
                    if t > 0:
                        k.copy(U[:, 0:2], U[:, TT:TT + 2], [U], [U])
                    k.tt(U[:, 2:TT + 2], pc[:, :], cxs[:], ALU.mult, [pc, cxs, U], [U])
                    k.ts(yv[:], U[:, 2:TT + 2], cw[:, ch, 2:3], ALU.mult, [U, cw], [yv])
                    k.stt(yv[:], U[:, 1:TT + 1], cw[:, ch, 1:2], yv[:], ALU.mult, ALU.add, [U, cw, yv], [yv])
                    k.stt(yv[:], U[:, 0:TT], cw[:, ch, 0:1], yv[:], ALU.mult, ALU.add, [U, cw, yv], [yv])
                    Z = zc[ch]
                    k.tt(Z[:], pb[:, :], yv[:], ALU.mult, [pb, yv], [Z])
                    k.dma('pool', A['ZTc'][ch * 128:(ch + 1) * 128, cs], Z[:], reads=[Z], writes=[ZT], partial=True)

        if 'B' in phases:
          with k.scope():
            NB = 1536
            w_b = k.sb('w_b', [128, 8, NB], BF16)
            wbv = A['w_b'].rearrange("(kc p) n -> p kc n", p=128)
            for kc in range(8):
                load_cast(k, w_b, lambda c, n: w_b[:, kc, c:c + n], lambda c, n: wbv[:, kc, c:c + n], NB, step=768)
            hTs = [k.sb('hTb%d' % i, [128, 8, TT], BF16) for i in range(2)]
            rmask = k.sb('rmask', [128, 2, 128], F32)
            k.dma('sp', rmask[:], A['rmask'], writes=[rmask])
            qdec = k.sb('qdec', [128, 2, TT], F32)
            k.dma('sp', qdec[:], A['qdec'], writes=[qdec])
            kdec = k.sb('kdec', [128, 2], F32)
            k.dma('sp', kdec[:], A['kdec'], writes=[kdec])
            sdec = k.sb('sdec', [128, 2], F32)
            k.dma('sp', sdec[:], A['sdec'], writes=[sdec])
            cosr = k.sb('cosr', [128, TT], F32); sinr = k.sb('sinr', [128, TT], F32)
            W = rope_work(k)
            qf = k.sb('qf', [128, TT], F32); qb = k.sb('qb', [128, TT], BF16)
            t1 = k.sb('t1b', [128, TT], F32); t2 = k.sb('t2b', [128, TT], F32)
            RQT = [k.sb('RQT%d' % i, [128, TT], BF16) for i in range(2)]
            RQd = [k.sb('RQd%d' % i, [128, TT], BF16) for i in range(2)]
            RKT = [k.sb('RKT%d' % i, [128, TT], BF16) for i in range(2)]
            RKd = [k.sb('RKd%d' % i, [128, 4, 128], BF16) for i in range(2)]
            RV = k.sb('RV', [128, 4, 512], BF16); G = k.sb('G', [128, 4, 512], BF16)
            AT = [k.sb('AT%d' % i, [128, 128], BF16) for i in range(2)]
            st_f = [k.sb('stf%d' % i, [128, 256], F32) for i in range(2)]
            st_b = [k.sb('stb%d' % i, [128, 256], BF16) for i in range(2)]
            junk = k.sb('junk', [128, 256], BF16)
            ssq = k.sb('ssq', [128, 1], F32); sd = k.sb('sd', [128, 1], F32); rinv = k.sb('rinv', [128, 1], F32)
            zr = [k.sb('zr%d' % i, [128, 256], BF16) for i in range(2)]
            zT = [k.sb('zT%d' % i, [128, 2, TT], BF16) for i in range(2)]
            for hr in range(2):
                k.memset(st_f[hr][:], 0.0, [st_f[hr]]); k.memset(st_b[hr][:], 0.0, [st_b[hr]])

            def load_h(t):
                hb = hTs[t % 2]
                k.dma('sp', hb[:], HTv[:, :, t * TT:(t + 1) * TT], reads=[HT], writes=[hb])

            load_h(0)
            for t in range(NT):
                cs = slice(t * TT, (t + 1) * TT)
                if t + 1 < NT:
                    load_h(t + 1)
                hT = hTs[t % 2]
                rope_tables(k, W, A['pos'][0:1, cs], invf[:, 0:1], cosr, sinr)
                for hr in range(2):
                    ps = rot.next()
                    for kc in range(8):
                        k.mm(ps[:, :], w_b[:, kc, hr * 128:(hr + 1) * 128], hT[:, kc, :], kc == 0, kc == 7, [w_b, hT], [ps])
                    k.act(qf[:], ps[:, :], AF.Copy, [ps], [qf])
                    k.act(qb[:], ps[:, :], AF.Copy, [ps], [qb])
                    ps2 = rot.next()
                    k.mm(ps2[:, :], prot[:, 0, :], qb[:], True, True, [prot, qb], [ps2])
                    k.tt(t1[:], qf[:], cosr[:], ALU.mult, [qf, cosr], [t1])
                    k.tt(t2[:], ps2[:, :], sinr[:], ALU.mult, [ps2, sinr], [t2])
                    k.tt(t1[:], t1[:], t2[:], ALU.add, [t1, t2], [t1], eng='pool')
                    k.act(RQT[hr][:], t1[:], AF.Copy, [t1], [RQT[hr]])
                    k.tt(RQd[hr][:], t1[:], qdec[:, hr, :], ALU.mult, [t1, qdec], [RQd[hr]])
                    ps = rot.next()
                    for kc in range(8):
                        k.mm(ps[:, :], w_b[:, kc, 256 + hr * 128:256 + (hr + 1) * 128], hT[:, kc, :], kc == 0, kc == 7, [w_b, hT], [ps])
                    k.act(qf[:], ps[:, :], AF.Copy, [ps], [qf], scale=RET_KS)
                    k.act(qb[:], ps[:, :], AF.Copy, [ps], [qb], scale=RET_KS)
                    ps2 = rot.next()
                    k.mm(ps2[:, :], prot[:, 0, :], qb[:], True, True, [prot, qb], [ps2])
                    k.tt(t1[:], qf[:], cosr[:], ALU.mult, [qf, cosr], [t1])
                    k.tt(t2[:], ps2[:, :], sinr[:], ALU.mult, [ps2, sinr], [t2])
                    k.tt(RKT[hr][:], t1[:], t2[:], ALU.add, [t1, t2], [RKT[hr]])
                    pT = rot.next()
                    for blk in range(4):
                        k.tr(pT.bf[:, blk * 128:(blk + 1) * 128], RKT[hr][:, blk * 128:(blk + 1) * 128], X.id_bf[:, :],
                             [RKT[hr], X.id_bf], [pT], partial=(blk > 0))
                    for blk in range(4):
                        k.act(RKd[hr][:, blk, :], pT.bf[:, blk * 128:(blk + 1) * 128], AF.Copy, [pT, kdec], [RKd[hr]],
                              scale=kdec[:, hr:hr + 1], partial=(blk > 0))
                for blk in range(4):
                    ps = rot.next()
                    for kc in range(8):
                        k.mm(ps[:, :], hT[:, kc, blk * 128:(blk + 1) * 128], w_b[:, kc, 512:1024], kc == 0, kc == 7, [w_b, hT], [ps])
                    k.act(RV[:, blk, :], ps[:, :], AF.Copy, [ps], [RV], partial=(blk > 0))
                    ps = rot.next()
                    for kc in range(8):
                        k.mm(ps[:, :], hT[:, kc, blk * 128:(blk + 1) * 128], w_b[:, kc, 1024:1536], kc == 0, kc == 7, [w_b, hT], [ps])
                    k.act(G[:, blk, :], ps[:, :], AF.Silu, [ps], [G], partial=(blk > 0))
                for blk in range(4):
                    bs = slice(blk * 128, (blk + 1) * 128)
                    for hr in range(2):
                        vs = slice(hr * 256, (hr + 1) * 256)
                        pS = rot.next()
                        k.mm(pS[:, 0:128], RKT[hr][:, bs], RQT[hr][:, bs], True, True, [RKT[hr], RQT[hr]], [pS])
                        a = AT[hr]
                        k.tt(a[:], pS[:, 0:128], rmask[:, hr, :], ALU.mult, [pS, rmask], [a])
                        pO = rot.next()
                        k.mm(pO[:, 0:256], a[:], RV[:, blk, vs], True, False, [a, RV], [pO])
                        k.mm(pO[:, 0:256], RQd[hr][:, bs], st_b[hr][:], False, True, [RQd[hr], st_b[hr]], [pO])
                        pN = rot.next()
                        k.mm(pN[:, 0:256], RKd[hr][:, blk, :], RV[:, blk, vs], True, True, [RKd[hr], RV], [pN])
                        k.stt(st_f[hr][:], st_f[hr][:], sdec[:, hr:hr + 1], pN[:, 0:256], ALU.mult, ALU.add,
                              [st_f[hr], sdec, pN], [st_f[hr]])
                        k.act(st_b[hr][:], st_f[hr][:], AF.Copy, [st_f[hr]], [st_b[hr]])
                        k.act(junk[:], pO[:, 0:256], AF.Square, [pO], [junk, ssq], accum_out=ssq[:, 0:1])
                        k.act(sd[:], ssq[:], AF.Sqrt, [ssq], [sd], bias=EPS, scale=1.0 / 256.0)
                        k.recip(rinv[:], sd[:], [sd], [rinv])
                        z = zr[hr]
                        k.stt(z[:], pO[:, 0:256], rinv[:, 0:1], G[:, blk, vs], ALU.mult, ALU.mult, [pO, rinv, G], [z])
                        pT = rot.next()
                        for vc in range(2):
                            k.tr(pT.bf[:, vc * 128:(vc + 1) * 128], z[:, vc * 128:(vc + 1) * 128], X.id_bf[:, :],
                                 [z, X.id_bf], [pT], partial=(vc > 0))
                        k.act(zT[hr][:, :, bs], pT.bf[:, 0:256].rearrange("p (v i) -> p v i", v=2), AF.Copy, [pT], [zT[hr]],
                              partial=True)
                for hr in range(2):
                    k.dma('pool', A['ZTr'][hr * 256:(hr + 1) * 256, cs].rearrange("(v p) c -> p v c", p=128),
                          zT[hr][:], reads=[zT[hr]], writes=[ZT], partial=True)

        if 'S2' in phases:
          with k.scope():
            kts = [k.sb('kt%d' % i, [128, S], BF16) for i in range(2)]
            qts = [k.sb('qt%d' % i, [128, TT], BF16) for i in range(3)]
            PTs = [k.sb('PT%d' % i, [128, TT], BF16) for i in range(3)]
            of = [k.sb('of%d' % i, [128, TT], F32) for i in range(2)]
            rb = k.sb('rb', [128, TT], F32)
            zm = [k.sb('zm%d' % i, [128, TT], BF16) for i in range(2)]
            sel = k.sb('sel', [128, 64], F32)
            for o_ in of:
                k.memset(o_[:], 0.0, [o_])
            k.dma('sp', sel[:], A['sel65'], writes=[sel])
            srot = Rot(banks[0:4]); orot = Rot(banks[4:6]); brot = Rot(banks[6:8])
            qi = 0
            for h in range(4):
                kt = kts[h % 2]
                for c4 in range(4):
                    k.dma('sp', kt[0:96, c4 * 2048:(c4 + 1) * 2048], A['KTd'][h, :, c4 * 2048:(c4 + 1) * 2048],
                          reads=[KTd], writes=[kt], partial=(c4 > 0))
                for qt in range(NT):
                    q = qts[qi % 3]; qi += 1
                    k.dma('sp', q[0:96, :], A['QTd'][h, :, qt * TT:(qt + 1) * TT], reads=[QTd], writes=[q])
                    nkb = 4 * qt + 4
                    O = orot.next()
                    pend = []

                    def emit_s(kb):
                        d = kb - 4 * qt
                        qlo = 0 if d < 0 else 128 * d
                        pS = srot.next()
                        k.mm(pS[:, qlo:TT], kt[0:96, kb * 128:(kb + 1) * 128], q[0:96, qlo:TT], True, True, [kt, q], [pS])
                        P = PTs[kb % 3]
                        k.act(P[:, qlo:TT], pS[:, qlo:TT], AF.Exp, [pS], [P])
                        if d >= 0:
                            k.memset(P[64:128, qlo:qlo + 64], 0.0, [P], eng='pool', partial=True, reads=[P])
                        return P, d, qlo

                    def emit_pv(kb, P, d, qlo):
                        k.mm(O[0:65, qlo:TT], V_sb[:, kb, h, :], P[:, qlo:TT], kb == 0, kb == nkb - 1, [V_sb, P], [O])

                    for kb in range(nkb):
                        pend.append((kb,) + emit_s(kb))
                        if len(pend) > 1:
                            a = pend.pop(0); emit_pv(*a)
                    while pend:
                        a = pend.pop(0); emit_pv(*a)
                    o = of[qt % 2]
                    k.act(o[0:65, :], O[0:65, :], AF.Copy, [O], [o], partial=True)
                    pB = brot.next()
                    k.mm(pB[0:64, :], sel[:, 0:64], o[:, :], True, True, [sel, o], [pB])
                    k.recip(rb[0:64, :], pB[0:64, :], [pB], [rb])
                    z = zm[qt % 2]
                    k.tt(z[0:64, :], o[0:64, :], rb[0:64, :], ALU.mult, [o, rb], [z])
                    k.dma('pool', A['ZTm'][h * 64:(h + 1) * 64, qt * TT:(qt + 1) * TT], z[0:64, :], reads=[z], writes=[ZT], partial=True)


OFF = {'q_lat': 0, 'kv_lat': 384, 'k_rope': 640, 'cb': 672, 'cc': 1184, 'cx': 1696, 'rq': 2208, 'rk': 2720,
       'rv': 3232, 'rg': 4256, 'gl': 5280}


def col8(v):
    return np.ascontiguousarray(v.reshape(-1, 128).T)


def consts_for(hh):
    C = {}
    C['ident'] = np.eye(128, dtype=np.float32)
    prot = np.zeros((128, 2, 128), np.float32)
    for r in range(64):
        prot[r + 64, 0, r] = -1.0
        prot[r, 0, r + 64] = 1.0
    for r in range(64, 80):
        prot[r + 16, 1, r] = -1.0
        prot[r, 1, r + 16] = 1.0
    C['prot'] = prot
    invf = np.zeros((128, 2), np.float32)
    inv_ret = (10000.0 ** (-np.arange(0, 128, 2, dtype=np.float32) / 128)).astype(np.float32)
    inv_mla = (10000.0 ** (-np.arange(0, 32, 2, dtype=np.float32) / 32)).astype(np.float32)
    for p in range(128):
        invf[p, 0] = inv_ret[p % 64]
    for p in range(64, 96):
        invf[p, 1] = inv_mla[(p - 64) % 16]
    C['invf'] = invf
    rmask = np.zeros((128, 2, 128), np.float64); qdec = np.zeros((128, 2, TT), np.float64)
    kdec = np.zeros((128, 2), np.float64); sdec = np.zeros((128, 2), np.float64)
    for hr in range(2):
        H = hh * 2 + hr
        g = 1.0 - 2.0 ** (-5.0 - H)
        for j in range(128):
            for i in range(128):
                cj, ci = j // 64, i // 64
                if cj == ci:
                    rmask[j, hr, i] = g ** abs(i - j)
                elif cj < ci:
                    rmask[j, hr, i] = g ** (i - j)
        qdec[:, hr, :] = (g ** ((np.arange(TT) % 128) + 1.0))[None, :]
        kdec[:, hr] = g ** (127.0 - np.arange(128))
        sdec[:, hr] = g ** 128.0
    C['rmask'] = rmask.astype(np.float32); C['qdec'] = qdec.astype(np.float32)
    C['kdec'] = kdec.astype(np.float32); C['sdec'] = sdec.astype(np.float32)
    sel = np.zeros((128, 64), np.float32); sel[64, :] = 1.0
    C['sel65'] = sel
    return C


def mixer_inputs(inp, l, b, hh, xT_b):
    w_in = inp['w_in'][l]
    m = dict(consts_for(hh))
    if xT_b is not None:
        m['xT'] = xT_b
    m['cc8'] = col8(inp['c'][b])
    m['b_ada8'] = col8(inp['b_ada'][l])
    m['gmix8'] = col8(inp['norm_mix_g'][l])
    wa = np.zeros((D, 1504), np.float32)
    wa[:, 0:640] = w_in[:, 0:640]
    wa[:, 704:736] = w_in[:, 640:672]
    for i, nm in enumerate(('cb', 'cc', 'cx')):
        wa[:, 736 + i * 256:736 + (i + 1) * 256] = w_in[:, OFF[nm] + hh * 256:OFF[nm] + (hh + 1) * 256]
    m['w_a'] = wa
    wb = np.empty((D, 1536), np.float32)
    wb[:, 0:256] = w_in[:, OFF['rq'] + hh * 256:OFF['rq'] + (hh + 1) * 256]
    wb[:, 256:512] = w_in[:, OFF['rk'] + hh * 256:OFF['rk'] + (hh + 1) * 256]
    wb[:, 512:1024] = w_in[:, OFF['rv'] + hh * 512:OFF['rv'] + (hh + 1) * 512]
    wb[:, 1024:1536] = w_in[:, OFF['rg'] + hh * 512:OFF['rg'] + (hh + 1) * 512]
    m['w_b'] = wb
    m['pos'] = np.ascontiguousarray(inp['positions'][b:b + 1].astype(np.int32))
    m['qg3'] = col8(inp['mla_q_norm_g'][l]); m['kvg2'] = col8(inp['mla_kv_norm_g'][l])
    m['wuq'] = np.ascontiguousarray(inp['w_uq'][l][:, hh * 384:(hh + 1) * 384])
    wukv = inp['w_ukv'][l].reshape(256, 8, 128)[:, hh * 4:(hh + 1) * 4, :]
    m['wuk'] = np.ascontiguousarray(wukv[:, :, 0:64].reshape(256, 256))
    m['wuv'] = np.ascontiguousarray(wukv[:, :, 64:128].reshape(256, 256))
    cwv = inp['conv_w'][l][:, hh * 256:(hh + 1) * 256]
    m['convw'] = np.ascontiguousarray(cwv.reshape(3, 2, 128).transpose(2, 1, 0))
    return m


SO = 4096; NTO = SO // TT
BIG = 1.0e30


def emit_ffn(k, X, A, modT, last, tok):
    if True:
        banks = X.banks
        rot = Rot(banks[0:7])
        pL = banks[7]
        gffn = k.sb('gffn', [128, 8], F32)
        k.dma('sp', gffn[:], A['gffn8'], writes=[gffn])
        gsc2 = k.sb('gsc2', [128, 8], F32)
        k.stt(gsc2[:], modT[:, 32:40], 1.0, gffn[:], ALU.add, ALU.mult, [modT, gffn], [gsc2])
        X1T = A['X1T_buf']; H2K = A['H2K_buf']; HSb = A['HS_buf']; YSb = A['YS_buf']; XO = A['xo_buf']
        NBLK = SO // 128; TS = 256; NTILE = 64
        rc = k.sb('rconst', [128, 128 + 128 + 64 + 16 + 32 + 2], F32)
        k.dma('sp', rc[:], A['rconst'], writes=[rc])
        tri = rc[:, 0:128]; ones_f = rc[:, 128:256]; iota64 = rc[:, 256:320]; thr16 = rc[:, 320:336]
        iota32 = rc[:, 336:368]; pbase2 = rc[:, 368:370]
        e1s = k.sb('e1s', [128, NBLK], F32); e2s = k.sb('e2s', [128, NBLK], F32)
        r1s = k.sb('r1s', [128, NBLK], F32); r2s = k.sb('r2s', [128, NBLK], F32)
        w1s = k.sb('w1s', [128, NBLK], F32); w2s = k.sb('w2s', [128, NBLK], F32)
        run_bc = k.sb('run_bc', [128, 32], F32)
        k.memset(run_bc[:], 0.0, [run_bc])
        pos1i = k.sb('pos1i', [128, NBLK], I32); pos2i = k.sb('pos2i', [128, NBLK], I32)
        widx = k.sb('widx', [128, NTILE, 2], I32)
        HTb = A['HT_buf']; ZTb = A['ZT_buf']; XSb = A['xs_buf']
        xTv = A['xT'].rearrange("(kc p) c -> p kc c", p=128)
        HTv = A['HT'].rearrange("(kc p) c -> p kc c", p=128)
        ZTv = A['ZT'].rearrange("(kc p) c -> p kc c", p=128)
        X1v = A['X1T'].rearrange("(kc p) c -> p kc c", p=128)
        XOv = A['xoT'].rearrange("(kc p) c -> p kc c", p=128)

        with k.scope():
            w_g = k.sb('w_g', [128, 8, 3072], BF16)
            wgv = A['w_g'].rearrange("(kc p) n -> p kc n", p=128)
            for kc in range(8):
                load_cast(k, w_g, lambda c, n: w_g[:, kc, c:c + n], lambda c, n: wgv[:, kc, c:c + n], 3072, step=1024)
            w_o = k.sb('w_o', [128, 16, 1024], BF16)
            wov = A['w_o'].rearrange("(kc p) n -> p kc n", p=128)
            for kc in range(16):
                k.dma('pool', w_o[:, kc, :], wov[:, kc, :], writes=[w_o], partial=True)
            w_m = k.sb('w_m', [128, 8, 1024], BF16)
            wmv = A['w_mix'].rearrange("(kc p) n -> p kc n", p=128)
            for kc in range(8):
                k.dma('pool', w_m[:, kc, :], wmv[:, kc, :], writes=[w_m], partial=True)
            w_r = k.sb('w_r', [128, 8, 36], F32)
            k.dma('sp', w_r[:], A['w_r'].rearrange("(kc p) n -> p kc n", p=128), writes=[w_r])
            b_r = k.sb('b_r', [128, 36], F32)
            k.dma('sp', b_r[:], A['b_r'].to_broadcast([128, 36]), writes=[b_r])
            hT = k.sb('hTc', [128, 8, TT], BF16)
            Zt = k.sb('Zt', [128, 16, TT], BF16)
            xt = k.sb('xtc', [128, 8, TT], F32)
            gt0 = k.sb('gt0', [128, 3, TT], BF16); gt = [gt0, gt0]
            tA = k.sb('tA', [128, TT], F32); tB = k.sb('tB', [128, TT], F32)
            mg = k.sb('mg', [128, 8, TT], BF16)
            rtmp = k.sb('rtmpc', [128, TT], F32); rstd = k.sb('rstdc', [128, TT], F32)
            h2f0 = k.sb('h2f0', [128, TT], F32); h2f = [h2f0, h2f0]
            h2b = k.sb('h2b', [128, 8, TT], BF16)
            lt = tA
            lg = k.sb('lg', [128, 36], F32); gmax = k.sb('gmax', [128, 1], F32); ngmax = k.sb('ngmax', [128, 1], F32)
            g1h = k.sb('g1h', [128, 4], F32); pen = k.sb('pen', [128, 4], F32)
            ej = k.sb('ej', [128, 4], F32); gsum = k.sb('gsum', [128, 1], F32); gval = k.sb('gval', [128, 1], F32)
            lem = k.sb('lem', [128, 32], F32); top8 = k.sb('top8', [128, 8], F32)
            m1 = k.sb('m1', [128, 32], F32); m2 = k.sb('m2', [128, 32], F32)
            dd = k.sb('dd', [128, 1], F32); ee = k.sb('ee', [128, 1], F32); den = k.sb('den', [128, 1], F32)
            oh = k.sb('oh', [128, 32], F32); rk = k.sb('rk', [128, 32], F32); jk = k.sb('jk', [128, 32], F32)
            cT = tB
            for t in range(NTO):
                cs = slice(t * TT, (t + 1) * TT)
                tsl = tok('sp', t * TT, TT)
                k.dma('sp', hT[:], HTv[:, :, tsl], reads=[HTb], writes=[hT])
                k.dma('sp', Zt[:], ZTv[:, :, tsl], reads=[ZTb], writes=[Zt])
                for kc in range(8):
                    k.dma('sp', xt[:, kc, :], xTv[:, kc, tsl], reads=[XSb], writes=[xt], partial=(kc > 0))
                for dc in range(8):
                    ds_ = slice(dc * 128, (dc + 1) * 128)
                    g = gt[dc % 2]
                    for br in range(3):
                        ps = rot.next()
                        for kc in range(8):
                            k.mm(ps[:, :], w_g[:, kc, br * 1024 + dc * 128: br * 1024 + (dc + 1) * 128], hT[:, kc, :],
                                 kc == 0, kc == 7, [w_g, hT], [ps])
                        k.act(g[:, br, :], ps[:, :], AF.Sigmoid, [ps], [g], partial=(br > 0))
                    pys = []
                    for (k0, k1) in ((0, 4), (4, 8), (8, 16)):
                        ps = rot.next()
                        for kc in range(k0, k1):
                            k.mm(ps[:, :], w_o[:, kc, ds_], Zt[:, kc, :], kc == k0, kc == k1 - 1, [w_o, Zt], [ps])
                        pys.append(ps)
                    k.tt(tA[:], pys[0][:, :], g[:, 0, :], ALU.mult, [pys[0], g], [tA])
                    k.tt(tB[:], pys[1][:, :], g[:, 1, :], ALU.mult, [pys[1], g], [tB])
                    k.tt(tA[:], tA[:], tB[:], ALU.add, [tA, tB], [tA], eng='pool')
                    k.tt(tB[:], pys[2][:, :], g[:, 2, :], ALU.mult, [pys[2], g], [tB])
                    k.tt(mg[:, dc, :], tA[:], tB[:], ALU.add, [tA, tB], [mg], eng='pool', partial=(dc > 0))
                for dc in range(8):
                    ps = rot.next()
                    for kc in range(8):
                        k.mm(ps[:, :], w_m[:, kc, dc * 128:(dc + 1) * 128], mg[:, kc, :], kc == 0, kc == 7, [w_m, mg], [ps])
                    k.stt(xt[:, dc, :], ps[:, :], modT[:, 16 + dc:17 + dc], xt[:, dc, :], ALU.mult, ALU.add,
                          [ps, modT, xt], [xt], partial=True)
                k.dma('pool', X1v[:, :, cs], xt[:], reads=[xt], writes=[X1T], partial=True)
                for kc in range(8):
                    k.act(mg[:, kc, :], xt[:, kc, :], AF.Square, [xt], [mg], partial=(kc > 0))
                pss = rot.next()
                for kc in range(8):
                    k.mm(pss[:, :], X.ones_bf[:, :], mg[:, kc, :], kc == 0, kc == 7, [X.ones_bf, mg], [pss])
                k.act(rtmp[:], pss[:, :], AF.Sqrt, [pss], [rtmp], bias=EPS, scale=1.0 / D)
                k.recip(rstd[:], rtmp[:], [rtmp], [rstd])
                for kc in range(8):
                    hf = h2f[kc % 2]
                    k.stt(hf[:], xt[:, kc, :], gsc2[:, kc:kc + 1], rstd[:], ALU.mult, ALU.mult, [xt, gsc2, rstd], [hf])
                    k.act(hf[:], hf[:], AF.Identity, [hf, modT], [hf], bias=modT[:, 24 + kc:25 + kc])
                    k.copy(h2b[:, kc, :], hf[:], [hf], [h2b], eng='pool', partial=(kc > 0))
                    k.mm(pL[0:36, :], w_r[:, kc, :], hf[:], kc == 0, kc == 7, [w_r, hf], [pL])
                for blk in range(4):
                    pH = rot.next()
                    for kc in range(8):
                        k.tr(pH.bf[:, kc * 128:(kc + 1) * 128], h2b[:, kc, blk * 128:(blk + 1) * 128], X.id_bf[:, :],
                             [h2b, X.id_bf], [pH], partial=(kc > 0))
                    hk = gt0
                    k.act(hk[:, 0:2, :], pH.bf[:, :].rearrange("p (a b) -> p a b", a=2), AF.Copy, [pH], [hk])
                    k.dma('pool', A['H2K'][(t * 4 + blk) * 128:(t * 4 + blk + 1) * 128, :].rearrange("p (a b) -> p a b", a=2),
                          hk[:, 0:2, :], reads=[hk], writes=[H2K], partial=True)
                k.act(lt[0:36, :], pL[0:36, :], AF.Copy, [pL], [lt])
                pT = rot.next()
                for blk in range(4):
                    k.tr(pT[:, blk * 36:(blk + 1) * 36], lt[0:36, blk * 128:(blk + 1) * 128], X.id_f[0:36, 0:36],
                         [lt, X.id_f], [pT], partial=(blk > 0))
                for blk in range(4):
                    gb = t * 4 + blk
                    w1 = w1s[:, gb:gb + 1]; w2 = w2s[:, gb:gb + 1]
                    k.tt(lg[:], pT[:, blk * 36:(blk + 1) * 36], b_r[:], ALU.add, [pT, b_r], [lg])
                    k.op('dve', lambda e: e.reduce_max(out=gmax[:], in_=lg[:, 0:4], axis=mybir.AxisListType.X),
                         reads=[lg], writes=[gmax])
                    k.ts(g1h[:], lg[:, 0:4], gmax[:, 0:1], ALU.is_equal, [lg, gmax], [g1h])
                    k.ts(ngmax[:], gmax[:], -1.0, ALU.mult, [gmax], [ngmax])
                    k.act(ej[:], lg[:, 0:4], AF.Exp, [lg, ngmax], [ej, gsum], bias=ngmax[:, 0:1], accum_out=gsum[:, 0:1])
                    k.recip(gval[:], gsum[:], [gsum], [gval])
                    k.ts(pen[:], g1h[:], -1.0, ALU.add, [g1h], [pen], s2=BIG, op1=ALU.mult)
                    for g_ in range(4):
                        k.ts(lem[:, g_ * 8:(g_ + 1) * 8], lg[:, 4 + g_ * 8:4 + (g_ + 1) * 8], pen[:, g_:g_ + 1], ALU.add,
                             [lg, pen], [lem], partial=(g_ > 0))
                    k.op('dve', lambda e: e.max(out=top8[:], in_=lem[:]), reads=[lem], writes=[top8])
                    k.ts(m1[:], lem[:], top8[:, 0:1], ALU.is_equal, [lem, top8], [m1])
                    k.ts(m2[:], lem[:], top8[:, 1:2], ALU.is_equal, [lem, top8], [m2])
                    k.tt(dd[:], top8[:, 1:2], top8[:, 0:1], ALU.subtract, [top8], [dd])
                    k.act(ee[:], dd[:], AF.Exp, [dd], [ee])
                    k.ts(den[:], ee[:], 1.0, ALU.add, [ee], [den])
                    k.recip(den[:], den[:], [den], [den])
                    k.tt(w1, den[:], gval[:], ALU.mult, [den, gval], [w1s], partial=True)
                    k.tt(w2, w1, ee[:], ALU.mult, [w1s, ee], [w2s], partial=True)
                    k.tt(oh[:], m1[:], m2[:], ALU.add, [m1, m2], [oh])
                    pP = rot.next()
                    k.mm(pP[:, 0:32], tri, oh[:], True, True, [rc, oh], [pP])
                    k.tt(rk[:], pP[:, 0:32], run_bc[:], ALU.add, [pP, run_bc], [rk])
                    k.op('dve', lambda e: e.scalar_tensor_tensor(out=jk[:], in0=rk[:], scalar=1.0, in1=m1[:], op0=ALU.mult, op1=ALU.mult,
                                                              accum_out=r1s[:, gb:gb + 1]), reads=[rk, m1], writes=[jk, r1s], partial=True)
                    k.op('dve', lambda e: e.scalar_tensor_tensor(out=jk[:], in0=rk[:], scalar=1.0, in1=m2[:], op0=ALU.mult, op1=ALU.mult,
                                                              accum_out=r2s[:, gb:gb + 1]), reads=[rk, m2], writes=[jk, r2s], partial=True)
                    k.op('dve', lambda e: e.scalar_tensor_tensor(out=jk[:], in0=iota32, scalar=1.0, in1=m1[:], op0=ALU.mult, op1=ALU.mult,
                                                              accum_out=e1s[:, gb:gb + 1]), reads=[rc, m1], writes=[jk, e1s], partial=True)
                    k.op('dve', lambda e: e.scalar_tensor_tensor(out=jk[:], in0=iota32, scalar=1.0, in1=m2[:], op0=ALU.mult, op1=ALU.mult,
                                                              accum_out=e2s[:, gb:gb + 1]), reads=[rc, m2], writes=[jk, e2s], partial=True)
                    pQ = rot.next()
                    k.mm(pQ[:, 0:32], ones_f, oh[:], True, True, [rc, oh], [pQ])
                    k.tt(run_bc[:], run_bc[:], pQ[:, 0:32], ALU.add, [run_bc, pQ], [run_bc])
            ptile = k.sb('ptile', [128, 32], F32); cmp16 = k.sb('cmp16', [128, 16], F32)
            start = k.sb('start', [128, 32], F32); endt = k.sb('endt', [128, 32], F32); sstart = k.sb('sstart', [128, 32], F32)
            et = k.sb('et', [128, NTILE], F32); wf = k.sb('wf', [128, NTILE, 2], F32)
            p1f = k.sb('p1f', [128, NBLK], F32); p2f = k.sb('p2f', [128, NBLK], F32)
            for e_ in range(32):
                k.op('dve', lambda e: e.tensor_scalar(out=cmp16[:], in0=thr16, scalar1=run_bc[:, e_:e_ + 1], scalar2=None,
                                                      op0=ALU.is_lt, op1=ALU.add, accum_out=ptile[:, e_:e_ + 1]),
                     reads=[rc, run_bc], writes=[cmp16, ptile], partial=True)
            k.memset(start[:, 0:1], 0.0, [start], partial=True)
            for e_ in range(1, 32):
                k.tt(start[:, e_:e_ + 1], start[:, e_ - 1:e_], ptile[:, e_ - 1:e_], ALU.add, [start, ptile], [start], partial=True)
            k.tt(endt[:], start[:], ptile[:], ALU.add, [start, ptile], [endt])
            k.ts(sstart[:], start[:], float(TS), ALU.mult, [start], [sstart])
            k.memset(et[:], 0.0, [et])
            for e_ in range(32):
                k.stt(et[:], iota64, endt[:, e_:e_ + 1], et[:], ALU.is_ge, ALU.add, [rc, endt, et], [et])
            k.ts(et[:], et[:], 31.0, ALU.min, [et], [et])
            for j in range(2):
                k.ts(wf[:, :, j], et[:], 256.0, ALU.mult, [et, rc], [wf], s2=pbase2[:, j:j + 1], op1=ALU.add, partial=True)
            k.copy(widx[:], wf[:], [wf], [widx])
            for gb in range(NBLK):
                k.ts(m1[:], iota32, e1s[:, gb:gb + 1], ALU.is_equal, [rc, e1s], [m1])
                k.op('dve', lambda e: e.scalar_tensor_tensor(out=jk[:], in0=m1[:], scalar=1.0, in1=sstart[:], op0=ALU.mult, op1=ALU.mult,
                                                          accum_out=p1f[:, gb:gb + 1]), reads=[m1, sstart], writes=[jk, p1f], partial=True)
                k.ts(m2[:], iota32, e2s[:, gb:gb + 1], ALU.is_equal, [rc, e2s], [m2])
                k.op('dve', lambda e: e.scalar_tensor_tensor(out=jk[:], in0=m2[:], scalar=1.0, in1=sstart[:], op0=ALU.mult, op1=ALU.mult,
                                                          accum_out=p2f[:, gb:gb + 1]), reads=[m2, sstart], writes=[jk, p2f], partial=True)
            k.tt(p1f[:], p1f[:], r1s[:], ALU.add, [p1f, r1s], [p1f])
            k.tt(p2f[:], p2f[:], r2s[:], ALU.add, [p2f, r2s], [p2f])
            k.copy(pos1i[:], p1f[:], [p1f], [pos1i]); k.copy(pos2i[:], p2f[:], [p2f], [pos2i])

        if last:
            fin = k.sb('fin', [128, 8], F32)
            k.dma('sp', fin[:], A['fin8'], writes=[fin])
        with k.scope():
            hr_ = [k.sb('hr%d' % i, [128, 1024], BF16) for i in range(2)]
            for gb in range(NBLK):
                hb_ = hr_[gb % 2]
                k.dma('sp', hb_[:], A['H2K'][gb * 128:(gb + 1) * 128, :], reads=[H2K], writes=[hb_])
                k.idma(A['HS'][:, :], hb_[:, :], pos1i[:, gb:gb + 1], True, [hb_, pos1i], [HSb], hb_)
                k.idma(A['HS'][:, :], hb_[:, :], pos2i[:, gb:gb + 1], True, [hb_, pos2i], [HSb], hb_)
        with k.scope():
            wg = [k.sb('wg%d' % i, [128, 4096], BF16) for i in range(2)]
            wu = [k.sb('wu%d' % i, [128, 4096], BF16) for i in range(2)]
            wd = [k.sb('wd%d' % i, [128, 4096], BF16) for i in range(2)]
            hst = [k.sb('hst%d' % i, [128, 2, 1024], BF16) for i in range(2)]
            hTs = [k.sb('hTs%d' % i, [128, 8, TS], BF16) for i in range(2)]
            sg = [k.sb('sg%d' % i, [128, TS], F32) for i in range(2)]
            hid = [k.sb('hid%d' % i, [128, 4, TS], BF16) for i in range(2)]
            ys = [k.sb('ys%d' % i, [128, 2, 1024], F32) for i in range(2)]
            HSv = A['HS'].rearrange("(i sb p) n -> i p sb n", sb=2, p=128)
            YSv = A['YS'].rearrange("(i sb p) n -> i p sb n", sb=2, p=128)

            def load_w(i):
                bi = i % 2
                for (wt, src) in ((wg[bi], A['w_eg']), (wu[bi], A['w_eu']), (wd[bi], A['w_ed'])):
                    for j in range(2):
                        k.idma(wt[:, j * 2048:(j + 1) * 2048], src[:, :], widx[:, i, j:j + 1], False, [widx], [wt], wt, partial=(j > 0))

            load_w(0)
            for i in range(NTILE):
                if i + 1 < NTILE:
                    load_w(i + 1)
                bi = i % 2
                hs_ = hst[bi]; hT_ = hTs[bi]; hd = hid[bi]; y_ = ys[bi]
                k.dma('sp', hs_[:], HSv[i], reads=[HSb], writes=[hs_])
                for sb in range(2):
                    pH = rot.next()
                    for kc in range(8):
                        k.tr(pH.bf[:, kc * 128:(kc + 1) * 128], hs_[:, sb, kc * 128:(kc + 1) * 128], X.id_bf[:, :],
                             [hs_, X.id_bf], [pH], partial=(kc > 0))
                    k.act(hT_[:, :, sb * 128:(sb + 1) * 128], pH.bf[:, :].rearrange("p (kc s) -> p kc s", kc=8), AF.Copy,
                          [pH], [hT_], partial=(sb > 0))
                for fc in range(4):
                    pg = rot.next(); pu = rot.next()
                    for kc in range(8):
                        k.mm(pg[:, 0:TS], wg[bi][:, kc * 512 + fc * 128: kc * 512 + (fc + 1) * 128], hT_[:, kc, :], kc == 0, kc == 7, [wg[bi], hT_], [pg])
                    for kc in range(8):
                        k.mm(pu[:, 0:TS], wu[bi][:, kc * 512 + fc * 128: kc * 512 + (fc + 1) * 128], hT_[:, kc, :], kc == 0, kc == 7, [wu[bi], hT_], [pu])
                    s_ = sg[fc % 2]
                    k.act(s_[:], pg[:, 0:TS], AF.Silu, [pg], [s_])
                    k.tt(hd[:, fc, :], pu[:, 0:TS], s_[:], ALU.mult, [pu, s_], [hd], partial=(fc > 0))
                for sb in range(2):
                    for dh in range(2):
                        pd = rot.next()
                        for fc in range(4):
                            k.mm(pd[:, :], hd[:, fc, sb * 128:(sb + 1) * 128], wd[bi][:, fc * 1024 + dh * 512: fc * 1024 + (dh + 1) * 512],
                                 fc == 0, fc == 3, [hd, wd[bi]], [pd])
                        if (sb + dh) % 2 == 0:
                            k.act(y_[:, sb, dh * 512:(dh + 1) * 512], pd[:, :], AF.Copy, [pd], [y_], partial=True)
                        else:
                            k.copy(y_[:, sb, dh * 512:(dh + 1) * 512], pd[:, :], [pd], [y_], partial=True)
                k.dma('sp', YSv[i], y_[:], reads=[y_], writes=[YSb], partial=True)
        with k.scope():
            y1 = [k.sb('y1_%d' % i, [128, 1024], F32) for i in range(2)]
            y2 = [k.sb('y2_%d' % i, [128, 1024], F32) for i in range(2)]
            mo = k.sb('mo', [128, 4, 1024], F32)
            x1 = [k.sb('x1_%d' % i, [128, 8, TT], F32) for i in range(2)]
            sqf = k.sb('sqf', [128, 8, TT], BF16)
            rt2 = k.sb('rt2', [128, TT], F32); rs2 = k.sb('rs2', [128, TT], F32)
            for t in range(NTO):
                gs = slice(t * TT, (t + 1) * TT)
                xb = x1[t % 2]
                k.dma('sp', xb[:], X1v[:, :, gs], reads=[X1T], writes=[xb])
                for blk in range(4):
                    gb = t * 4 + blk
                    a1 = y1[blk % 2]; a2 = y2[blk % 2]
                    k.idma(a1[:, :], A['YS'][:, :], pos1i[:, gb:gb + 1], False, [YSb, pos1i], [a1], a1, partial=False)
                    k.idma(a2[:, :], A['YS'][:, :], pos2i[:, gb:gb + 1], False, [YSb, pos2i], [a2], a2, partial=False)
                    k.ts(mo[:, blk, :], a1[:], w1s[:, gb:gb + 1], ALU.mult, [a1, w1s], [mo], partial=(blk > 0))
                    k.stt(mo[:, blk, :], a2[:], w2s[:, gb:gb + 1], mo[:, blk, :], ALU.mult, ALU.add, [a2, w2s, mo], [mo], partial=True)
                for dc in range(8):
                    pM = rot.next()
                    for blk in range(4):
                        k.tr(pM[:, blk * 128:(blk + 1) * 128], mo[:, blk, dc * 128:(dc + 1) * 128], X.id_f[:, :],
                             [mo, X.id_f], [pM], partial=(blk > 0))
                    k.stt(xb[:, dc, :], pM[:, :], modT[:, 40 + dc:41 + dc], xb[:, dc, :], ALU.mult, ALU.add,
                          [pM, modT, xb], [xb], partial=True)
                if last:
                    for kc in range(8):
                        k.act(sqf[:, kc, :], xb[:, kc, :], AF.Square, [xb], [sqf], partial=(kc > 0))
                    pss = rot.next()
                    for kc in range(8):
                        k.mm(pss[:, :], X.ones_bf[:, :], sqf[:, kc, :], kc == 0, kc == 7, [X.ones_bf, sqf], [pss])
                    k.act(rt2[:], pss[:, :], AF.Sqrt, [pss], [rt2], bias=EPS, scale=1.0 / D)
                    k.recip(rs2[:], rt2[:], [rt2], [rs2])
                    for kc in range(8):
                        k.stt(xb[:, kc, :], xb[:, kc, :], fin[:, kc:kc + 1], rs2[:], ALU.mult, ALU.mult,
                              [xb, fin, rs2], [xb], partial=True)
                k.dma('pool', XOv[:, :, gs], xb[:], reads=[xb], writes=[XO], partial=True)


FUSED_IN = {
    'xT': ([D, S], F32), 'cc8': ([128, 8], F32), 'w_ada': ([2, D, 6 * D], F32), 'b_ada8': ([2, 128, 48], F32),
    'gmix8': ([2, 128, 8], F32), 'gffn8': ([2, 128, 8], F32), 'fin8': ([128, 8], F32),
    'w_a': ([2, 2, D, 1504], F32), 'w_b': ([2, 2, D, 1536], F32), 'pos': ([1, S], I32),
    'qg3': ([2, 128, 3], F32), 'kvg2': ([2, 128, 2], F32), 'wuq': ([2, 2, 384, 384], F32),
    'wuk': ([2, 2, 256, 256], F32), 'wuv': ([2, 2, 256, 256], F32), 'convw': ([2, 2, 128, 2, 3], F32),
    'ident': ([128, 128], F32), 'prot': ([128, 2, 128], F32), 'invf': ([128, 2], F32),
    'rmask': ([2, 128, 2, 128], F32), 'qdec': ([2, 128, 2, TT], F32), 'kdec': ([2, 128, 2], F32),
    'sdec': ([2, 128, 2], F32), 'sel65': ([128, 64], F32),
    'w_g': ([2, D, 3072], F32), 'w_o': ([2, 2048, D], F32), 'w_mix': ([2, D, D], F32), 'w_r': ([2, D, 36], F32),
    'b_r': ([2, 1, 36], F32), 'w_eg0': ([8192, 2048], F32), 'w_eu0': ([8192, 2048], F32), 'w_ed0': ([8192, 2048], F32),
    'w_eg1': ([8192, 2048], F32), 'w_eu1': ([8192, 2048], F32), 'w_ed1': ([8192, 2048], F32), 'rconst': ([128, 370], F32),
}


def build_fused(nc, A):
    root = ExitStack()
    with root:
        k = K(nc, root)
        X = setup_common(k, A)
        V_sb = k.sb('V_sb', [128, 64, 4, 65], BF16)
        k.memset(V_sb[:, :, :, 64:65], 1.0, [V_sb], eng='pool')
        bufs = {n: Buf(n, A[n], dram=True) for n in ('HT', 'ZT', 'QTd', 'KTd', 'XO', 'X1T', 'H2K', 'HS', 'YS', 'xT', 'out', 'HTo', 'ZTo', 'XOo')}
        modT = [k.sb('modT%d' % l, [128, 48], F32) for l in range(2)]
        for l in range(2):
            emit_mod(k, X, {'cc8': A['cc8'], 'b_ada8': A['b_ada8'][l], 'w_ada': A['w_ada'][l]}, modT[l])
        pid = nc.sync.partition_id()
        for l in range(2):
            xsrc, xsb = (A['xT'], bufs['xT']) if l == 0 else (A['XO'], bufs['XO'])
            for hh in range(2):
                Am = dict(gmix8=A['gmix8'][l], invf=A['invf'], prot=A['prot'], HT=A['HT'], xT=xsrc,
                          HT_buf=bufs['HT'], ZT_buf=bufs['ZT'], QTd_buf=bufs['QTd'], KTd_buf=bufs['KTd'],
                          QTd=A['QTd'], KTd=A['KTd'], w_a=A['w_a'][l, hh], w_b=A['w_b'][l, hh],
                          qg3=A['qg3'][l], kvg2=A['kvg2'][l], wuq=A['wuq'][l, hh], wuk=A['wuk'][l, hh], wuv=A['wuv'][l, hh],
                          convw=A['convw'][l, hh], pos=A['pos'], rmask=A['rmask'][hh], qdec=A['qdec'][hh],
                          kdec=A['kdec'][hh], sdec=A['sdec'][hh], sel65=A['sel65'],
                          ZTm=A['ZT'][hh * 256:(hh + 1) * 256, :], ZTc=A['ZT'][512 + hh * 256:512 + (hh + 1) * 256, :],
                          ZTr=A['ZT'][1024 + hh * 512:1024 + (hh + 1) * 512, :])
                with k.scope():
                    emit_mixer(k, X, Am, modT[l], V_sb, write_ht=(hh == 0))
            last = (l == 1)
            if last:
                stg = Buf('stage')
                hoff = pid % 2 * SO
                for (dst, src) in (('HTo', 'HT'), ('ZTo', 'ZT'), ('XOo', 'XO')):
                    k.dma('sp', A[dst][:, :], A[src][:, bass.ds(hoff, SO)], reads=[bufs[src]], writes=[bufs[dst]], owner=stg)
            for th in ((None,) if last else (0, 1)):
                if last:
                    tok = lambda q, c0, n: slice(c0, c0 + n)
                    xo = A['out']; xob = bufs['out']
                    srcs = dict(xs_buf=bufs['XOo'], xT=A['XOo'], HT=A['HTo'], ZT=A['ZTo'], HT_buf=bufs['HTo'], ZT_buf=bufs['ZTo'])
                else:
                    tok = (lambda th_: (lambda q, c0, n: slice(th_ * SO + c0, th_ * SO + c0 + n)))(th)
                    xo = A['XO'][:, th * SO:(th + 1) * SO]; xob = bufs['XO']
                    srcs = dict(xs_buf=xsb, xT=xsrc, HT=A['HT'], ZT=A['ZT'], HT_buf=bufs['HT'], ZT_buf=bufs['ZT'])
                Af = dict(gffn8=A['gffn8'][l], X1T_buf=bufs['X1T'], H2K_buf=bufs['H2K'], HS_buf=bufs['HS'], YS_buf=bufs['YS'], xo_buf=xob,
                          rconst=A['rconst'], H2K=A['H2K'], HS=A['HS'], YS=A['YS'],
                          X1T=A['X1T'], xoT=xo, w_g=A['w_g'][l], w_o=A['w_o'][l],
                          w_mix=A['w_mix'][l], w_r=A['w_r'][l], b_r=A['b_r'][l], w_eg=A['w_eg%d' % l], w_eu=A['w_eu%d' % l],
                          w_ed=A['w_ed%d' % l], fin8=A['fin8'], **srcs)
                with k.scope():
                    emit_ffn(k, X, Af, modT[l], last, tok)
        k.barrier()
    return nc


def make_fused_nc():
    nc = bass.Bass("TRN2", target_bir_lowering=False)
    A = {}
    for name, (shape, dt) in FUSED_IN.items():
        A[name] = nc.dram_tensor(name, shape, dt, kind="ExternalInput").ap()
    A['out'] = nc.dram_tensor('out', [D, SO], F32, kind="ExternalOutput").ap()
    for name, shape, dt in (('HT', [D, S], BF16), ('ZT', [2048, S], BF16), ('QTd', [4, 96, S], BF16),
                            ('KTd', [4, 96, S], BF16), ('XO', [D, S], F32), ('X1T', [D, SO], F32),
                            ('H2K', [SO, D], BF16), ('HS', [16384, D], BF16), ('YS', [16384, D], F32), ('HTo', [D, SO], BF16),
                            ('ZTo', [2048, SO], BF16), ('XOo', [D, SO], F32)):
        A[name] = nc.dram_tensor(name, shape, dt, kind="Internal").ap()
    build_fused(nc, A)
    return nc


def fused_inputs(inp, b):
    L = range(2)
    m = {}
    c0 = consts_for(0); c1 = consts_for(1)
    for kk in ('ident', 'prot', 'invf', 'sel65'):
        m[kk] = c0[kk]
    for kk in ('rmask', 'qdec', 'kdec', 'sdec'):
        m[kk] = np.stack([c0[kk], c1[kk]], axis=0)
    mi = [[mixer_inputs(inp, l, b, hh, None) for hh in range(2)] for l in L]
    m['xT'] = np.ascontiguousarray(inp['x'][b].T)
    m['cc8'] = mi[0][0]['cc8']; m['pos'] = mi[0][0]['pos']
    m['w_ada'] = np.ascontiguousarray(inp['w_ada'])
    for kk in ('b_ada8', 'gmix8', 'qg3', 'kvg2'):
        m[kk] = np.stack([mi[l][0][kk] for l in L], axis=0)
    for kk in ('w_a', 'w_b', 'wuq', 'wuk', 'wuv', 'convw'):
        m[kk] = np.stack([np.stack([mi[l][hh][kk] for hh in range(2)], axis=0) for l in L], axis=0)
    m['gffn8'] = np.stack([col8(inp['norm_ffn_g'][l]) for l in L], axis=0)
    m['fin8'] = col8(inp['final_g'])
    m['w_g'] = np.ascontiguousarray(inp['w_in'][:, :, OFF['gl']:OFF['gl'] + 3072])
    m['w_o'] = np.concatenate([inp['w_o_mla'], inp['w_o_conv'], inp['w_o_ret']], axis=1)
    m['w_mix'] = np.ascontiguousarray(inp['w_mix_out'])
    m['w_r'] = np.concatenate([inp['w_route_group'], inp['w_route_expert']], axis=2)
    m['b_r'] = np.concatenate([inp['b_route_group'], inp['b_route_expert']], axis=1)[:, None, :].astype(np.float32)
    for l in L:
        m['w_eg%d' % l] = np.ascontiguousarray(inp['w_exp_gate'][l].reshape(32, 8, 128, 512).transpose(0, 2, 1, 3)).reshape(8192, 2048)
        m['w_eu%d' % l] = np.ascontiguousarray(inp['w_exp_up'][l].reshape(32, 8, 128, 512).transpose(0, 2, 1, 3)).reshape(8192, 2048)
        m['w_ed%d' % l] = np.ascontiguousarray(inp['w_exp_down'][l].reshape(32, 4, 128, 1024).transpose(0, 2, 1, 3)).reshape(8192, 2048)
    rcst = np.zeros((128, 370), np.float32)
    rcst[:, 0:128] = np.triu(np.ones((128, 128), np.float32), 1)
    rcst[:, 128:256] = 1.0
    rcst[:, 256:320] = np.arange(64, dtype=np.float32)[None, :]
    rcst[:, 320:336] = (np.arange(16, dtype=np.float32) * 256.0)[None, :]
    rcst[:, 336:368] = np.arange(32, dtype=np.float32)[None, :]
    rcst[:, 368] = 2.0 * np.arange(128); rcst[:, 369] = 2.0 * np.arange(128) + 1.0
    m['rconst'] = rcst
    return m


_NC_CACHE = {}


def kernel(**inp):
    inp = {k_: np.asarray(v) for k_, v in inp.items()}
    B = inp['x'].shape[0]
    cores = list(range(8))
    if 'fused' not in _NC_CACHE:
        _NC_CACHE['fused'] = make_fused_nc()
    per_b = [fused_inputs(inp, b) for b in range(B)]
    maps = [per_b[c // 2] for c in cores]
    res = run_bass_kernel_spmd(_NC_CACHE['fused'], maps, core_ids=cores).results
    out = np.empty((B, S, D), np.float32)
    for c in cores:
        b, th = c // 2, c % 2
        out[b, th * SO:(th + 1) * SO, :] = np.asarray(res[c]['out']).T
    return out
```

```python
import math
from contextlib import ExitStack, contextmanager
import numpy as np
import ml_dtypes
import concourse.bass as bass
import concourse.mybir as mybir
from concourse.bass_utils import run_bass_kernel_spmd

F32 = mybir.dt.float32; BF16 = mybir.dt.bfloat16; I32 = mybir.dt.int32
ALU = mybir.AluOpType; AF = mybir.ActivationFunctionType

D = 1024; S = 8192; TT = 512; NT = S // TT
EPS = 1e-6
TWO_PI = 2.0 * math.pi
CW1 = 6.28125
CW2 = TWO_PI - CW1
PI_LO = 3.1415925
MLA_SCALE = 96.0 ** -0.5
RET_KS = 128.0 ** -0.5


class Buf:
    def __init__(self, name, ap=None, dram=False):
        self.name = name; self.ap = ap; self.dram = dram
        self.writers = {}; self.readers = {}
        self.dsem = None; self.dval = 0; self.dkey = None; self.psum = False

    def __getitem__(self, idx):
        return self.ap[idx]


class Rot:
    def __init__(self, bufs):
        self.bufs = bufs; self.i = 0

    def next(self):
        b = self.bufs[self.i % len(self.bufs)]; self.i += 1
        return b


class K:
    def __init__(self, nc, root):
        self.nc = nc; self.root = root; self.stacks = [root]
        self.eng = {'pe': nc.tensor, 'dve': nc.vector, 'act': nc.scalar, 'pool': nc.gpsimd, 'sp': nc.sync}
        self.sem = {}; self.cnt = {}
        for e in self.eng:
            self.sem[e] = root.enter_context(nc.semaphore('s_' + e)); self.cnt[e] = 0
        self.waited = {e: {} for e in self.eng}
        self.dma_latest = {}
        self.uid = 0
        self.free_dsems = []
        self.scope_bufs = [[]]

    def sb(self, name, shape, dt):
        self.uid += 1
        t = self.stacks[-1].enter_context(self.nc.sbuf_tensor('%s_%d' % (name, self.uid), shape, dt))
        b = Buf(name, t)
        self.scope_bufs[-1].append(b)
        return b

    def ps(self, name, shape, dt=F32):
        t = self.root.enter_context(self.nc.psum_tensor(name, shape, dt))
        b = Buf(name, t); b.psum = True
        return b

    def dram(self, name, shape, dt, kind="Internal"):
        t = self.nc.dram_tensor(name, shape, dt, kind=kind).ap()
        return Buf(name, t, dram=True)

    @contextmanager
    def scope(self):
        st = ExitStack()
        self.stacks.append(st)
        self.scope_bufs.append([])
        try:
            yield
        finally:
            self.barrier()
            for b in self.scope_bufs.pop():
                if b.dsem is not None:
                    self.free_dsems.append((b.dsem, b.dval, b.dkey))
                    b.dsem = None
            self.stacks.pop()
            st.close()

    def _wait(self, e, sem, val, key):
        if self.waited[e].get(key, 0) >= val:
            return
        self.waited[e][key] = val
        self.eng[e].wait_ge(sem, val)

    def _deps(self, e, reads, writes, partial):
        for b in reads:
            for key, (sem, val) in b.writers.items():
                if e == 'pe' and key == 'pe':
                    continue
                self._wait(e, sem, val, key)
            if b.psum:
                for key, (sem, val) in b.readers.items():
                    if key != e:
                        self._wait(e, sem, val, key)
        for b in writes:
            if not partial:
                for key, (sem, val) in b.writers.items():
                    if e == 'pe' and key == 'pe':
                        continue
                    self._wait(e, sem, val, key)
            for key, (sem, val) in b.readers.items():
                if key == e:
                    continue
                self._wait(e, sem, val, key)

    def _mark(self, sem, val, key, reads, writes, partial):
        for b in reads:
            b.readers[key] = (sem, val)
        for b in writes:
            if partial:
                b.writers[key] = (sem, val)
            else:
                b.writers = {key: (sem, val)}

    def op(self, e, fn, reads=(), writes=(), partial=False):
        self._deps(e, reads, writes, partial)
        ins = fn(self.eng[e])
        self.cnt[e] += 1
        ins.then_inc(self.sem[e], 1)
        self._mark(self.sem[e], self.cnt[e], e, reads, writes, partial)
        return ins

    def dma(self, q, out_ap, in_ap, reads=(), writes=(), owner=None, partial=False, **kw):
        if owner is None:
            owner = [b for b in list(writes) + list(reads) if not b.dram][0]
        if owner.dsem is None:
            if self.free_dsems:
                owner.dsem, owner.dval, owner.dkey = self.free_dsems.pop()
            else:
                self.uid += 1
                owner.dkey = 'dsem%d' % self.uid
                owner.dsem = self.root.enter_context(self.nc.semaphore(owner.dkey))
        self._deps(q, reads, writes, partial)
        if owner.dval > 0:
            self._wait(q, owner.dsem, owner.dval, owner.dkey)
        ins = self.eng[q].dma_start(out=out_ap, in_=in_ap, **kw)
        owner.dval += 16
        ins.then_inc(owner.dsem, 16)
        self._mark(owner.dsem, owner.dval, owner.dkey, reads, writes, partial)
        self.dma_latest[owner.dkey] = (owner.dsem, owner.dval)
        return ins

    def idma(self, out_ap, in_ap, idx_ap, scatter, reads, writes, owner, partial=True):
        q = 'pool'
        if owner.dsem is None:
            if self.free_dsems:
                owner.dsem, owner.dval, owner.dkey = self.free_dsems.pop()
            else:
                self.uid += 1
                owner.dkey = 'dsem%d' % self.uid
                owner.dsem = self.root.enter_context(self.nc.semaphore(owner.dkey))
        self._deps(q, reads, writes, partial)
        if owner.dval > 0:
            self._wait(q, owner.dsem, owner.dval, owner.dkey)
        off = bass.IndirectOffsetOnAxis(ap=idx_ap, axis=0)
        if scatter:
            ins = self.nc.gpsimd.indirect_dma_start(out=out_ap, out_offset=off, in_=in_ap, in_offset=None)
        else:
            ins = self.nc.gpsimd.indirect_dma_start(out=out_ap, out_offset=None, in_=in_ap, in_offset=off)
        owner.dval += 16
        ins.then_inc(owner.dsem, 16)
        self._mark(owner.dsem, owner.dval, owner.dkey, reads, writes, partial)
        self.dma_latest[owner.dkey] = (owner.dsem, owner.dval)
        return ins

    def barrier(self):
        for e in self.eng:
            for e2 in self.eng:
                if e2 != e and self.cnt[e2] > 0:
                    self._wait(e, self.sem[e2], self.cnt[e2], e2)
            for key, (sem, val) in self.dma_latest.items():
                self._wait(e, sem, val, key)

    def mm(self, out, lhsT, rhs, start, stop, reads, writes):
        return self.op('pe', lambda e: e.matmul(out, lhsT, rhs, start=start, stop=stop),
                       reads=reads, writes=writes, partial=not start)

    def tr(self, out, in_, ident, reads, writes, partial=True):
        return self.op('pe', lambda e: e.transpose(out, in_, ident), reads=reads, writes=writes, partial=partial)

    def act(self, out, in_, func, reads, writes, bias=None, scale=None, accum_out=None, partial=False, eng='act'):
        kw = {}
        if bias is not None: kw['bias'] = bias
        if scale is not None: kw['scale'] = scale
        if accum_out is not None: kw['accum_out'] = accum_out
        return self.op(eng, lambda e: e.activation(out=out, in_=in_, func=func, **kw),
                       reads=reads, writes=writes, partial=partial)

    def tt(self, out, in0, in1, op, reads, writes, partial=False, eng='dve'):
        return self.op(eng, lambda e: e.tensor_tensor(out=out, in0=in0, in1=in1, op=op),
                       reads=reads, writes=writes, partial=partial)

    def ts(self, out, in0, s1, op0, reads, writes, s2=None, op1=None, partial=False, eng='dve'):
        if op1 is None:
            return self.op(eng, lambda e: e.tensor_scalar(out=out, in0=in0, scalar1=s1, scalar2=None, op0=op0),
                           reads=reads, writes=writes, partial=partial)
        return self.op(eng, lambda e: e.tensor_scalar(out=out, in0=in0, scalar1=s1, scalar2=s2, op0=op0, op1=op1),
                       reads=reads, writes=writes, partial=partial)

    def stt(self, out, in0, scalar, in1, op0, op1, reads, writes, partial=False):
        return self.op('dve', lambda e: e.scalar_tensor_tensor(out=out, in0=in0, scalar=scalar, in1=in1, op0=op0, op1=op1),
                       reads=reads, writes=writes, partial=partial)

    def copy(self, out, in_, reads, writes, partial=False, eng='dve'):
        return self.op(eng, lambda e: e.tensor_copy(out=out, in_=in_), reads=reads, writes=writes, partial=partial)

    def recip(self, out, in_, reads, writes, partial=False):
        return self.op('dve', lambda e: e.reciprocal(out=out, in_=in_), reads=reads, writes=writes, partial=partial)

    def memset(self, ap, val, writes, eng='dve', partial=False, reads=()):
        return self.op(eng, lambda e: e.memset(ap, val), reads=reads, writes=writes, partial=partial)


def load_cast(k, dst, dst_ap_fn, src_ap_fn, ncols, step=2048):
    c = 0
    while c < ncols:
        n = min(step, ncols - c)
        k.dma('pool', dst_ap_fn(c, n), src_ap_fn(c, n), writes=[dst], partial=True)
        c += n


class Ctx:
    pass


def setup_common(k, A):
    X = Ctx()
    X.banks = [k.ps('bank%d' % i, [128, 512], F32) for i in range(8)]
    for b in X.banks:
        b.bf = b.ap.bitcast(BF16)
    X.ones_bf = k.sb('ones_bf', [128, 128], BF16)
    k.memset(X.ones_bf[:], 1.0, [X.ones_bf])
    X.id_f = k.sb('id_f', [128, 128], F32)
    k.dma('sp', X.id_f[:], A['ident'][:, :], writes=[X.id_f])
    X.id_bf = k.sb('id_bf', [128, 128], BF16)
    k.copy(X.id_bf[:], X.id_f[:], [X.id_f], [X.id_bf])
    return X


def rope_tables(k, W, pos_ap, invcol, cosb, sinb):
    k.dma('sp', W.posi[:], pos_ap.to_broadcast([128, TT]), writes=[W.posi])
    k.copy(W.ang[:], W.posi[:], [W.posi], [W.ang])
    k.ts(W.ang[:], W.ang[:], invcol, ALU.mult, [W.ang], [W.ang])
    k.ts(W.kq[:], W.ang[:], 1.0 / TWO_PI, ALU.mult, [W.ang], [W.kq])
    k.copy(W.kf[:], W.kq[:], [W.kq], [W.kf])
    k.stt(W.r1[:], W.kf[:], -CW1, W.ang[:], ALU.mult, ALU.add, [W.kf, W.ang], [W.r1])
    k.stt(W.r1[:], W.kf[:], -CW2, W.r1[:], ALU.mult, ALU.add, [W.kf, W.r1], [W.r1])
    k.ts(W.r1[:], W.r1[:], PI_LO, ALU.min, [W.r1], [W.r1], s2=-PI_LO, op1=ALU.max)
    k.act(sinb[:], W.r1[:], AF.Sin, [W.r1], [sinb])
    k.stt(W.kf[:], W.r1[:], -1.0, W.r1[:], ALU.mult, ALU.max, [W.r1], [W.kf])
    k.act(cosb[:], W.kf[:], AF.Sin, [W.kf], [cosb], bias=W.halfpi[:, 0:1], scale=-1.0)


def rope_work(k):
    W = Ctx()
    W.posi = k.sb('posi', [128, TT], I32)
    W.ang = k.sb('ang', [128, TT], F32)
    W.kq = k.sb('kq', [128, TT], I32)
    W.kf = k.sb('kf', [128, TT], F32)
    W.r1 = k.sb('r1', [128, TT], F32)
    W.halfpi = k.sb('halfpi', [128, 1], F32)
    k.memset(W.halfpi[:], math.pi / 2.0, [W.halfpi])
    return W


def emit_mod(k, X, A, modT):
    with k.scope():
        cc = k.sb('cc', [128, 8], F32)
        k.dma('sp', cc[:], A['cc8'][:, :], writes=[cc])
        cact = k.sb('cact', [128, 8], F32)
        k.act(cact[:], cc[:], AF.Silu, [cc], [cact])
        bada = k.sb('bada', [128, 48], F32)
        k.dma('sp', bada[:], A['b_ada8'][:, :], writes=[bada])
        wv = A['w_ada'].rearrange("(kc p) n -> p kc n", p=128)
        wb = [k.sb('wada%d' % i, [128, 8, 768], F32) for i in range(2)]
        pm = X.banks[0]
        for blk in range(8):
            w = wb[blk % 2]
            for kc in range(8):
                k.dma('sp', w[:, kc, :], wv[:, kc, blk * 768:(blk + 1) * 768], writes=[w], partial=(kc > 0))
            for j in range(6):
                jj = blk * 6 + j
                for kc in range(8):
                    k.mm(pm[:, jj:jj + 1], w[:, kc, j * 128:(j + 1) * 128], cact[:, kc:kc + 1],
                         kc == 0, kc == 7, [w, cact], [pm])
        k.tt(modT[:], pm[:, 0:48], bada[:], ALU.add, [pm, bada], [modT])


def emit_mixer(k, X, A, modT, V_sb, write_ht, phases=('A', 'B', 'S2')):
    if True:
        banks = X.banks
        rot = Rot(banks)
        gmix = k.sb('gmix', [128, 8], F32)
        k.dma('sp', gmix[:], A['gmix8'], writes=[gmix])
        gsc = k.sb('gsc', [128, 8], F32)
        k.stt(gsc[:], modT[:, 8:16], 1.0, gmix[:], ALU.add, ALU.mult, [modT, gmix], [gsc])
        invf = k.sb('invf', [128, 2], F32)
        k.dma('sp', invf[:], A['invf'], writes=[invf])
        prot_f = k.sb('prot_f', [128, 2, 128], F32)
        k.dma('sp', prot_f[:], A['prot'], writes=[prot_f])
        prot = k.sb('prot', [128, 2, 128], BF16)
        k.copy(prot[:], prot_f[:], [prot_f], [prot])
        HT = A['HT_buf']; ZT = A['ZT_buf']; QTd = A['QTd_buf']; KTd = A['KTd_buf']
        HTv = A['HT'].rearrange("(kc p) c -> p kc c", p=128)
        xTv = A['xT'].rearrange("(kc p) c -> p kc c", p=128)

        if 'A' in phases:
          with k.scope():
            NA = 1504
            w_a = k.sb('w_a', [128, 8, NA], BF16)
            wav = A['w_a'].rearrange("(kc p) n -> p kc n", p=128)
            for kc in range(8):
                load_cast(k, w_a, lambda c, n: w_a[:, kc, c:c + n], lambda c, n: wav[:, kc, c:c + n], NA, step=752)
            st_f = k.sb('st_f', [128, 3, 384], F32)
            qg = k.sb('qg', [128, 3], F32); kvg = k.sb('kvg', [128, 2], F32)
            k.dma('sp', qg[:], A['qg3'], writes=[qg]); k.dma('sp', kvg[:], A['kvg2'], writes=[kvg])
            wuq = k.sb('wuq', [128, 3, 384], BF16)
            k.dma('sp', st_f[:], A['wuq'].rearrange("(rc p) n -> p rc n", p=128), writes=[st_f])
            for rc in range(3):
                k.ts(wuq[:, rc, :], st_f[:, rc, :], qg[:, rc:rc + 1], ALU.mult, [st_f, qg], [wuq], partial=True)
            wuk = k.sb('wuk', [128, 2, 256], BF16); wuv = k.sb('wuv', [128, 2, 256], BF16)
            st2 = k.sb('st2', [128, 2, 256], F32); st3 = k.sb('st3', [128, 2, 256], F32)
            k.dma('sp', st2[:], A['wuk'].rearrange("(rc p) n -> p rc n", p=128), writes=[st2])
            k.dma('sp', st3[:], A['wuv'].rearrange("(rc p) n -> p rc n", p=128), writes=[st3])
            for rc in range(2):
                k.ts(wuk[:, rc, :], st2[:, rc, :], kvg[:, rc:rc + 1], ALU.mult, [st2, kvg], [wuk], partial=True)
                k.ts(wuv[:, rc, :], st3[:, rc, :], kvg[:, rc:rc + 1], ALU.mult, [st3, kvg], [wuv], partial=True)
            cw = k.sb('cw', [128, 2, 3], F32)
            k.dma('sp', cw[:], A['convw'], writes=[cw])
            xts = [k.sb('xt%d' % i, [128, 8, TT], F32) for i in range(2)]
            sq = k.sb('sq', [128, 8, TT], BF16)
            hTa = [k.sb('hT%d' % i, [128, 8, TT], BF16) for i in range(2)]
            rtmp = k.sb('rtmp', [128, TT], F32)
            rstd = k.sb('rstd', [128, TT], F32)
            hx = [k.sb('hx%d' % i, [128, TT], F32) for i in range(2)]
            lat_f = k.sb('lat_f', [128, 5, TT], F32)
            sql = k.sb('sql', [128, 5, TT], BF16)
            rq_bc = k.sb('rq_bc', [128, TT], F32); rkv_bc = k.sb('rkv_bc', [128, TT], F32)
            qn = k.sb('qn', [128, 3, TT], BF16); kvn = k.sb('kvn', [128, 2, TT], BF16)
            QTs = [k.sb('QTs%d' % i, [128, TT], BF16) for i in range(4)]
            KTs = [k.sb('KTs%d' % i, [128, TT], BF16) for i in range(4)]
            qr_f = k.sb('qr_f', [128, TT], F32)
            kr_f = k.sb('kr_f', [128, TT], F32); kr_b = k.sb('kr_b', [128, TT], BF16)
            kpe = k.sb('kpe', [128, TT], BF16)
            t1 = k.sb('t1', [128, TT], F32); t2 = k.sb('t2', [128, TT], F32)
            cosm = k.sb('cosm', [128, TT], F32); sinm = k.sb('sinm', [128, TT], F32)
            W = rope_work(k)
            u = [k.sb('u%d' % i, [128, TT + 2], F32) for i in range(2)]
            cxs = k.sb('cxs', [128, TT], F32); yv = k.sb('yv', [128, TT], F32)
            zc = [k.sb('zc%d' % i, [128, TT], BF16) for i in range(2)]
            for ch in range(2):
                k.memset(u[ch][:], 0.0, [u[ch]])

            def load_x(t):
                xt = xts[t % 2]
                for kc in range(8):
                    k.dma('sp', xt[:, kc, :], xTv[:, kc, t * TT:(t + 1) * TT], writes=[xt], partial=(kc > 0))

            load_x(0)
            for t in range(NT):
                cs = slice(t * TT, (t + 1) * TT)
                if t + 1 < NT:
                    load_x(t + 1)
                xt = xts[t % 2]
                hT = hTa[t % 2]
                for kc in range(8):
                    k.act(sq[:, kc, :], xt[:, kc, :], AF.Square, [xt], [sq], partial=(kc > 0))
                pss = rot.next()
                for kc in range(8):
                    k.mm(pss[:, :], X.ones_bf[:, :], sq[:, kc, :], kc == 0, kc == 7, [X.ones_bf, sq], [pss])
                k.act(rtmp[:], pss[:, :], AF.Sqrt, [pss], [rtmp], bias=EPS, scale=1.0 / D)
                k.recip(rstd[:], rtmp[:], [rtmp], [rstd])
                for kc in range(8):
                    hb = hx[kc % 2]
                    k.stt(hb[:], xt[:, kc, :], gsc[:, kc:kc + 1], rstd[:], ALU.mult, ALU.mult, [xt, gsc, rstd], [hb])
                    k.act(hT[:, kc, :], hb[:], AF.Identity, [hb, modT], [hT], bias=modT[:, kc:kc + 1], partial=(kc > 0))
                if write_ht:
                    k.dma('pool', HTv[:, :, cs], hT[:], reads=[hT], writes=[HT], partial=True)
                rope_tables(k, W, A['pos'][0:1, cs], invf[:, 1:2], cosm, sinm)
                for j in range(5):
                    ps = rot.next()
                    for kc in range(8):
                        k.mm(ps[:, :], w_a[:, kc, j * 128:(j + 1) * 128], hT[:, kc, :], kc == 0, kc == 7, [w_a, hT], [ps])
                    k.act(lat_f[:, j, :], ps[:, :], AF.Copy, [ps], [lat_f], partial=(j > 0))
                    k.act(sql[:, j, :], ps[:, :], AF.Square, [ps], [sql], partial=(j > 0))
                for ch in range(2):
                    pb = rot.next(); pc = rot.next(); px = rot.next()
                    for (pp, base) in ((pb, 736), (pc, 992), (px, 1248)):
                        for kc in range(8):
                            k.mm(pp[:, :], w_a[:, kc, base + ch * 128: base + (ch + 1) * 128], hT[:, kc, :],
                                 kc == 0, kc == 7, [w_a, hT], [pp])
                    k.act(cxs[:], px[:, :], AF.Copy, [px], [cxs])
                    U = u[ch]
                    if t > 0:
                        k.copy(U[:, 0:2], U[:, TT:TT + 2], [U], [U])
                    k.tt(U[:, 2:TT + 2], pc[:, :], cxs[:], ALU.mult, [pc, cxs, U], [U])
                    k.ts(yv[:], U[:, 2:TT + 2], cw[:, ch, 2:3], ALU.mult, [U, cw], [yv])
                    k.stt(yv[:], U[:, 1:TT + 1], cw[:, ch, 1:2], yv[:], ALU.mult, ALU.add, [U, cw, yv], [yv])
                    k.stt(yv[:], U[:, 0:TT], cw[:, ch, 0:1], yv[:], ALU.mult, ALU.add, [U, cw, yv], [yv])
                    Z = zc[ch]
                    k.tt(Z[:], pb[:, :], yv[:], ALU.mult, [pb, yv], [Z])
                    k.dma('pool', A['ZTc'][ch * 128:(ch + 1) * 128, cs], Z[:], reads=[Z], writes=[ZT], partial=True)
                for (j0, j1, n, dst) in ((0, 3, 384.0, rq_bc), (3, 5, 256.0, rkv_bc)):
                    ps = rot.next()
                    for j in range(j0, j1):
                        k.mm(ps[:, :], X.ones_bf[:, :], sql[:, j, :], j == j0, j == j1 - 1, [X.ones_bf, sql], [ps])
                    k.act(rtmp[:], ps[:, :], AF.Sqrt, [ps], [rtmp], bias=EPS, scale=1.0 / n)
                    k.recip(dst[:], rtmp[:], [rtmp], [dst])
                for j in range(3):
                    k.tt(qn[:, j, :], lat_f[:, j, :], rq_bc[:], ALU.mult, [lat_f, rq_bc], [qn], partial=(j > 0))
                for j in range(2):
                    k.tt(kvn[:, j, :], lat_f[:, 3 + j, :], rkv_bc[:], ALU.mult, [lat_f, rkv_bc], [kvn], partial=(j > 0))
                ps = rot.next()
                for kc in range(8):
                    k.mm(ps[0:96, :], w_a[:, kc, 640:736], hT[:, kc, :], kc == 0, kc == 7, [w_a, hT], [ps])
                k.act(kr_f[64:96, :], ps[64:96, :], AF.Copy, [ps], [kr_f])
                k.act(kr_b[0:96, :], ps[0:96, :], AF.Copy, [ps], [kr_b])
                ps2 = rot.next()
                k.mm(ps2[0:96, :], prot[0:96, 1, 0:96], kr_b[0:96, :], True, True, [prot, kr_b], [ps2])
                k.tt(t1[64:96, :], kr_f[64:96, :], cosm[64:96, :], ALU.mult, [kr_f, cosm], [t1])
                k.tt(t2[64:96, :], ps2[64:96, :], sinm[64:96, :], ALU.mult, [ps2, sinm], [t2])
                k.tt(kpe[64:96, :], t1[64:96, :], t2[64:96, :], ALU.add, [t1, t2], [kpe])
                for h in range(4):
                    k.dma('pool', A['KTd'][h, 64:96, cs], kpe[64:96, :], reads=[kpe], writes=[KTd], partial=True)
                for h in range(4):
                    ps = rot.next()
                    for rc in range(3):
                        k.mm(ps[0:96, :], wuq[:, rc, h * 96:(h + 1) * 96], qn[:, rc, :], rc == 0, rc == 2, [wuq, qn], [ps])
                    Q = QTs[h]
                    k.act(Q[0:96, :], ps[0:96, :], AF.Copy, [ps], [Q], scale=MLA_SCALE)
                    k.act(qr_f[64:96, :], ps[64:96, :], AF.Copy, [ps], [qr_f], scale=MLA_SCALE)
                    ps2 = rot.next()
                    k.mm(ps2[0:96, :], prot[0:96, 1, 0:96], Q[0:96, :], True, True, [prot, Q], [ps2])
                    k.tt(t1[64:96, :], qr_f[64:96, :], cosm[64:96, :], ALU.mult, [qr_f, cosm], [t1])
                    k.tt(t2[64:96, :], ps2[64:96, :], sinm[64:96, :], ALU.mult, [ps2, sinm], [t2])
                    k.tt(Q[64:96, :], t1[64:96, :], t2[64:96, :], ALU.add, [t1, t2], [Q])
                    k.dma('pool', A['QTd'][h, :, cs], Q[0:96, :], reads=[Q], writes=[QTd], partial=True)
                for h in range(4):
                    ps = rot.next()
                    for rc in range(2):
                        k.mm(ps[0:64, :], wuk[:, rc, h * 64:(h + 1) * 64], kvn[:, rc, :], rc == 0, rc == 1, [wuk, kvn], [ps])
                    Kt = KTs[h]
                    k.act(Kt[0:64, :], ps[0:64, :], AF.Copy, [ps], [Kt])
                    k.dma('pool', A['KTd'][h, 0:64, cs], Kt[0:64, :], reads=[Kt], writes=[KTd], partial=True)
                for blk in range(4):
                    ps = rot.next()
                    for rc in range(2):
                        k.mm(ps[:, 0:256], kvn[:, rc, blk * 128:(blk + 1) * 128], wuv[:, rc, :], rc == 0, rc == 1, [kvn, wuv], [ps])
                    k.act(V_sb[:, t * 4 + blk, :, 0:64], ps[:, 0:256].rearrange("p (h v) -> p h v", h=4), AF.Copy,
                          [ps], [V_sb], partial=True)

        if 'B' in phases:
          with k.scope():
            NB = 1536
            w_b = k.sb('w_b', [128, 8, NB], BF16)
            wbv = A['w_b'].rearrange("(kc p) n -> p kc n", p=128)
            for kc in range(8):
                load_cast(k, w_b, lambda c, n: w_b[:, kc, c:c + n], lambda c, n: wbv[:, kc, c:c + n], NB, step=768)
            hTs = [k.sb('hTb%d' % i, [128, 8, TT], BF16) for i in range(2)]
            rmask = k.sb('rmask', [128, 2, 128], F32)
            k.dma('sp', rmask[:], A['rmask'], writes=[rmask])
            qdec = k.sb('qdec', [128, 2, TT], F32)
            k.dma('sp', qdec[:], A['qdec'], writes=[qdec])
            kdec = k.sb('kdec', [128, 2], F32)
            k.dma('sp', kdec[:], A['kdec'], writes=[kdec])
            sdec = k.sb('sdec', [128, 2], F32)
            k.dma('sp', sdec[:], A['sdec'], writes=[sdec])
            cosr = k.sb('cosr', [128, TT], F32); sinr = k.sb('sinr', [128, TT], F32)
            W = rope_work(k)
            qf = k.sb('qf', [128, TT], F32); qb = k.sb('qb', [128, TT], BF16)
            t1 = k.sb('t1b', [128, TT], F32); t2 = k.sb('t2b', [128, TT], F32)
            RQT = [k.sb('RQT%d' % i, [128, TT], BF16) for i in range(2)]
            RQd = [k.sb('RQd%d' % i, [128, TT], BF16) for i in range(2)]
            RKT = [k.sb('RKT%d' % i, [128, TT], BF16) for i in range(2)]
            RKd = [k.sb('RKd%d' % i, [128, 4, 128], BF16) for i in range(2)]
            RVb = [k.sb('RV%d' % i, [128, 512], BF16) for i in range(4)]; Gb = [k.sb('G%d' % i, [128, 512], BF16) for i in range(4)]
            AT = [k.sb('AT%d' % i, [128, 128], BF16) for i in range(2)]
            st_f = [k.sb('stf%d' % i, [128, 256], F32) for i in range(2)]
            st_b = [k.sb('stb%d' % i, [128, 256], BF16) for i in range(2)]
            junk = k.sb('junk', [128, 256], BF16)
            ssq = k.sb('ssq', [128, 1], F32); sd = k.sb('sd', [128, 1], F32); rinv = k.sb('rinv', [128, 1], F32)
            zr = [k.sb('zr%d' % i, [128, 256], BF16) for i in range(2)]
            zT = [k.sb('zT%d' % i, [128, 2, TT], BF16) for i in range(2)]
            for hr in range(2):
                k.memset(st_f[hr][:], 0.0, [st_f[hr]]); k.memset(st_b[hr][:], 0.0, [st_b[hr]])

            def load_h(t):
                hb = hTs[t % 2]
                k.dma('sp', hb[:], HTv[:, :, t * TT:(t + 1) * TT], reads=[HT], writes=[hb])

            load_h(0)
            for t in range(NT):
                cs = slice(t * TT, (t + 1) * TT)
                if t + 1 < NT:
                    load_h(t + 1)
                hT = hTs[t % 2]
                rope_tables(k, W, A['pos'][0:1, cs], invf[:, 0:1], cosr, sinr)
                for hr in range(2):
                    ps = rot.next()
                    for kc in range(8):
                        k.mm(ps[:, :], w_b[:, kc, hr * 128:(hr + 1) * 128], hT[:, kc, :], kc == 0, kc == 7, [w_b, hT], [ps])
                    k.act(qf[:], ps[:, :], AF.Copy, [ps], [qf])
                    k.act(qb[:], ps[:, :], AF.Copy, [ps], [qb])
                    ps2 = rot.next()
                    k.mm(ps2[:, :], prot[:, 0, :], qb[:], True, True, [prot, qb], [ps2])
                    k.tt(t1[:], qf[:], cosr[:], ALU.mult, [qf, cosr], [t1])
                    k.tt(t2[:], ps2[:, :], sinr[:], ALU.mult, [ps2, sinr], [t2])
                    k.tt(t1[:], t1[:], t2[:], ALU.add, [t1, t2], [t1], eng='pool')
                    k.act(RQT[hr][:], t1[:], AF.Copy, [t1], [RQT[hr]])
                    k.tt(RQd[hr][:], t1[:], qdec[:, hr, :], ALU.mult, [t1, qdec], [RQd[hr]])
                    ps = rot.next()
                    for kc in range(8):
                        k.mm(ps[:, :], w_b[:, kc, 256 + hr * 128:256 + (hr + 1) * 128], hT[:, kc, :], kc == 0, kc == 7, [w_b, hT], [ps])
                    k.act(qf[:], ps[:, :], AF.Copy, [ps], [qf], scale=RET_KS)
                    k.act(qb[:], ps[:, :], AF.Copy, [ps], [qb], scale=RET_KS)
                    ps2 = rot.next()
                    k.mm(ps2[:, :], prot[:, 0, :], qb[:], True, True, [prot, qb], [ps2])
                    k.tt(t1[:], qf[:], cosr[:], ALU.mult, [qf, cosr], [t1])
                    k.tt(t2[:], ps2[:, :], sinr[:], ALU.mult, [ps2, sinr], [t2])
                    k.tt(RKT[hr][:], t1[:], t2[:], ALU.add, [t1, t2], [RKT[hr]])
                    pT = rot.next()
                    for blk in range(4):
                        k.tr(pT.bf[:, blk * 128:(blk + 1) * 128], RKT[hr][:, blk * 128:(blk + 1) * 128], X.id_bf[:, :],
                             [RKT[hr], X.id_bf], [pT], partial=(blk > 0))
                    for blk in range(4):
                        k.act(RKd[hr][:, blk, :], pT.bf[:, blk * 128:(blk + 1) * 128], AF.Copy, [pT, kdec], [RKd[hr]],
                              scale=kdec[:, hr:hr + 1], partial=(blk > 0))
                def proj_vg(blk):
                    ps = rot.next()
                    for kc in range(8):
                        k.mm(ps[:, :], hT[:, kc, blk * 128:(blk + 1) * 128], w_b[:, kc, 512:1024], kc == 0, kc == 7, [w_b, hT], [ps])
                    k.act(RVb[blk][:], ps[:, :], AF.Copy, [ps], [RVb[blk]])
                    ps = rot.next()
                    for kc in range(8):
                        k.mm(ps[:, :], hT[:, kc, blk * 128:(blk + 1) * 128], w_b[:, kc, 1024:1536], kc == 0, kc == 7, [w_b, hT], [ps])
                    k.act(Gb[blk][:], ps[:, :], AF.Silu, [ps], [Gb[blk]])

                proj_vg(0)
                for blk in range(4):
                    if blk + 1 < 4:
                        proj_vg(blk + 1)
                    RV = RVb[blk]; G = Gb[blk]
                    bs = slice(blk * 128, (blk + 1) * 128)
                    for hr in range(2):
                        vs = slice(hr * 256, (hr + 1) * 256)
                        pS = rot.next()
                        k.mm(pS[:, 0:128], RKT[hr][:, bs], RQT[hr][:, bs], True, True, [RKT[hr], RQT[hr]], [pS])
                        a = AT[hr]
                        k.tt(a[:], pS[:, 0:128], rmask[:, hr, :], ALU.mult, [pS, rmask], [a])
                        pO = rot.next()
                        k.mm(pO[:, 0:256], a[:], RV[:, vs], True, False, [a, RV], [pO])
                        k.mm(pO[:, 0:256], RQd[hr][:, bs], st_b[hr][:], False, True, [RQd[hr], st_b[hr]], [pO])
                        pN = rot.next()
                        k.mm(pN[:, 0:256], RKd[hr][:, blk, :], RV[:, vs], True, True, [RKd[hr], RV], [pN])
                        k.stt(st_f[hr][:], st_f[hr][:], sdec[:, hr:hr + 1], pN[:, 0:256], ALU.mult, ALU.add,
                              [st_f[hr], sdec, pN], [st_f[hr]])
                        k.act(st_b[hr][:], st_f[hr][:], AF.Copy, [st_f[hr]], [st_b[hr]])
                        k.act(junk[:], pO[:, 0:256], AF.Square, [pO], [junk, ssq], accum_out=ssq[:, 0:1])
                        k.act(sd[:], ssq[:], AF.Sqrt, [ssq], [sd], bias=EPS, scale=1.0 / 256.0)
                        k.recip(rinv[:], sd[:], [sd], [rinv])
                        z = zr[hr]
                        k.stt(z[:], pO[:, 0:256], rinv[:, 0:1], G[:, vs], ALU.mult, ALU.mult, [pO, rinv, G], [z])
                        pT = rot.next()
                        for vc in range(2):
                            k.tr(pT.bf[:, vc * 128:(vc + 1) * 128], z[:, vc * 128:(vc + 1) * 128], X.id_bf[:, :],
                                 [z, X.id_bf], [pT], partial=(vc > 0))
                        k.act(zT[hr][:, :, bs], pT.bf[:, 0:256].rearrange("p (v i) -> p v i", v=2), AF.Copy, [pT], [zT[hr]],
                              partial=True)
                for hr in range(2):
                    k.dma('pool', A['ZTr'][hr * 256:(hr + 1) * 256, cs].rearrange("(v p) c -> p v c", p=128),
                          zT[hr][:], reads=[zT[hr]], writes=[ZT], partial=True)

        if 'S2' in phases:
          with k.scope():
            kts = [k.sb('kt%d' % i, [128, S], BF16) for i in range(2)]
            qts = [k.sb('qt%d' % i, [128, TT], BF16) for i in range(3)]
            PTs = [k.sb('PT%d' % i, [128, TT], BF16) for i in range(3)]
            of = [k.sb('of%d' % i, [128, TT], F32) for i in range(2)]
            rb = k.sb('rb', [128, TT], F32)
            zm = [k.sb('zm%d' % i, [128, TT], BF16) for i in range(2)]
            sel = k.sb('sel', [128, 64], F32)
            for o_ in of:
                k.memset(o_[:], 0.0, [o_])
            k.dma('sp', sel[:], A['sel65'], writes=[sel])
            srot = Rot(banks[0:4]); orot = Rot(banks[4:6]); brot = Rot(banks[6:8])
            qi = 0
            for h in range(4):
                kt = kts[h % 2]
                for c4 in range(4):
                    k.dma('sp', kt[0:96, c4 * 2048:(c4 + 1) * 2048], A['KTd'][h, :, c4 * 2048:(c4 + 1) * 2048],
                          reads=[KTd], writes=[kt], partial=(c4 > 0))
                for qt in range(NT):
                    q = qts[qi % 3]; qi += 1
                    k.dma('sp', q[0:96, :], A['QTd'][h, :, qt * TT:(qt + 1) * TT], reads=[QTd], writes=[q])
                    nkb = 4 * qt + 4
                    O = orot.next()
                    pend = []

                    def emit_s(kb):
                        d = kb - 4 * qt
                        qlo = 0 if d < 0 else 128 * d
                        pS = srot.next()
                        k.mm(pS[:, qlo:TT], kt[0:96, kb * 128:(kb + 1) * 128], q[0:96, qlo:TT], True, True, [kt, q], [pS])
                        P = PTs[kb % 3]
                        k.act(P[:, qlo:TT], pS[:, qlo:TT], AF.Exp, [pS], [P])
                        if d >= 0:
                            k.memset(P[64:128, qlo:qlo + 64], 0.0, [P], eng='pool', partial=True, reads=[P])
                        return P, d, qlo

                    def emit_pv(kb, P, d, qlo):
                        k.mm(O[0:65, qlo:TT], V_sb[:, kb, h, :], P[:, qlo:TT], kb == 0, kb == nkb - 1, [V_sb, P], [O])

                    for kb in range(nkb):
                        pend.append((kb,) + emit_s(kb))
                        if len(pend) > 2:
                            a = pend.pop(0); emit_pv(*a)
                    while pend:
                        a = pend.pop(0); emit_pv(*a)
                    o = of[qt % 2]
                    k.act(o[0:65, :], O[0:65, :], AF.Copy, [O], [o], partial=True)
                    pB = brot.next()
                    k.mm(pB[0:64, :], sel[:, 0:64], o[:, :], True, True, [sel, o], [pB])
                    k.recip(rb[0:64, :], pB[0:64, :], [pB], [rb])
                    z = zm[qt % 2]
                    k.tt(z[0:64, :], o[0:64, :], rb[0:64, :], ALU.mult, [o, rb], [z])
                    k.dma('pool', A['ZTm'][h * 64:(h + 1) * 64, qt * TT:(qt + 1) * TT], z[0:64, :], reads=[z], writes=[ZT], partial=True)


OFF = {'q_lat': 0, 'kv_lat': 384, 'k_rope': 640, 'cb': 672, 'cc': 1184, 'cx': 1696, 'rq': 2208, 'rk': 2720,
       'rv': 3232, 'rg': 4256, 'gl': 5280}


def col8(v):
    return np.ascontiguousarray(v.reshape(-1, 128).T)


def consts_for(hh):
    C = {}
    C['ident'] = np.eye(128, dtype=np.float32)
    prot = np.zeros((128, 2, 128), np.float32)
    for r in range(64):
        prot[r + 64, 0, r] = -1.0
        prot[r, 0, r + 64] = 1.0
    for r in range(64, 80):
        prot[r + 16, 1, r] = -1.0
        prot[r, 1, r + 16] = 1.0
    C['prot'] = prot
    invf = np.zeros((128, 2), np.float32)
    inv_ret = (10000.0 ** (-np.arange(0, 128, 2, dtype=np.float32) / 128)).astype(np.float32)
    inv_mla = (10000.0 ** (-np.arange(0, 32, 2, dtype=np.float32) / 32)).astype(np.float32)
    for p in range(128):
        invf[p, 0] = inv_ret[p % 64]
    for p in range(64, 96):
        invf[p, 1] = inv_mla[(p - 64) % 16]
    C['invf'] = invf
    rmask = np.zeros((128, 2, 128), np.float64); qdec = np.zeros((128, 2, TT), np.float64)
    kdec = np.zeros((128, 2), np.float64); sdec = np.zeros((128, 2), np.float64)
    for hr in range(2):
        H = hh * 2 + hr
        g = 1.0 - 2.0 ** (-5.0 - H)
        for j in range(128):
            for i in range(128):
                cj, ci = j // 64, i // 64
                if cj == ci:
                    rmask[j, hr, i] = g ** abs(i - j)
                elif cj < ci:
                    rmask[j, hr, i] = g ** (i - j)
        qdec[:, hr, :] = (g ** ((np.arange(TT) % 128) + 1.0))[None, :]
        kdec[:, hr] = g ** (127.0 - np.arange(128))
        sdec[:, hr] = g ** 128.0
    C['rmask'] = rmask.astype(np.float32); C['qdec'] = qdec.astype(np.float32)
    C['kdec'] = kdec.astype(np.float32); C['sdec'] = sdec.astype(np.float32)
    sel = np.zeros((128, 64), np.float32); sel[64, :] = 1.0
    C['sel65'] = sel
    return C


def mixer_inputs(inp, l, b, hh, xT_b):
    w_in = inp['w_in'][l]
    m = dict(consts_for(hh))
    if xT_b is not None:
        m['xT'] = xT_b
    m['cc8'] = col8(inp['c'][b])
    m['b_ada8'] = col8(inp['b_ada'][l])
    m['gmix8'] = col8(inp['norm_mix_g'][l])
    wa = np.zeros((D, 1504), np.float32)
    wa[:, 0:640] = w_in[:, 0:640]
    wa[:, 704:736] = w_in[:, 640:672]
    for i, nm in enumerate(('cb', 'cc', 'cx')):
        wa[:, 736 + i * 256:736 + (i + 1) * 256] = w_in[:, OFF[nm] + hh * 256:OFF[nm] + (hh + 1) * 256]
    m['w_a'] = wa
    wb = np.empty((D, 1536), np.float32)
    wb[:, 0:256] = w_in[:, OFF['rq'] + hh * 256:OFF['rq'] + (hh + 1) * 256]
    wb[:, 256:512] = w_in[:, OFF['rk'] + hh * 256:OFF['rk'] + (hh + 1) * 256]
    wb[:, 512:1024] = w_in[:, OFF['rv'] + hh * 512:OFF['rv'] + (hh + 1) * 512]
    wb[:, 1024:1536] = w_in[:, OFF['rg'] + hh * 512:OFF['rg'] + (hh + 1) * 512]
    m['w_b'] = wb
    m['pos'] = np.ascontiguousarray(inp['positions'][b:b + 1].astype(np.int32))
    m['qg3'] = col8(inp['mla_q_norm_g'][l]); m['kvg2'] = col8(inp['mla_kv_norm_g'][l])
    m['wuq'] = np.ascontiguousarray(inp['w_uq'][l][:, hh * 384:(hh + 1) * 384])
    wukv = inp['w_ukv'][l].reshape(256, 8, 128)[:, hh * 4:(hh + 1) * 4, :]
    m['wuk'] = np.ascontiguousarray(wukv[:, :, 0:64].reshape(256, 256))
    m['wuv'] = np.ascontiguousarray(wukv[:, :, 64:128].reshape(256, 256))
    cwv = inp['conv_w'][l][:, hh * 256:(hh + 1) * 256]
    m['convw'] = np.ascontiguousarray(cwv.reshape(3, 2, 128).transpose(2, 1, 0))
    return m


SO = 4096; NTO = SO // TT
BIG = 1.0e30


def emit_ffn(k, X, A, modT, last, tok):
    if True:
        banks = X.banks
        rot = Rot(banks[0:7])
        pL = banks[7]
        gffn = k.sb('gffn', [128, 8], F32)
        k.dma('sp', gffn[:], A['gffn8'], writes=[gffn])
        gsc2 = k.sb('gsc2', [128, 8], F32)
        k.stt(gsc2[:], modT[:, 32:40], 1.0, gffn[:], ALU.add, ALU.mult, [modT, gffn], [gsc2])
        X1T = A['X1T_buf']; H2K = A['H2K_buf']; HSb = A['HS_buf']; YSb = A['YS_buf']; XO = A['xo_buf']
        NBLK = SO // 128; TS = 256; NTILE = 64
        rc = k.sb('rconst', [128, 128 + 128 + 64 + 16 + 32 + 2], F32)
        k.dma('sp', rc[:], A['rconst'], writes=[rc])
        tri = rc[:, 0:128]; ones_f = rc[:, 128:256]; iota64 = rc[:, 256:320]; thr16 = rc[:, 320:336]
        iota32 = rc[:, 336:368]; pbase2 = rc[:, 368:370]
        e1s = k.sb('e1s', [128, NBLK], F32); e2s = k.sb('e2s', [128, NBLK], F32)
        r1s = k.sb('r1s', [128, NBLK], F32); r2s = k.sb('r2s', [128, NBLK], F32)
        w1s = k.sb('w1s', [128, NBLK], F32); w2s = k.sb('w2s', [128, NBLK], F32)
        run_bc = k.sb('run_bc', [128, 32], F32)
        k.memset(run_bc[:], 0.0, [run_bc])
        pos1i = k.sb('pos1i', [128, NBLK], I32); pos2i = k.sb('pos2i', [128, NBLK], I32)
        widx = k.sb('widx', [128, NTILE, 2], I32)
        HTb = A['HT_buf']; ZTb = A['ZT_buf']; XSb = A['xs_buf']
        xTv = A['xT'].rearrange("(kc p) c -> p kc c", p=128)
        HTv = A['HT'].rearrange("(kc p) c -> p kc c", p=128)
        ZTv = A['ZT'].rearrange("(kc p) c -> p kc c", p=128)
        X1v = A['X1T'].rearrange("(kc p) c -> p kc c", p=128)
        XOv = A['xoT'].rearrange("(kc p) c -> p kc c", p=128)

        with k.scope():
            w_g = k.sb('w_g', [128, 8, 3072], BF16)
            wgv = A['w_g'].rearrange("(kc p) n -> p kc n", p=128)
            for kc in range(8):
                load_cast(k, w_g, lambda c, n: w_g[:, kc, c:c + n], lambda c, n: wgv[:, kc, c:c + n], 3072, step=1024)
            w_o = k.sb('w_o', [128, 16, 1024], BF16)
            wov = A['w_o'].rearrange("(kc p) n -> p kc n", p=128)
            for kc in range(16):
                k.dma('pool', w_o[:, kc, :], wov[:, kc, :], writes=[w_o], partial=True)
            w_m = k.sb('w_m', [128, 8, 1024], BF16)
            wmv = A['w_mix'].rearrange("(kc p) n -> p kc n", p=128)
            for kc in range(8):
                k.dma('pool', w_m[:, kc, :], wmv[:, kc, :], writes=[w_m], partial=True)
            w_r = k.sb('w_r', [128, 8, 36], F32)
            k.dma('sp', w_r[:], A['w_r'].rearrange("(kc p) n -> p kc n", p=128), writes=[w_r])
            b_r = k.sb('b_r', [128, 36], F32)
            k.dma('sp', b_r[:], A['b_r'].to_broadcast([128, 36]), writes=[b_r])
            hT = k.sb('hTc', [128, 8, TT], BF16)
            Zt = k.sb('Zt', [128, 16, TT], BF16)
            xt = k.sb('xtc', [128, 8, TT], F32)
            gt0 = k.sb('gt0', [128, 3, TT], BF16); gt = [gt0, gt0]
            tA = k.sb('tA', [128, TT], F32); tB = k.sb('tB', [128, TT], F32)
            mg = k.sb('mg', [128, 8, TT], BF16)
            rtmp = k.sb('rtmpc', [128, TT], F32); rstd = k.sb('rstdc', [128, TT], F32)
            h2f0 = k.sb('h2f0', [128, TT], F32); h2f = [h2f0, h2f0]
            h2b = k.sb('h2b', [128, 8, TT], BF16)
            lt = tA
            lg = k.sb('lg', [128, 36], F32); gmax = k.sb('gmax', [128, 1], F32); ngmax = k.sb('ngmax', [128, 1], F32)
            g1h = k.sb('g1h', [128, 4], F32); pen = k.sb('pen', [128, 4], F32)
            ej = k.sb('ej', [128, 4], F32); gsum = k.sb('gsum', [128, 1], F32); gval = k.sb('gval', [128, 1], F32)
            lem = k.sb('lem', [128, 32], F32); top8 = k.sb('top8', [128, 8], F32)
            m1 = k.sb('m1', [128, 32], F32); m2 = k.sb('m2', [128, 32], F32)
            dd = k.sb('dd', [128, 1], F32); ee = k.sb('ee', [128, 1], F32); den = k.sb('den', [128, 1], F32)
            oh = k.sb('oh', [128, 32], F32); rk = k.sb('rk', [128, 32], F32); jk = k.sb('jk', [128, 32], F32)
            cT = tB
            for t in range(NTO):
                cs = slice(t * TT, (t + 1) * TT)
                tsl = tok('sp', t * TT, TT)
                k.dma('sp', hT[:], HTv[:, :, tsl], reads=[HTb], writes=[hT])
                k.dma('sp', Zt[:], ZTv[:, :, tsl], reads=[ZTb], writes=[Zt])
                for kc in range(8):
                    k.dma('sp', xt[:, kc, :], xTv[:, kc, tsl], reads=[XSb], writes=[xt], partial=(kc > 0))
                for dc in range(8):
                    ds_ = slice(dc * 128, (dc + 1) * 128)
                    g = gt[dc % 2]
                    for br in range(3):
                        ps = rot.next()
                        for kc in range(8):
                            k.mm(ps[:, :], w_g[:, kc, br * 1024 + dc * 128: br * 1024 + (dc + 1) * 128], hT[:, kc, :],
                                 kc == 0, kc == 7, [w_g, hT], [ps])
                        k.act(g[:, br, :], ps[:, :], AF.Sigmoid, [ps], [g], partial=(br > 0))
                    pys = []
                    for (k0, k1) in ((0, 4), (4, 8), (8, 16)):
                        ps = rot.next()
                        for kc in range(k0, k1):
                            k.mm(ps[:, :], w_o[:, kc, ds_], Zt[:, kc, :], kc == k0, kc == k1 - 1, [w_o, Zt], [ps])
                        pys.append(ps)
                    k.tt(tA[:], pys[0][:, :], g[:, 0, :], ALU.mult, [pys[0], g], [tA])
                    k.tt(tB[:], pys[1][:, :], g[:, 1, :], ALU.mult, [pys[1], g], [tB])
                    k.tt(tA[:], tA[:], tB[:], ALU.add, [tA, tB], [tA], eng='pool')
                    k.tt(tB[:], pys[2][:, :], g[:, 2, :], ALU.mult, [pys[2], g], [tB])
                    k.tt(mg[:, dc, :], tA[:], tB[:], ALU.add, [tA, tB], [mg], eng='pool', partial=(dc > 0))
                for dc in range(8):
                    ps = rot.next()
                    for kc in range(8):
                        k.mm(ps[:, :], w_m[:, kc, dc * 128:(dc + 1) * 128], mg[:, kc, :], kc == 0, kc == 7, [w_m, mg], [ps])
                    k.stt(xt[:, dc, :], ps[:, :], modT[:, 16 + dc:17 + dc], xt[:, dc, :], ALU.mult, ALU.add,
                          [ps, modT, xt], [xt], partial=True)
                k.dma('pool', X1v[:, :, cs], xt[:], reads=[xt], writes=[X1T], partial=True)
                for kc in range(8):
                    k.act(mg[:, kc, :], xt[:, kc, :], AF.Square, [xt], [mg], partial=(kc > 0))
                pss = rot.next()
                for kc in range(8):
                    k.mm(pss[:, :], X.ones_bf[:, :], mg[:, kc, :], kc == 0, kc == 7, [X.ones_bf, mg], [pss])
                k.act(rtmp[:], pss[:, :], AF.Sqrt, [pss], [rtmp], bias=EPS, scale=1.0 / D)
                k.recip(rstd[:], rtmp[:], [rtmp], [rstd])
                for kc in range(8):
                    hf = h2f[kc % 2]
                    k.stt(hf[:], xt[:, kc, :], gsc2[:, kc:kc + 1], rstd[:], ALU.mult, ALU.mult, [xt, gsc2, rstd], [hf])
                    k.act(hf[:], hf[:], AF.Identity, [hf, modT], [hf], bias=modT[:, 24 + kc:25 + kc])
                    k.copy(h2b[:, kc, :], hf[:], [hf], [h2b], eng='pool', partial=(kc > 0))
                    k.mm(pL[0:36, :], w_r[:, kc, :], hf[:], kc == 0, kc == 7, [w_r, hf], [pL])
                for blk in range(4):
                    pH = rot.next()
                    for kc in range(8):
                        k.tr(pH.bf[:, kc * 128:(kc + 1) * 128], h2b[:, kc, blk * 128:(blk + 1) * 128], X.id_bf[:, :],
                             [h2b, X.id_bf], [pH], partial=(kc > 0))
                    hk = gt0
                    k.act(hk[:, 0:2, :], pH.bf[:, :].rearrange("p (a b) -> p a b", a=2), AF.Copy, [pH], [hk])
                    k.dma('pool', A['H2K'][(t * 4 + blk) * 128:(t * 4 + blk + 1) * 128, :].rearrange("p (a b) -> p a b", a=2),
                          hk[:, 0:2, :], reads=[hk], writes=[H2K], partial=True)
                k.act(lt[0:36, :], pL[0:36, :], AF.Copy, [pL], [lt])
                pT = rot.next()
                for blk in range(4):
                    k.tr(pT[:, blk * 36:(blk + 1) * 36], lt[0:36, blk * 128:(blk + 1) * 128], X.id_f[0:36, 0:36],
                         [lt, X.id_f], [pT], partial=(blk > 0))
                for blk in range(4):
                    gb = t * 4 + blk
                    w1 = w1s[:, gb:gb + 1]; w2 = w2s[:, gb:gb + 1]
                    k.tt(lg[:], pT[:, blk * 36:(blk + 1) * 36], b_r[:], ALU.add, [pT, b_r], [lg])
                    k.op('dve', lambda e: e.reduce_max(out=gmax[:], in_=lg[:, 0:4], axis=mybir.AxisListType.X),
                         reads=[lg], writes=[gmax])
                    k.ts(g1h[:], lg[:, 0:4], gmax[:, 0:1], ALU.is_equal, [lg, gmax], [g1h])
                    k.ts(ngmax[:], gmax[:], -1.0, ALU.mult, [gmax], [ngmax])
                    k.act(ej[:], lg[:, 0:4], AF.Exp, [lg, ngmax], [ej, gsum], bias=ngmax[:, 0:1], accum_out=gsum[:, 0:1])
                    k.recip(gval[:], gsum[:], [gsum], [gval])
                    k.ts(pen[:], g1h[:], -1.0, ALU.add, [g1h], [pen], s2=BIG, op1=ALU.mult)
                    for g_ in range(4):
                        k.ts(lem[:, g_ * 8:(g_ + 1) * 8], lg[:, 4 + g_ * 8:4 + (g_ + 1) * 8], pen[:, g_:g_ + 1], ALU.add,
                             [lg, pen], [lem], partial=(g_ > 0))
                    k.op('dve', lambda e: e.max(out=top8[:], in_=lem[:]), reads=[lem], writes=[top8])
                    k.ts(m1[:], lem[:], top8[:, 0:1], ALU.is_equal, [lem, top8], [m1])
                    k.ts(m2[:], lem[:], top8[:, 1:2], ALU.is_equal, [lem, top8], [m2])
                    k.tt(dd[:], top8[:, 1:2], top8[:, 0:1], ALU.subtract, [top8], [dd])
                    k.act(ee[:], dd[:], AF.Exp, [dd], [ee])
                    k.ts(den[:], ee[:], 1.0, ALU.add, [ee], [den])
                    k.recip(den[:], den[:], [den], [den])
                    k.tt(w1, den[:], gval[:], ALU.mult, [den, gval], [w1s], partial=True)
                    k.tt(w2, w1, ee[:], ALU.mult, [w1s, ee], [w2s], partial=True)
                    k.tt(oh[:], m1[:], m2[:], ALU.add, [m1, m2], [oh])
                    pP = rot.next()
                    k.mm(pP[:, 0:32], tri, oh[:], True, True, [rc, oh], [pP])
                    k.tt(rk[:], pP[:, 0:32], run_bc[:], ALU.add, [pP, run_bc], [rk])
                    k.op('dve', lambda e: e.scalar_tensor_tensor(out=jk[:], in0=rk[:], scalar=1.0, in1=m1[:], op0=ALU.mult, op1=ALU.mult,
                                                              accum_out=r1s[:, gb:gb + 1]), reads=[rk, m1], writes=[jk, r1s], partial=True)
                    k.op('dve', lambda e: e.scalar_tensor_tensor(out=jk[:], in0=rk[:], scalar=1.0, in1=m2[:], op0=ALU.mult, op1=ALU.mult,
                                                              accum_out=r2s[:, gb:gb + 1]), reads=[rk, m2], writes=[jk, r2s], partial=True)
                    k.op('dve', lambda e: e.scalar_tensor_tensor(out=jk[:], in0=iota32, scalar=1.0, in1=m1[:], op0=ALU.mult, op1=ALU.mult,
                                                              accum_out=e1s[:, gb:gb + 1]), reads=[rc, m1], writes=[jk, e1s], partial=True)
                    k.op('dve', lambda e: e.scalar_tensor_tensor(out=jk[:], in0=iota32, scalar=1.0, in1=m2[:], op0=ALU.mult, op1=ALU.mult,
                                                              accum_out=e2s[:, gb:gb + 1]), reads=[rc, m2], writes=[jk, e2s], partial=True)
                    pQ = rot.next()
                    k.mm(pQ[:, 0:32], ones_f, oh[:], True, True, [rc, oh], [pQ])
                    k.tt(run_bc[:], run_bc[:], pQ[:, 0:32], ALU.add, [run_bc, pQ], [run_bc])
            ptile = k.sb('ptile', [128, 32], F32); cmp16 = k.sb('cmp16', [128, 16], F32)
            start = k.sb('start', [128, 32], F32); endt = k.sb('endt', [128, 32], F32); sstart = k.sb('sstart', [128, 32], F32)
            et = k.sb('et', [128, NTILE], F32); wf = k.sb('wf', [128, NTILE, 2], F32)
            p1f = k.sb('p1f', [128, NBLK], F32); p2f = k.sb('p2f', [128, NBLK], F32)
            for e_ in range(32):
                k.op('dve', lambda e: e.tensor_scalar(out=cmp16[:], in0=thr16, scalar1=run_bc[:, e_:e_ + 1], scalar2=None,
                                                      op0=ALU.is_lt, op1=ALU.add, accum_out=ptile[:, e_:e_ + 1]),
                     reads=[rc, run_bc], writes=[cmp16, ptile], partial=True)
            k.memset(start[:, 0:1], 0.0, [start], partial=True)
            for e_ in range(1, 32):
                k.tt(start[:, e_:e_ + 1], start[:, e_ - 1:e_], ptile[:, e_ - 1:e_], ALU.add, [start, ptile], [start], partial=True)
            k.tt(endt[:], start[:], ptile[:], ALU.add, [start, ptile], [endt])
            k.ts(sstart[:], start[:], float(TS), ALU.mult, [start], [sstart])
            k.memset(et[:], 0.0, [et])
            for e_ in range(32):
                k.stt(et[:], iota64, endt[:, e_:e_ + 1], et[:], ALU.is_ge, ALU.add, [rc, endt, et], [et])
            k.ts(et[:], et[:], 31.0, ALU.min, [et], [et])
            for j in range(2):
                k.ts(wf[:, :, j], et[:], 256.0, ALU.mult, [et, rc], [wf], s2=pbase2[:, j:j + 1], op1=ALU.add, partial=True)
            k.copy(widx[:], wf[:], [wf], [widx])
            for gb in range(NBLK):
                k.ts(m1[:], iota32, e1s[:, gb:gb + 1], ALU.is_equal, [rc, e1s], [m1])
                k.op('dve', lambda e: e.scalar_tensor_tensor(out=jk[:], in0=m1[:], scalar=1.0, in1=sstart[:], op0=ALU.mult, op1=ALU.mult,
                                                          accum_out=p1f[:, gb:gb + 1]), reads=[m1, sstart], writes=[jk, p1f], partial=True)
                k.ts(m2[:], iota32, e2s[:, gb:gb + 1], ALU.is_equal, [rc, e2s], [m2])
                k.op('dve', lambda e: e.scalar_tensor_tensor(out=jk[:], in0=m2[:], scalar=1.0, in1=sstart[:], op0=ALU.mult, op1=ALU.mult,
                                                          accum_out=p2f[:, gb:gb + 1]), reads=[m2, sstart], writes=[jk, p2f], partial=True)
            k.tt(p1f[:], p1f[:], r1s[:], ALU.add, [p1f, r1s], [p1f])
            k.tt(p2f[:], p2f[:], r2s[:], ALU.add, [p2f, r2s], [p2f])
            k.copy(pos1i[:], p1f[:], [p1f], [pos1i]); k.copy(pos2i[:], p2f[:], [p2f], [pos2i])

        if last:
            fin = k.sb('fin', [128, 8], F32)
            k.dma('sp', fin[:], A['fin8'], writes=[fin])
        with k.scope():
            hr_ = [k.sb('hr%d' % i, [128, 1024], BF16) for i in range(2)]
            for gb in range(NBLK):
                hb_ = hr_[gb % 2]
                k.dma('sp', hb_[:], A['H2K'][gb * 128:(gb + 1) * 128, :], reads=[H2K], writes=[hb_])
                k.idma(A['HS'][:, :], hb_[:, :], pos1i[:, gb:gb + 1], True, [hb_, pos1i], [HSb], hb_)
                k.idma(A['HS'][:, :], hb_[:, :], pos2i[:, gb:gb + 1], True, [hb_, pos2i], [HSb], hb_)
        with k.scope():
            wg = [k.sb('wg%d' % i, [128, 4096], BF16) for i in range(2)]
            wu = [k.sb('wu%d' % i, [128, 4096], BF16) for i in range(2)]
            wd = [k.sb('wd%d' % i, [128, 4096], BF16) for i in range(2)]
            hst = [k.sb('hst%d' % i, [128, 2, 1024], BF16) for i in range(2)]
            hTs = [k.sb('hTs%d' % i, [128, 8, TS], BF16) for i in range(2)]
            sg = [k.sb('sg%d' % i, [128, TS], F32) for i in range(2)]
            hid = [k.sb('hid%d' % i, [128, 4, TS], BF16) for i in range(2)]
            ys = [k.sb('ys%d' % i, [128, 2, 1024], F32) for i in range(2)]
            HSv = A['HS'].rearrange("(i sb p) n -> i p sb n", sb=2, p=128)
            YSv = A['YS'].rearrange("(i sb p) n -> i p sb n", sb=2, p=128)

            def load_w(i):
                bi = i % 2
                for (wt, src) in ((wg[bi], A['w_eg']), (wu[bi], A['w_eu']), (wd[bi], A['w_ed'])):
                    for j in range(2):
                        k.idma(wt[:, j * 2048:(j + 1) * 2048], src[:, :], widx[:, i, j:j + 1], False, [widx], [wt], wt, partial=(j > 0))

            load_w(0)
            for i in range(NTILE):
                if i + 1 < NTILE:
                    load_w(i + 1)
                bi = i % 2
                hs_ = hst[bi]; hT_ = hTs[bi]; hd = hid[bi]; y_ = ys[bi]
                k.dma('sp', hs_[:], HSv[i], reads=[HSb], writes=[hs_])
                for sb in range(2):
                    pH = rot.next()
                    for kc in range(8):
                        k.tr(pH.bf[:, kc * 128:(kc + 1) * 128], hs_[:, sb, kc * 128:(kc + 1) * 128], X.id_bf[:, :],
                             [hs_, X.id_bf], [pH], partial=(kc > 0))
                    k.act(hT_[:, :, sb * 128:(sb + 1) * 128], pH.bf[:, :].rearrange("p (kc s) -> p kc s", kc=8), AF.Copy,
                          [pH], [hT_], partial=(sb > 0))
                for fc in range(4):
                    pg = rot.next(); pu = rot.next()
                    for kc in range(8):
                        k.mm(pg[:, 0:TS], wg[bi][:, kc * 512 + fc * 128: kc * 512 + (fc + 1) * 128], hT_[:, kc, :], kc == 0, kc == 7, [wg[bi], hT_], [pg])
                    for kc in range(8):
                        k.mm(pu[:, 0:TS], wu[bi][:, kc * 512 + fc * 128: kc * 512 + (fc + 1) * 128], hT_[:, kc, :], kc == 0, kc == 7, [wu[bi], hT_], [pu])
                    s_ = sg[fc % 2]
                    k.act(s_[:], pg[:, 0:TS], AF.Silu, [pg], [s_])
                    k.tt(hd[:, fc, :], pu[:, 0:TS], s_[:], ALU.mult, [pu, s_], [hd], partial=(fc > 0))
                for sb in range(2):
                    for dh in range(2):
                        pd = rot.next()
                        for fc in range(4):
                            k.mm(pd[:, :], hd[:, fc, sb * 128:(sb + 1) * 128], wd[bi][:, fc * 1024 + dh * 512: fc * 1024 + (dh + 1) * 512],
                                 fc == 0, fc == 3, [hd, wd[bi]], [pd])
                        if (sb + dh) % 2 == 0:
                            k.act(y_[:, sb, dh * 512:(dh + 1) * 512], pd[:, :], AF.Copy, [pd], [y_], partial=True)
                        else:
                            k.copy(y_[:, sb, dh * 512:(dh + 1) * 512], pd[:, :], [pd], [y_], partial=True)
                k.dma('sp', YSv[i], y_[:], reads=[y_], writes=[YSb], partial=True)
        with k.scope():
            y1 = [k.sb('y1_%d' % i, [128, 1024], F32) for i in range(2)]
            y2 = [k.sb('y2_%d' % i, [128, 1024], F32) for i in range(2)]
            mo = k.sb('mo', [128, 4, 1024], F32)
            x1 = [k.sb('x1_%d' % i, [128, 8, TT], F32) for i in range(2)]
            sqf = k.sb('sqf', [128, 8, TT], BF16)
            rt2 = k.sb('rt2', [128, TT], F32); rs2 = k.sb('rs2', [128, TT], F32)
            for t in range(NTO):
                gs = slice(t * TT, (t + 1) * TT)
                xb = x1[t % 2]
                k.dma('sp', xb[:], X1v[:, :, gs], reads=[X1T], writes=[xb])
                for blk in range(4):
                    gb = t * 4 + blk
                    a1 = y1[blk % 2]; a2 = y2[blk % 2]
                    k.idma(a1[:, :], A['YS'][:, :], pos1i[:, gb:gb + 1], False, [YSb, pos1i], [a1], a1, partial=False)
                    k.idma(a2[:, :], A['YS'][:, :], pos2i[:, gb:gb + 1], False, [YSb, pos2i], [a2], a2, partial=False)
                    k.ts(mo[:, blk, :], a1[:], w1s[:, gb:gb + 1], ALU.mult, [a1, w1s], [mo], partial=(blk > 0))
                    k.stt(mo[:, blk, :], a2[:], w2s[:, gb:gb + 1], mo[:, blk, :], ALU.mult, ALU.add, [a2, w2s, mo], [mo], partial=True)
                for dc in range(8):
                    pM = rot.next()
                    for blk in range(4):
                        k.tr(pM[:, blk * 128:(blk + 1) * 128], mo[:, blk, dc * 128:(dc + 1) * 128], X.id_f[:, :],
                             [mo, X.id_f], [pM], partial=(blk > 0))
                    k.stt(xb[:, dc, :], pM[:, :], modT[:, 40 + dc:41 + dc], xb[:, dc, :], ALU.mult, ALU.add,
                          [pM, modT, xb], [xb], partial=True)
                if last:
                    for kc in range(8):
                        k.act(sqf[:, kc, :], xb[:, kc, :], AF.Square, [xb], [sqf], partial=(kc > 0))
                    pss = rot.next()
                    for kc in range(8):
                        k.mm(pss[:, :], X.ones_bf[:, :], sqf[:, kc, :], kc == 0, kc == 7, [X.ones_bf, sqf], [pss])
                    k.act(rt2[:], pss[:, :], AF.Sqrt, [pss], [rt2], bias=EPS, scale=1.0 / D)
                    k.recip(rs2[:], rt2[:], [rt2], [rs2])
                    for kc in range(8):
                        k.stt(xb[:, kc, :], xb[:, kc, :], fin[:, kc:kc + 1], rs2[:], ALU.mult, ALU.mult,
                              [xb, fin, rs2], [xb], partial=True)
                k.dma('pool', XOv[:, :, gs], xb[:], reads=[xb], writes=[XO], partial=True)


FUSED_IN = {
    'xT': ([D, S], F32), 'cc8': ([128, 8], F32), 'w_ada': ([2, D, 6 * D], F32), 'b_ada8': ([2, 128, 48], F32),
    'gmix8': ([2, 128, 8], F32), 'gffn8': ([2, 128, 8], F32), 'fin8': ([128, 8], F32),
    'w_a': ([2, 2, D, 1504], F32), 'w_b': ([2, 2, D, 1536], F32), 'pos': ([1, S], I32),
    'qg3': ([2, 128, 3], F32), 'kvg2': ([2, 128, 2], F32), 'wuq': ([2, 2, 384, 384], F32),
    'wuk': ([2, 2, 256, 256], F32), 'wuv': ([2, 2, 256, 256], F32), 'convw': ([2, 2, 128, 2, 3], F32),
    'ident': ([128, 128], F32), 'prot': ([128, 2, 128], F32), 'invf': ([128, 2], F32),
    'rmask': ([2, 128, 2, 128], F32), 'qdec': ([2, 128, 2, TT], F32), 'kdec': ([2, 128, 2], F32),
    'sdec': ([2, 128, 2], F32), 'sel65': ([128, 64], F32),
    'w_g': ([2, D, 3072], F32), 'w_o': ([2, 2048, D], F32), 'w_mix': ([2, D, D], F32), 'w_r': ([2, D, 36], F32),
    'b_r': ([2, 1, 36], F32), 'w_eg0': ([8192, 2048], F32), 'w_eu0': ([8192, 2048], F32), 'w_ed0': ([8192, 2048], F32),
    'w_eg1': ([8192, 2048], F32), 'w_eu1': ([8192, 2048], F32), 'w_ed1': ([8192, 2048], F32), 'rconst': ([128, 370], F32),
}


def build_fused(nc, A):
    root = ExitStack()
    with root:
        k = K(nc, root)
        X = setup_common(k, A)
        V_sb = k.sb('V_sb', [128, 64, 4, 65], BF16)
        k.memset(V_sb[:, :, :, 64:65], 1.0, [V_sb], eng='pool')
        bufs = {n: Buf(n, A[n], dram=True) for n in ('HT', 'ZT', 'QTd', 'KTd', 'XO', 'X1T', 'H2K', 'HS', 'YS', 'xT', 'out', 'HTo', 'ZTo', 'XOo')}
        modT = [k.sb('modT%d' % l, [128, 48], F32) for l in range(2)]
        for l in range(2):
            emit_mod(k, X, {'cc8': A['cc8'], 'b_ada8': A['b_ada8'][l], 'w_ada': A['w_ada'][l]}, modT[l])
        pid = nc.sync.partition_id()
        for l in range(2):
            xsrc, xsb = (A['xT'], bufs['xT']) if l == 0 else (A['XO'], bufs['XO'])
            for hh in range(2):
                Am = dict(gmix8=A['gmix8'][l], invf=A['invf'], prot=A['prot'], HT=A['HT'], xT=xsrc,
                          HT_buf=bufs['HT'], ZT_buf=bufs['ZT'], QTd_buf=bufs['QTd'], KTd_buf=bufs['KTd'],
                          QTd=A['QTd'], KTd=A['KTd'], w_a=A['w_a'][l, hh], w_b=A['w_b'][l, hh],
                          qg3=A['qg3'][l], kvg2=A['kvg2'][l], wuq=A['wuq'][l, hh], wuk=A['wuk'][l, hh], wuv=A['wuv'][l, hh],
                          convw=A['convw'][l, hh], pos=A['pos'], rmask=A['rmask'][hh], qdec=A['qdec'][hh],
                          kdec=A['kdec'][hh], sdec=A['sdec'][hh], sel65=A['sel65'],
                          ZTm=A['ZT'][hh * 256:(hh + 1) * 256, :], ZTc=A['ZT'][512 + hh * 256:512 + (hh + 1) * 256, :],
                          ZTr=A['ZT'][1024 + hh * 512:1024 + (hh + 1) * 512, :])
                with k.scope():
                    emit_mixer(k, X, Am, modT[l], V_sb, write_ht=(hh == 0))
            last = (l == 1)
            if last:
                stg = Buf('stage')
                hoff = pid % 2 * SO
                for (dst, src) in (('HTo', 'HT'), ('ZTo', 'ZT'), ('XOo', 'XO')):
                    k.dma('sp', A[dst][:, :], A[src][:, bass.ds(hoff, SO)], reads=[bufs[src]], writes=[bufs[dst]], owner=stg)
            for th in ((None,) if last else (0, 1)):
                if last:
                    tok = lambda q, c0, n: slice(c0, c0 + n)
                    xo = A['out']; xob = bufs['out']
                    srcs = dict(xs_buf=bufs['XOo'], xT=A['XOo'], HT=A['HTo'], ZT=A['ZTo'], HT_buf=bufs['HTo'], ZT_buf=bufs['ZTo'])
                else:
                    tok = (lambda th_: (lambda q, c0, n: slice(th_ * SO + c0, th_ * SO + c0 + n)))(th)
                    xo = A['XO'][:, th * SO:(th + 1) * SO]; xob = bufs['XO']
                    srcs = dict(xs_buf=xsb, xT=xsrc, HT=A['HT'], ZT=A['ZT'], HT_buf=bufs['HT'], ZT_buf=bufs['ZT'])
                Af = dict(gffn8=A['gffn8'][l], X1T_buf=bufs['X1T'], H2K_buf=bufs['H2K'], HS_buf=bufs['HS'], YS_buf=bufs['YS'], xo_buf=xob,
                          rconst=A['rconst'], H2K=A['H2K'], HS=A['HS'], YS=A['YS'],
                          X1T=A['X1T'], xoT=xo, w_g=A['w_g'][l], w_o=A['w_o'][l],
                          w_mix=A['w_mix'][l], w_r=A['w_r'][l], b_r=A['b_r'][l], w_eg=A['w_eg%d' % l], w_eu=A['w_eu%d' % l],
                          w_ed=A['w_ed%d' % l], fin8=A['fin8'], **srcs)
                with k.scope():
                    emit_ffn(k, X, Af, modT[l], last, tok)
        k.barrier()
    return nc


def make_fused_nc():
    nc = bass.Bass("TRN2", target_bir_lowering=False)
    A = {}
    for name, (shape, dt) in FUSED_IN.items():
        A[name] = nc.dram_tensor(name, shape, dt, kind="ExternalInput").ap()
    A['out'] = nc.dram_tensor('out', [D, SO], F32, kind="ExternalOutput").ap()
    for name, shape, dt in (('HT', [D, S], BF16), ('ZT', [2048, S], BF16), ('QTd', [4, 96, S], BF16),
                            ('KTd', [4, 96, S], BF16), ('XO', [D, S], F32), ('X1T', [D, SO], F32),
                            ('H2K', [SO, D], BF16), ('HS', [16384, D], BF16), ('YS', [16384, D], F32), ('HTo', [D, SO], BF16),
                            ('ZTo', [2048, SO], BF16), ('XOo', [D, SO], F32)):
        A[name] = nc.dram_tensor(name, shape, dt, kind="Internal").ap()
    build_fused(nc, A)
    return nc


def fused_inputs(inp, b):
    L = range(2)
    m = {}
    c0 = consts_for(0); c1 = consts_for(1)
    for kk in ('ident', 'prot', 'invf', 'sel65'):
        m[kk] = c0[kk]
    for kk in ('rmask', 'qdec', 'kdec', 'sdec'):
        m[kk] = np.stack([c0[kk], c1[kk]], axis=0)
    mi = [[mixer_inputs(inp, l, b, hh, None) for hh in range(2)] for l in L]
    m['xT'] = np.ascontiguousarray(inp['x'][b].T)
    m['cc8'] = mi[0][0]['cc8']; m['pos'] = mi[0][0]['pos']
    m['w_ada'] = np.ascontiguousarray(inp['w_ada'])
    for kk in ('b_ada8', 'gmix8', 'qg3', 'kvg2'):
        m[kk] = np.stack([mi[l][0][kk] for l in L], axis=0)
    for kk in ('w_a', 'w_b', 'wuq', 'wuk', 'wuv', 'convw'):
        m[kk] = np.stack([np.stack([mi[l][hh][kk] for hh in range(2)], axis=0) for l in L], axis=0)
    m['gffn8'] = np.stack([col8(inp['norm_ffn_g'][l]) for l in L], axis=0)
    m['fin8'] = col8(inp['final_g'])
    m['w_g'] = np.ascontiguousarray(inp['w_in'][:, :, OFF['gl']:OFF['gl'] + 3072])
    m['w_o'] = np.concatenate([inp['w_o_mla'], inp['w_o_conv'], inp['w_o_ret']], axis=1)
    m['w_mix'] = np.ascontiguousarray(inp['w_mix_out'])
    m['w_r'] = np.concatenate([inp['w_route_group'], inp['w_route_expert']], axis=2)
    m['b_r'] = np.concatenate([inp['b_route_group'], inp['b_route_expert']], axis=1)[:, None, :].astype(np.float32)
    for l in L:
        m['w_eg%d' % l] = np.ascontiguousarray(inp['w_exp_gate'][l].reshape(32, 8, 128, 512).transpose(0, 2, 1, 3)).reshape(8192, 2048)
        m['w_eu%d' % l] = np.ascontiguousarray(inp['w_exp_up'][l].reshape(32, 8, 128, 512).transpose(0, 2, 1, 3)).reshape(8192, 2048)
        m['w_ed%d' % l] = np.ascontiguousarray(inp['w_exp_down'][l].reshape(32, 4, 128, 1024).transpose(0, 2, 1, 3)).reshape(8192, 2048)
    rcst = np.zeros((128, 370), np.float32)
    rcst[:, 0:128] = np.triu(np.ones((128, 128), np.float32), 1)
    rcst[:, 128:256] = 1.0
    rcst[:, 256:320] = np.arange(64, dtype=np.float32)[None, :]
    rcst[:, 320:336] = (np.arange(16, dtype=np.float32) * 256.0)[None, :]
    rcst[:, 336:368] = np.arange(32, dtype=np.float32)[None, :]
    rcst[:, 368] = 2.0 * np.arange(128); rcst[:, 369] = 2.0 * np.arange(128) + 1.0
    m['rconst'] = rcst
    return m


_NC_CACHE = {}


def kernel(**inp):
    inp = {k_: np.asarray(v) for k_, v in inp.items()}
    B = inp['x'].shape[0]
    cores = list(range(8))
    if 'fused' not in _NC_CACHE:
        _NC_CACHE['fused'] = make_fused_nc()
    per_b = [fused_inputs(inp, b) for b in range(B)]
    maps = [per_b[c // 2] for c in cores]
    res = run_bass_kernel_spmd(_NC_CACHE['fused'], maps, core_ids=cores).results
    out = np.empty((B, S, D), np.float32)
    for c in cores:
        b, th = c // 2, c % 2
        out[b, th * SO:(th + 1) * SO, :] = np.asarray(res[c]['out']).T
    return out
```

```python
import math
from contextlib import ExitStack, contextmanager
import numpy as np
import ml_dtypes
import concourse.bass as bass
import concourse.mybir as mybir
from concourse.bass_utils import run_bass_kernel_spmd

F32 = mybir.dt.float32; BF16 = mybir.dt.bfloat16; I32 = mybir.dt.int32
ALU = mybir.AluOpType; AF = mybir.ActivationFunctionType

D = 1024; S = 8192; TT = 512; NT = S // TT
EPS = 1e-6
TWO_PI = 2.0 * math.pi
CW1 = 6.28125
CW2 = TWO_PI - CW1
PI_LO = 3.1415925
MLA_SCALE = 96.0 ** -0.5
RET_KS = 128.0 ** -0.5


class Buf:
    def __init__(self, name, ap=None, dram=False):
        self.name = name; self.ap = ap; self.dram = dram
        self.writers = {}; self.readers = {}
        self.dsem = None; self.dval = 0; self.dkey = None; self.psum = False

    def __getitem__(self, idx):
        return self.ap[idx]


class Rot:
    def __init__(self, bufs):
        self.bufs = bufs; self.i = 0

    def next(self):
        b = self.bufs[self.i % len(self.bufs)]; self.i += 1
        return b


class K:
    def __init__(self, nc, root):
        self.nc = nc; self.root = root; self.stacks = [root]
        self.eng = {'pe': nc.tensor, 'dve': nc.vector, 'act': nc.scalar, 'pool': nc.gpsimd, 'sp': nc.sync}
        self.sem = {}; self.cnt = {}
        for e in self.eng:
            self.sem[e] = root.enter_context(nc.semaphore('s_' + e)); self.cnt[e] = 0
        self.waited = {e: {} for e in self.eng}
        self.dma_latest = {}
        self.uid = 0
        self.free_dsems = []
        self.bound_regs = {}
        self.scope_bufs = [[]]

    def sb(self, name, shape, dt):
        self.uid += 1
        t = self.stacks[-1].enter_context(self.nc.sbuf_tensor('%s_%d' % (name, self.uid), shape, dt))
        b = Buf(name, t)
        self.scope_bufs[-1].append(b)
        return b

    def ps(self, name, shape, dt=F32):
        t = self.root.enter_context(self.nc.psum_tensor(name, shape, dt))
        b = Buf(name, t); b.psum = True
        return b

    def dram(self, name, shape, dt, kind="Internal"):
        t = self.nc.dram_tensor(name, shape, dt, kind=kind).ap()
        return Buf(name, t, dram=True)

    @contextmanager
    def scope(self):
        st = ExitStack()
        self.stacks.append(st)
        self.scope_bufs.append([])
        try:
            yield
        finally:
            self.barrier()
            for b in self.scope_bufs.pop():
                if b.dsem is not None:
                    self.free_dsems.append((b.dsem, b.dval, b.dkey))
                    b.dsem = None
            self.stacks.pop()
            st.close()

    def _wait(self, e, sem, val, key):
        if self.waited[e].get(key, 0) >= val:
            return
        self.waited[e][key] = val
        self.eng[e].wait_ge(sem, val)

    def _deps(self, e, reads, writes, partial):
        for b in reads:
            for key, (sem, val) in b.writers.items():
                if e == 'pe' and key == 'pe':
                    continue
                self._wait(e, sem, val, key)
            if b.psum:
                for key, (sem, val) in b.readers.items():
                    if key != e:
                        self._wait(e, sem, val, key)
        for b in writes:
            if not partial:
                for key, (sem, val) in b.writers.items():
                    if e == 'pe' and key == 'pe':
                        continue
                    self._wait(e, sem, val, key)
            for key, (sem, val) in b.readers.items():
                if key == e:
                    continue
                self._wait(e, sem, val, key)

    def _mark(self, sem, val, key, reads, writes, partial):
        for b in reads:
            b.readers[key] = (sem, val)
        for b in writes:
            if partial:
                b.writers[key] = (sem, val)
            else:
                b.writers = {key: (sem, val)}

    def op(self, e, fn, reads=(), writes=(), partial=False):
        self._deps(e, reads, writes, partial)
        ins = fn(self.eng[e])
        self.cnt[e] += 1
        ins.then_inc(self.sem[e], 1)
        self._mark(self.sem[e], self.cnt[e], e, reads, writes, partial)
        return ins

    def dma(self, q, out_ap, in_ap, reads=(), writes=(), owner=None, partial=False, **kw):
        if owner is None:
            owner = [b for b in list(writes) + list(reads) if not b.dram][0]
        if owner.dsem is None:
            if self.free_dsems:
                owner.dsem, owner.dval, owner.dkey = self.free_dsems.pop()
            else:
                self.uid += 1
                owner.dkey = 'dsem%d' % self.uid
                owner.dsem = self.root.enter_context(self.nc.semaphore(owner.dkey))
        self._deps(q, reads, writes, partial)
        if owner.dval > 0:
            self._wait(q, owner.dsem, owner.dval, owner.dkey)
        ins = self.eng[q].dma_start(out=out_ap, in_=in_ap, **kw)
        owner.dval += 16
        ins.then_inc(owner.dsem, 16)
        self._mark(owner.dsem, owner.dval, owner.dkey, reads, writes, partial)
        self.dma_latest[owner.dkey] = (owner.dsem, owner.dval)
        return ins

    def idma(self, out_ap, in_ap, idx_ap, scatter, reads, writes, owner, partial=True, bound=None):
        q = 'pool'
        if owner.dsem is None:
            if self.free_dsems:
                owner.dsem, owner.dval, owner.dkey = self.free_dsems.pop()
            else:
                self.uid += 1
                owner.dkey = 'dsem%d' % self.uid
                owner.dsem = self.root.enter_context(self.nc.semaphore(owner.dkey))
        self._deps(q, reads, writes, partial)
        if owner.dval > 0:
            self._wait(q, owner.dsem, owner.dval, owner.dkey)
        off = bass.IndirectOffsetOnAxis(ap=idx_ap, axis=0)
        if scatter:
            ins = self.nc.gpsimd.indirect_dma_start(out=out_ap, out_offset=off, in_=in_ap, in_offset=None)
        else:
            if bound is None:
                ins = self.nc.gpsimd.indirect_dma_start(out=out_ap, out_offset=None, in_=in_ap, in_offset=off)
            else:
                if bound not in self.bound_regs:
                    self.bound_regs[bound] = self.nc.gpsimd.to_reg(bound)
                ins = self.nc.gpsimd.indirect_dma_start(out=out_ap, out_offset=None, in_=in_ap, in_offset=off,
                                                        bounds_check=self.bound_regs[bound], oob_is_err=False)
        owner.dval += 16
        ins.then_inc(owner.dsem, 16)
        self._mark(owner.dsem, owner.dval, owner.dkey, reads, writes, partial)
        self.dma_latest[owner.dkey] = (owner.dsem, owner.dval)
        return ins

    def barrier(self):
        for e in self.eng:
            for e2 in self.eng:
                if e2 != e and self.cnt[e2] > 0:
                    self._wait(e, self.sem[e2], self.cnt[e2], e2)
            for key, (sem, val) in self.dma_latest.items():
                self._wait(e, sem, val, key)

    def mm(self, out, lhsT, rhs, start, stop, reads, writes):
        return self.op('pe', lambda e: e.matmul(out, lhsT, rhs, start=start, stop=stop),
                       reads=reads, writes=writes, partial=not start)

    def tr(self, out, in_, ident, reads, writes, partial=True):
        return self.op('pe', lambda e: e.transpose(out, in_, ident), reads=reads, writes=writes, partial=partial)

    def act(self, out, in_, func, reads, writes, bias=None, scale=None, accum_out=None, partial=False, eng='act'):
        kw = {}
        if bias is not None: kw['bias'] = bias
        if scale is not None: kw['scale'] = scale
        if accum_out is not None: kw['accum_out'] = accum_out
        return self.op(eng, lambda e: e.activation(out=out, in_=in_, func=func, **kw),
                       reads=reads, writes=writes, partial=partial)

    def tt(self, out, in0, in1, op, reads, writes, partial=False, eng='dve'):
        return self.op(eng, lambda e: e.tensor_tensor(out=out, in0=in0, in1=in1, op=op),
                       reads=reads, writes=writes, partial=partial)

    def ts(self, out, in0, s1, op0, reads, writes, s2=None, op1=None, partial=False, eng='dve'):
        if op1 is None:
            return self.op(eng, lambda e: e.tensor_scalar(out=out, in0=in0, scalar1=s1, scalar2=None, op0=op0),
                           reads=reads, writes=writes, partial=partial)
        return self.op(eng, lambda e: e.tensor_scalar(out=out, in0=in0, scalar1=s1, scalar2=s2, op0=op0, op1=op1),
                       reads=reads, writes=writes, partial=partial)

    def stt(self, out, in0, scalar, in1, op0, op1, reads, writes, partial=False):
        return self.op('dve', lambda e: e.scalar_tensor_tensor(out=out, in0=in0, scalar=scalar, in1=in1, op0=op0, op1=op1),
                       reads=reads, writes=writes, partial=partial)

    def copy(self, out, in_, reads, writes, partial=False, eng='dve'):
        return self.op(eng, lambda e: e.tensor_copy(out=out, in_=in_), reads=reads, writes=writes, partial=partial)

    def recip(self, out, in_, reads, writes, partial=False):
        return self.op('dve', lambda e: e.reciprocal(out=out, in_=in_), reads=reads, writes=writes, partial=partial)

    def memset(self, ap, val, writes, eng='dve', partial=False, reads=()):
        return self.op(eng, lambda e: e.memset(ap, val), reads=reads, writes=writes, partial=partial)


def load_cast(k, dst, dst_ap_fn, src_ap_fn, ncols, step=2048):
    c = 0
    while c < ncols:
        n = min(step, ncols - c)
        k.dma('pool', dst_ap_fn(c, n), src_ap_fn(c, n), writes=[dst], partial=True)
        c += n


class Ctx:
    pass


def setup_common(k, A):
    X = Ctx()
    X.banks = [k.ps('bank%d' % i, [128, 512], F32) for i in range(8)]
    for b in X.banks:
        b.bf = b.ap.bitcast(BF16)
    X.ones_bf = k.sb('ones_bf', [128, 128], BF16)
    k.memset(X.ones_bf[:], 1.0, [X.ones_bf])
    X.id_f = k.sb('id_f', [128, 128], F32)
    k.dma('sp', X.id_f[:], A['ident'][:, :], writes=[X.id_f])
    X.id_bf = k.sb('id_bf', [128, 128], BF16)
    k.copy(X.id_bf[:], X.id_f[:], [X.id_f], [X.id_bf])
    return X


def rope_tables(k, W, pos_ap, invcol, cosb, sinb):
    k.dma('sp', W.posi[:], pos_ap.to_broadcast([128, TT]), writes=[W.posi])
    k.copy(W.ang[:], W.posi[:], [W.posi], [W.ang])
    k.ts(W.ang[:], W.ang[:], invcol, ALU.mult, [W.ang], [W.ang])
    k.ts(W.kq[:], W.ang[:], 1.0 / TWO_PI, ALU.mult, [W.ang], [W.kq])
    k.copy(W.kf[:], W.kq[:], [W.kq], [W.kf])
    k.stt(W.r1[:], W.kf[:], -CW1, W.ang[:], ALU.mult, ALU.add, [W.kf, W.ang], [W.r1])
    k.stt(W.r1[:], W.kf[:], -CW2, W.r1[:], ALU.mult, ALU.add, [W.kf, W.r1], [W.r1])
    k.ts(W.r1[:], W.r1[:], PI_LO, ALU.min, [W.r1], [W.r1], s2=-PI_LO, op1=ALU.max)
    k.act(sinb[:], W.r1[:], AF.Sin, [W.r1], [sinb])
    k.stt(W.kf[:], W.r1[:], -1.0, W.r1[:], ALU.mult, ALU.max, [W.r1], [W.kf])
    k.act(cosb[:], W.kf[:], AF.Sin, [W.kf], [cosb], bias=W.halfpi[:, 0:1], scale=-1.0)


def rope_work(k):
    W = Ctx()
    W.posi = k.sb('posi', [128, TT], I32)
    W.ang = k.sb('ang', [128, TT], F32)
    W.kq = k.sb('kq', [128, TT], I32)
    W.kf = k.sb('kf', [128, TT], F32)
    W.r1 = k.sb('r1', [128, TT], F32)
    W.halfpi = k.sb('halfpi', [128, 1], F32)
    k.memset(W.halfpi[:], math.pi / 2.0, [W.halfpi])
    return W


def emit_mod(k, X, A, modT):
    with k.scope():
        cc = k.sb('cc', [128, 8], F32)
        k.dma('sp', cc[:], A['cc8'][:, :], writes=[cc])
        cact = k.sb('cact', [128, 8], F32)
        k.act(cact[:], cc[:], AF.Silu, [cc], [cact])
        bada = k.sb('bada', [128, 48], F32)
        k.dma('sp', bada[:], A['b_ada8'][:, :], writes=[bada])
        wv = A['w_ada'].rearrange("(kc p) n -> p kc n", p=128)
        wb = [k.sb('wada%d' % i, [128, 8, 768], F32) for i in range(2)]
        pm = X.banks[0]
        for blk in range(8):
            w = wb[blk % 2]
            for kc in range(8):
                k.dma('sp', w[:, kc, :], wv[:, kc, blk * 768:(blk + 1) * 768], writes=[w], partial=(kc > 0))
            for j in range(6):
                jj = blk * 6 + j
                for kc in range(8):
                    k.mm(pm[:, jj:jj + 1], w[:, kc, j * 128:(j + 1) * 128], cact[:, kc:kc + 1],
                         kc == 0, kc == 7, [w, cact], [pm])
        k.tt(modT[:], pm[:, 0:48], bada[:], ALU.add, [pm, bada], [modT])


def emit_mixer(k, X, A, modT, V_sb, write_ht, phases=('A', 'B', 'S2')):
    if True:
        banks = X.banks
        rot = Rot(banks)
        gmix = k.sb('gmix', [128, 8], F32)
        k.dma('sp', gmix[:], A['gmix8'], writes=[gmix])
        gsc = k.sb('gsc', [128, 8], F32)
        k.stt(gsc[:], modT[:, 8:16], 1.0, gmix[:], ALU.add, ALU.mult, [modT, gmix], [gsc])
        invf = k.sb('invf', [128, 2], F32)
        k.dma('sp', invf[:], A['invf'], writes=[invf])
        prot_f = k.sb('prot_f', [128, 2, 128], F32)
        k.dma('sp', prot_f[:], A['prot'], writes=[prot_f])
        prot = k.sb('prot', [128, 2, 128], BF16)
        k.copy(prot[:], prot_f[:], [prot_f], [prot])
        HT = A['HT_buf']; ZT = A['ZT_buf']; QTd = A['QTd_buf']; KTd = A['KTd_buf']
        HTv = A['HT'].rearrange("(kc p) c -> p kc c", p=128)
        xTv = A['xT'].rearrange("(kc p) c -> p kc c", p=128)

        if 'A' in phases:
          with k.scope():
            NA = 1504
            w_a = k.sb('w_a', [128, 8, NA], BF16)
            wav = A['w_a'].rearrange("(kc p) n -> p kc n", p=128)
            for kc in range(8):
                load_cast(k, w_a, lambda c, n: w_a[:, kc, c:c + n], lambda c, n: wav[:, kc, c:c + n], NA, step=752)
            st_f = k.sb('st_f', [128, 3, 384], F32)
            qg = k.sb('qg', [128, 3], F32); kvg = k.sb('kvg', [128, 2], F32)
            k.dma('sp', qg[:], A['qg3'], writes=[qg]); k.dma('sp', kvg[:], A['kvg2'], writes=[kvg])
            wuq = k.sb('wuq', [128, 3, 384], BF16)
            k.dma('sp', st_f[:], A['wuq'].rearrange("(rc p) n -> p rc n", p=128), writes=[st_f])
            for rc in range(3):
                k.ts(wuq[:, rc, :], st_f[:, rc, :], qg[:, rc:rc + 1], ALU.mult, [st_f, qg], [wuq], partial=True)
            wuk = k.sb('wuk', [128, 2, 256], BF16); wuv = k.sb('wuv', [128, 2, 256], BF16)
            st2 = k.sb('st2', [128, 2, 256], F32); st3 = k.sb('st3', [128, 2, 256], F32)
            k.dma('sp', st2[:], A['wuk'].rearrange("(rc p) n -> p rc n", p=128), writes=[st2])
            k.dma('sp', st3[:], A['wuv'].rearrange("(rc p) n -> p rc n", p=128), writes=[st3])
            for rc in range(2):
                k.ts(wuk[:, rc, :], st2[:, rc, :], kvg[:, rc:rc + 1], ALU.mult, [st2, kvg], [wuk], partial=True)
                k.ts(wuv[:, rc, :], st3[:, rc, :], kvg[:, rc:rc + 1], ALU.mult, [st3, kvg], [wuv], partial=True)
            cw = k.sb('cw', [128, 2, 3], F32)
            k.dma('sp', cw[:], A['convw'], writes=[cw])
            xts = [k.sb('xt%d' % i, [128, 8, TT], F32) for i in range(2)]
            sq = k.sb('sq', [128, 8, TT], BF16)
            hTa = [k.sb('hT%d' % i, [128, 8, TT], BF16) for i in range(2)]
            rtmp = k.sb('rtmp', [128, TT], F32)
            rstd = k.sb('rstd', [128, TT], F32)
            hx = [k.sb('hx%d' % i, [128, TT], F32) for i in range(2)]
            lat_f = k.sb('lat_f', [128, 5, TT], F32)
            sql = k.sb('sql', [128, 5, TT], BF16)
            rq_bc = k.sb('rq_bc', [128, TT], F32); rkv_bc = k.sb('rkv_bc', [128, TT], F32)
            qn = k.sb('qn', [128, 3, TT], BF16); kvn = k.sb('kvn', [128, 2, TT], BF16)
            QTs = [k.sb('QTs%d' % i, [128, TT], BF16) for i in range(4)]
            KTs = [k.sb('KTs%d' % i, [128, TT], BF16) for i in range(4)]
            qr_f = k.sb('qr_f', [128, TT], F32)
            kr_f = k.sb('kr_f', [128, TT], F32); kr_b = k.sb('kr_b', [128, TT], BF16)
            kpe = k.sb('kpe', [128, TT], BF16)
            t1 = k.sb('t1', [128, TT], F32); t2 = k.sb('t2', [128, TT], F32)
            cosm = k.sb('cosm', [128, TT], F32); sinm = k.sb('sinm', [128, TT], F32)
            W = rope_work(k)
            u = [k.sb('u%d' % i, [128, TT + 2], F32) for i in range(2)]
            cxs = k.sb('cxs', [128, TT], F32); yv = k.sb('yv', [128, TT], F32)
            zc = [k.sb('zc%d' % i, [128, TT], BF16) for i in range(2)]
            for ch in range(2):
                k.memset(u[ch][:], 0.0, [u[ch]])

            def load_x(t):
                xt = xts[t % 2]
                for kc in range(8):
                    k.dma('sp', xt[:, kc, :], xTv[:, kc, t * TT:(t + 1) * TT], writes=[xt], partial=(kc > 0))

            load_x(0)
            for t in range(NT):
                cs = slice(t * TT, (t + 1) * TT)
                if t + 1 < NT:
                    load_x(t + 1)
                xt = xts[t % 2]
                hT = hTa[t % 2]
                for kc in range(8):
                    k.act(sq[:, kc, :], xt[:, kc, :], AF.Square, [xt], [sq], partial=(kc > 0))
                pss = rot.next()
                for kc in range(8):
                    k.mm(pss[:, :], X.ones_bf[:, :], sq[:, kc, :], kc == 0, kc == 7, [X.ones_bf, sq], [pss])
                k.act(rtmp[:], pss[:, :], AF.Sqrt, [pss], [rtmp], bias=EPS, scale=1.0 / D)
                k.recip(rstd[:], rtmp[:], [rtmp], [rstd])
                for kc in range(8):
                    hb = hx[kc % 2]
                    k.stt(hb[:], xt[:, kc, :], gsc[:, kc:kc + 1], rstd[:], ALU.mult, ALU.mult, [xt, gsc, rstd], [hb])
                    k.act(hT[:, kc, :], hb[:], AF.Identity, [hb, modT], [hT], bias=modT[:, kc:kc + 1], partial=(kc > 0))
                if write_ht:
                    k.dma('pool', HTv[:, :, cs], hT[:], reads=[hT], writes=[HT], partial=True)
                rope_tables(k, W, A['pos'][0:1, cs], invf[:, 1:2], cosm, sinm)
                for j in range(5):
                    ps = rot.next()
                    for kc in range(8):
                        k.mm(ps[:, :], w_a[:, kc, j * 128:(j + 1) * 128], hT[:, kc, :], kc == 0, kc == 7, [w_a, hT], [ps])
                    k.act(lat_f[:, j, :], ps[:, :], AF.Copy, [ps], [lat_f], partial=(j > 0))
                    k.act(sql[:, j, :], ps[:, :], AF.Square, [ps], [sql], partial=(j > 0))
                for ch in range(2):
                    pb = rot.next(); pc = rot.next(); px = rot.next()
                    for (pp, base) in ((pb, 736), (pc, 992), (px, 1248)):
                        for kc in range(8):
                            k.mm(pp[:, :], w_a[:, kc, base + ch * 128: base + (ch + 1) * 128], hT[:, kc, :],
                                 kc == 0, kc == 7, [w_a, hT], [pp])
                    k.act(cxs[:], px[:, :], AF.Copy, [px], [cxs])
                    U = u[ch]
                    if t > 0:
                        k.copy(U[:, 0:2], U[:, TT:TT + 2], [U], [U])
                    k.tt(U[:, 2:TT + 2], pc[:, :], cxs[:], ALU.mult, [pc, cxs, U], [U])
                    k.ts(yv[:], U[:, 2:TT + 2], cw[:, ch, 2:3], ALU.mult, [U, cw], [yv])
                    k.stt(yv[:], U[:, 1:TT + 1], cw[:, ch, 1:2], yv[:], ALU.mult, ALU.add, [U, cw, yv], [yv])
                    k.stt(yv[:], U[:, 0:TT], cw[:, ch, 0:1], yv[:], ALU.mult, ALU.add, [U, cw, yv], [yv])
                    Z = zc[ch]
                    k.tt(Z[:], pb[:, :], yv[:], ALU.mult, [pb, yv], [Z])
                    k.dma('pool', A['ZTc'][ch * 128:(ch + 1) * 128, cs], Z[:], reads=[Z], writes=[ZT], partial=True)
                for (j0, j1, n, dst) in ((0, 3, 384.0, rq_bc), (3, 5, 256.0, rkv_bc)):
                    ps = rot.next()
                    for j in range(j0, j1):
                        k.mm(ps[:, :], X.ones_bf[:, :], sql[:, j, :], j == j0, j == j1 - 1, [X.ones_bf, sql], [ps])
                    k.act(rtmp[:], ps[:, :], AF.Sqrt, [ps], [rtmp], bias=EPS, scale=1.0 / n)
                    k.recip(dst[:], rtmp[:], [rtmp], [dst])
                for j in range(3):
                    k.tt(qn[:, j, :], lat_f[:, j, :], rq_bc[:], ALU.mult, [lat_f, rq_bc], [qn], partial=(j > 0))
                for j in range(2):
                    k.tt(kvn[:, j, :], lat_f[:, 3 + j, :], rkv_bc[:], ALU.mult, [lat_f, rkv_bc], [kvn], partial=(j > 0))
                ps = rot.next()
                for kc in range(8):
                    k.mm(ps[0:96, :], w_a[:, kc, 640:736], hT[:, kc, :], kc == 0, kc == 7, [w_a, hT], [ps])
                k.act(kr_f[64:96, :], ps[64:96, :], AF.Copy, [ps], [kr_f])
                k.act(kr_b[0:96, :], ps[0:96, :], AF.Copy, [ps], [kr_b])
                ps2 = rot.next()
                k.mm(ps2[0:96, :], prot[0:96, 1, 0:96], kr_b[0:96, :], True, True, [prot, kr_b], [ps2])
                k.tt(t1[64:96, :], kr_f[64:96, :], cosm[64:96, :], ALU.mult, [kr_f, cosm], [t1])
                k.tt(t2[64:96, :], ps2[64:96, :], sinm[64:96, :], ALU.mult, [ps2, sinm], [t2])
                k.tt(kpe[64:96, :], t1[64:96, :], t2[64:96, :], ALU.add, [t1, t2], [kpe])
                for h in range(4):
                    k.dma('pool', A['KTd'][h, 64:96, cs], kpe[64:96, :], reads=[kpe], writes=[KTd], partial=True)
                for h in range(4):
                    ps = rot.next()
                    for rc in range(3):
                        k.mm(ps[0:96, :], wuq[:, rc, h * 96:(h + 1) * 96], qn[:, rc, :], rc == 0, rc == 2, [wuq, qn], [ps])
                    Q = QTs[h]
                    k.act(Q[0:96, :], ps[0:96, :], AF.Copy, [ps], [Q], scale=MLA_SCALE)
                    k.act(qr_f[64:96, :], ps[64:96, :], AF.Copy, [ps], [qr_f], scale=MLA_SCALE)
                    ps2 = rot.next()
                    k.mm(ps2[0:96, :], prot[0:96, 1, 0:96], Q[0:96, :], True, True, [prot, Q], [ps2])
                    k.tt(t1[64:96, :], qr_f[64:96, :], cosm[64:96, :], ALU.mult, [qr_f, cosm], [t1])
                    k.tt(t2[64:96, :], ps2[64:96, :], sinm[64:96, :], ALU.mult, [ps2, sinm], [t2])
                    k.tt(Q[64:96, :], t1[64:96, :], t2[64:96, :], ALU.add, [t1, t2], [Q])
                    k.dma('pool', A['QTd'][h, :, cs], Q[0:96, :], reads=[Q], writes=[QTd], partial=True)
                for h in range(4):
                    ps = rot.next()
                    for rc in range(2):
                        k.mm(ps[0:64, :], wuk[:, rc, h * 64:(h + 1) * 64], kvn[:, rc, :], rc == 0, rc == 1, [wuk, kvn], [ps])
                    Kt = KTs[h]
                    k.act(Kt[0:64, :], ps[0:64, :], AF.Copy, [ps], [Kt])
                    k.dma('pool', A['KTd'][h, 0:64, cs], Kt[0:64, :], reads=[Kt], writes=[KTd], partial=True)
                for blk in range(4):
                    ps = rot.next()
                    for rc in range(2):
                        k.mm(ps[:, 0:256], kvn[:, rc, blk * 128:(blk + 1) * 128], wuv[:, rc, :], rc == 0, rc == 1, [kvn, wuv], [ps])
                    k.act(V_sb[:, t * 4 + blk, :, 0:64], ps[:, 0:256].rearrange("p (h v) -> p h v", h=4), AF.Copy,
                          [ps], [V_sb], partial=True)

        if 'B' in phases:
          with k.scope():
            NB = 1536
            w_b = k.sb('w_b', [128, 8, NB], BF16)
            wbv = A['w_b'].rearrange("(kc p) n -> p kc n", p=128)
            for kc in range(8):
                load_cast(k, w_b, lambda c, n: w_b[:, kc, c:c + n], lambda c, n: wbv[:, kc, c:c + n], NB, step=768)
            hTs = [k.sb('hTb%d' % i, [128, 8, TT], BF16) for i in range(2)]
            rmask = k.sb('rmask', [128, 2, 128], F32)
            k.dma('sp', rmask[:], A['rmask'], writes=[rmask])
            qdec = k.sb('qdec', [128, 2, TT], F32)
            k.dma('sp', qdec[:], A['qdec'], writes=[qdec])
            kdec = k.sb('kdec', [128, 2], F32)
            k.dma('sp', kdec[:], A['kdec'], writes=[kdec])
            sdec = k.sb('sdec', [128, 2], F32)
            k.dma('sp', sdec[:], A['sdec'], writes=[sdec])
            cosr = k.sb('cosr', [128, TT], F32); sinr = k.sb('sinr', [128, TT], F32)
            W = rope_work(k)
            qf = k.sb('qf', [128, TT], F32); qb = k.sb('qb', [128, TT], BF16)
            t1 = k.sb('t1b', [128, TT], F32); t2 = k.sb('t2b', [128, TT], F32)
            RQT = [k.sb('RQT%d' % i, [128, TT], BF16) for i in range(2)]
            RQd = [k.sb('RQd%d' % i, [128, TT], BF16) for i in range(2)]
            RKT = [k.sb('RKT%d' % i, [128, TT], BF16) for i in range(2)]
            RKd = [k.sb('RKd%d' % i, [128, 4, 128], BF16) for i in range(2)]
            RVb = [k.sb('RV%d' % i, [128, 512], BF16) for i in range(4)]; Gb = [k.sb('G%d' % i, [128, 512], BF16) for i in range(4)]
            AT = [k.sb('AT%d' % i, [128, 128], BF16) for i in range(2)]
            st_f = [k.sb('stf%d' % i, [128, 256], F32) for i in range(2)]
            st_b = [k.sb('stb%d' % i, [128, 256], BF16) for i in range(2)]
            junk = k.sb('junk', [128, 256], BF16)
            ssq = k.sb('ssq', [128, 1], F32); sd = k.sb('sd', [128, 1], F32); rinv = k.sb('rinv', [128, 1], F32)
            zr = [k.sb('zr%d' % i, [128, 256], BF16) for i in range(2)]
            zT = [k.sb('zT%d' % i, [128, 2, TT], BF16) for i in range(2)]
            for hr in range(2):
                k.memset(st_f[hr][:], 0.0, [st_f[hr]]); k.memset(st_b[hr][:], 0.0, [st_b[hr]])

            def load_h(t):
                hb = hTs[t % 2]
                k.dma('sp', hb[:], HTv[:, :, t * TT:(t + 1) * TT], reads=[HT], writes=[hb])

            load_h(0)
            for t in range(NT):
                cs = slice(t * TT, (t + 1) * TT)
                if t + 1 < NT:
                    load_h(t + 1)
                hT = hTs[t % 2]
                rope_tables(k, W, A['pos'][0:1, cs], invf[:, 0:1], cosr, sinr)
                for hr in range(2):
                    ps = rot.next()
                    for kc in range(8):
                        k.mm(ps[:, :], w_b[:, kc, hr * 128:(hr + 1) * 128], hT[:, kc, :], kc == 0, kc == 7, [w_b, hT], [ps])
                    k.act(qf[:], ps[:, :], AF.Copy, [ps], [qf])
                    k.act(qb[:], ps[:, :], AF.Copy, [ps], [qb])
                    ps2 = rot.next()
                    k.mm(ps2[:, :], prot[:, 0, :], qb[:], True, True, [prot, qb], [ps2])
                    k.tt(t1[:], qf[:], cosr[:], ALU.mult, [qf, cosr], [t1])
                    k.tt(t2[:], ps2[:, :], sinr[:], ALU.mult, [ps2, sinr], [t2])
                    k.tt(t1[:], t1[:], t2[:], ALU.add, [t1, t2], [t1], eng='pool')
                    k.act(RQT[hr][:], t1[:], AF.Copy, [t1], [RQT[hr]])
                    k.tt(RQd[hr][:], t1[:], qdec[:, hr, :], ALU.mult, [t1, qdec], [RQd[hr]])
                    ps = rot.next()
                    for kc in range(8):
                        k.mm(ps[:, :], w_b[:, kc, 256 + hr * 128:256 + (hr + 1) * 128], hT[:, kc, :], kc == 0, kc == 7, [w_b, hT], [ps])
                    k.act(qf[:], ps[:, :], AF.Copy, [ps], [qf], scale=RET_KS)
                    k.act(qb[:], ps[:, :], AF.Copy, [ps], [qb], scale=RET_KS)
                    ps2 = rot.next()
                    k.mm(ps2[:, :], prot[:, 0, :], qb[:], True, True, [prot, qb], [ps2])
                    k.tt(t1[:], qf[:], cosr[:], ALU.mult, [qf, cosr], [t1])
                    k.tt(t2[:], ps2[:, :], sinr[:], ALU.mult, [ps2, sinr], [t2])
                    k.tt(RKT[hr][:], t1[:], t2[:], ALU.add, [t1, t2], [RKT[hr]])
                    pT = rot.next()
                    for blk in range(4):
                        k.tr(pT.bf[:, blk * 128:(blk + 1) * 128], RKT[hr][:, blk * 128:(blk + 1) * 128], X.id_bf[:, :],
                             [RKT[hr], X.id_bf], [pT], partial=(blk > 0))
                    for blk in range(4):
                        k.act(RKd[hr][:, blk, :], pT.bf[:, blk * 128:(blk + 1) * 128], AF.Copy, [pT, kdec], [RKd[hr]],
                              scale=kdec[:, hr:hr + 1], partial=(blk > 0))
                def proj_vg(blk):
                    ps = rot.next()
                    for kc in range(8):
                        k.mm(ps[:, :], hT[:, kc, blk * 128:(blk + 1) * 128], w_b[:, kc, 512:1024], kc == 0, kc == 7, [w_b, hT], [ps])
                    k.act(RVb[blk][:], ps[:, :], AF.Copy, [ps], [RVb[blk]])
                    ps = rot.next()
                    for kc in range(8):
                        k.mm(ps[:, :], hT[:, kc, blk * 128:(blk + 1) * 128], w_b[:, kc, 1024:1536], kc == 0, kc == 7, [w_b, hT], [ps])
                    k.act(Gb[blk][:], ps[:, :], AF.Silu, [ps], [Gb[blk]])

                proj_vg(0)
                for blk in range(4):
                    if blk + 1 < 4:
                        proj_vg(blk + 1)
                    RV = RVb[blk]; G = Gb[blk]
                    bs = slice(blk * 128, (blk + 1) * 128)
                    for hr in range(2):
                        vs = slice(hr * 256, (hr + 1) * 256)
                        pS = rot.next()
                        k.mm(pS[:, 0:128], RKT[hr][:, bs], RQT[hr][:, bs], True, True, [RKT[hr], RQT[hr]], [pS])
                        a = AT[hr]
                        k.tt(a[:], pS[:, 0:128], rmask[:, hr, :], ALU.mult, [pS, rmask], [a])
                        pO = rot.next()
                        k.mm(pO[:, 0:256], a[:], RV[:, vs], True, False, [a, RV], [pO])
                        k.mm(pO[:, 0:256], RQd[hr][:, bs], st_b[hr][:], False, True, [RQd[hr], st_b[hr]], [pO])
                        pN = rot.next()
                        k.mm(pN[:, 0:256], RKd[hr][:, blk, :], RV[:, vs], True, True, [RKd[hr], RV], [pN])
                        k.stt(st_f[hr][:], st_f[hr][:], sdec[:, hr:hr + 1], pN[:, 0:256], ALU.mult, ALU.add,
                              [st_f[hr], sdec, pN], [st_f[hr]])
                        k.act(st_b[hr][:], st_f[hr][:], AF.Copy, [st_f[hr]], [st_b[hr]])
                        k.act(junk[:], pO[:, 0:256], AF.Square, [pO], [junk, ssq], accum_out=ssq[:, 0:1])
                        k.act(sd[:], ssq[:], AF.Sqrt, [ssq], [sd], bias=EPS, scale=1.0 / 256.0)
                        k.recip(rinv[:], sd[:], [sd], [rinv])
                        z = zr[hr]
                        k.stt(z[:], pO[:, 0:256], rinv[:, 0:1], G[:, vs], ALU.mult, ALU.mult, [pO, rinv, G], [z])
                        pT = rot.next()
                        for vc in range(2):
                            k.tr(pT.bf[:, vc * 128:(vc + 1) * 128], z[:, vc * 128:(vc + 1) * 128], X.id_bf[:, :],
                                 [z, X.id_bf], [pT], partial=(vc > 0))
                        k.act(zT[hr][:, :, bs], pT.bf[:, 0:256].rearrange("p (v i) -> p v i", v=2), AF.Copy, [pT], [zT[hr]],
                              partial=True)
                for hr in range(2):
                    k.dma('pool', A['ZTr'][hr * 256:(hr + 1) * 256, cs].rearrange("(v p) c -> p v c", p=128),
                          zT[hr][:], reads=[zT[hr]], writes=[ZT], partial=True)

        if 'S2' in phases:
          with k.scope():
            kts = [k.sb('kt%d' % i, [128, S], BF16) for i in range(2)]
            qts = [k.sb('qt%d' % i, [128, TT], BF16) for i in range(3)]
            PTs = [k.sb('PT%d' % i, [128, TT], BF16) for i in range(3)]
            of = [k.sb('of%d' % i, [128, TT], F32) for i in range(2)]
            rb = k.sb('rb', [128, TT], F32)
            zm = [k.sb('zm%d' % i, [128, TT], BF16) for i in range(2)]
            sel = k.sb('sel', [128, 64], F32)
            for o_ in of:
                k.memset(o_[:], 0.0, [o_])
            k.dma('sp', sel[:], A['sel65'], writes=[sel])
            srot = Rot(banks[0:4]); orot = Rot(banks[4:6]); brot = Rot(banks[6:8])
            qi = 0
            for h in range(4):
                kt = kts[h % 2]
                for c4 in range(4):
                    k.dma('sp', kt[0:96, c4 * 2048:(c4 + 1) * 2048], A['KTd'][h, :, c4 * 2048:(c4 + 1) * 2048],
                          reads=[KTd], writes=[kt], partial=(c4 > 0))
                for qt in range(NT):
                    q = qts[qi % 3]; qi += 1
                    k.dma('sp', q[0:96, :], A['QTd'][h, :, qt * TT:(qt + 1) * TT], reads=[QTd], writes=[q])
                    nkb = 4 * qt + 4
                    O = orot.next()
                    pend = []

                    def emit_s(kb):
                        d = kb - 4 * qt
                        qlo = 0 if d < 0 else 128 * d
                        pS = srot.next()
                        k.mm(pS[:, qlo:TT], kt[0:96, kb * 128:(kb + 1) * 128], q[0:96, qlo:TT], True, True, [kt, q], [pS])
                        P = PTs[kb % 3]
                        k.act(P[:, qlo:TT], pS[:, qlo:TT], AF.Exp, [pS], [P])
                        if d >= 0:
                            k.memset(P[64:128, qlo:qlo + 64], 0.0, [P], eng='pool', partial=True, reads=[P])
                        return P, d, qlo

                    def emit_pv(kb, P, d, qlo):
                        k.mm(O[0:65, qlo:TT], V_sb[:, kb, h, :], P[:, qlo:TT], kb == 0, kb == nkb - 1, [V_sb, P], [O])

                    for kb in range(nkb):
                        pend.append((kb,) + emit_s(kb))
                        if len(pend) > 2:
                            a = pend.pop(0); emit_pv(*a)
                    while pend:
                        a = pend.pop(0); emit_pv(*a)
                    o = of[qt % 2]
                    k.act(o[0:65, :], O[0:65, :], AF.Copy, [O], [o], partial=True)
                    pB = brot.next()
                    k.mm(pB[0:64, :], sel[:, 0:64], o[:, :], True, True, [sel, o], [pB])
                    k.recip(rb[0:64, :], pB[0:64, :], [pB], [rb])
                    z = zm[qt % 2]
                    k.tt(z[0:64, :], o[0:64, :], rb[0:64, :], ALU.mult, [o, rb], [z])
                    k.dma('pool', A['ZTm'][h * 64:(h + 1) * 64, qt * TT:(qt + 1) * TT], z[0:64, :], reads=[z], writes=[ZT], partial=True)


OFF = {'q_lat': 0, 'kv_lat': 384, 'k_rope': 640, 'cb': 672, 'cc': 1184, 'cx': 1696, 'rq': 2208, 'rk': 2720,
       'rv': 3232, 'rg': 4256, 'gl': 5280}


def col8(v):
    return np.ascontiguousarray(v.reshape(-1, 128).T)


def consts_for(hh):
    C = {}
    C['ident'] = np.eye(128, dtype=np.float32)
    prot = np.zeros((128, 2, 128), np.float32)
    for r in range(64):
        prot[r + 64, 0, r] = -1.0
        prot[r, 0, r + 64] = 1.0
    for r in range(64, 80):
        prot[r + 16, 1, r] = -1.0
        prot[r, 1, r + 16] = 1.0
    C['prot'] = prot
    invf = np.zeros((128, 2), np.float32)
    inv_ret = (10000.0 ** (-np.arange(0, 128, 2, dtype=np.float32) / 128)).astype(np.float32)
    inv_mla = (10000.0 ** (-np.arange(0, 32, 2, dtype=np.float32) / 32)).astype(np.float32)
    for p in range(128):
        invf[p, 0] = inv_ret[p % 64]
    for p in range(64, 96):
        invf[p, 1] = inv_mla[(p - 64) % 16]
    C['invf'] = invf
    rmask = np.zeros((128, 2, 128), np.float64); qdec = np.zeros((128, 2, TT), np.float64)
    kdec = np.zeros((128, 2), np.float64); sdec = np.zeros((128, 2), np.float64)
    for hr in range(2):
        H = hh * 2 + hr
        g = 1.0 - 2.0 ** (-5.0 - H)
        for j in range(128):
            for i in range(128):
                cj, ci = j // 64, i // 64
                if cj == ci:
                    rmask[j, hr, i] = g ** abs(i - j)
                elif cj < ci:
                    rmask[j, hr, i] = g ** (i - j)
        qdec[:, hr, :] = (g ** ((np.arange(TT) % 128) + 1.0))[None, :]
        kdec[:, hr] = g ** (127.0 - np.arange(128))
        sdec[:, hr] = g ** 128.0
    C['rmask'] = rmask.astype(np.float32); C['qdec'] = qdec.astype(np.float32)
    C['kdec'] = kdec.astype(np.float32); C['sdec'] = sdec.astype(np.float32)
    sel = np.zeros((128, 64), np.float32); sel[64, :] = 1.0
    C['sel65'] = sel
    return C


def mixer_inputs(inp, l, b, hh, xT_b):
    w_in = inp['w_in'][l]
    m = dict(consts_for(hh))
    if xT_b is not None:
        m['xT'] = xT_b
    m['cc8'] = col8(inp['c'][b])
    m['b_ada8'] = col8(inp['b_ada'][l])
    m['gmix8'] = col8(inp['norm_mix_g'][l])
    wa = np.zeros((D, 1504), np.float32)
    wa[:, 0:640] = w_in[:, 0:640]
    wa[:, 704:736] = w_in[:, 640:672]
    for i, nm in enumerate(('cb', 'cc', 'cx')):
        wa[:, 736 + i * 256:736 + (i + 1) * 256] = w_in[:, OFF[nm] + hh * 256:OFF[nm] + (hh + 1) * 256]
    m['w_a'] = wa
    wb = np.empty((D, 1536), np.float32)
    wb[:, 0:256] = w_in[:, OFF['rq'] + hh * 256:OFF['rq'] + (hh + 1) * 256]
    wb[:, 256:512] = w_in[:, OFF['rk'] + hh * 256:OFF['rk'] + (hh + 1) * 256]
    wb[:, 512:1024] = w_in[:, OFF['rv'] + hh * 512:OFF['rv'] + (hh + 1) * 512]
    wb[:, 1024:1536] = w_in[:, OFF['rg'] + hh * 512:OFF['rg'] + (hh + 1) * 512]
    m['w_b'] = wb
    m['pos'] = np.ascontiguousarray(inp['positions'][b:b + 1].astype(np.int32))
    m['qg3'] = col8(inp['mla_q_norm_g'][l]); m['kvg2'] = col8(inp['mla_kv_norm_g'][l])
    m['wuq'] = np.ascontiguousarray(inp['w_uq'][l][:, hh * 384:(hh + 1) * 384])
    wukv = inp['w_ukv'][l].reshape(256, 8, 128)[:, hh * 4:(hh + 1) * 4, :]
    m['wuk'] = np.ascontiguousarray(wukv[:, :, 0:64].reshape(256, 256))
    m['wuv'] = np.ascontiguousarray(wukv[:, :, 64:128].reshape(256, 256))
    cwv = inp['conv_w'][l][:, hh * 256:(hh + 1) * 256]
    m['convw'] = np.ascontiguousarray(cwv.reshape(3, 2, 128).transpose(2, 1, 0))
    return m


SO = 4096; NTO = SO // TT
BIG = 1.0e30


def emit_ffn(k, X, A, modT, last, tok):
    if True:
        banks = X.banks
        rot = Rot(banks[0:7])
        pL = banks[7]
        gffn = k.sb('gffn', [128, 8], F32)
        k.dma('sp', gffn[:], A['gffn8'], writes=[gffn])
        gsc2 = k.sb('gsc2', [128, 8], F32)
        k.stt(gsc2[:], modT[:, 32:40], 1.0, gffn[:], ALU.add, ALU.mult, [modT, gffn], [gsc2])
        X1T = A['X1T_buf']; H2K = A['H2K_buf']; HSb = A['HS_buf']; YSb = A['YS_buf']; XO = A['xo_buf']
        NBLK = SO // 128; TS = 256; NTILE = 64
        rc = k.sb('rconst', [128, 128 + 128 + 64 + 16 + 32 + 2], F32)
        k.dma('sp', rc[:], A['rconst'], writes=[rc])
        tri = rc[:, 0:128]; ones_f = rc[:, 128:256]; iota64 = rc[:, 256:320]; thr16 = rc[:, 320:336]
        iota32 = rc[:, 336:368]; pbase2 = rc[:, 368:370]
        e1s = k.sb('e1s', [128, NBLK], F32); e2s = k.sb('e2s', [128, NBLK], F32)
        r1s = k.sb('r1s', [128, NBLK], F32); r2s = k.sb('r2s', [128, NBLK], F32)
        w1s = k.sb('w1s', [128, NBLK], F32); w2s = k.sb('w2s', [128, NBLK], F32)
        run_bc = k.sb('run_bc', [128, 32], F32)
        k.memset(run_bc[:], 0.0, [run_bc])
        pos1i = k.sb('pos1i', [128, NBLK], I32); pos2i = k.sb('pos2i', [128, NBLK], I32)
        widx = k.sb('widx', [128, NTILE, 2], I32)
        HTb = A['HT_buf']; ZTb = A['ZT_buf']; XSb = A['xs_buf']
        xTv = A['xT'].rearrange("(kc p) c -> p kc c", p=128)
        HTv = A['HT'].rearrange("(kc p) c -> p kc c", p=128)
        ZTv = A['ZT'].rearrange("(kc p) c -> p kc c", p=128)
        X1v = A['X1T'].rearrange("(kc p) c -> p kc c", p=128)
        XOv = A['xoT'].rearrange("(kc p) c -> p kc c", p=128)

        with k.scope():
            w_g = k.sb('w_g', [128, 8, 3072], BF16)
            wgv = A['w_g'].rearrange("(kc p) n -> p kc n", p=128)
            for kc in range(8):
                load_cast(k, w_g, lambda c, n: w_g[:, kc, c:c + n], lambda c, n: wgv[:, kc, c:c + n], 3072, step=1024)
            w_o = k.sb('w_o', [128, 16, 1024], BF16)
            wov = A['w_o'].rearrange("(kc p) n -> p kc n", p=128)
            for kc in range(16):
                k.dma('pool', w_o[:, kc, :], wov[:, kc, :], writes=[w_o], partial=True)
            w_m = k.sb('w_m', [128, 8, 1024], BF16)
            wmv = A['w_mix'].rearrange("(kc p) n -> p kc n", p=128)
            for kc in range(8):
                k.dma('pool', w_m[:, kc, :], wmv[:, kc, :], writes=[w_m], partial=True)
            w_r = k.sb('w_r', [128, 8, 36], F32)
            k.dma('sp', w_r[:], A['w_r'].rearrange("(kc p) n -> p kc n", p=128), writes=[w_r])
            b_r = k.sb('b_r', [128, 36], F32)
            k.dma('sp', b_r[:], A['b_r'].to_broadcast([128, 36]), writes=[b_r])
            hT = k.sb('hTc', [128, 8, TT], BF16)
            Zt = k.sb('Zt', [128, 16, TT], BF16)
            xt = k.sb('xtc', [128, 8, TT], F32)
            gt0 = k.sb('gt0', [128, 3, TT], BF16); gt = [gt0, gt0]
            tA = k.sb('tA', [128, TT], F32); tB = k.sb('tB', [128, TT], F32)
            mg = k.sb('mg', [128, 8, TT], BF16)
            rtmp = k.sb('rtmpc', [128, TT], F32); rstd = k.sb('rstdc', [128, TT], F32)
            h2f0 = k.sb('h2f0', [128, TT], F32); h2f = [h2f0, h2f0]
            h2b = k.sb('h2b', [128, 8, TT], BF16)
            lt = tA
            lg = k.sb('lg', [128, 36], F32); gmax = k.sb('gmax', [128, 1], F32); ngmax = k.sb('ngmax', [128, 1], F32)
            g1h = k.sb('g1h', [128, 4], F32); pen = k.sb('pen', [128, 4], F32)
            ej = k.sb('ej', [128, 4], F32); gsum = k.sb('gsum', [128, 1], F32); gval = k.sb('gval', [128, 1], F32)
            lem = k.sb('lem', [128, 32], F32); top8 = k.sb('top8', [128, 8], F32)
            m1 = k.sb('m1', [128, 32], F32); m2 = k.sb('m2', [128, 32], F32)
            dd = k.sb('dd', [128, 1], F32); ee = k.sb('ee', [128, 1], F32); den = k.sb('den', [128, 1], F32)
            oh = k.sb('oh', [128, 32], F32); rk = k.sb('rk', [128, 32], F32); jk = k.sb('jk', [128, 32], F32)
            cT = tB
            for t in range(NTO):
                cs = slice(t * TT, (t + 1) * TT)
                tsl = tok('sp', t * TT, TT)
                k.dma('sp', hT[:], HTv[:, :, tsl], reads=[HTb], writes=[hT])
                k.dma('sp', Zt[:], ZTv[:, :, tsl], reads=[ZTb], writes=[Zt])
                for kc in range(8):
                    k.dma('sp', xt[:, kc, :], xTv[:, kc, tsl], reads=[XSb], writes=[xt], partial=(kc > 0))
                for dc in range(8):
                    ds_ = slice(dc * 128, (dc + 1) * 128)
                    g = gt[dc % 2]
                    for br in range(3):
                        ps = rot.next()
                        for kc in range(8):
                            k.mm(ps[:, :], w_g[:, kc, br * 1024 + dc * 128: br * 1024 + (dc + 1) * 128], hT[:, kc, :],
                                 kc == 0, kc == 7, [w_g, hT], [ps])
                        k.act(g[:, br, :], ps[:, :], AF.Sigmoid, [ps], [g], partial=(br > 0))
                    pys = []
                    for (k0, k1) in ((0, 4), (4, 8), (8, 16)):
                        ps = rot.next()
                        for kc in range(k0, k1):
                            k.mm(ps[:, :], w_o[:, kc, ds_], Zt[:, kc, :], kc == k0, kc == k1 - 1, [w_o, Zt], [ps])
                        pys.append(ps)
                    k.tt(tA[:], pys[0][:, :], g[:, 0, :], ALU.mult, [pys[0], g], [tA])
                    k.tt(tB[:], pys[1][:, :], g[:, 1, :], ALU.mult, [pys[1], g], [tB])
                    k.tt(tA[:], tA[:], tB[:], ALU.add, [tA, tB], [tA], eng='pool')
                    k.tt(tB[:], pys[2][:, :], g[:, 2, :], ALU.mult, [pys[2], g], [tB])
                    k.tt(mg[:, dc, :], tA[:], tB[:], ALU.add, [tA, tB], [mg], eng='pool', partial=(dc > 0))
                for dc in range(8):
                    ps = rot.next()
                    for kc in range(8):
                        k.mm(ps[:, :], w_m[:, kc, dc * 128:(dc + 1) * 128], mg[:, kc, :], kc == 0, kc == 7, [w_m, mg], [ps])
                    k.stt(xt[:, dc, :], ps[:, :], modT[:, 16 + dc:17 + dc], xt[:, dc, :], ALU.mult, ALU.add,
                          [ps, modT, xt], [xt], partial=True)
                k.dma('pool', X1v[:, :, cs], xt[:], reads=[xt], writes=[X1T], partial=True)
                for kc in range(8):
                    k.act(mg[:, kc, :], xt[:, kc, :], AF.Square, [xt], [mg], partial=(kc > 0))
                pss = rot.next()
                for kc in range(8):
                    k.mm(pss[:, :], X.ones_bf[:, :], mg[:, kc, :], kc == 0, kc == 7, [X.ones_bf, mg], [pss])
                k.act(rtmp[:], pss[:, :], AF.Sqrt, [pss], [rtmp], bias=EPS, scale=1.0 / D)
                k.recip(rstd[:], rtmp[:], [rtmp], [rstd])
                for kc in range(8):
                    hf = h2f[kc % 2]
                    k.stt(hf[:], xt[:, kc, :], gsc2[:, kc:kc + 1], rstd[:], ALU.mult, ALU.mult, [xt, gsc2, rstd], [hf])
                    k.act(hf[:], hf[:], AF.Identity, [hf, modT], [hf], bias=modT[:, 24 + kc:25 + kc])
                    k.copy(h2b[:, kc, :], hf[:], [hf], [h2b], eng='pool', partial=(kc > 0))
                    k.mm(pL[0:36, :], w_r[:, kc, :], hf[:], kc == 0, kc == 7, [w_r, hf], [pL])
                for blk in range(4):
                    pH = rot.next()
                    for kc in range(8):
                        k.tr(pH.bf[:, kc * 128:(kc + 1) * 128], h2b[:, kc, blk * 128:(blk + 1) * 128], X.id_bf[:, :],
                             [h2b, X.id_bf], [pH], partial=(kc > 0))
                    hk = gt0
                    k.act(hk[:, 0:2, :], pH.bf[:, :].rearrange("p (a b) -> p a b", a=2), AF.Copy, [pH], [hk])
                    k.dma('pool', A['H2K'][(t * 4 + blk) * 128:(t * 4 + blk + 1) * 128, :].rearrange("p (a b) -> p a b", a=2),
                          hk[:, 0:2, :], reads=[hk], writes=[H2K], partial=True)
                k.act(lt[0:36, :], pL[0:36, :], AF.Copy, [pL], [lt])
                pT = rot.next()
                for blk in range(4):
                    k.tr(pT[:, blk * 36:(blk + 1) * 36], lt[0:36, blk * 128:(blk + 1) * 128], X.id_f[0:36, 0:36],
                         [lt, X.id_f], [pT], partial=(blk > 0))
                for blk in range(4):
                    gb = t * 4 + blk
                    w1 = w1s[:, gb:gb + 1]; w2 = w2s[:, gb:gb + 1]
                    k.tt(lg[:], pT[:, blk * 36:(blk + 1) * 36], b_r[:], ALU.add, [pT, b_r], [lg])
                    k.op('dve', lambda e: e.reduce_max(out=gmax[:], in_=lg[:, 0:4], axis=mybir.AxisListType.X),
                         reads=[lg], writes=[gmax])
                    k.ts(g1h[:], lg[:, 0:4], gmax[:, 0:1], ALU.is_equal, [lg, gmax], [g1h])
                    k.ts(ngmax[:], gmax[:], -1.0, ALU.mult, [gmax], [ngmax])
                    k.act(ej[:], lg[:, 0:4], AF.Exp, [lg, ngmax], [ej, gsum], bias=ngmax[:, 0:1], accum_out=gsum[:, 0:1])
                    k.recip(gval[:], gsum[:], [gsum], [gval])
                    k.ts(pen[:], g1h[:], -1.0, ALU.add, [g1h], [pen], s2=BIG, op1=ALU.mult)
                    for g_ in range(4):
                        k.ts(lem[:, g_ * 8:(g_ + 1) * 8], lg[:, 4 + g_ * 8:4 + (g_ + 1) * 8], pen[:, g_:g_ + 1], ALU.add,
                             [lg, pen], [lem], partial=(g_ > 0))
                    k.op('dve', lambda e: e.max(out=top8[:], in_=lem[:]), reads=[lem], writes=[top8])
                    k.ts(m1[:], lem[:], top8[:, 0:1], ALU.is_equal, [lem, top8], [m1])
                    k.ts(m2[:], lem[:], top8[:, 1:2], ALU.is_equal, [lem, top8], [m2])
                    k.tt(dd[:], top8[:, 1:2], top8[:, 0:1], ALU.subtract, [top8], [dd])
                    k.act(ee[:], dd[:], AF.Exp, [dd], [ee])
                    k.ts(den[:], ee[:], 1.0, ALU.add, [ee], [den])
                    k.recip(den[:], den[:], [den], [den])
                    k.tt(w1, den[:], gval[:], ALU.mult, [den, gval], [w1s], partial=True)
                    k.tt(w2, w1, ee[:], ALU.mult, [w1s, ee], [w2s], partial=True)
                    k.tt(oh[:], m1[:], m2[:], ALU.add, [m1, m2], [oh])
                    pP = rot.next()
                    k.mm(pP[:, 0:32], tri, oh[:], True, True, [rc, oh], [pP])
                    k.tt(rk[:], pP[:, 0:32], run_bc[:], ALU.add, [pP, run_bc], [rk])
                    k.op('dve', lambda e: e.scalar_tensor_tensor(out=jk[:], in0=rk[:], scalar=1.0, in1=m1[:], op0=ALU.mult, op1=ALU.mult,
                                                              accum_out=r1s[:, gb:gb + 1]), reads=[rk, m1], writes=[jk, r1s], partial=True)
                    k.op('dve', lambda e: e.scalar_tensor_tensor(out=jk[:], in0=rk[:], scalar=1.0, in1=m2[:], op0=ALU.mult, op1=ALU.mult,
                                                              accum_out=r2s[:, gb:gb + 1]), reads=[rk, m2], writes=[jk, r2s], partial=True)
                    k.op('dve', lambda e: e.scalar_tensor_tensor(out=jk[:], in0=iota32, scalar=1.0, in1=m1[:], op0=ALU.mult, op1=ALU.mult,
                                                              accum_out=e1s[:, gb:gb + 1]), reads=[rc, m1], writes=[jk, e1s], partial=True)
                    k.op('dve', lambda e: e.scalar_tensor_tensor(out=jk[:], in0=iota32, scalar=1.0, in1=m2[:], op0=ALU.mult, op1=ALU.mult,
                                                              accum_out=e2s[:, gb:gb + 1]), reads=[rc, m2], writes=[jk, e2s], partial=True)
                    pQ = rot.next()
                    k.mm(pQ[:, 0:32], ones_f, oh[:], True, True, [rc, oh], [pQ])
                    k.tt(run_bc[:], run_bc[:], pQ[:, 0:32], ALU.add, [run_bc, pQ], [run_bc])
            ptile = k.sb('ptile', [128, 32], F32); cmp16 = k.sb('cmp16', [128, 16], F32)
            start = k.sb('start', [128, 32], F32); endt = k.sb('endt', [128, 32], F32); sstart = k.sb('sstart', [128, 32], F32)
            et = k.sb('et', [128, NTILE], F32); wf = k.sb('wf', [128, NTILE, 2], F32)
            p1f = k.sb('p1f', [128, NBLK], F32); p2f = k.sb('p2f', [128, NBLK], F32)
            for e_ in range(32):
                k.op('dve', lambda e: e.tensor_scalar(out=cmp16[:], in0=thr16, scalar1=run_bc[:, e_:e_ + 1], scalar2=None,
                                                      op0=ALU.is_lt, op1=ALU.add, accum_out=ptile[:, e_:e_ + 1]),
                     reads=[rc, run_bc], writes=[cmp16, ptile], partial=True)
            k.memset(start[:, 0:1], 0.0, [start], partial=True)
            for e_ in range(1, 32):
                k.tt(start[:, e_:e_ + 1], start[:, e_ - 1:e_], ptile[:, e_ - 1:e_], ALU.add, [start, ptile], [start], partial=True)
            k.tt(endt[:], start[:], ptile[:], ALU.add, [start, ptile], [endt])
            k.ts(sstart[:], start[:], float(TS), ALU.mult, [start], [sstart])
            k.memset(et[:], 0.0, [et])
            for e_ in range(32):
                k.stt(et[:], iota64, endt[:, e_:e_ + 1], et[:], ALU.is_ge, ALU.add, [rc, endt, et], [et])
            k.ts(et[:], et[:], 31.0, ALU.min, [et], [et])
            same = k.sb('same', [128, NTILE], F32)
            k.memset(same[:, 0:1], 0.0, [same], partial=True)
            k.tt(same[:, 1:NTILE], et[:, 1:NTILE], et[:, 0:NTILE - 1], ALU.is_equal, [et], [same], partial=True)
            for j in range(2):
                k.ts(wf[:, :, j], et[:], 256.0, ALU.mult, [et, rc], [wf], s2=pbase2[:, j:j + 1], op1=ALU.add, partial=True)
                k.stt(wf[:, :, j], same[:], 1.0e7, wf[:, :, j], ALU.mult, ALU.add, [same, wf], [wf], partial=True)
            k.copy(widx[:], wf[:], [wf], [widx])
            for gb in range(NBLK):
                k.ts(m1[:], iota32, e1s[:, gb:gb + 1], ALU.is_equal, [rc, e1s], [m1])
                k.op('dve', lambda e: e.scalar_tensor_tensor(out=jk[:], in0=m1[:], scalar=1.0, in1=sstart[:], op0=ALU.mult, op1=ALU.mult,
                                                          accum_out=p1f[:, gb:gb + 1]), reads=[m1, sstart], writes=[jk, p1f], partial=True)
                k.ts(m2[:], iota32, e2s[:, gb:gb + 1], ALU.is_equal, [rc, e2s], [m2])
                k.op('dve', lambda e: e.scalar_tensor_tensor(out=jk[:], in0=m2[:], scalar=1.0, in1=sstart[:], op0=ALU.mult, op1=ALU.mult,
                                                          accum_out=p2f[:, gb:gb + 1]), reads=[m2, sstart], writes=[jk, p2f], partial=True)
            k.tt(p1f[:], p1f[:], r1s[:], ALU.add, [p1f, r1s], [p1f])
            k.tt(p2f[:], p2f[:], r2s[:], ALU.add, [p2f, r2s], [p2f])
            k.copy(pos1i[:], p1f[:], [p1f], [pos1i]); k.copy(pos2i[:], p2f[:], [p2f], [pos2i])

        if last:
            fin = k.sb('fin', [128, 8], F32)
            k.dma('sp', fin[:], A['fin8'], writes=[fin])
        with k.scope():
            hr_ = [k.sb('hr%d' % i, [128, 1024], BF16) for i in range(2)]
            for gb in range(NBLK):
                hb_ = hr_[gb % 2]
                k.dma('sp', hb_[:], A['H2K'][gb * 128:(gb + 1) * 128, :], reads=[H2K], writes=[hb_])
                k.idma(A['HS'][:, :], hb_[:, :], pos1i[:, gb:gb + 1], True, [hb_, pos1i], [HSb], hb_)
                k.idma(A['HS'][:, :], hb_[:, :], pos2i[:, gb:gb + 1], True, [hb_, pos2i], [HSb], hb_)
        with k.scope():
            wg0 = k.sb('wg0', [128, 4096], BF16); wu0 = k.sb('wu0', [128, 4096], BF16); wd0 = k.sb('wd0', [128, 4096], BF16)
            wg = [wg0, wg0]; wu = [wu0, wu0]; wd = [wd0, wd0]
            hst = [k.sb('hst%d' % i, [128, 2, 1024], BF16) for i in range(2)]
            hTs = [k.sb('hTs%d' % i, [128, 8, TS], BF16) for i in range(2)]
            sg = [k.sb('sg%d' % i, [128, TS], F32) for i in range(2)]
            hid = [k.sb('hid%d' % i, [128, 4, TS], BF16) for i in range(2)]
            ys = [k.sb('ys%d' % i, [128, 2, 1024], F32) for i in range(2)]
            HSv = A['HS'].rearrange("(i sb p) n -> i p sb n", sb=2, p=128)
            YSv = A['YS'].rearrange("(i sb p) n -> i p sb n", sb=2, p=128)

            def load_w(i, which):
                for (wt, src) in which:
                    for j in range(2):
                        k.idma(wt[:, j * 2048:(j + 1) * 2048], src[:, :], widx[:, i, j:j + 1], False, [widx], [wt], wt, partial=(j > 0), bound=8191)

            GU = ((wg0, A['w_eg']), (wu0, A['w_eu'])); DN = ((wd0, A['w_ed']),)
            load_w(0, GU); load_w(0, DN)
            for i in range(NTILE):
                bi = i % 2
                hs_ = hst[bi]; hT_ = hTs[bi]; hd = hid[bi]; y_ = ys[bi]
                k.dma('sp', hs_[:], HSv[i], reads=[HSb], writes=[hs_])
                for sb in range(2):
                    pH = rot.next()
                    for kc in range(8):
                        k.tr(pH.bf[:, kc * 128:(kc + 1) * 128], hs_[:, sb, kc * 128:(kc + 1) * 128], X.id_bf[:, :],
                             [hs_, X.id_bf], [pH], partial=(kc > 0))
                    k.act(hT_[:, :, sb * 128:(sb + 1) * 128], pH.bf[:, :].rearrange("p (kc s) -> p kc s", kc=8), AF.Copy,
                          [pH], [hT_], partial=(sb > 0))
                for fc in range(4):
                    pg = rot.next(); pu = rot.next()
                    for kc in range(8):
                        k.mm(pg[:, 0:TS], wg[bi][:, kc * 512 + fc * 128: kc * 512 + (fc + 1) * 128], hT_[:, kc, :], kc == 0, kc == 7, [wg[bi], hT_], [pg])
                    for kc in range(8):
                        k.mm(pu[:, 0:TS], wu[bi][:, kc * 512 + fc * 128: kc * 512 + (fc + 1) * 128], hT_[:, kc, :], kc == 0, kc == 7, [wu[bi], hT_], [pu])
                    s_ = sg[fc % 2]
                    k.act(s_[:], pg[:, 0:TS], AF.Silu, [pg], [s_])
                    k.tt(hd[:, fc, :], pu[:, 0:TS], s_[:], ALU.mult, [pu, s_], [hd], partial=(fc > 0))
                if i + 1 < NTILE:
                    load_w(i + 1, GU)
                for sb in range(2):
                    for dh in range(2):
                        pd = rot.next()
                        for fc in range(4):
                            k.mm(pd[:, :], hd[:, fc, sb * 128:(sb + 1) * 128], wd[bi][:, fc * 1024 + dh * 512: fc * 1024 + (dh + 1) * 512],
                                 fc == 0, fc == 3, [hd, wd[bi]], [pd])
                        if (sb + dh) % 2 == 0:
                            k.act(y_[:, sb, dh * 512:(dh + 1) * 512], pd[:, :], AF.Copy, [pd], [y_], partial=True)
                        else:
                            k.copy(y_[:, sb, dh * 512:(dh + 1) * 512], pd[:, :], [pd], [y_], partial=True)
                if i + 1 < NTILE:
                    load_w(i + 1, DN)
                k.dma('sp', YSv[i], y_[:], reads=[y_], writes=[YSb], partial=True)
        with k.scope():
            y1 = [k.sb('y1_%d' % i, [128, 1024], F32) for i in range(2)]
            y2 = [k.sb('y2_%d' % i, [128, 1024], F32) for i in range(2)]
            mo = k.sb('mo', [128, 4, 1024], F32)
            x1 = [k.sb('x1_%d' % i, [128, 8, TT], F32) for i in range(2)]
            sqf = k.sb('sqf', [128, 8, TT], BF16)
            rt2 = k.sb('rt2', [128, TT], F32); rs2 = k.sb('rs2', [128, TT], F32)
            for t in range(NTO):
                gs = slice(t * TT, (t + 1) * TT)
                xb = x1[t % 2]
                k.dma('sp', xb[:], X1v[:, :, gs], reads=[X1T], writes=[xb])
                for blk in range(4):
                    gb = t * 4 + blk
                    a1 = y1[blk % 2]; a2 = y2[blk % 2]
                    k.idma(a1[:, :], A['YS'][:, :], pos1i[:, gb:gb + 1], False, [YSb, pos1i], [a1], a1, partial=False)
                    k.idma(a2[:, :], A['YS'][:, :], pos2i[:, gb:gb + 1], False, [YSb, pos2i], [a2], a2, partial=False)
                    k.ts(mo[:, blk, :], a1[:], w1s[:, gb:gb + 1], ALU.mult, [a1, w1s], [mo], partial=(blk > 0))
                    k.stt(mo[:, blk, :], a2[:], w2s[:, gb:gb + 1], mo[:, blk, :], ALU.mult, ALU.add, [a2, w2s, mo], [mo], partial=True)
                for dc in range(8):
                    pM = rot.next()
                    for blk in range(4):
                        k.tr(pM[:, blk * 128:(blk + 1) * 128], mo[:, blk, dc * 128:(dc + 1) * 128], X.id_f[:, :],
                             [mo, X.id_f], [pM], partial=(blk > 0))
                    k.stt(xb[:, dc, :], pM[:, :], modT[:, 40 + dc:41 + dc], xb[:, dc, :], ALU.mult, ALU.add,
                          [pM, modT, xb], [xb], partial=True)
                if last:
                    for kc in range(8):
                        k.act(sqf[:, kc, :], xb[:, kc, :], AF.Square, [xb], [sqf], partial=(kc > 0))
                    pss = rot.next()
                    for kc in range(8):
                        k.mm(pss[:, :], X.ones_bf[:, :], sqf[:, kc, :], kc == 0, kc == 7, [X.ones_bf, sqf], [pss])
                    k.act(rt2[:], pss[:, :], AF.Sqrt, [pss], [rt2], bias=EPS, scale=1.0 / D)
                    k.recip(rs2[:], rt2[:], [rt2], [rs2])
                    for kc in range(8):
                        k.stt(xb[:, kc, :], xb[:, kc, :], fin[:, kc:kc + 1], rs2[:], ALU.mult, ALU.mult,
                              [xb, fin, rs2], [xb], partial=True)
                k.dma('pool', XOv[:, :, gs], xb[:], reads=[xb], writes=[XO], partial=True)


FUSED_IN = {
    'xT': ([D, S], F32), 'cc8': ([128, 8], F32), 'w_ada': ([2, D, 6 * D], F32), 'b_ada8': ([2, 128, 48], F32),
    'gmix8': ([2, 128, 8], F32), 'gffn8': ([2, 128, 8], F32), 'fin8': ([128, 8], F32),
    'w_a': ([2, 2, D, 1504], F32), 'w_b': ([2, 2, D, 1536], F32), 'pos': ([1, S], I32),
    'qg3': ([2, 128, 3], F32), 'kvg2': ([2, 128, 2], F32), 'wuq': ([2, 2, 384, 384], F32),
    'wuk': ([2, 2, 256, 256], F32), 'wuv': ([2, 2, 256, 256], F32), 'convw': ([2, 2, 128, 2, 3], F32),
    'ident': ([128, 128], F32), 'prot': ([128, 2, 128], F32), 'invf': ([128, 2], F32),
    'rmask': ([2, 128, 2, 128], F32), 'qdec': ([2, 128, 2, TT], F32), 'kdec': ([2, 128, 2], F32),
    'sdec': ([2, 128, 2], F32), 'sel65': ([128, 64], F32),
    'w_g': ([2, D, 3072], F32), 'w_o': ([2, 2048, D], F32), 'w_mix': ([2, D, D], F32), 'w_r': ([2, D, 36], F32),
    'b_r': ([2, 1, 36], F32), 'w_eg0': ([8192, 2048], F32), 'w_eu0': ([8192, 2048], F32), 'w_ed0': ([8192, 2048], F32),
    'w_eg1': ([8192, 2048], F32), 'w_eu1': ([8192, 2048], F32), 'w_ed1': ([8192, 2048], F32), 'rconst': ([128, 370], F32),
}


def build_fused(nc, A):
    root = ExitStack()
    with root:
        k = K(nc, root)
        X = setup_common(k, A)
        V_sb = k.sb('V_sb', [128, 64, 4, 65], BF16)
        k.memset(V_sb[:, :, :, 64:65], 1.0, [V_sb], eng='pool')
        bufs = {n: Buf(n, A[n], dram=True) for n in ('HT', 'ZT', 'QTd', 'KTd', 'XO', 'X1T', 'H2K', 'HS', 'YS', 'xT', 'out', 'HTo', 'ZTo', 'XOo')}
        modT = [k.sb('modT%d' % l, [128, 48], F32) for l in range(2)]
        for l in range(2):
            emit_mod(k, X, {'cc8': A['cc8'], 'b_ada8': A['b_ada8'][l], 'w_ada': A['w_ada'][l]}, modT[l])
        pid = nc.sync.partition_id()
        for l in range(2):
            xsrc, xsb = (A['xT'], bufs['xT']) if l == 0 else (A['XO'], bufs['XO'])
            for hh in range(2):
                Am = dict(gmix8=A['gmix8'][l], invf=A['invf'], prot=A['prot'], HT=A['HT'], xT=xsrc,
                          HT_buf=bufs['HT'], ZT_buf=bufs['ZT'], QTd_buf=bufs['QTd'], KTd_buf=bufs['KTd'],
                          QTd=A['QTd'], KTd=A['KTd'], w_a=A['w_a'][l, hh], w_b=A['w_b'][l, hh],
                          qg3=A['qg3'][l], kvg2=A['kvg2'][l], wuq=A['wuq'][l, hh], wuk=A['wuk'][l, hh], wuv=A['wuv'][l, hh],
                          convw=A['convw'][l, hh], pos=A['pos'], rmask=A['rmask'][hh], qdec=A['qdec'][hh],
                          kdec=A['kdec'][hh], sdec=A['sdec'][hh], sel65=A['sel65'],
                          ZTm=A['ZT'][hh * 256:(hh + 1) * 256, :], ZTc=A['ZT'][512 + hh * 256:512 + (hh + 1) * 256, :],
                          ZTr=A['ZT'][1024 + hh * 512:1024 + (hh + 1) * 512, :])
                with k.scope():
                    emit_mixer(k, X, Am, modT[l], V_sb, write_ht=(hh == 0))
            last = (l == 1)
            if last:
                stg = Buf('stage')
                hoff = pid % 2 * SO
                for (dst, src) in (('HTo', 'HT'), ('ZTo', 'ZT'), ('XOo', 'XO')):
                    k.dma('sp', A[dst][:, :], A[src][:, bass.ds(hoff, SO)], reads=[bufs[src]], writes=[bufs[dst]], owner=stg)
            for th in ((None,) if last else (0, 1)):
                if last:
                    tok = lambda q, c0, n: slice(c0, c0 + n)
                    xo = A['out']; xob = bufs['out']
                    srcs = dict(xs_buf=bufs['XOo'], xT=A['XOo'], HT=A['HTo'], ZT=A['ZTo'], HT_buf=bufs['HTo'], ZT_buf=bufs['ZTo'])
                else:
                    tok = (lambda th_: (lambda q, c0, n: slice(th_ * SO + c0, th_ * SO + c0 + n)))(th)
                    xo = A['XO'][:, th * SO:(th + 1) * SO]; xob = bufs['XO']
                    srcs = dict(xs_buf=xsb, xT=xsrc, HT=A['HT'], ZT=A['ZT'], HT_buf=bufs['HT'], ZT_buf=bufs['ZT'])
                Af = dict(gffn8=A['gffn8'][l], X1T_buf=bufs['X1T'], H2K_buf=bufs['H2K'], HS_buf=bufs['HS'], YS_buf=bufs['YS'], xo_buf=xob,
                          rconst=A['rconst'], H2K=A['H2K'], HS=A['HS'], YS=A['YS'],
                          X1T=A['X1T'], xoT=xo, w_g=A['w_g'][l], w_o=A['w_o'][l],
                          w_mix=A['w_mix'][l], w_r=A['w_r'][l], b_r=A['b_r'][l], w_eg=A['w_eg%d' % l], w_eu=A['w_eu%d' % l],
                          w_ed=A['w_ed%d' % l], fin8=A['fin8'], **srcs)
                with k.scope():
                    emit_ffn(k, X, Af, modT[l], last, tok)
        k.barrier()
    return nc


def make_fused_nc():
    nc = bass.Bass("TRN2", target_bir_lowering=False)
    A = {}
    for name, (shape, dt) in FUSED_IN.items():
        A[name] = nc.dram_tensor(name, shape, dt, kind="ExternalInput").ap()
    A['out'] = nc.dram_tensor('out', [D, SO], F32, kind="ExternalOutput").ap()
    for name, shape, dt in (('HT', [D, S], BF16), ('ZT', [2048, S], BF16), ('QTd', [4, 96, S], BF16),
                            ('KTd', [4, 96, S], BF16), ('XO', [D, S], F32), ('X1T', [D, SO], F32),
                            ('H2K', [SO, D], BF16), ('HS', [16384, D], BF16), ('YS', [16384, D], F32), ('HTo', [D, SO], BF16),
                            ('ZTo', [2048, SO], BF16), ('XOo', [D, SO], F32)):
        A[name] = nc.dram_tensor(name, shape, dt, kind="Internal").ap()
    build_fused(nc, A)
    return nc


def fused_inputs(inp, b):
    L = range(2)
    m = {}
    c0 = consts_for(0); c1 = consts_for(1)
    for kk in ('ident', 'prot', 'invf', 'sel65'):
        m[kk] = c0[kk]
    for kk in ('rmask', 'qdec', 'kdec', 'sdec'):
        m[kk] = np.stack([c0[kk], c1[kk]], axis=0)
    mi = [[mixer_inputs(inp, l, b, hh, None) for hh in range(2)] for l in L]
    m['xT'] = np.ascontiguousarray(inp['x'][b].T)
    m['cc8'] = mi[0][0]['cc8']; m['pos'] = mi[0][0]['pos']
    m['w_ada'] = np.ascontiguousarray(inp['w_ada'])
    for kk in ('b_ada8', 'gmix8', 'qg3', 'kvg2'):
        m[kk] = np.stack([mi[l][0][kk] for l in L], axis=0)
    for kk in ('w_a', 'w_b', 'wuq', 'wuk', 'wuv', 'convw'):
        m[kk] = np.stack([np.stack([mi[l][hh][kk] for hh in range(2)], axis=0) for l in L], axis=0)
    m['gffn8'] = np.stack([col8(inp['norm_ffn_g'][l]) for l in L], axis=0)
    m['fin8'] = col8(inp['final_g'])
    m['w_g'] = np.ascontiguousarray(inp['w_in'][:, :, OFF['gl']:OFF['gl'] + 3072])
    m['w_o'] = np.concatenate([inp['w_o_mla'], inp['w_o_conv'], inp['w_o_ret']], axis=1)
    m['w_mix'] = np.ascontiguousarray(inp['w_mix_out'])
    m['w_r'] = np.concatenate([inp['w_route_group'], inp['w_route_expert']], axis=2)
    m['b_r'] = np.concatenate([inp['b_route_group'], inp['b_route_expert']], axis=1)[:, None, :].astype(np.float32)
    for l in L:
        m['w_eg%d' % l] = np.ascontiguousarray(inp['w_exp_gate'][l].reshape(32, 8, 128, 512).transpose(0, 2, 1, 3)).reshape(8192, 2048)
        m['w_eu%d' % l] = np.ascontiguousarray(inp['w_exp_up'][l].reshape(32, 8, 128, 512).transpose(0, 2, 1, 3)).reshape(8192, 2048)
        m['w_ed%d' % l] = np.ascontiguousarray(inp['w_exp_down'][l].reshape(32, 4, 128, 1024).transpose(0, 2, 1, 3)).reshape(8192, 2048)
    rcst = np.zeros((128, 370), np.float32)
    rcst[:, 0:128] = np.triu(np.ones((128, 128), np.float32), 1)
    rcst[:, 128:256] = 1.0
    rcst[:, 256:320] = np.arange(64, dtype=np.float32)[None, :]
    rcst[:, 320:336] = (np.arange(16, dtype=np.float32) * 256.0)[None, :]
    rcst[:, 336:368] = np.arange(32, dtype=np.float32)[None, :]
    rcst[:, 368] = 2.0 * np.arange(128); rcst[:, 369] = 2.0 * np.arange(128) + 1.0
    m['rconst'] = rcst
    return m


_NC_CACHE = {}


def kernel(**inp):
    inp = {k_: np.asarray(v) for k_, v in inp.items()}
    B = inp['x'].shape[0]
    cores = list(range(8))
    if 'fused' not in _NC_CACHE:
        _NC_CACHE['fused'] = make_fused_nc()
    per_b = [fused_inputs(inp, b) for b in range(B)]
    maps = [per_b[c // 2] for c in cores]
    res = run_bass_kernel_spmd(_NC_CACHE['fused'], maps, core_ids=cores).results
    out = np.empty((B, S, D), np.float32)
    for c in cores:
        b, th = c // 2, c % 2
        out[b, th * SO:(th + 1) * SO, :] = np.asarray(res[c]['out']).T
    return out
```

```python
import math
from contextlib import ExitStack, contextmanager
import numpy as np
import ml_dtypes
import concourse.bass as bass
import concourse.mybir as mybir
from concourse.bass_utils import run_bass_kernel_spmd

F32 = mybir.dt.float32; BF16 = mybir.dt.bfloat16; I32 = mybir.dt.int32
ALU = mybir.AluOpType; AF = mybir.ActivationFunctionType

D = 1024; S = 8192; TT = 512; NT = S // TT
EPS = 1e-6
TWO_PI = 2.0 * math.pi
CW1 = 6.28125
CW2 = TWO_PI - CW1
PI_LO = 3.1415925
MLA_SCALE = 96.0 ** -0.5
RET_KS = 128.0 ** -0.5


class Buf:
    def __init__(self, name, ap=None, dram=False):
        self.name = name; self.ap = ap; self.dram = dram
        self.writers = {}; self.readers = {}
        self.dsem = None; self.dval = 0; self.dkey = None; self.psum = False

    def __getitem__(self, idx):
        return self.ap[idx]


class Rot:
    def __init__(self, bufs):
        self.bufs = bufs; self.i = 0

    def next(self):
        b = self.bufs[self.i % len(self.bufs)]; self.i += 1
        return b


class K:
    def __init__(self, nc, root):
        self.nc = nc; self.root = root; self.stacks = [root]
        self.eng = {'pe': nc.tensor, 'dve': nc.vector, 'act': nc.scalar, 'pool': nc.gpsimd, 'sp': nc.sync}
        self.sem = {}; self.cnt = {}
        for e in self.eng:
            self.sem[e] = root.enter_context(nc.semaphore('s_' + e)); self.cnt[e] = 0
        self.waited = {e: {} for e in self.eng}
        self.dma_latest = {}
        self.uid = 0
        self.free_dsems = []
        self.bound_regs = {}
        self.scope_bufs = [[]]

    def sb(self, name, shape, dt):
        self.uid += 1
        t = self.stacks[-1].enter_context(self.nc.sbuf_tensor('%s_%d' % (name, self.uid), shape, dt))
        b = Buf(name, t)
        self.scope_bufs[-1].append(b)
        return b

    def ps(self, name, shape, dt=F32):
        t = self.root.enter_context(self.nc.psum_tensor(name, shape, dt))
        b = Buf(name, t); b.psum = True
        return b

    def dram(self, name, shape, dt, kind="Internal"):
        t = self.nc.dram_tensor(name, shape, dt, kind=kind).ap()
        return Buf(name, t, dram=True)

    @contextmanager
    def scope(self):
        st = ExitStack()
        self.stacks.append(st)
        self.scope_bufs.append([])
        try:
            yield
        finally:
            self.barrier()
            for b in self.scope_bufs.pop():
                if b.dsem is not None:
                    self.free_dsems.append((b.dsem, b.dval, b.dkey))
                    b.dsem = None
            self.stacks.pop()
            st.close()

    def _wait(self, e, sem, val, key):
        if self.waited[e].get(key, 0) >= val:
            return
        self.waited[e][key] = val
        self.eng[e].wait_ge(sem, val)

    def _deps(self, e, reads, writes, partial):
        for b in reads:
            for key, (sem, val) in b.writers.items():
                if e == 'pe' and key == 'pe':
                    continue
                self._wait(e, sem, val, key)
            if b.psum:
                for key, (sem, val) in b.readers.items():
                    if key != e:
                        self._wait(e, sem, val, key)
        for b in writes:
            if not partial:
                for key, (sem, val) in b.writers.items():
                    if e == 'pe' and key == 'pe':
                        continue
                    self._wait(e, sem, val, key)
            for key, (sem, val) in b.readers.items():
                if key == e:
                    continue
                self._wait(e, sem, val, key)

    def _mark(self, sem, val, key, reads, writes, partial):
        for b in reads:
            b.readers[key] = (sem, val)
        for b in writes:
            if partial:
                b.writers[key] = (sem, val)
            else:
                b.writers = {key: (sem, val)}

    def op(self, e, fn, reads=(), writes=(), partial=False):
        self._deps(e, reads, writes, partial)
        ins = fn(self.eng[e])
        self.cnt[e] += 1
        ins.then_inc(self.sem[e], 1)
        self._mark(self.sem[e], self.cnt[e], e, reads, writes, partial)
        return ins

    def dma(self, q, out_ap, in_ap, reads=(), writes=(), owner=None, partial=False, **kw):
        if owner is None:
            owner = [b for b in list(writes) + list(reads) if not b.dram][0]
        if owner.dsem is None:
            if self.free_dsems:
                owner.dsem, owner.dval, owner.dkey = self.free_dsems.pop()
            else:
                self.uid += 1
                owner.dkey = 'dsem%d' % self.uid
                owner.dsem = self.root.enter_context(self.nc.semaphore(owner.dkey))
        self._deps(q, reads, writes, partial)
        if owner.dval > 0:
            self._wait(q, owner.dsem, owner.dval, owner.dkey)
        ins = self.eng[q].dma_start(out=out_ap, in_=in_ap, **kw)
        owner.dval += 16
        ins.then_inc(owner.dsem, 16)
        self._mark(owner.dsem, owner.dval, owner.dkey, reads, writes, partial)
        self.dma_latest[owner.dkey] = (owner.dsem, owner.dval)
        return ins

    def idma(self, out_ap, in_ap, idx_ap, scatter, reads, writes, owner, partial=True, bound=None):
        q = 'pool'
        if owner.dsem is None:
            if self.free_dsems:
                owner.dsem, owner.dval, owner.dkey = self.free_dsems.pop()
            else:
                self.uid += 1
                owner.dkey = 'dsem%d' % self.uid
                owner.dsem = self.root.enter_context(self.nc.semaphore(owner.dkey))
        self._deps(q, reads, writes, partial)
        if owner.dval > 0:
            self._wait(q, owner.dsem, owner.dval, owner.dkey)
        off = bass.IndirectOffsetOnAxis(ap=idx_ap, axis=0)
        if scatter:
            ins = self.nc.gpsimd.indirect_dma_start(out=out_ap, out_offset=off, in_=in_ap, in_offset=None)
        else:
            if bound is None:
                ins = self.nc.gpsimd.indirect_dma_start(out=out_ap, out_offset=None, in_=in_ap, in_offset=off)
            else:
                if bound not in self.bound_regs:
                    self.bound_regs[bound] = self.nc.gpsimd.to_reg(bound)
                ins = self.nc.gpsimd.indirect_dma_start(out=out_ap, out_offset=None, in_=in_ap, in_offset=off,
                                                        bounds_check=self.bound_regs[bound], oob_is_err=False)
        owner.dval += 16
        ins.then_inc(owner.dsem, 16)
        self._mark(owner.dsem, owner.dval, owner.dkey, reads, writes, partial)
        self.dma_latest[owner.dkey] = (owner.dsem, owner.dval)
        return ins

    def barrier(self):
        for e in self.eng:
            for e2 in self.eng:
                if e2 != e and self.cnt[e2] > 0:
                    self._wait(e, self.sem[e2], self.cnt[e2], e2)
            for key, (sem, val) in self.dma_latest.items():
                self._wait(e, sem, val, key)

    def mm(self, out, lhsT, rhs, start, stop, reads, writes):
        return self.op('pe', lambda e: e.matmul(out, lhsT, rhs, start=start, stop=stop),
                       reads=reads, writes=writes, partial=not start)

    def tr(self, out, in_, ident, reads, writes, partial=True):
        return self.op('pe', lambda e: e.transpose(out, in_, ident), reads=reads, writes=writes, partial=partial)

    def act(self, out, in_, func, reads, writes, bias=None, scale=None, accum_out=None, partial=False, eng='act'):
        kw = {}
        if bias is not None: kw['bias'] = bias
        if scale is not None: kw['scale'] = scale
        if accum_out is not None: kw['accum_out'] = accum_out
        return self.op(eng, lambda e: e.activation(out=out, in_=in_, func=func, **kw),
                       reads=reads, writes=writes, partial=partial)

    def tt(self, out, in0, in1, op, reads, writes, partial=False, eng='dve'):
        return self.op(eng, lambda e: e.tensor_tensor(out=out, in0=in0, in1=in1, op=op),
                       reads=reads, writes=writes, partial=partial)

    def ts(self, out, in0, s1, op0, reads, writes, s2=None, op1=None, partial=False, eng='dve'):
        if op1 is None:
            return self.op(eng, lambda e: e.tensor_scalar(out=out, in0=in0, scalar1=s1, scalar2=None, op0=op0),
                           reads=reads, writes=writes, partial=partial)
        return self.op(eng, lambda e: e.tensor_scalar(out=out, in0=in0, scalar1=s1, scalar2=s2, op0=op0, op1=op1),
                       reads=reads, writes=writes, partial=partial)

    def stt(self, out, in0, scalar, in1, op0, op1, reads, writes, partial=False):
        return self.op('dve', lambda e: e.scalar_tensor_tensor(out=out, in0=in0, scalar=scalar, in1=in1, op0=op0, op1=op1),
                       reads=reads, writes=writes, partial=partial)

    def copy(self, out, in_, reads, writes, partial=False, eng='dve'):
        return self.op(eng, lambda e: e.tensor_copy(out=out, in_=in_), reads=reads, writes=writes, partial=partial)

    def recip(self, out, in_, reads, writes, partial=False):
        return self.op('dve', lambda e: e.reciprocal(out=out, in_=in_), reads=reads, writes=writes, partial=partial)

    def memset(self, ap, val, writes, eng='dve', partial=False, reads=()):
        return self.op(eng, lambda e: e.memset(ap, val), reads=reads, writes=writes, partial=partial)


def load_cast(k, dst, dst_ap_fn, src_ap_fn, ncols, step=2048):
    c = 0
    while c < ncols:
        n = min(step, ncols - c)
        k.dma('pool', dst_ap_fn(c, n), src_ap_fn(c, n), writes=[dst], partial=True)
        c += n


class Ctx:
    pass


def setup_common(k, A):
    X = Ctx()
    X.banks = [k.ps('bank%d' % i, [128, 512], F32) for i in range(8)]
    for b in X.banks:
        b.bf = b.ap.bitcast(BF16)
    X.ones_bf = k.sb('ones_bf', [128, 128], BF16)
    k.memset(X.ones_bf[:], 1.0, [X.ones_bf])
    X.id_f = k.sb('id_f', [128, 128], F32)
    k.dma('sp', X.id_f[:], A['ident'][:, :], writes=[X.id_f])
    X.id_bf = k.sb('id_bf', [128, 128], BF16)
    k.copy(X.id_bf[:], X.id_f[:], [X.id_f], [X.id_bf])
    return X


def rope_tables(k, W, pos_ap, invcol, cosb, sinb):
    k.dma('sp', W.posi[:], pos_ap.to_broadcast([128, TT]), writes=[W.posi])
    k.copy(W.ang[:], W.posi[:], [W.posi], [W.ang])
    k.ts(W.ang[:], W.ang[:], invcol, ALU.mult, [W.ang], [W.ang])
    k.ts(W.kq[:], W.ang[:], 1.0 / TWO_PI, ALU.mult, [W.ang], [W.kq])
    k.copy(W.kf[:], W.kq[:], [W.kq], [W.kf])
    k.stt(W.r1[:], W.kf[:], -CW1, W.ang[:], ALU.mult, ALU.add, [W.kf, W.ang], [W.r1])
    k.stt(W.r1[:], W.kf[:], -CW2, W.r1[:], ALU.mult, ALU.add, [W.kf, W.r1], [W.r1])
    k.ts(W.r1[:], W.r1[:], PI_LO, ALU.min, [W.r1], [W.r1], s2=-PI_LO, op1=ALU.max)
    k.act(sinb[:], W.r1[:], AF.Sin, [W.r1], [sinb])
    k.stt(W.kf[:], W.r1[:], -1.0, W.r1[:], ALU.mult, ALU.max, [W.r1], [W.kf])
    k.act(cosb[:], W.kf[:], AF.Sin, [W.kf], [cosb], bias=W.halfpi[:, 0:1], scale=-1.0)


def rope_work(k):
    W = Ctx()
    W.posi = k.sb('posi', [128, TT], I32)
    W.ang = k.sb('ang', [128, TT], F32)
    W.kq = k.sb('kq', [128, TT], I32)
    W.kf = k.sb('kf', [128, TT], F32)
    W.r1 = k.sb('r1', [128, TT], F32)
    W.halfpi = k.sb('halfpi', [128, 1], F32)
    k.memset(W.halfpi[:], math.pi / 2.0, [W.halfpi])
    return W


def emit_mod(k, X, A, modT):
    with k.scope():
        cc = k.sb('cc', [128, 8], F32)
        k.dma('sp', cc[:], A['cc8'][:, :], writes=[cc])
        cact = k.sb('cact', [128, 8], F32)
        k.act(cact[:], cc[:], AF.Silu, [cc], [cact])
        bada = k.sb('bada', [128, 48], F32)
        k.dma('sp', bada[:], A['b_ada8'][:, :], writes=[bada])
        wv = A['w_ada'].rearrange("(kc p) n -> p kc n", p=128)
        wb = [k.sb('wada%d' % i, [128, 8, 768], F32) for i in range(2)]
        pm = X.banks[0]
        for blk in range(8):
            w = wb[blk % 2]
            for kc in range(8):
                k.dma('sp', w[:, kc, :], wv[:, kc, blk * 768:(blk + 1) * 768], writes=[w], partial=(kc > 0))
            for j in range(6):
                jj = blk * 6 + j
                for kc in range(8):
                    k.mm(pm[:, jj:jj + 1], w[:, kc, j * 128:(j + 1) * 128], cact[:, kc:kc + 1],
                         kc == 0, kc == 7, [w, cact], [pm])
        k.tt(modT[:], pm[:, 0:48], bada[:], ALU.add, [pm, bada], [modT])


def emit_mixer(k, X, A, modT, V_sb, write_ht, phases=('A', 'B', 'S2')):
    if True:
        banks = X.banks
        rot = Rot(banks)
        gmix = k.sb('gmix', [128, 8], F32)
        k.dma('sp', gmix[:], A['gmix8'], writes=[gmix])
        gsc = k.sb('gsc', [128, 8], F32)
        k.stt(gsc[:], modT[:, 8:16], 1.0, gmix[:], ALU.add, ALU.mult, [modT, gmix], [gsc])
        invf = k.sb('invf', [128, 2], F32)
        k.dma('sp', invf[:], A['invf'], writes=[invf])
        prot_f = k.sb('prot_f', [128, 2, 128], F32)
        k.dma('sp', prot_f[:], A['prot'], writes=[prot_f])
        prot = k.sb('prot', [128, 2, 128], BF16)
        k.copy(prot[:], prot_f[:], [prot_f], [prot])
        HT = A['HT_buf']; ZT = A['ZT_buf']; QTd = A['QTd_buf']; KTd = A['KTd_buf']
        HTv = A['HT'].rearrange("(kc p) c -> p kc c", p=128)
        xTv = A['xT'].rearrange("(kc p) c -> p kc c", p=128)

        if 'A' in phases:
          with k.scope():
            NA = 1504
            w_a = k.sb('w_a', [128, 8, NA], BF16)
            wav = A['w_a'].rearrange("(kc p) n -> p kc n", p=128)
            for kc in range(8):
                load_cast(k, w_a, lambda c, n: w_a[:, kc, c:c + n], lambda c, n: wav[:, kc, c:c + n], NA, step=752)
            st_f = k.sb('st_f', [128, 3, 384], F32)
            qg = k.sb('qg', [128, 3], F32); kvg = k.sb('kvg', [128, 2], F32)
            k.dma('sp', qg[:], A['qg3'], writes=[qg]); k.dma('sp', kvg[:], A['kvg2'], writes=[kvg])
            wuq = k.sb('wuq', [128, 3, 384], BF16)
            k.dma('sp', st_f[:], A['wuq'].rearrange("(rc p) n -> p rc n", p=128), writes=[st_f])
            for rc in range(3):
                k.ts(wuq[:, rc, :], st_f[:, rc, :], qg[:, rc:rc + 1], ALU.mult, [st_f, qg], [wuq], partial=True)
            wuk = k.sb('wuk', [128, 2, 256], BF16); wuv = k.sb('wuv', [128, 2, 256], BF16)
            st2 = k.sb('st2', [128, 2, 256], F32); st3 = k.sb('st3', [128, 2, 256], F32)
            k.dma('sp', st2[:], A['wuk'].rearrange("(rc p) n -> p rc n", p=128), writes=[st2])
            k.dma('sp', st3[:], A['wuv'].rearrange("(rc p) n -> p rc n", p=128), writes=[st3])
            for rc in range(2):
                k.ts(wuk[:, rc, :], st2[:, rc, :], kvg[:, rc:rc + 1], ALU.mult, [st2, kvg], [wuk], partial=True)
                k.ts(wuv[:, rc, :], st3[:, rc, :], kvg[:, rc:rc + 1], ALU.mult, [st3, kvg], [wuv], partial=True)
            cw = k.sb('cw', [128, 2, 3], F32)
            k.dma('sp', cw[:], A['convw'], writes=[cw])
            xts = [k.sb('xt%d' % i, [128, 8, TT], F32) for i in range(2)]
            sq = k.sb('sq', [128, 8, TT], BF16)
            hTa = [k.sb('hT%d' % i, [128, 8, TT], BF16) for i in range(2)]
            rtmp = k.sb('rtmp', [128, TT], F32)
            rstd = k.sb('rstd', [128, TT], F32)
            hx = [k.sb('hx%d' % i, [128, TT], F32) for i in range(2)]
            lat_f = k.sb('lat_f', [128, 5, TT], F32)
            sql = k.sb('sql', [128, 5, TT], BF16)
            rq_bc = k.sb('rq_bc', [128, TT], F32); rkv_bc = k.sb('rkv_bc', [128, TT], F32)
            qn = k.sb('qn', [128, 3, TT], BF16); kvn = k.sb('kvn', [128, 2, TT], BF16)
            QTs = [k.sb('QTs%d' % i, [128, TT], BF16) for i in range(4)]
            KTs = [k.sb('KTs%d' % i, [128, TT], BF16) for i in range(4)]
            qr_f = k.sb('qr_f', [128, TT], F32)
            kr_f = k.sb('kr_f', [128, TT], F32); kr_b = k.sb('kr_b', [128, TT], BF16)
            kpe = k.sb('kpe', [128, TT], BF16)
            t1 = k.sb('t1', [128, TT], F32); t2 = k.sb('t2', [128, TT], F32)
            cosm = k.sb('cosm', [128, TT], F32); sinm = k.sb('sinm', [128, TT], F32)
            W = rope_work(k)
            u = [k.sb('u%d' % i, [128, TT + 2], F32) for i in range(2)]
            cxs = k.sb('cxs', [128, TT], F32); yv = k.sb('yv', [128, TT], F32)
            zc = [k.sb('zc%d' % i, [128, TT], BF16) for i in range(2)]
            for ch in range(2):
                k.memset(u[ch][:], 0.0, [u[ch]])

            def load_x(t):
                xt = xts[t % 2]
                for kc in range(8):
                    k.dma('sp', xt[:, kc, :], xTv[:, kc, t * TT:(t + 1) * TT], writes=[xt], partial=(kc > 0))

            load_x(0)
            for t in range(NT):
                cs = slice(t * TT, (t + 1) * TT)
                if t + 1 < NT:
                    load_x(t + 1)
                xt = xts[t % 2]
                hT = hTa[t % 2]
                for kc in range(8):
                    k.act(sq[:, kc, :], xt[:, kc, :], AF.Square, [xt], [sq], partial=(kc > 0))
                pss = rot.next()
                for kc in range(8):
                    k.mm(pss[:, :], X.ones_bf[:, :], sq[:, kc, :], kc == 0, kc == 7, [X.ones_bf, sq], [pss])
                k.act(rtmp[:], pss[:, :], AF.Sqrt, [pss], [rtmp], bias=EPS, scale=1.0 / D)
                k.recip(rstd[:], rtmp[:], [rtmp], [rstd])
                for kc in range(8):
                    hb = hx[kc % 2]
                    k.stt(hb[:], xt[:, kc, :], gsc[:, kc:kc + 1], rstd[:], ALU.mult, ALU.mult, [xt, gsc, rstd], [hb])
                    k.act(hT[:, kc, :], hb[:], AF.Identity, [hb, modT], [hT], bias=modT[:, kc:kc + 1], partial=(kc > 0))
                if write_ht:
                    k.dma('pool', HTv[:, :, cs], hT[:], reads=[hT], writes=[HT], partial=True)
                rope_tables(k, W, A['pos'][0:1, cs], invf[:, 1:2], cosm, sinm)
                for j in range(5):
                    ps = rot.next()
                    for kc in range(8):
                        k.mm(ps[:, :], w_a[:, kc, j * 128:(j + 1) * 128], hT[:, kc, :], kc == 0, kc == 7, [w_a, hT], [ps])
                    k.act(lat_f[:, j, :], ps[:, :], AF.Copy, [ps], [lat_f], partial=(j > 0))
                    k.act(sql[:, j, :], ps[:, :], AF.Square, [ps], [sql], partial=(j > 0))
                for ch in range(2):
                    pb = rot.next(); pc = rot.next(); px = rot.next()
                    for (pp, base) in ((pb, 736), (pc, 992), (px, 1248)):
                        for kc in range(8):
                            k.mm(pp[:, :], w_a[:, kc, base + ch * 128: base + (ch + 1) * 128], hT[:, kc, :],
                                 kc == 0, kc == 7, [w_a, hT], [pp])
                    k.act(cxs[:], px[:, :], AF.Copy, [px], [cxs])
                    U = u[ch]
                    if t > 0:
                        k.copy(U[:, 0:2], U[:, TT:TT + 2], [U], [U])
                    k.tt(U[:, 2:TT + 2], pc[:, :], cxs[:], ALU.mult, [pc, cxs, U], [U])
                    k.ts(yv[:], U[:, 2:TT + 2], cw[:, ch, 2:3], ALU.mult, [U, cw], [yv])
                    k.stt(yv[:], U[:, 1:TT + 1], cw[:, ch, 1:2], yv[:], ALU.mult, ALU.add, [U, cw, yv], [yv])
                    k.stt(yv[:], U[:, 0:TT], cw[:, ch, 0:1], yv[:], ALU.mult, ALU.add, [U, cw, yv], [yv])
                    Z = zc[ch]
                    k.tt(Z[:], pb[:, :], yv[:], ALU.mult, [pb, yv], [Z])
                    k.dma('pool', A['ZTc'][ch * 128:(ch + 1) * 128, cs], Z[:], reads=[Z], writes=[ZT], partial=True)
                for (j0, j1, n, dst) in ((0, 3, 384.0, rq_bc), (3, 5, 256.0, rkv_bc)):
                    ps = rot.next()
                    for j in range(j0, j1):
                        k.mm(ps[:, :], X.ones_bf[:, :], sql[:, j, :], j == j0, j == j1 - 1, [X.ones_bf, sql], [ps])
                    k.act(rtmp[:], ps[:, :], AF.Sqrt, [ps], [rtmp], bias=EPS, scale=1.0 / n)
                    k.recip(dst[:], rtmp[:], [rtmp], [dst])
                for j in range(3):
                    k.tt(qn[:, j, :], lat_f[:, j, :], rq_bc[:], ALU.mult, [lat_f, rq_bc], [qn], partial=(j > 0))
                for j in range(2):
                    k.tt(kvn[:, j, :], lat_f[:, 3 + j, :], rkv_bc[:], ALU.mult, [lat_f, rkv_bc], [kvn], partial=(j > 0))
                ps = rot.next()
                for kc in range(8):
                    k.mm(ps[0:96, :], w_a[:, kc, 640:736], hT[:, kc, :], kc == 0, kc == 7, [w_a, hT], [ps])
                k.act(kr_f[64:96, :], ps[64:96, :], AF.Copy, [ps], [kr_f])
                k.act(kr_b[0:96, :], ps[0:96, :], AF.Copy, [ps], [kr_b])
                ps2 = rot.next()
                k.mm(ps2[0:96, :], prot[0:96, 1, 0:96], kr_b[0:96, :], True, True, [prot, kr_b], [ps2])
                k.tt(t1[64:96, :], kr_f[64:96, :], cosm[64:96, :], ALU.mult, [kr_f, cosm], [t1])
                k.tt(t2[64:96, :], ps2[64:96, :], sinm[64:96, :], ALU.mult, [ps2, sinm], [t2])
                k.tt(kpe[64:96, :], t1[64:96, :], t2[64:96, :], ALU.add, [t1, t2], [kpe])
                for h in range(4):
                    k.dma('pool', A['KTd'][h, 64:96, cs], kpe[64:96, :], reads=[kpe], writes=[KTd], partial=True)
                for h in range(4):
                    ps = rot.next()
                    for rc in range(3):
                        k.mm(ps[0:96, :], wuq[:, rc, h * 96:(h + 1) * 96], qn[:, rc, :], rc == 0, rc == 2, [wuq, qn], [ps])
                    Q = QTs[h]
                    k.act(Q[0:96, :], ps[0:96, :], AF.Copy, [ps], [Q], scale=MLA_SCALE)
                    k.act(qr_f[64:96, :], ps[64:96, :], AF.Copy, [ps], [qr_f], scale=MLA_SCALE)
                    ps2 = rot.next()
                    k.mm(ps2[0:96, :], prot[0:96, 1, 0:96], Q[0:96, :], True, True, [prot, Q], [ps2])
                    k.tt(t1[64:96, :], qr_f[64:96, :], cosm[64:96, :], ALU.mult, [qr_f, cosm], [t1])
                    k.tt(t2[64:96, :], ps2[64:96, :], sinm[64:96, :], ALU.mult, [ps2, sinm], [t2])
                    k.tt(Q[64:96, :], t1[64:96, :], t2[64:96, :], ALU.add, [t1, t2], [Q])
                    k.dma('pool', A['QTd'][h, :, cs], Q[0:96, :], reads=[Q], writes=[QTd], partial=True)
                for h in range(4):
                    ps = rot.next()
                    for rc in range(2):
                        k.mm(ps[0:64, :], wuk[:, rc, h * 64:(h + 1) * 64], kvn[:, rc, :], rc == 0, rc == 1, [wuk, kvn], [ps])
                    Kt = KTs[h]
                    k.act(Kt[0:64, :], ps[0:64, :], AF.Copy, [ps], [Kt])
                    k.dma('pool', A['KTd'][h, 0:64, cs], Kt[0:64, :], reads=[Kt], writes=[KTd], partial=True)
                for blk in range(4):
                    ps = rot.next()
                    for rc in range(2):
                        k.mm(ps[:, 0:256], kvn[:, rc, blk * 128:(blk + 1) * 128], wuv[:, rc, :], rc == 0, rc == 1, [kvn, wuv], [ps])
                    k.act(V_sb[:, t * 4 + blk, :, 0:64], ps[:, 0:256].rearrange("p (h v) -> p h v", h=4), AF.Copy,
                          [ps], [V_sb], partial=True)

        if 'B' in phases:
          with k.scope():
            NB = 1536
            w_b = k.sb('w_b', [128, 8, NB], BF16)
            wbv = A['w_b'].rearrange("(kc p) n -> p kc n", p=128)
            for kc in range(8):
                load_cast(k, w_b, lambda c, n: w_b[:, kc, c:c + n], lambda c, n: wbv[:, kc, c:c + n], NB, step=768)
            hTs = [k.sb('hTb%d' % i, [128, 8, TT], BF16) for i in range(2)]
            rmask = k.sb('rmask', [128, 2, 128], F32)
            k.dma('sp', rmask[:], A['rmask'], writes=[rmask])
            qdec = k.sb('qdec', [128, 2, TT], F32)
            k.dma('sp', qdec[:], A['qdec'], writes=[qdec])
            kdec = k.sb('kdec', [128, 2], F32)
            k.dma('sp', kdec[:], A['kdec'], writes=[kdec])
            sdec = k.sb('sdec', [128, 2], F32)
            k.dma('sp', sdec[:], A['sdec'], writes=[sdec])
            cosr = k.sb('cosr', [128, TT], F32); sinr = k.sb('sinr', [128, TT], F32)
            W = rope_work(k)
            qf = k.sb('qf', [128, TT], F32); qb = k.sb('qb', [128, TT], BF16)
            t1 = k.sb('t1b', [128, TT], F32); t2 = k.sb('t2b', [128, TT], F32)
            RQT = [k.sb('RQT%d' % i, [128, TT], BF16) for i in range(2)]
            RQd = [k.sb('RQd%d' % i, [128, TT], BF16) for i in range(2)]
            RKT = [k.sb('RKT%d' % i, [128, TT], BF16) for i in range(2)]
            RKd = [k.sb('RKd%d' % i, [128, 4, 128], BF16) for i in range(2)]
            RVb = [k.sb('RV%d' % i, [128, 512], BF16) for i in range(4)]; Gb = [k.sb('G%d' % i, [128, 512], BF16) for i in range(4)]
            AT = [k.sb('AT%d' % i, [128, 128], BF16) for i in range(2)]
            st_f = [k.sb('stf%d' % i, [128, 256], F32) for i in range(2)]
            st_b = [k.sb('stb%d' % i, [128, 256], BF16) for i in range(2)]
            junk = k.sb('junk', [128, 256], BF16)
            ssq = k.sb('ssq', [128, 1], F32); sd = k.sb('sd', [128, 1], F32); rinv = k.sb('rinv', [128, 1], F32)
            zr = [k.sb('zr%d' % i, [128, 256], BF16) for i in range(2)]
            zT = [k.sb('zT%d' % i, [128, 2, TT], BF16) for i in range(2)]
            for hr in range(2):
                k.memset(st_f[hr][:], 0.0, [st_f[hr]]); k.memset(st_b[hr][:], 0.0, [st_b[hr]])

            def load_h(t):
                hb = hTs[t % 2]
                k.dma('sp', hb[:], HTv[:, :, t * TT:(t + 1) * TT], reads=[HT], writes=[hb])

            load_h(0)
            for t in range(NT):
                cs = slice(t * TT, (t + 1) * TT)
                if t + 1 < NT:
                    load_h(t + 1)
                hT = hTs[t % 2]
                rope_tables(k, W, A['pos'][0:1, cs], invf[:, 0:1], cosr, sinr)
                for hr in range(2):
                    ps = rot.next()
                    for kc in range(8):
                        k.mm(ps[:, :], w_b[:, kc, hr * 128:(hr + 1) * 128], hT[:, kc, :], kc == 0, kc == 7, [w_b, hT], [ps])
                    k.act(qf[:], ps[:, :], AF.Copy, [ps], [qf])
                    k.act(qb[:], ps[:, :], AF.Copy, [ps], [qb])
                    ps2 = rot.next()
                    k.mm(ps2[:, :], prot[:, 0, :], qb[:], True, True, [prot, qb], [ps2])
                    k.tt(t1[:], qf[:], cosr[:], ALU.mult, [qf, cosr], [t1])
                    k.tt(t2[:], ps2[:, :], sinr[:], ALU.mult, [ps2, sinr], [t2])
                    k.tt(t1[:], t1[:], t2[:], ALU.add, [t1, t2], [t1], eng='pool')
                    k.act(RQT[hr][:], t1[:], AF.Copy, [t1], [RQT[hr]])
                    k.tt(RQd[hr][:], t1[:], qdec[:, hr, :], ALU.mult, [t1, qdec], [RQd[hr]])
                    ps = rot.next()
                    for kc in range(8):
                        k.mm(ps[:, :], w_b[:, kc, 256 + hr * 128:256 + (hr + 1) * 128], hT[:, kc, :], kc == 0, kc == 7, [w_b, hT], [ps])
                    k.act(qf[:], ps[:, :], AF.Copy, [ps], [qf], scale=RET_KS)
                    k.act(qb[:], ps[:, :], AF.Copy, [ps], [qb], scale=RET_KS)
                    ps2 = rot.next()
                    k.mm(ps2[:, :], prot[:, 0, :], qb[:], True, True, [prot, qb], [ps2])
                    k.tt(t1[:], qf[:], cosr[:], ALU.mult, [qf, cosr], [t1])
                    k.tt(t2[:], ps2[:, :], sinr[:], ALU.mult, [ps2, sinr], [t2])
                    k.tt(RKT[hr][:], t1[:], t2[:], ALU.add, [t1, t2], [RKT[hr]])
                    pT = rot.next()
                    for blk in range(4):
                        k.tr(pT.bf[:, blk * 128:(blk + 1) * 128], RKT[hr][:, blk * 128:(blk + 1) * 128], X.id_bf[:, :],
                             [RKT[hr], X.id_bf], [pT], partial=(blk > 0))
                    for blk in range(4):
                        k.act(RKd[hr][:, blk, :], pT.bf[:, blk * 128:(blk + 1) * 128], AF.Copy, [pT, kdec], [RKd[hr]],
                              scale=kdec[:, hr:hr + 1], partial=(blk > 0))
                def proj_vg(blk):
                    ps = rot.next()
                    for kc in range(8):
                        k.mm(ps[:, :], hT[:, kc, blk * 128:(blk + 1) * 128], w_b[:, kc, 512:1024], kc == 0, kc == 7, [w_b, hT], [ps])
                    k.act(RVb[blk][:], ps[:, :], AF.Copy, [ps], [RVb[blk]])
                    ps = rot.next()
                    for kc in range(8):
                        k.mm(ps[:, :], hT[:, kc, blk * 128:(blk + 1) * 128], w_b[:, kc, 1024:1536], kc == 0, kc == 7, [w_b, hT], [ps])
                    k.act(Gb[blk][:], ps[:, :], AF.Silu, [ps], [Gb[blk]])

                proj_vg(0)
                for blk in range(4):
                    if blk + 1 < 4:
                        proj_vg(blk + 1)
                    RV = RVb[blk]; G = Gb[blk]
                    bs = slice(blk * 128, (blk + 1) * 128)
                    for hr in range(2):
                        vs = slice(hr * 256, (hr + 1) * 256)
                        pS = rot.next()
                        k.mm(pS[:, 0:128], RKT[hr][:, bs], RQT[hr][:, bs], True, True, [RKT[hr], RQT[hr]], [pS])
                        a = AT[hr]
                        k.tt(a[:], pS[:, 0:128], rmask[:, hr, :], ALU.mult, [pS, rmask], [a])
                        pO = rot.next()
                        k.mm(pO[:, 0:256], a[:], RV[:, vs], True, False, [a, RV], [pO])
                        k.mm(pO[:, 0:256], RQd[hr][:, bs], st_b[hr][:], False, True, [RQd[hr], st_b[hr]], [pO])
                        pN = rot.next()
                        k.mm(pN[:, 0:256], RKd[hr][:, blk, :], RV[:, vs], True, True, [RKd[hr], RV], [pN])
                        k.stt(st_f[hr][:], st_f[hr][:], sdec[:, hr:hr + 1], pN[:, 0:256], ALU.mult, ALU.add,
                              [st_f[hr], sdec, pN], [st_f[hr]])
                        k.act(st_b[hr][:], st_f[hr][:], AF.Copy, [st_f[hr]], [st_b[hr]])
                        k.act(junk[:], pO[:, 0:256], AF.Square, [pO], [junk, ssq], accum_out=ssq[:, 0:1])
                        k.act(sd[:], ssq[:], AF.Sqrt, [ssq], [sd], bias=EPS, scale=1.0 / 256.0)
                        k.recip(rinv[:], sd[:], [sd], [rinv])
                        z = zr[hr]
                        k.stt(z[:], pO[:, 0:256], rinv[:, 0:1], G[:, vs], ALU.mult, ALU.mult, [pO, rinv, G], [z])
                        pT = rot.next()
                        for vc in range(2):
                            k.tr(pT.bf[:, vc * 128:(vc + 1) * 128], z[:, vc * 128:(vc + 1) * 128], X.id_bf[:, :],
                                 [z, X.id_bf], [pT], partial=(vc > 0))
                        k.act(zT[hr][:, :, bs], pT.bf[:, 0:256].rearrange("p (v i) -> p v i", v=2), AF.Copy, [pT], [zT[hr]],
                              partial=True)
                for hr in range(2):
                    k.dma('pool', A['ZTr'][hr * 256:(hr + 1) * 256, cs].rearrange("(v p) c -> p v c", p=128),
                          zT[hr][:], reads=[zT[hr]], writes=[ZT], partial=True)

        if 'S2' in phases:
          with k.scope():
            kts = [k.sb('kt%d' % i, [128, S], BF16) for i in range(2)]
            qts = [k.sb('qt%d' % i, [128, TT], BF16) for i in range(3)]
            PTs = [k.sb('PT%d' % i, [128, TT], BF16) for i in range(3)]
            of = [k.sb('of%d' % i, [128, TT], F32) for i in range(2)]
            rb = k.sb('rb', [128, TT], F32)
            zm = [k.sb('zm%d' % i, [128, TT], BF16) for i in range(2)]
            sel = k.sb('sel', [128, 64], F32)
            for o_ in of:
                k.memset(o_[:], 0.0, [o_])
            k.dma('sp', sel[:], A['sel65'], writes=[sel])
            srot = Rot(banks[0:4]); orot = Rot(banks[4:6]); brot = Rot(banks[6:8])
            qi = 0
            for h in range(4):
                kt = kts[h % 2]
                for c4 in range(4):
                    k.dma('sp', kt[0:96, c4 * 2048:(c4 + 1) * 2048], A['KTd'][h, :, c4 * 2048:(c4 + 1) * 2048],
                          reads=[KTd], writes=[kt], partial=(c4 > 0))
                for qt in range(NT):
                    q = qts[qi % 3]; qi += 1
                    k.dma('sp', q[0:96, :], A['QTd'][h, :, qt * TT:(qt + 1) * TT], reads=[QTd], writes=[q])
                    nkb = 4 * qt + 4
                    O = orot.next()
                    pend = []

                    def emit_s(kb):
                        d = kb - 4 * qt
                        qlo = 0 if d < 0 else 128 * d
                        pS = srot.next()
                        k.mm(pS[:, qlo:TT], kt[0:96, kb * 128:(kb + 1) * 128], q[0:96, qlo:TT], True, True, [kt, q], [pS])
                        P = PTs[kb % 3]
                        k.act(P[:, qlo:TT], pS[:, qlo:TT], AF.Exp, [pS], [P])
                        if d >= 0:
                            k.memset(P[64:128, qlo:qlo + 64], 0.0, [P], eng='pool', partial=True, reads=[P])
                        return P, d, qlo

                    def emit_pv(kb, P, d, qlo):
                        k.mm(O[0:65, qlo:TT], V_sb[:, kb, h, :], P[:, qlo:TT], kb == 0, kb == nkb - 1, [V_sb, P], [O])

                    for kb in range(nkb):
                        pend.append((kb,) + emit_s(kb))
                        if len(pend) > 2:
                            a = pend.pop(0); emit_pv(*a)
                    while pend:
                        a = pend.pop(0); emit_pv(*a)
                    o = of[qt % 2]
                    k.act(o[0:65, :], O[0:65, :], AF.Copy, [O], [o], partial=True)
                    pB = brot.next()
                    k.mm(pB[0:64, :], sel[:, 0:64], o[:, :], True, True, [sel, o], [pB])
                    k.recip(rb[0:64, :], pB[0:64, :], [pB], [rb])
                    z = zm[qt % 2]
                    k.tt(z[0:64, :], o[0:64, :], rb[0:64, :], ALU.mult, [o, rb], [z])
                    k.dma('pool', A['ZTm'][h * 64:(h + 1) * 64, qt * TT:(qt + 1) * TT], z[0:64, :], reads=[z], writes=[ZT], partial=True)


OFF = {'q_lat': 0, 'kv_lat': 384, 'k_rope': 640, 'cb': 672, 'cc': 1184, 'cx': 1696, 'rq': 2208, 'rk': 2720,
       'rv': 3232, 'rg': 4256, 'gl': 5280}


def col8(v):
    return np.ascontiguousarray(v.reshape(-1, 128).T)


def consts_for(hh):
    C = {}
    C['ident'] = np.eye(128, dtype=np.float32)
    prot = np.zeros((128, 2, 128), np.float32)
    for r in range(64):
        prot[r + 64, 0, r] = -1.0
        prot[r, 0, r + 64] = 1.0
    for r in range(64, 80):
        prot[r + 16, 1, r] = -1.0
        prot[r, 1, r + 16] = 1.0
    C['prot'] = prot
    invf = np.zeros((128, 2), np.float32)
    inv_ret = (10000.0 ** (-np.arange(0, 128, 2, dtype=np.float32) / 128)).astype(np.float32)
    inv_mla = (10000.0 ** (-np.arange(0, 32, 2, dtype=np.float32) / 32)).astype(np.float32)
    for p in range(128):
        invf[p, 0] = inv_ret[p % 64]
    for p in range(64, 96):
        invf[p, 1] = inv_mla[(p - 64) % 16]
    C['invf'] = invf
    rmask = np.zeros((128, 2, 128), np.float64); qdec = np.zeros((128, 2, TT), np.float64)
    kdec = np.zeros((128, 2), np.float64); sdec = np.zeros((128, 2), np.float64)
    for hr in range(2):
        H = hh * 2 + hr
        g = 1.0 - 2.0 ** (-5.0 - H)
        for j in range(128):
            for i in range(128):
                cj, ci = j // 64, i // 64
                if cj == ci:
                    rmask[j, hr, i] = g ** abs(i - j)
                elif cj < ci:
                    rmask[j, hr, i] = g ** (i - j)
        qdec[:, hr, :] = (g ** ((np.arange(TT) % 128) + 1.0))[None, :]
        kdec[:, hr] = g ** (127.0 - np.arange(128))
        sdec[:, hr] = g ** 128.0
    C['rmask'] = rmask.astype(np.float32); C['qdec'] = qdec.astype(np.float32)
    C['kdec'] = kdec.astype(np.float32); C['sdec'] = sdec.astype(np.float32)
    sel = np.zeros((128, 64), np.float32); sel[64, :] = 1.0
    C['sel65'] = sel
    return C


def mixer_inputs(inp, l, b, hh, xT_b):
    w_in = inp['w_in'][l]
    m = dict(consts_for(hh))
    if xT_b is not None:
        m['xT'] = xT_b
    m['cc8'] = col8(inp['c'][b])
    m['b_ada8'] = col8(inp['b_ada'][l])
    m['gmix8'] = col8(inp['norm_mix_g'][l])
    wa = np.zeros((D, 1504), np.float32)
    wa[:, 0:640] = w_in[:, 0:640]
    wa[:, 704:736] = w_in[:, 640:672]
    for i, nm in enumerate(('cb', 'cc', 'cx')):
        wa[:, 736 + i * 256:736 + (i + 1) * 256] = w_in[:, OFF[nm] + hh * 256:OFF[nm] + (hh + 1) * 256]
    m['w_a'] = wa
    wb = np.empty((D, 1536), np.float32)
    wb[:, 0:256] = w_in[:, OFF['rq'] + hh * 256:OFF['rq'] + (hh + 1) * 256]
    wb[:, 256:512] = w_in[:, OFF['rk'] + hh * 256:OFF['rk'] + (hh + 1) * 256]
    wb[:, 512:1024] = w_in[:, OFF['rv'] + hh * 512:OFF['rv'] + (hh + 1) * 512]
    wb[:, 1024:1536] = w_in[:, OFF['rg'] + hh * 512:OFF['rg'] + (hh + 1) * 512]
    m['w_b'] = wb
    m['pos'] = np.ascontiguousarray(inp['positions'][b:b + 1].astype(np.int32))
    m['qg3'] = col8(inp['mla_q_norm_g'][l]); m['kvg2'] = col8(inp['mla_kv_norm_g'][l])
    m['wuq'] = np.ascontiguousarray(inp['w_uq'][l][:, hh * 384:(hh + 1) * 384])
    wukv = inp['w_ukv'][l].reshape(256, 8, 128)[:, hh * 4:(hh + 1) * 4, :]
    m['wuk'] = np.ascontiguousarray(wukv[:, :, 0:64].reshape(256, 256))
    m['wuv'] = np.ascontiguousarray(wukv[:, :, 64:128].reshape(256, 256))
    cwv = inp['conv_w'][l][:, hh * 256:(hh + 1) * 256]
    m['convw'] = np.ascontiguousarray(cwv.reshape(3, 2, 128).transpose(2, 1, 0))
    return m


SO = 4096; NTO = SO // TT
BIG = 1.0e30


def emit_ffn(k, X, A, modT, last, tok):
    if True:
        banks = X.banks
        rot = Rot(banks[0:7])
        pL = banks[7]
        gffn = k.sb('gffn', [128, 8], F32)
        k.dma('sp', gffn[:], A['gffn8'], writes=[gffn])
        gsc2 = k.sb('gsc2', [128, 8], F32)
        k.stt(gsc2[:], modT[:, 32:40], 1.0, gffn[:], ALU.add, ALU.mult, [modT, gffn], [gsc2])
        X1T = A['X1T_buf']; H2K = A['H2K_buf']; HSb = A['HS_buf']; YSb = A['YS_buf']; XO = A['xo_buf']
        NBLK = SO // 128; TS = 256; NTILE = 64
        rc = k.sb('rconst', [128, 128 + 128 + 64 + 16 + 32 + 2], F32)
        k.dma('sp', rc[:], A['rconst'], writes=[rc])
        tri = rc[:, 0:128]; ones_f = rc[:, 128:256]; iota64 = rc[:, 256:320]; thr16 = rc[:, 320:336]
        iota32 = rc[:, 336:368]; pbase2 = rc[:, 368:370]
        e1s = k.sb('e1s', [128, NBLK], F32); e2s = k.sb('e2s', [128, NBLK], F32)
        r1s = k.sb('r1s', [128, NBLK], F32); r2s = k.sb('r2s', [128, NBLK], F32)
        w1s = k.sb('w1s', [128, NBLK], F32); w2s = k.sb('w2s', [128, NBLK], F32)
        run_bc = k.sb('run_bc', [128, 32], F32)
        k.memset(run_bc[:], 0.0, [run_bc])
        pos1i = k.sb('pos1i', [128, NBLK], I32); pos2i = k.sb('pos2i', [128, NBLK], I32)
        widx = k.sb('widx', [128, NTILE, 2], I32)
        HTb = A['HT_buf']; ZTb = A['ZT_buf']; XSb = A['xs_buf']
        xTv = A['xT'].rearrange("(kc p) c -> p kc c", p=128)
        HTv = A['HT'].rearrange("(kc p) c -> p kc c", p=128)
        ZTv = A['ZT'].rearrange("(kc p) c -> p kc c", p=128)
        X1v = A['X1T'].rearrange("(kc p) c -> p kc c", p=128)
        XOv = A['xoT'].rearrange("(kc p) c -> p kc c", p=128)

        with k.scope():
            w_g = k.sb('w_g', [128, 8, 3072], BF16)
            wgv = A['w_g'].rearrange("(kc p) n -> p kc n", p=128)
            for kc in range(8):
                load_cast(k, w_g, lambda c, n: w_g[:, kc, c:c + n], lambda c, n: wgv[:, kc, c:c + n], 3072, step=1024)
            w_o = k.sb('w_o', [128, 16, 1024], BF16)
            wov = A['w_o'].rearrange("(kc p) n -> p kc n", p=128)
            for kc in range(16):
                k.dma('pool', w_o[:, kc, :], wov[:, kc, :], writes=[w_o], partial=True)
            w_m = k.sb('w_m', [128, 8, 1024], BF16)
            wmv = A['w_mix'].rearrange("(kc p) n -> p kc n", p=128)
            for kc in range(8):
                k.dma('pool', w_m[:, kc, :], wmv[:, kc, :], writes=[w_m], partial=True)
            w_r = k.sb('w_r', [128, 8, 36], F32)
            k.dma('sp', w_r[:], A['w_r'].rearrange("(kc p) n -> p kc n", p=128), writes=[w_r])
            b_r = k.sb('b_r', [128, 36], F32)
            k.dma('sp', b_r[:], A['b_r'].to_broadcast([128, 36]), writes=[b_r])
            hT = k.sb('hTc', [128, 8, TT], BF16)
            Zt = k.sb('Zt', [128, 16, TT], BF16)
            xt = k.sb('xtc', [128, 8, TT], F32)
            gt0 = k.sb('gt0', [128, 3, TT], BF16); gt = [gt0, gt0]
            tA = k.sb('tA', [128, TT], F32); tB = k.sb('tB', [128, TT], F32)
            mg = k.sb('mg', [128, 8, TT], BF16)
            rtmp = k.sb('rtmpc', [128, TT], F32); rstd = k.sb('rstdc', [128, TT], F32)
            h2f0 = k.sb('h2f0', [128, TT], F32); h2f = [h2f0, h2f0]
            h2b = k.sb('h2b', [128, 8, TT], BF16)
            lt = tA
            lg = k.sb('lg', [128, 36], F32); gmax = k.sb('gmax', [128, 1], F32); ngmax = k.sb('ngmax', [128, 1], F32)
            g1h = k.sb('g1h', [128, 4], F32); pen = k.sb('pen', [128, 4], F32)
            ej = k.sb('ej', [128, 4], F32); gsum = k.sb('gsum', [128, 1], F32); gval = k.sb('gval', [128, 1], F32)
            lem = k.sb('lem', [128, 32], F32); top8 = k.sb('top8', [128, 8], F32)
            m1 = k.sb('m1', [128, 32], F32); m2 = k.sb('m2', [128, 32], F32)
            dd = k.sb('dd', [128, 1], F32); ee = k.sb('ee', [128, 1], F32); den = k.sb('den', [128, 1], F32)
            oh = k.sb('oh', [128, 32], F32); rk = k.sb('rk', [128, 32], F32); jk = k.sb('jk', [128, 32], F32)
            cT = tB
            for t in range(NTO):
                cs = slice(t * TT, (t + 1) * TT)
                tsl = tok('sp', t * TT, TT)
                k.dma('sp', hT[:], HTv[:, :, tsl], reads=[HTb], writes=[hT])
                k.dma('sp', Zt[:], ZTv[:, :, tsl], reads=[ZTb], writes=[Zt])
                for kc in range(8):
                    k.dma('sp', xt[:, kc, :], xTv[:, kc, tsl], reads=[XSb], writes=[xt], partial=(kc > 0))
                for dc in range(8):
                    ds_ = slice(dc * 128, (dc + 1) * 128)
                    g = gt[dc % 2]
                    for br in range(3):
                        ps = rot.next()
                        for kc in range(8):
                            k.mm(ps[:, :], w_g[:, kc, br * 1024 + dc * 128: br * 1024 + (dc + 1) * 128], hT[:, kc, :],
                                 kc == 0, kc == 7, [w_g, hT], [ps])
                        k.act(g[:, br, :], ps[:, :], AF.Sigmoid, [ps], [g], partial=(br > 0))
                    pys = []
                    for (k0, k1) in ((0, 4), (4, 8), (8, 16)):
                        ps = rot.next()
                        for kc in range(k0, k1):
                            k.mm(ps[:, :], w_o[:, kc, ds_], Zt[:, kc, :], kc == k0, kc == k1 - 1, [w_o, Zt], [ps])
                        pys.append(ps)
                    k.tt(tA[:], pys[0][:, :], g[:, 0, :], ALU.mult, [pys[0], g], [tA])
                    k.tt(tB[:], pys[1][:, :], g[:, 1, :], ALU.mult, [pys[1], g], [tB])
                    k.tt(tA[:], tA[:], tB[:], ALU.add, [tA, tB], [tA], eng='pool')
                    k.tt(tB[:], pys[2][:, :], g[:, 2, :], ALU.mult, [pys[2], g], [tB])
                    k.tt(mg[:, dc, :], tA[:], tB[:], ALU.add, [tA, tB], [mg], eng='pool', partial=(dc > 0))
                for dc in range(8):
                    ps = rot.next()
                    for kc in range(8):
                        k.mm(ps[:, :], w_m[:, kc, dc * 128:(dc + 1) * 128], mg[:, kc, :], kc == 0, kc == 7, [w_m, mg], [ps])
                    k.stt(xt[:, dc, :], ps[:, :], modT[:, 16 + dc:17 + dc], xt[:, dc, :], ALU.mult, ALU.add,
                          [ps, modT, xt], [xt], partial=True)
                k.dma('pool', X1v[:, :, cs], xt[:], reads=[xt], writes=[X1T], partial=True)
                for kc in range(8):
                    k.act(mg[:, kc, :], xt[:, kc, :], AF.Square, [xt], [mg], partial=(kc > 0))
                pss = rot.next()
                for kc in range(8):
                    k.mm(pss[:, :], X.ones_bf[:, :], mg[:, kc, :], kc == 0, kc == 7, [X.ones_bf, mg], [pss])
                k.act(rtmp[:], pss[:, :], AF.Sqrt, [pss], [rtmp], bias=EPS, scale=1.0 / D)
                k.recip(rstd[:], rtmp[:], [rtmp], [rstd])
                for kc in range(8):
                    hf = h2f[kc % 2]
                    k.stt(hf[:], xt[:, kc, :], gsc2[:, kc:kc + 1], rstd[:], ALU.mult, ALU.mult, [xt, gsc2, rstd], [hf])
                    k.act(hf[:], hf[:], AF.Identity, [hf, modT], [hf], bias=modT[:, 24 + kc:25 + kc])
                    k.copy(h2b[:, kc, :], hf[:], [hf], [h2b], eng='pool', partial=(kc > 0))
                    k.mm(pL[0:36, :], w_r[:, kc, :], hf[:], kc == 0, kc == 7, [w_r, hf], [pL])
                for blk in range(4):
                    pH = rot.next()
                    for kc in range(8):
                        k.tr(pH.bf[:, kc * 128:(kc + 1) * 128], h2b[:, kc, blk * 128:(blk + 1) * 128], X.id_bf[:, :],
                             [h2b, X.id_bf], [pH], partial=(kc > 0))
                    hk = gt0
                    k.act(hk[:, 0:2, :], pH.bf[:, :].rearrange("p (a b) -> p a b", a=2), AF.Copy, [pH], [hk])
                    k.dma('pool', A['H2K'][(t * 4 + blk) * 128:(t * 4 + blk + 1) * 128, :].rearrange("p (a b) -> p a b", a=2),
                          hk[:, 0:2, :], reads=[hk], writes=[H2K], partial=True)
                k.act(lt[0:36, :], pL[0:36, :], AF.Copy, [pL], [lt])
                pT = rot.next()
                for blk in range(4):
                    k.tr(pT[:, blk * 36:(blk + 1) * 36], lt[0:36, blk * 128:(blk + 1) * 128], X.id_f[0:36, 0:36],
                         [lt, X.id_f], [pT], partial=(blk > 0))
                for blk in range(4):
                    gb = t * 4 + blk
                    w1 = w1s[:, gb:gb + 1]; w2 = w2s[:, gb:gb + 1]
                    k.tt(lg[:], pT[:, blk * 36:(blk + 1) * 36], b_r[:], ALU.add, [pT, b_r], [lg])
                    k.op('dve', lambda e: e.reduce_max(out=gmax[:], in_=lg[:, 0:4], axis=mybir.AxisListType.X),
                         reads=[lg], writes=[gmax])
                    k.ts(g1h[:], lg[:, 0:4], gmax[:, 0:1], ALU.is_equal, [lg, gmax], [g1h])
                    k.ts(ngmax[:], gmax[:], -1.0, ALU.mult, [gmax], [ngmax])
                    k.act(ej[:], lg[:, 0:4], AF.Exp, [lg, ngmax], [ej, gsum], bias=ngmax[:, 0:1], accum_out=gsum[:, 0:1])
                    k.recip(gval[:], gsum[:], [gsum], [gval])
                    k.ts(pen[:], g1h[:], -1.0, ALU.add, [g1h], [pen], s2=BIG, op1=ALU.mult)
                    for g_ in range(4):
                        k.ts(lem[:, g_ * 8:(g_ + 1) * 8], lg[:, 4 + g_ * 8:4 + (g_ + 1) * 8], pen[:, g_:g_ + 1], ALU.add,
                             [lg, pen], [lem], partial=(g_ > 0))
                    k.op('dve', lambda e: e.max(out=top8[:], in_=lem[:]), reads=[lem], writes=[top8])
                    k.ts(m1[:], lem[:], top8[:, 0:1], ALU.is_equal, [lem, top8], [m1])
                    k.ts(m2[:], lem[:], top8[:, 1:2], ALU.is_equal, [lem, top8], [m2])
                    k.tt(dd[:], top8[:, 1:2], top8[:, 0:1], ALU.subtract, [top8], [dd])
                    k.act(ee[:], dd[:], AF.Exp, [dd], [ee])
                    k.ts(den[:], ee[:], 1.0, ALU.add, [ee], [den])
                    k.recip(den[:], den[:], [den], [den])
                    k.tt(w1, den[:], gval[:], ALU.mult, [den, gval], [w1s], partial=True)
                    k.tt(w2, w1, ee[:], ALU.mult, [w1s, ee], [w2s], partial=True)
                    k.tt(oh[:], m1[:], m2[:], ALU.add, [m1, m2], [oh])
                    pP = rot.next()
                    k.mm(pP[:, 0:32], tri, oh[:], True, True, [rc, oh], [pP])
                    k.tt(rk[:], pP[:, 0:32], run_bc[:], ALU.add, [pP, run_bc], [rk])
                    k.op('dve', lambda e: e.scalar_tensor_tensor(out=jk[:], in0=rk[:], scalar=1.0, in1=m1[:], op0=ALU.mult, op1=ALU.mult,
                                                              accum_out=r1s[:, gb:gb + 1]), reads=[rk, m1], writes=[jk, r1s], partial=True)
                    k.op('dve', lambda e: e.scalar_tensor_tensor(out=jk[:], in0=rk[:], scalar=1.0, in1=m2[:], op0=ALU.mult, op1=ALU.mult,
                                                              accum_out=r2s[:, gb:gb + 1]), reads=[rk, m2], writes=[jk, r2s], partial=True)
                    k.op('dve', lambda e: e.scalar_tensor_tensor(out=jk[:], in0=iota32, scalar=1.0, in1=m1[:], op0=ALU.mult, op1=ALU.mult,
                                                              accum_out=e1s[:, gb:gb + 1]), reads=[rc, m1], writes=[jk, e1s], partial=True)
                    k.op('dve', lambda e: e.scalar_tensor_tensor(out=jk[:], in0=iota32, scalar=1.0, in1=m2[:], op0=ALU.mult, op1=ALU.mult,
                                                              accum_out=e2s[:, gb:gb + 1]), reads=[rc, m2], writes=[jk, e2s], partial=True)
                    pQ = rot.next()
                    k.mm(pQ[:, 0:32], ones_f, oh[:], True, True, [rc, oh], [pQ])
                    k.tt(run_bc[:], run_bc[:], pQ[:, 0:32], ALU.add, [run_bc, pQ], [run_bc])
            ptile = k.sb('ptile', [128, 32], F32); cmp16 = k.sb('cmp16', [128, 16], F32)
            start = k.sb('start', [128, 32], F32); endt = k.sb('endt', [128, 32], F32); sstart = k.sb('sstart', [128, 32], F32)
            et = k.sb('et', [128, NTILE], F32); wf = k.sb('wf', [128, NTILE, 2], F32)
            p1f = k.sb('p1f', [128, NBLK], F32); p2f = k.sb('p2f', [128, NBLK], F32)
            for e_ in range(32):
                k.op('dve', lambda e: e.tensor_scalar(out=cmp16[:], in0=thr16, scalar1=run_bc[:, e_:e_ + 1], scalar2=None,
                                                      op0=ALU.is_lt, op1=ALU.add, accum_out=ptile[:, e_:e_ + 1]),
                     reads=[rc, run_bc], writes=[cmp16, ptile], partial=True)
            k.memset(start[:, 0:1], 0.0, [start], partial=True)
            for e_ in range(1, 32):
                k.tt(start[:, e_:e_ + 1], start[:, e_ - 1:e_], ptile[:, e_ - 1:e_], ALU.add, [start, ptile], [start], partial=True)
            k.tt(endt[:], start[:], ptile[:], ALU.add, [start, ptile], [endt])
            k.ts(sstart[:], start[:], float(TS), ALU.mult, [start], [sstart])
            k.memset(et[:], 0.0, [et])
            for e_ in range(32):
                k.stt(et[:], iota64, endt[:, e_:e_ + 1], et[:], ALU.is_ge, ALU.add, [rc, endt, et], [et])
            k.ts(et[:], et[:], 31.0, ALU.min, [et], [et])
            same = k.sb('same', [128, NTILE], F32)
            k.memset(same[:, 0:1], 0.0, [same], partial=True)
            k.tt(same[:, 1:NTILE], et[:, 1:NTILE], et[:, 0:NTILE - 1], ALU.is_equal, [et], [same], partial=True)
            for j in range(2):
                k.ts(wf[:, :, j], et[:], 256.0, ALU.mult, [et, rc], [wf], s2=pbase2[:, j:j + 1], op1=ALU.add, partial=True)
                k.stt(wf[:, :, j], same[:], 1.0e7, wf[:, :, j], ALU.mult, ALU.add, [same, wf], [wf], partial=True)
            k.copy(widx[:], wf[:], [wf], [widx])
            for gb in range(NBLK):
                k.ts(m1[:], iota32, e1s[:, gb:gb + 1], ALU.is_equal, [rc, e1s], [m1])
                k.op('dve', lambda e: e.scalar_tensor_tensor(out=jk[:], in0=m1[:], scalar=1.0, in1=sstart[:], op0=ALU.mult, op1=ALU.mult,
                                                          accum_out=p1f[:, gb:gb + 1]), reads=[m1, sstart], writes=[jk, p1f], partial=True)
                k.ts(m2[:], iota32, e2s[:, gb:gb + 1], ALU.is_equal, [rc, e2s], [m2])
                k.op('dve', lambda e: e.scalar_tensor_tensor(out=jk[:], in0=m2[:], scalar=1.0, in1=sstart[:], op0=ALU.mult, op1=ALU.mult,
                                                          accum_out=p2f[:, gb:gb + 1]), reads=[m2, sstart], writes=[jk, p2f], partial=True)
            k.tt(p1f[:], p1f[:], r1s[:], ALU.add, [p1f, r1s], [p1f])
            k.tt(p2f[:], p2f[:], r2s[:], ALU.add, [p2f, r2s], [p2f])
            k.copy(pos1i[:], p1f[:], [p1f], [pos1i]); k.copy(pos2i[:], p2f[:], [p2f], [pos2i])

        if last:
            fin = k.sb('fin', [128, 8], F32)
            k.dma('sp', fin[:], A['fin8'], writes=[fin])
        with k.scope():
            hr_ = [k.sb('hr%d' % i, [128, 1024], BF16) for i in range(2)]
            for gb in range(NBLK):
                hb_ = hr_[gb % 2]
                k.dma('sp', hb_[:], A['H2K'][gb * 128:(gb + 1) * 128, :], reads=[H2K], writes=[hb_])
                k.idma(A['HS'][:, :], hb_[:, :], pos1i[:, gb:gb + 1], True, [hb_, pos1i], [HSb], hb_)
                k.idma(A['HS'][:, :], hb_[:, :], pos2i[:, gb:gb + 1], True, [hb_, pos2i], [HSb], hb_)
        with k.scope():
            wg0 = k.sb('wg0', [128, 4096], BF16); wu0 = k.sb('wu0', [128, 4096], BF16); wd0 = k.sb('wd0', [128, 4096], BF16)
            wg = [wg0, wg0]; wu = [wu0, wu0]; wd = [wd0, wd0]
            hst = [k.sb('hst%d' % i, [128, 2, 1024], BF16) for i in range(2)]
            hTs = [k.sb('hTs%d' % i, [128, 8, TS], BF16) for i in range(2)]
            sg = [k.sb('sg%d' % i, [128, TS], F32) for i in range(2)]
            hid = [k.sb('hid%d' % i, [128, 4, TS], BF16) for i in range(2)]
            ys = [k.sb('ys%d' % i, [128, 2, 1024], F32) for i in range(2)]
            HSv = A['HS'].rearrange("(i sb p) n -> i p sb n", sb=2, p=128)
            YSv = A['YS'].rearrange("(i sb p) n -> i p sb n", sb=2, p=128)

            def load_w(i, which):
                for (wt, src) in which:
                    for j in range(2):
                        k.idma(wt[:, j * 2048:(j + 1) * 2048], src[:, :], widx[:, i, j:j + 1], False, [widx], [wt], wt, partial=(j > 0), bound=8191)

            GU = ((wg0, A['w_eg']), (wu0, A['w_eu'])); DN = ((wd0, A['w_ed']),)
            load_w(0, GU); load_w(0, DN)
            for i in range(NTILE):
                bi = i % 2
                hs_ = hst[bi]; hT_ = hTs[bi]; hd = hid[bi]; y_ = ys[bi]
                if i == 0:
                    k.dma('sp', hs_[:], HSv[0], reads=[HSb], writes=[hs_])
                if i + 1 < NTILE:
                    k.dma('sp', hst[(i + 1) % 2][:], HSv[i + 1], reads=[HSb], writes=[hst[(i + 1) % 2]])
                for sb in range(2):
                    pH = rot.next()
                    for kc in range(8):
                        k.tr(pH.bf[:, kc * 128:(kc + 1) * 128], hs_[:, sb, kc * 128:(kc + 1) * 128], X.id_bf[:, :],
                             [hs_, X.id_bf], [pH], partial=(kc > 0))
                    k.act(hT_[:, :, sb * 128:(sb + 1) * 128], pH.bf[:, :].rearrange("p (kc s) -> p kc s", kc=8), AF.Copy,
                          [pH], [hT_], partial=(sb > 0))
                for fc in range(4):
                    pg = rot.next(); pu = rot.next()
                    for kc in range(8):
                        k.mm(pg[:, 0:TS], wg[bi][:, kc * 512 + fc * 128: kc * 512 + (fc + 1) * 128], hT_[:, kc, :], kc == 0, kc == 7, [wg[bi], hT_], [pg])
                    for kc in range(8):
                        k.mm(pu[:, 0:TS], wu[bi][:, kc * 512 + fc * 128: kc * 512 + (fc + 1) * 128], hT_[:, kc, :], kc == 0, kc == 7, [wu[bi], hT_], [pu])
                    s_ = sg[fc % 2]
                    k.act(s_[:], pg[:, 0:TS], AF.Silu, [pg], [s_])
                    k.tt(hd[:, fc, :], pu[:, 0:TS], s_[:], ALU.mult, [pu, s_], [hd], partial=(fc > 0))
                if i + 1 < NTILE:
                    load_w(i + 1, GU)
                for sb in range(2):
                    for dh in range(2):
                        pd = rot.next()
                        for fc in range(4):
                            k.mm(pd[:, :], hd[:, fc, sb * 128:(sb + 1) * 128], wd[bi][:, fc * 1024 + dh * 512: fc * 1024 + (dh + 1) * 512],
                                 fc == 0, fc == 3, [hd, wd[bi]], [pd])
                        if (sb + dh) % 2 == 0:
                            k.act(y_[:, sb, dh * 512:(dh + 1) * 512], pd[:, :], AF.Copy, [pd], [y_], partial=True)
                        else:
                            k.copy(y_[:, sb, dh * 512:(dh + 1) * 512], pd[:, :], [pd], [y_], partial=True)
                if i + 1 < NTILE:
                    load_w(i + 1, DN)
                k.dma('act', YSv[i], y_[:], reads=[y_], writes=[YSb], partial=True)
        with k.scope():
            y1 = [k.sb('y1_%d' % i, [128, 1024], F32) for i in range(2)]
            y2 = [k.sb('y2_%d' % i, [128, 1024], F32) for i in range(2)]
            mo = k.sb('mo', [128, 4, 1024], F32)
            x1 = [k.sb('x1_%d' % i, [128, 8, TT], F32) for i in range(2)]
            sqf = k.sb('sqf', [128, 8, TT], BF16)
            rt2 = k.sb('rt2', [128, TT], F32); rs2 = k.sb('rs2', [128, TT], F32)
            for t in range(NTO):
                gs = slice(t * TT, (t + 1) * TT)
                xb = x1[t % 2]
                k.dma('sp', xb[:], X1v[:, :, gs], reads=[X1T], writes=[xb])
                for blk in range(4):
                    gb = t * 4 + blk
                    a1 = y1[blk % 2]; a2 = y2[blk % 2]
                    k.idma(a1[:, :], A['YS'][:, :], pos1i[:, gb:gb + 1], False, [YSb, pos1i], [a1], a1, partial=False)
                    k.idma(a2[:, :], A['YS'][:, :], pos2i[:, gb:gb + 1], False, [YSb, pos2i], [a2], a2, partial=False)
                    k.ts(mo[:, blk, :], a1[:], w1s[:, gb:gb + 1], ALU.mult, [a1, w1s], [mo], partial=(blk > 0))
                    k.stt(mo[:, blk, :], a2[:], w2s[:, gb:gb + 1], mo[:, blk, :], ALU.mult, ALU.add, [a2, w2s, mo], [mo], partial=True)
                for dc in range(8):
                    pM = rot.next()
                    for blk in range(4):
                        k.tr(pM[:, blk * 128:(blk + 1) * 128], mo[:, blk, dc * 128:(dc + 1) * 128], X.id_f[:, :],
                             [mo, X.id_f], [pM], partial=(blk > 0))
                    k.stt(xb[:, dc, :], pM[:, :], modT[:, 40 + dc:41 + dc], xb[:, dc, :], ALU.mult, ALU.add,
                          [pM, modT, xb], [xb], partial=True)
                if last:
                    for kc in range(8):
                        k.act(sqf[:, kc, :], xb[:, kc, :], AF.Square, [xb], [sqf], partial=(kc > 0))
                    pss = rot.next()
                    for kc in range(8):
                        k.mm(pss[:, :], X.ones_bf[:, :], sqf[:, kc, :], kc == 0, kc == 7, [X.ones_bf, sqf], [pss])
                    k.act(rt2[:], pss[:, :], AF.Sqrt, [pss], [rt2], bias=EPS, scale=1.0 / D)
                    k.recip(rs2[:], rt2[:], [rt2], [rs2])
                    for kc in range(8):
                        k.stt(xb[:, kc, :], xb[:, kc, :], fin[:, kc:kc + 1], rs2[:], ALU.mult, ALU.mult,
                              [xb, fin, rs2], [xb], partial=True)
                k.dma('pool', XOv[:, :, gs], xb[:], reads=[xb], writes=[XO], partial=True)


FUSED_IN = {
    'xT': ([D, S], F32), 'cc8': ([128, 8], F32), 'w_ada': ([2, D, 6 * D], F32), 'b_ada8': ([2, 128, 48], F32),
    'gmix8': ([2, 128, 8], F32), 'gffn8': ([2, 128, 8], F32), 'fin8': ([128, 8], F32),
    'w_a': ([2, 2, D, 1504], F32), 'w_b': ([2, 2, D, 1536], F32), 'pos': ([1, S], I32),
    'qg3': ([2, 128, 3], F32), 'kvg2': ([2, 128, 2], F32), 'wuq': ([2, 2, 384, 384], F32),
    'wuk': ([2, 2, 256, 256], F32), 'wuv': ([2, 2, 256, 256], F32), 'convw': ([2, 2, 128, 2, 3], F32),
    'ident': ([128, 128], F32), 'prot': ([128, 2, 128], F32), 'invf': ([128, 2], F32),
    'rmask': ([2, 128, 2, 128], F32), 'qdec': ([2, 128, 2, TT], F32), 'kdec': ([2, 128, 2], F32),
    'sdec': ([2, 128, 2], F32), 'sel65': ([128, 64], F32),
    'w_g': ([2, D, 3072], F32), 'w_o': ([2, 2048, D], F32), 'w_mix': ([2, D, D], F32), 'w_r': ([2, D, 36], F32),
    'b_r': ([2, 1, 36], F32), 'w_eg0': ([8192, 2048], F32), 'w_eu0': ([8192, 2048], F32), 'w_ed0': ([8192, 2048], F32),
    'w_eg1': ([8192, 2048], F32), 'w_eu1': ([8192, 2048], F32), 'w_ed1': ([8192, 2048], F32), 'rconst': ([128, 370], F32),
}


def build_fused(nc, A):
    root = ExitStack()
    with root:
        k = K(nc, root)
        X = setup_common(k, A)
        V_sb = k.sb('V_sb', [128, 64, 4, 65], BF16)
        k.memset(V_sb[:, :, :, 64:65], 1.0, [V_sb], eng='pool')
        bufs = {n: Buf(n, A[n], dram=True) for n in ('HT', 'ZT', 'QTd', 'KTd', 'XO', 'X1T', 'H2K', 'HS', 'YS', 'xT', 'out', 'HTo', 'ZTo', 'XOo')}
        modT = [k.sb('modT%d' % l, [128, 48], F32) for l in range(2)]
        for l in range(2):
            emit_mod(k, X, {'cc8': A['cc8'], 'b_ada8': A['b_ada8'][l], 'w_ada': A['w_ada'][l]}, modT[l])
        pid = nc.sync.partition_id()
        for l in range(2):
            xsrc, xsb = (A['xT'], bufs['xT']) if l == 0 else (A['XO'], bufs['XO'])
            for hh in range(2):
                Am = dict(gmix8=A['gmix8'][l], invf=A['invf'], prot=A['prot'], HT=A['HT'], xT=xsrc,
                          HT_buf=bufs['HT'], ZT_buf=bufs['ZT'], QTd_buf=bufs['QTd'], KTd_buf=bufs['KTd'],
                          QTd=A['QTd'], KTd=A['KTd'], w_a=A['w_a'][l, hh], w_b=A['w_b'][l, hh],
                          qg3=A['qg3'][l], kvg2=A['kvg2'][l], wuq=A['wuq'][l, hh], wuk=A['wuk'][l, hh], wuv=A['wuv'][l, hh],
                          convw=A['convw'][l, hh], pos=A['pos'], rmask=A['rmask'][hh], qdec=A['qdec'][hh],
                          kdec=A['kdec'][hh], sdec=A['sdec'][hh], sel65=A['sel65'],
                          ZTm=A['ZT'][hh * 256:(hh + 1) * 256, :], ZTc=A['ZT'][512 + hh * 256:512 + (hh + 1) * 256, :],
                          ZTr=A['ZT'][1024 + hh * 512:1024 + (hh + 1) * 512, :])
                with k.scope():
                    emit_mixer(k, X, Am, modT[l], V_sb, write_ht=(hh == 0))
            last = (l == 1)
            if last:
                stg = Buf('stage')
                hoff = pid % 2 * SO
                for (dst, src) in (('HTo', 'HT'), ('ZTo', 'ZT'), ('XOo', 'XO')):
                    k.dma('sp', A[dst][:, :], A[src][:, bass.ds(hoff, SO)], reads=[bufs[src]], writes=[bufs[dst]], owner=stg)
            for th in ((None,) if last else (0, 1)):
                if last:
                    tok = lambda q, c0, n: slice(c0, c0 + n)
                    xo = A['out']; xob = bufs['out']
                    srcs = dict(xs_buf=bufs['XOo'], xT=A['XOo'], HT=A['HTo'], ZT=A['ZTo'], HT_buf=bufs['HTo'], ZT_buf=bufs['ZTo'])
                else:
                    tok = (lambda th_: (lambda q, c0, n: slice(th_ * SO + c0, th_ * SO + c0 + n)))(th)
                    xo = A['XO'][:, th * SO:(th + 1) * SO]; xob = bufs['XO']
                    srcs = dict(xs_buf=xsb, xT=xsrc, HT=A['HT'], ZT=A['ZT'], HT_buf=bufs['HT'], ZT_buf=bufs['ZT'])
                Af = dict(gffn8=A['gffn8'][l], X1T_buf=bufs['X1T'], H2K_buf=bufs['H2K'], HS_buf=bufs['HS'], YS_buf=bufs['YS'], xo_buf=xob,
                          rconst=A['rconst'], H2K=A['H2K'], HS=A['HS'], YS=A['YS'],
                          X1T=A['X1T'], xoT=xo, w_g=A['w_g'][l], w_o=A['w_o'][l],
                          w_mix=A['w_mix'][l], w_r=A['w_r'][l], b_r=A['b_r'][l], w_eg=A['w_eg%d' % l], w_eu=A['w_eu%d' % l],
                          w_ed=A['w_ed%d' % l], fin8=A['fin8'], **srcs)
                with k.scope():
                    emit_ffn(k, X, Af, modT[l], last, tok)
        k.barrier()
    return nc


def make_fused_nc():
    nc = bass.Bass("TRN2", target_bir_lowering=False)
    A = {}
    for name, (shape, dt) in FUSED_IN.items():
        A[name] = nc.dram_tensor(name, shape, dt, kind="ExternalInput").ap()
    A['out'] = nc.dram_tensor('out', [D, SO], F32, kind="ExternalOutput").ap()
    for name, shape, dt in (('HT', [D, S], BF16), ('ZT', [2048, S], BF16), ('QTd', [4, 96, S], BF16),
                            ('KTd', [4, 96, S], BF16), ('XO', [D, S], F32), ('X1T', [D, SO], F32),
                            ('H2K', [SO, D], BF16), ('HS', [16384, D], BF16), ('YS', [16384, D], F32), ('HTo', [D, SO], BF16),
                            ('ZTo', [2048, SO], BF16), ('XOo', [D, SO], F32)):
        A[name] = nc.dram_tensor(name, shape, dt, kind="Internal").ap()
    build_fused(nc, A)
    return nc


def fused_inputs(inp, b):
    L = range(2)
    m = {}
    c0 = consts_for(0); c1 = consts_for(1)
    for kk in ('ident', 'prot', 'invf', 'sel65'):
        m[kk] = c0[kk]
    for kk in ('rmask', 'qdec', 'kdec', 'sdec'):
        m[kk] = np.stack([c0[kk], c1[kk]], axis=0)
    mi = [[mixer_inputs(inp, l, b, hh, None) for hh in range(2)] for l in L]
    m['xT'] = np.ascontiguousarray(inp['x'][b].T)
    m['cc8'] = mi[0][0]['cc8']; m['pos'] = mi[0][0]['pos']
    m['w_ada'] = np.ascontiguousarray(inp['w_ada'])
    for kk in ('b_ada8', 'gmix8', 'qg3', 'kvg2'):
        m[kk] = np.stack([mi[l][0][kk] for l in L], axis=0)
    for kk in ('w_a', 'w_b', 'wuq', 'wuk', 'wuv', 'convw'):
        m[kk] = np.stack([np.stack([mi[l][hh][kk] for hh in range(2)], axis=0) for l in L], axis=0)
    m['gffn8'] = np.stack([col8(inp['norm_ffn_g'][l]) for l in L], axis=0)
    m['fin8'] = col8(inp['final_g'])
    m['w_g'] = np.ascontiguousarray(inp['w_in'][:, :, OFF['gl']:OFF['gl'] + 3072])
    m['w_o'] = np.concatenate([inp['w_o_mla'], inp['w_o_conv'], inp['w_o_ret']], axis=1)
    m['w_mix'] = np.ascontiguousarray(inp['w_mix_out'])
    m['w_r'] = np.concatenate([inp['w_route_group'], inp['w_route_expert']], axis=2)
    m['b_r'] = np.concatenate([inp['b_route_group'], inp['b_route_expert']], axis=1)[:, None, :].astype(np.float32)
    for l in L:
        m['w_eg%d' % l] = np.ascontiguousarray(inp['w_exp_gate'][l].reshape(32, 8, 128, 512).transpose(0, 2, 1, 3)).reshape(8192, 2048)
        m['w_eu%d' % l] = np.ascontiguousarray(inp['w_exp_up'][l].reshape(32, 8, 128, 512).transpose(0, 2, 1, 3)).reshape(8192, 2048)
        m['w_ed%d' % l] = np.ascontiguousarray(inp['w_exp_down'][l].reshape(32, 4, 128, 1024).transpose(0, 2, 1, 3)).reshape(8192, 2048)
    rcst = np.zeros((128, 370), np.float32)
    rcst[:, 0:128] = np.triu(np.ones((128, 128), np.float32), 1)
    rcst[:, 128:256] = 1.0
    rcst[:, 256:320] = np.arange(64, dtype=np.float32)[None, :]
    rcst[:, 320:336] = (np.arange(16, dtype=np.float32) * 256.0)[None, :]
    rcst[:, 336:368] = np.arange(32, dtype=np.float32)[None, :]
    rcst[:, 368] = 2.0 * np.arange(128); rcst[:, 369] = 2.0 * np.arange(128) + 1.0
    m['rconst'] = rcst
    return m


_NC_CACHE = {}


def kernel(**inp):
    inp = {k_: np.asarray(v) for k_, v in inp.items()}
    B = inp['x'].shape[0]
    cores = list(range(8))
    if 'fused' not in _NC_CACHE:
        _NC_CACHE['fused'] = make_fused_nc()
    per_b = [fused_inputs(inp, b) for b in range(B)]
    maps = [per_b[c // 2] for c in cores]
    res = run_bass_kernel_spmd(_NC_CACHE['fused'], maps, core_ids=cores).results
    out = np.empty((B, S, D), np.float32)
    for c in cores:
        b, th = c // 2, c % 2
        out[b, th * SO:(th + 1) * SO, :] = np.asarray(res[c]['out']).T
    return out
```

```python
import math
from contextlib import ExitStack, contextmanager
import numpy as np
import ml_dtypes
import concourse.bass as bass
import concourse.mybir as mybir
from concourse.bass_utils import run_bass_kernel_spmd

F32 = mybir.dt.float32; BF16 = mybir.dt.bfloat16; I32 = mybir.dt.int32
ALU = mybir.AluOpType; AF = mybir.ActivationFunctionType

D = 1024; S = 8192; TT = 512; NT = S // TT
EPS = 1e-6
TWO_PI = 2.0 * math.pi
CW1 = 6.28125
CW2 = TWO_PI - CW1
PI_LO = 3.1415925
MLA_SCALE = 96.0 ** -0.5
RET_KS = 128.0 ** -0.5


class Buf:
    def __init__(self, name, ap=None, dram=False):
        self.name = name; self.ap = ap; self.dram = dram
        self.writers = {}; self.readers = {}
        self.dsem = None; self.dval = 0; self.dkey = None; self.psum = False

    def __getitem__(self, idx):
        return self.ap[idx]


class Rot:
    def __init__(self, bufs):
        self.bufs = bufs; self.i = 0

    def next(self):
        b = self.bufs[self.i % len(self.bufs)]; self.i += 1
        return b


class K:
    def __init__(self, nc, root):
        self.nc = nc; self.root = root; self.stacks = [root]
        self.eng = {'pe': nc.tensor, 'dve': nc.vector, 'act': nc.scalar, 'pool': nc.gpsimd, 'sp': nc.sync}
        self.sem = {}; self.cnt = {}
        for e in self.eng:
            self.sem[e] = root.enter_context(nc.semaphore('s_' + e)); self.cnt[e] = 0
        self.waited = {e: {} for e in self.eng}
        self.dma_latest = {}
        self.uid = 0
        self.free_dsems = []
        self.bound_regs = {}
        self.scope_bufs = [[]]

    def sb(self, name, shape, dt):
        self.uid += 1
        t = self.stacks[-1].enter_context(self.nc.sbuf_tensor('%s_%d' % (name, self.uid), shape, dt))
        b = Buf(name, t)
        self.scope_bufs[-1].append(b)
        return b

    def ps(self, name, shape, dt=F32):
        t = self.root.enter_context(self.nc.psum_tensor(name, shape, dt))
        b = Buf(name, t); b.psum = True
        return b

    def dram(self, name, shape, dt, kind="Internal"):
        t = self.nc.dram_tensor(name, shape, dt, kind=kind).ap()
        return Buf(name, t, dram=True)

    @contextmanager
    def scope(self):
        st = ExitStack()
        self.stacks.append(st)
        self.scope_bufs.append([])
        try:
            yield
        finally:
            self.barrier()
            for b in self.scope_bufs.pop():
                if b.dsem is not None:
                    self.free_dsems.append((b.dsem, b.dval, b.dkey))
                    b.dsem = None
            self.stacks.pop()
            st.close()

    def _wait(self, e, sem, val, key):
        if self.waited[e].get(key, 0) >= val:
            return
        self.waited[e][key] = val
        self.eng[e].wait_ge(sem, val)

    def _deps(self, e, reads, writes, partial):
        for b in reads:
            for key, (sem, val) in b.writers.items():
                if e == 'pe' and key == 'pe':
                    continue
                self._wait(e, sem, val, key)
            if b.psum:
                for key, (sem, val) in b.readers.items():
                    if key != e:
                        self._wait(e, sem, val, key)
        for b in writes:
            if not partial:
                for key, (sem, val) in b.writers.items():
                    if e == 'pe' and key == 'pe':
                        continue
                    self._wait(e, sem, val, key)
            for key, (sem, val) in b.readers.items():
                if key == e:
                    continue
                self._wait(e, sem, val, key)

    def _mark(self, sem, val, key, reads, writes, partial):
        for b in reads:
            b.readers[key] = (sem, val)
        for b in writes:
            if partial:
                b.writers[key] = (sem, val)
            else:
                b.writers = {key: (sem, val)}

    def op(self, e, fn, reads=(), writes=(), partial=False):
        self._deps(e, reads, writes, partial)
        ins = fn(self.eng[e])
        self.cnt[e] += 1
        ins.then_inc(self.sem[e], 1)
        self._mark(self.sem[e], self.cnt[e], e, reads, writes, partial)
        return ins

    def dma(self, q, out_ap, in_ap, reads=(), writes=(), owner=None, partial=False, **kw):
        if owner is None:
            owner = [b for b in list(writes) + list(reads) if not b.dram][0]
        if owner.dsem is None:
            if self.free_dsems:
                owner.dsem, owner.dval, owner.dkey = self.free_dsems.pop()
            else:
                self.uid += 1
                owner.dkey = 'dsem%d' % self.uid
                owner.dsem = self.root.enter_context(self.nc.semaphore(owner.dkey))
        self._deps(q, reads, writes, partial)
        if owner.dval > 0:
            self._wait(q, owner.dsem, owner.dval, owner.dkey)
        ins = self.eng[q].dma_start(out=out_ap, in_=in_ap, **kw)
        owner.dval += 16
        ins.then_inc(owner.dsem, 16)
        self._mark(owner.dsem, owner.dval, owner.dkey, reads, writes, partial)
        self.dma_latest[owner.dkey] = (owner.dsem, owner.dval)
        return ins

    def idma(self, out_ap, in_ap, idx_ap, scatter, reads, writes, owner, partial=True, bound=None):
        q = 'pool'
        if owner.dsem is None:
            if self.free_dsems:
                owner.dsem, owner.dval, owner.dkey = self.free_dsems.pop()
            else:
                self.uid += 1
                owner.dkey = 'dsem%d' % self.uid
                owner.dsem = self.root.enter_context(self.nc.semaphore(owner.dkey))
        self._deps(q, reads, writes, partial)
        if owner.dval > 0:
            self._wait(q, owner.dsem, owner.dval, owner.dkey)
        off = bass.IndirectOffsetOnAxis(ap=idx_ap, axis=0)
        if scatter:
            ins = self.nc.gpsimd.indirect_dma_start(out=out_ap, out_offset=off, in_=in_ap, in_offset=None)
        else:
            if bound is None:
                ins = self.nc.gpsimd.indirect_dma_start(out=out_ap, out_offset=None, in_=in_ap, in_offset=off)
            else:
                if bound not in self.bound_regs:
                    self.bound_regs[bound] = self.nc.gpsimd.to_reg(bound)
                ins = self.nc.gpsimd.indirect_dma_start(out=out_ap, out_offset=None, in_=in_ap, in_offset=off,
                                                        bounds_check=self.bound_regs[bound], oob_is_err=False)
        owner.dval += 16
        ins.then_inc(owner.dsem, 16)
        self._mark(owner.dsem, owner.dval, owner.dkey, reads, writes, partial)
        self.dma_latest[owner.dkey] = (owner.dsem, owner.dval)
        return ins

    def barrier(self):
        for e in self.eng:
            for e2 in self.eng:
                if e2 != e and self.cnt[e2] > 0:
                    self._wait(e, self.sem[e2], self.cnt[e2], e2)
            for key, (sem, val) in self.dma_latest.items():
                self._wait(e, sem, val, key)

    def mm(self, out, lhsT, rhs, start, stop, reads, writes):
        return self.op('pe', lambda e: e.matmul(out, lhsT, rhs, start=start, stop=stop),
                       reads=reads, writes=writes, partial=not start)

    def tr(self, out, in_, ident, reads, writes, partial=True):
        return self.op('pe', lambda e: e.transpose(out, in_, ident), reads=reads, writes=writes, partial=partial)

    def act(self, out, in_, func, reads, writes, bias=None, scale=None, accum_out=None, partial=False, eng='act'):
        kw = {}
        if bias is not None: kw['bias'] = bias
        if scale is not None: kw['scale'] = scale
        if accum_out is not None: kw['accum_out'] = accum_out
        return self.op(eng, lambda e: e.activation(out=out, in_=in_, func=func, **kw),
                       reads=reads, writes=writes, partial=partial)

    def tt(self, out, in0, in1, op, reads, writes, partial=False, eng='dve'):
        return self.op(eng, lambda e: e.tensor_tensor(out=out, in0=in0, in1=in1, op=op),
                       reads=reads, writes=writes, partial=partial)

    def ts(self, out, in0, s1, op0, reads, writes, s2=None, op1=None, partial=False, eng='dve'):
        if op1 is None:
            return self.op(eng, lambda e: e.tensor_scalar(out=out, in0=in0, scalar1=s1, scalar2=None, op0=op0),
                           reads=reads, writes=writes, partial=partial)
        return self.op(eng, lambda e: e.tensor_scalar(out=out, in0=in0, scalar1=s1, scalar2=s2, op0=op0, op1=op1),
                       reads=reads, writes=writes, partial=partial)

    def stt(self, out, in0, scalar, in1, op0, op1, reads, writes, partial=False):
        return self.op('dve', lambda e: e.scalar_tensor_tensor(out=out, in0=in0, scalar=scalar, in1=in1, op0=op0, op1=op1),
                       reads=reads, writes=writes, partial=partial)

    def copy(self, out, in_, reads, writes, partial=False, eng='dve'):
        return self.op(eng, lambda e: e.tensor_copy(out=out, in_=in_), reads=reads, writes=writes, partial=partial)

    def recip(self, out, in_, reads, writes, partial=False):
        return self.op('dve', lambda e: e.reciprocal(out=out, in_=in_), reads=reads, writes=writes, partial=partial)

    def memset(self, ap, val, writes, eng='dve', partial=False, reads=()):
        return self.op(eng, lambda e: e.memset(ap, val), reads=reads, writes=writes, partial=partial)


def load_cast(k, dst, dst_ap_fn, src_ap_fn, ncols, step=2048):
    c = 0
    while c < ncols:
        n = min(step, ncols - c)
        k.dma('pool', dst_ap_fn(c, n), src_ap_fn(c, n), writes=[dst], partial=True)
        c += n


class Ctx:
    pass


def setup_common(k, A):
    X = Ctx()
    X.banks = [k.ps('bank%d' % i, [128, 512], F32) for i in range(8)]
    for b in X.banks:
        b.bf = b.ap.bitcast(BF16)
    X.ones_bf = k.sb('ones_bf', [128, 128], BF16)
    k.memset(X.ones_bf[:], 1.0, [X.ones_bf])
    X.id_f = k.sb('id_f', [128, 128], F32)
    k.dma('sp', X.id_f[:], A['ident'][:, :], writes=[X.id_f])
    X.id_bf = k.sb('id_bf', [128, 128], BF16)
    k.copy(X.id_bf[:], X.id_f[:], [X.id_f], [X.id_bf])
    return X


def rope_tables(k, W, pos_ap, invcol, cosb, sinb):
    k.dma('sp', W.posi[:], pos_ap.to_broadcast([128, TT]), writes=[W.posi])
    k.copy(W.ang[:], W.posi[:], [W.posi], [W.ang])
    k.ts(W.ang[:], W.ang[:], invcol, ALU.mult, [W.ang], [W.ang])
    k.ts(W.kq[:], W.ang[:], 1.0 / TWO_PI, ALU.mult, [W.ang], [W.kq])
    k.copy(W.kf[:], W.kq[:], [W.kq], [W.kf])
    k.stt(W.r1[:], W.kf[:], -CW1, W.ang[:], ALU.mult, ALU.add, [W.kf, W.ang], [W.r1])
    k.stt(W.r1[:], W.kf[:], -CW2, W.r1[:], ALU.mult, ALU.add, [W.kf, W.r1], [W.r1])
    k.ts(W.r1[:], W.r1[:], PI_LO, ALU.min, [W.r1], [W.r1], s2=-PI_LO, op1=ALU.max)
    k.act(sinb[:], W.r1[:], AF.Sin, [W.r1], [sinb])
    k.stt(W.kf[:], W.r1[:], -1.0, W.r1[:], ALU.mult, ALU.max, [W.r1], [W.kf])
    k.act(cosb[:], W.kf[:], AF.Sin, [W.kf], [cosb], bias=W.halfpi[:, 0:1], scale=-1.0)


def rope_work(k):
    W = Ctx()
    W.posi = k.sb('posi', [128, TT], I32)
    W.ang = k.sb('ang', [128, TT], F32)
    W.kq = k.sb('kq', [128, TT], I32)
    W.kf = k.sb('kf', [128, TT], F32)
    W.r1 = k.sb('r1', [128, TT], F32)
    W.halfpi = k.sb('halfpi', [128, 1], F32)
    k.memset(W.halfpi[:], math.pi / 2.0, [W.halfpi])
    return W


def emit_mod(k, X, A, modT):
    with k.scope():
        cc = k.sb('cc', [128, 8], F32)
        k.dma('sp', cc[:], A['cc8'][:, :], writes=[cc])
        cact = k.sb('cact', [128, 8], F32)
        k.act(cact[:], cc[:], AF.Silu, [cc], [cact])
        bada = k.sb('bada', [128, 48], F32)
        k.dma('sp', bada[:], A['b_ada8'][:, :], writes=[bada])
        wv = A['w_ada'].rearrange("(kc p) n -> p kc n", p=128)
        wb = [k.sb('wada%d' % i, [128, 8, 768], F32) for i in range(2)]
        pm = X.banks[0]
        for blk in range(8):
            w = wb[blk % 2]
            for kc in range(8):
                k.dma('sp', w[:, kc, :], wv[:, kc, blk * 768:(blk + 1) * 768], writes=[w], partial=(kc > 0))
            for j in range(6):
                jj = blk * 6 + j
                for kc in range(8):
                    k.mm(pm[:, jj:jj + 1], w[:, kc, j * 128:(j + 1) * 128], cact[:, kc:kc + 1],
                         kc == 0, kc == 7, [w, cact], [pm])
        k.tt(modT[:], pm[:, 0:48], bada[:], ALU.add, [pm, bada], [modT])


def emit_mixer(k, X, A, modT, V_sb, write_ht, phases=('A', 'B', 'S2')):
    if True:
        banks = X.banks
        rot = Rot(banks)
        gmix = k.sb('gmix', [128, 8], F32)
        k.dma('sp', gmix[:], A['gmix8'], writes=[gmix])
        gsc = k.sb('gsc', [128, 8], F32)
        k.stt(gsc[:], modT[:, 8:16], 1.0, gmix[:], ALU.add, ALU.mult, [modT, gmix], [gsc])
        invf = k.sb('invf', [128, 2], F32)
        k.dma('sp', invf[:], A['invf'], writes=[invf])
        prot_f = k.sb('prot_f', [128, 2, 128], F32)
        k.dma('sp', prot_f[:], A['prot'], writes=[prot_f])
        prot = k.sb('prot', [128, 2, 128], BF16)
        k.copy(prot[:], prot_f[:], [prot_f], [prot])
        HT = A['HT_buf']; ZT = A['ZT_buf']; QTd = A['QTd_buf']; KTd = A['KTd_buf']
        HTv = A['HT'].rearrange("(kc p) c -> p kc c", p=128)
        xTv = A['xT'].rearrange("(kc p) c -> p kc c", p=128)

        if 'A' in phases:
          with k.scope():
            NA = 1504
            w_a = k.sb('w_a', [128, 8, NA], BF16)
            wav = A['w_a'].rearrange("(kc p) n -> p kc n", p=128)
            for kc in range(8):
                load_cast(k, w_a, lambda c, n: w_a[:, kc, c:c + n], lambda c, n: wav[:, kc, c:c + n], NA, step=752)
            st_f = k.sb('st_f', [128, 3, 384], F32)
            qg = k.sb('qg', [128, 3], F32); kvg = k.sb('kvg', [128, 2], F32)
            k.dma('sp', qg[:], A['qg3'], writes=[qg]); k.dma('sp', kvg[:], A['kvg2'], writes=[kvg])
            wuq = k.sb('wuq', [128, 3, 384], BF16)
            k.dma('sp', st_f[:], A['wuq'].rearrange("(rc p) n -> p rc n", p=128), writes=[st_f])
            for rc in range(3):
                k.ts(wuq[:, rc, :], st_f[:, rc, :], qg[:, rc:rc + 1], ALU.mult, [st_f, qg], [wuq], partial=True)
            wuk = k.sb('wuk', [128, 2, 256], BF16); wuv = k.sb('wuv', [128, 2, 256], BF16)
            st2 = k.sb('st2', [128, 2, 256], F32); st3 = k.sb('st3', [128, 2, 256], F32)
            k.dma('sp', st2[:], A['wuk'].rearrange("(rc p) n -> p rc n", p=128), writes=[st2])
            k.dma('sp', st3[:], A['wuv'].rearrange("(rc p) n -> p rc n", p=128), writes=[st3])
            for rc in range(2):
                k.ts(wuk[:, rc, :], st2[:, rc, :], kvg[:, rc:rc + 1], ALU.mult, [st2, kvg], [wuk], partial=True)
                k.ts(wuv[:, rc, :], st3[:, rc, :], kvg[:, rc:rc + 1], ALU.mult, [st3, kvg], [wuv], partial=True)
            cw = k.sb('cw', [128, 2, 3], F32)
            k.dma('sp', cw[:], A['convw'], writes=[cw])
            xts = [k.sb('xt%d' % i, [128, 8, TT], F32) for i in range(2)]
            sq = k.sb('sq', [128, 8, TT], BF16)
            hTa = [k.sb('hT%d' % i, [128, 8, TT], BF16) for i in range(2)]
            rtmp = k.sb('rtmp', [128, TT], F32)
            rstd = k.sb('rstd', [128, TT], F32)
            hx = [k.sb('hx%d' % i, [128, TT], F32) for i in range(2)]
            lat_f = k.sb('lat_f', [128, 5, TT], F32)
            sql = k.sb('sql', [128, 5, TT], BF16)
            rq_bc = k.sb('rq_bc', [128, TT], F32); rkv_bc = k.sb('rkv_bc', [128, TT], F32)
            qn = k.sb('qn', [128, 3, TT], BF16); kvn = k.sb('kvn', [128, 2, TT], BF16)
            QTs = [k.sb('QTs%d' % i, [128, TT], BF16) for i in range(4)]
            KTs = [k.sb('KTs%d' % i, [128, TT], BF16) for i in range(4)]
            qr_f = k.sb('qr_f', [128, TT], F32)
            kr_f = k.sb('kr_f', [128, TT], F32); kr_b = k.sb('kr_b', [128, TT], BF16)
            kpe = k.sb('kpe', [128, TT], BF16)
            t1 = k.sb('t1', [128, TT], F32); t2 = k.sb('t2', [128, TT], F32)
            cosm = k.sb('cosm', [128, TT], F32); sinm = k.sb('sinm', [128, TT], F32)
            W = rope_work(k)
            u = [k.sb('u%d' % i, [128, TT + 2], F32) for i in range(2)]
            cxs = k.sb('cxs', [128, TT], F32); yv = k.sb('yv', [128, TT], F32)
            zc = [k.sb('zc%d' % i, [128, TT], BF16) for i in range(2)]
            for ch in range(2):
                k.memset(u[ch][:], 0.0, [u[ch]])

            def load_x(t):
                xt = xts[t % 2]
                for kc in range(8):
                    k.dma('sp', xt[:, kc, :], xTv[:, kc, t * TT:(t + 1) * TT], writes=[xt], partial=(kc > 0))

            load_x(0)
            for t in range(NT):
                cs = slice(t * TT, (t + 1) * TT)
                if t + 1 < NT:
                    load_x(t + 1)
                xt = xts[t % 2]
                hT = hTa[t % 2]
                for kc in range(8):
                    k.act(sq[:, kc, :], xt[:, kc, :], AF.Square, [xt], [sq], partial=(kc > 0))
                pss = rot.next()
                for kc in range(8):
                    k.mm(pss[:, :], X.ones_bf[:, :], sq[:, kc, :], kc == 0, kc == 7, [X.ones_bf, sq], [pss])
                k.act(rtmp[:], pss[:, :], AF.Sqrt, [pss], [rtmp], bias=EPS, scale=1.0 / D)
                k.recip(rstd[:], rtmp[:], [rtmp], [rstd])
                for kc in range(8):
                    hb = hx[kc % 2]
                    k.stt(hb[:], xt[:, kc, :], gsc[:, kc:kc + 1], rstd[:], ALU.mult, ALU.mult, [xt, gsc, rstd], [hb])
                    k.act(hT[:, kc, :], hb[:], AF.Identity, [hb, modT], [hT], bias=modT[:, kc:kc + 1], partial=(kc > 0))
                if write_ht:
                    k.dma('pool', HTv[:, :, cs], hT[:], reads=[hT], writes=[HT], partial=True)
                rope_tables(k, W, A['pos'][0:1, cs], invf[:, 1:2], cosm, sinm)
                for j in range(5):
                    ps = rot.next()
                    for kc in range(8):
                        k.mm(ps[:, :], w_a[:, kc, j * 128:(j + 1) * 128], hT[:, kc, :], kc == 0, kc == 7, [w_a, hT], [ps])
                    k.act(lat_f[:, j, :], ps[:, :], AF.Copy, [ps], [lat_f], partial=(j > 0))
                    k.act(sql[:, j, :], ps[:, :], AF.Square, [ps], [sql], partial=(j > 0))
                for ch in range(2):
                    pb = rot.next(); pc = rot.next(); px = rot.next()
                    for (pp, base) in ((pb, 736), (pc, 992), (px, 1248)):
                        for kc in range(8):
                            k.mm(pp[:, :], w_a[:, kc, base + ch * 128: base + (ch + 1) * 128], hT[:, kc, :],
                                 kc == 0, kc == 7, [w_a, hT], [pp])
                    k.act(cxs[:], px[:, :], AF.Copy, [px], [cxs])
                    U = u[ch]
                    if t > 0:
                        k.copy(U[:, 0:2], U[:, TT:TT + 2], [U], [U])
                    k.tt(U[:, 2:TT + 2], pc[:, :], cxs[:], ALU.mult, [pc, cxs, U], [U])
                    k.ts(yv[:], U[:, 2:TT + 2], cw[:, ch, 2:3], ALU.mult, [U, cw], [yv])
                    k.stt(yv[:], U[:, 1:TT + 1], cw[:, ch, 1:2], yv[:], ALU.mult, ALU.add, [U, cw, yv], [yv])
                    k.stt(yv[:], U[:, 0:TT], cw[:, ch, 0:1], yv[:], ALU.mult, ALU.add, [U, cw, yv], [yv])
                    Z = zc[ch]
                    k.tt(Z[:], pb[:, :], yv[:], ALU.mult, [pb, yv], [Z])
                    k.dma('pool', A['ZTc'][ch * 128:(ch + 1) * 128, cs], Z[:], reads=[Z], writes=[ZT], partial=True)
                for (j0, j1, n, dst) in ((0, 3, 384.0, rq_bc), (3, 5, 256.0, rkv_bc)):
                    ps = rot.next()
                    for j in range(j0, j1):
                        k.mm(ps[:, :], X.ones_bf[:, :], sql[:, j, :], j == j0, j == j1 - 1, [X.ones_bf, sql], [ps])
                    k.act(rtmp[:], ps[:, :], AF.Sqrt, [ps], [rtmp], bias=EPS, scale=1.0 / n)
                    k.recip(dst[:], rtmp[:], [rtmp], [dst])
                for j in range(3):
                    k.tt(qn[:, j, :], lat_f[:, j, :], rq_bc[:], ALU.mult, [lat_f, rq_bc], [qn], partial=(j > 0))
                for j in range(2):
                    k.tt(kvn[:, j, :], lat_f[:, 3 + j, :], rkv_bc[:], ALU.mult, [lat_f, rkv_bc], [kvn], partial=(j > 0))
                ps = rot.next()
                for kc in range(8):
                    k.mm(ps[0:96, :], w_a[:, kc, 640:736], hT[:, kc, :], kc == 0, kc == 7, [w_a, hT], [ps])
                k.act(kr_f[64:96, :], ps[64:96, :], AF.Copy, [ps], [kr_f])
                k.act(kr_b[0:96, :], ps[0:96, :], AF.Copy, [ps], [kr_b])
                ps2 = rot.next()
                k.mm(ps2[0:96, :], prot[0:96, 1, 0:96], kr_b[0:96, :], True, True, [prot, kr_b], [ps2])
                k.tt(t1[64:96, :], kr_f[64:96, :], cosm[64:96, :], ALU.mult, [kr_f, cosm], [t1])
                k.tt(t2[64:96, :], ps2[64:96, :], sinm[64:96, :], ALU.mult, [ps2, sinm], [t2])
                k.tt(kpe[64:96, :], t1[64:96, :], t2[64:96, :], ALU.add, [t1, t2], [kpe])
                for h in range(4):
                    k.dma('pool', A['KTd'][h, 64:96, cs], kpe[64:96, :], reads=[kpe], writes=[KTd], partial=True)
                for h in range(4):
                    ps = rot.next()
                    for rc in range(3):
                        k.mm(ps[0:96, :], wuq[:, rc, h * 96:(h + 1) * 96], qn[:, rc, :], rc == 0, rc == 2, [wuq, qn], [ps])
                    Q = QTs[h]
                    k.act(Q[0:96, :], ps[0:96, :], AF.Copy, [ps], [Q], scale=MLA_SCALE)
                    k.act(qr_f[64:96, :], ps[64:96, :], AF.Copy, [ps], [qr_f], scale=MLA_SCALE)
                    ps2 = rot.next()
                    k.mm(ps2[0:96, :], prot[0:96, 1, 0:96], Q[0:96, :], True, True, [prot, Q], [ps2])
                    k.tt(t1[64:96, :], qr_f[64:96, :], cosm[64:96, :], ALU.mult, [qr_f, cosm], [t1])
                    k.tt(t2[64:96, :], ps2[64:96, :], sinm[64:96, :], ALU.mult, [ps2, sinm], [t2])
                    k.tt(Q[64:96, :], t1[64:96, :], t2[64:96, :], ALU.add, [t1, t2], [Q])
                    k.dma('pool', A['QTd'][h, :, cs], Q[0:96, :], reads=[Q], writes=[QTd], partial=True)
                for h in range(4):
                    ps = rot.next()
                    for rc in range(2):
                        k.mm(ps[0:64, :], wuk[:, rc, h * 64:(h + 1) * 64], kvn[:, rc, :], rc == 0, rc == 1, [wuk, kvn], [ps])
                    Kt = KTs[h]
                    k.act(Kt[0:64, :], ps[0:64, :], AF.Copy, [ps], [Kt])
                    k.dma('pool', A['KTd'][h, 0:64, cs], Kt[0:64, :], reads=[Kt], writes=[KTd], partial=True)
                for blk in range(4):
                    ps = rot.next()
                    for rc in range(2):
                        k.mm(ps[:, 0:256], kvn[:, rc, blk * 128:(blk + 1) * 128], wuv[:, rc, :], rc == 0, rc == 1, [kvn, wuv], [ps])
                    k.act(V_sb[:, t * 4 + blk, :, 0:64], ps[:, 0:256].rearrange("p (h v) -> p h v", h=4), AF.Copy,
                          [ps], [V_sb], partial=True)

        if 'B' in phases:
          with k.scope():
            NB = 1536
            w_b = k.sb('w_b', [128, 8, NB], BF16)
            wbv = A['w_b'].rearrange("(kc p) n -> p kc n", p=128)
            for kc in range(8):
                load_cast(k, w_b, lambda c, n: w_b[:, kc, c:c + n], lambda c, n: wbv[:, kc, c:c + n], NB, step=768)
            hTs = [k.sb('hTb%d' % i, [128, 8, TT], BF16) for i in range(2)]
            rmask = k.sb('rmask', [128, 2, 128], F32)
            k.dma('sp', rmask[:], A['rmask'], writes=[rmask])
            qdec = k.sb('qdec', [128, 2, TT], F32)
            k.dma('sp', qdec[:], A['qdec'], writes=[qdec])
            kdec = k.sb('kdec', [128, 2], F32)
            k.dma('sp', kdec[:], A['kdec'], writes=[kdec])
            sdec = k.sb('sdec', [128, 2], F32)
            k.dma('sp', sdec[:], A['sdec'], writes=[sdec])
            cosr = k.sb('cosr', [128, TT], F32); sinr = k.sb('sinr', [128, TT], F32)
            W = rope_work(k)
            qfs = [k.sb('qf%d' % i, [128, TT], F32) for i in range(4)]; qbs = [k.sb('qb%d' % i, [128, TT], BF16) for i in range(4)]
            t1s = [k.sb('t1b%d' % i, [128, TT], F32) for i in range(4)]; t2s = [k.sb('t2b%d' % i, [128, TT], F32) for i in range(4)]
            RQT = [k.sb('RQT%d' % i, [128, TT], BF16) for i in range(2)]
            RQd = [k.sb('RQd%d' % i, [128, TT], BF16) for i in range(2)]
            RKT = [k.sb('RKT%d' % i, [128, TT], BF16) for i in range(2)]
            RKd = [k.sb('RKd%d' % i, [128, 4, 128], BF16) for i in range(2)]
            RVb = [k.sb('RV%d' % i, [128, 512], BF16) for i in range(4)]; Gb = [k.sb('G%d' % i, [128, 512], BF16) for i in range(4)]
            AT = [k.sb('AT%d' % i, [128, 128], BF16) for i in range(2)]
            st_f = [k.sb('stf%d' % i, [128, 256], F32) for i in range(2)]
            st_b = [k.sb('stb%d' % i, [128, 256], BF16) for i in range(2)]
            junk = k.sb('junk', [128, 256], BF16)
            ssq = k.sb('ssq', [128, 1], F32); sd = k.sb('sd', [128, 1], F32); rinv = k.sb('rinv', [128, 1], F32)
            zr = [k.sb('zr%d' % i, [128, 256], BF16) for i in range(2)]
            zT = [k.sb('zT%d' % i, [128, 2, TT], BF16) for i in range(2)]
            for hr in range(2):
                k.memset(st_f[hr][:], 0.0, [st_f[hr]]); k.memset(st_b[hr][:], 0.0, [st_b[hr]])

            def load_h(t):
                hb = hTs[t % 2]
                k.dma('sp', hb[:], HTv[:, :, t * TT:(t + 1) * TT], reads=[HT], writes=[hb])

            load_h(0)
            for t in range(NT):
                cs = slice(t * TT, (t + 1) * TT)
                if t + 1 < NT:
                    load_h(t + 1)
                hT = hTs[t % 2]
                rope_tables(k, W, A['pos'][0:1, cs], invf[:, 0:1], cosr, sinr)
                pss_ = []
                for c in range(4):
                    hr, isk = c // 2, c % 2
                    ps = rot.next()
                    for kc in range(8):
                        k.mm(ps[:, :], w_b[:, kc, isk * 256 + hr * 128:isk * 256 + (hr + 1) * 128], hT[:, kc, :], kc == 0, kc == 7, [w_b, hT], [ps])
                    pss_.append(ps)
                for c in range(4):
                    sc_ = RET_KS if c % 2 else 1.0
                    k.act(qfs[c][:], pss_[c][:, :], AF.Copy, [pss_[c]], [qfs[c]], scale=sc_)
                    k.act(qbs[c][:], pss_[c][:, :], AF.Copy, [pss_[c]], [qbs[c]], scale=sc_)
                ps2s = []
                for c in range(4):
                    ps2 = rot.next()
                    k.mm(ps2[:, :], prot[:, 0, :], qbs[c][:], True, True, [prot, qbs[c]], [ps2])
                    ps2s.append(ps2)
                for c in range(4):
                    hr, isk = c // 2, c % 2
                    k.tt(t1s[c][:], qfs[c][:], cosr[:], ALU.mult, [qfs[c], cosr], [t1s[c]])
                    k.tt(t2s[c][:], ps2s[c][:, :], sinr[:], ALU.mult, [ps2s[c], sinr], [t2s[c]])
                    if isk:
                        k.tt(RKT[hr][:], t1s[c][:], t2s[c][:], ALU.add, [t1s[c], t2s[c]], [RKT[hr]], eng='pool')
                    else:
                        k.tt(t1s[c][:], t1s[c][:], t2s[c][:], ALU.add, [t1s[c], t2s[c]], [t1s[c]], eng='pool')
                        k.act(RQT[hr][:], t1s[c][:], AF.Copy, [t1s[c]], [RQT[hr]])
                        k.tt(RQd[hr][:], t1s[c][:], qdec[:, hr, :], ALU.mult, [t1s[c], qdec], [RQd[hr]])
                for hr in range(2):
                    pT = rot.next()
                    for blk in range(4):
                        k.tr(pT.bf[:, blk * 128:(blk + 1) * 128], RKT[hr][:, blk * 128:(blk + 1) * 128], X.id_bf[:, :],
                             [RKT[hr], X.id_bf], [pT], partial=(blk > 0))
                    for blk in range(4):
                        k.act(RKd[hr][:, blk, :], pT.bf[:, blk * 128:(blk + 1) * 128], AF.Copy, [pT, kdec], [RKd[hr]],
                              scale=kdec[:, hr:hr + 1], partial=(blk > 0))
                def proj_vg(blk):
                    ps = rot.next()
                    for kc in range(8):
                        k.mm(ps[:, :], hT[:, kc, blk * 128:(blk + 1) * 128], w_b[:, kc, 512:1024], kc == 0, kc == 7, [w_b, hT], [ps])
                    k.act(RVb[blk][:], ps[:, :], AF.Copy, [ps], [RVb[blk]])
                    ps = rot.next()
                    for kc in range(8):
                        k.mm(ps[:, :], hT[:, kc, blk * 128:(blk + 1) * 128], w_b[:, kc, 1024:1536], kc == 0, kc == 7, [w_b, hT], [ps])
                    k.act(Gb[blk][:], ps[:, :], AF.Silu, [ps], [Gb[blk]])

                proj_vg(0)
                for blk in range(4):
                    if blk + 1 < 4:
                        proj_vg(blk + 1)
                    RV = RVb[blk]; G = Gb[blk]
                    bs = slice(blk * 128, (blk + 1) * 128)
                    for hr in range(2):
                        vs = slice(hr * 256, (hr + 1) * 256)
                        pS = rot.next()
                        k.mm(pS[:, 0:128], RKT[hr][:, bs], RQT[hr][:, bs], True, True, [RKT[hr], RQT[hr]], [pS])
                        a = AT[hr]
                        k.tt(a[:], pS[:, 0:128], rmask[:, hr, :], ALU.mult, [pS, rmask], [a])
                        pO = rot.next()
                        k.mm(pO[:, 0:256], a[:], RV[:, vs], True, False, [a, RV], [pO])
                        k.mm(pO[:, 0:256], RQd[hr][:, bs], st_b[hr][:], False, True, [RQd[hr], st_b[hr]], [pO])
                        pN = rot.next()
                        k.mm(pN[:, 0:256], RKd[hr][:, blk, :], RV[:, vs], True, True, [RKd[hr], RV], [pN])
                        k.stt(st_f[hr][:], st_f[hr][:], sdec[:, hr:hr + 1], pN[:, 0:256], ALU.mult, ALU.add,
                              [st_f[hr], sdec, pN], [st_f[hr]])
                        k.act(st_b[hr][:], st_f[hr][:], AF.Copy, [st_f[hr]], [st_b[hr]])
                        k.act(junk[:], pO[:, 0:256], AF.Square, [pO], [junk, ssq], accum_out=ssq[:, 0:1])
                        k.act(sd[:], ssq[:], AF.Sqrt, [ssq], [sd], bias=EPS, scale=1.0 / 256.0)
                        k.recip(rinv[:], sd[:], [sd], [rinv])
                        z = zr[hr]
                        k.stt(z[:], pO[:, 0:256], rinv[:, 0:1], G[:, vs], ALU.mult, ALU.mult, [pO, rinv, G], [z])
                        pT = rot.next()
                        for vc in range(2):
                            k.tr(pT.bf[:, vc * 128:(vc + 1) * 128], z[:, vc * 128:(vc + 1) * 128], X.id_bf[:, :],
                                 [z, X.id_bf], [pT], partial=(vc > 0))
                        k.act(zT[hr][:, :, bs], pT.bf[:, 0:256].rearrange("p (v i) -> p v i", v=2), AF.Copy, [pT], [zT[hr]],
                              partial=True)
                for hr in range(2):
                    k.dma('pool', A['ZTr'][hr * 256:(hr + 1) * 256, cs].rearrange("(v p) c -> p v c", p=128),
                          zT[hr][:], reads=[zT[hr]], writes=[ZT], partial=True)

        if 'S2' in phases:
          with k.scope():
            kts = [k.sb('kt%d' % i, [128, S], BF16) for i in range(2)]
            qts = [k.sb('qt%d' % i, [128, TT], BF16) for i in range(3)]
            PTs = [k.sb('PT%d' % i, [128, TT], BF16) for i in range(3)]
            of = [k.sb('of%d' % i, [128, TT], F32) for i in range(2)]
            rb = k.sb('rb', [128, TT], F32)
            zm = [k.sb('zm%d' % i, [128, TT], BF16) for i in range(2)]
            sel = k.sb('sel', [128, 64], F32)
            for o_ in of:
                k.memset(o_[:], 0.0, [o_])
            k.dma('sp', sel[:], A['sel65'], writes=[sel])
            srot = Rot(banks[0:4]); orot = Rot(banks[4:6]); brot = Rot(banks[6:8])
            qi = 0
            for h in range(4):
                kt = kts[h % 2]
                for c4 in range(4):
                    k.dma('sp', kt[0:96, c4 * 2048:(c4 + 1) * 2048], A['KTd'][h, :, c4 * 2048:(c4 + 1) * 2048],
                          reads=[KTd], writes=[kt], partial=(c4 > 0))
                for qt in range(NT):
                    q = qts[qi % 3]; qi += 1
                    k.dma('sp', q[0:96, :], A['QTd'][h, :, qt * TT:(qt + 1) * TT], reads=[QTd], writes=[q])
                    nkb = 4 * qt + 4
                    O = orot.next()
                    pend = []

                    def emit_s(kb):
                        d = kb - 4 * qt
                        qlo = 0 if d < 0 else 128 * d
                        pS = srot.next()
                        k.mm(pS[:, qlo:TT], kt[0:96, kb * 128:(kb + 1) * 128], q[0:96, qlo:TT], True, True, [kt, q], [pS])
                        P = PTs[kb % 3]
                        k.act(P[:, qlo:TT], pS[:, qlo:TT], AF.Exp, [pS], [P])
                        if d >= 0:
                            k.memset(P[64:128, qlo:qlo + 64], 0.0, [P], eng='pool', partial=True, reads=[P])
                        return P, d, qlo

                    def emit_pv(kb, P, d, qlo):
                        k.mm(O[0:65, qlo:TT], V_sb[:, kb, h, :], P[:, qlo:TT], kb == 0, kb == nkb - 1, [V_sb, P], [O])

                    for kb in range(nkb):
                        pend.append((kb,) + emit_s(kb))
                        if len(pend) > 2:
                            a = pend.pop(0); emit_pv(*a)
                    while pend:
                        a = pend.pop(0); emit_pv(*a)
                    o = of[qt % 2]
                    k.act(o[0:65, :], O[0:65, :], AF.Copy, [O], [o], partial=True)
                    pB = brot.next()
                    k.mm(pB[0:64, :], sel[:, 0:64], o[:, :], True, True, [sel, o], [pB])
                    k.recip(rb[0:64, :], pB[0:64, :], [pB], [rb])
                    z = zm[qt % 2]
                    k.tt(z[0:64, :], o[0:64, :], rb[0:64, :], ALU.mult, [o, rb], [z])
                    k.dma('pool', A['ZTm'][h * 64:(h + 1) * 64, qt * TT:(qt + 1) * TT], z[0:64, :], reads=[z], writes=[ZT], partial=True)


OFF = {'q_lat': 0, 'kv_lat': 384, 'k_rope': 640, 'cb': 672, 'cc': 1184, 'cx': 1696, 'rq': 2208, 'rk': 2720,
       'rv': 3232, 'rg': 4256, 'gl': 5280}


def col8(v):
    return np.ascontiguousarray(v.reshape(-1, 128).T)


def consts_for(hh):
    C = {}
    C['ident'] = np.eye(128, dtype=np.float32)
    prot = np.zeros((128, 2, 128), np.float32)
    for r in range(64):
        prot[r + 64, 0, r] = -1.0
        prot[r, 0, r + 64] = 1.0
    for r in range(64, 80):
        prot[r + 16, 1, r] = -1.0
        prot[r, 1, r + 16] = 1.0
    C['prot'] = prot
    invf = np.zeros((128, 2), np.float32)
    inv_ret = (10000.0 ** (-np.arange(0, 128, 2, dtype=np.float32) / 128)).astype(np.float32)
    inv_mla = (10000.0 ** (-np.arange(0, 32, 2, dtype=np.float32) / 32)).astype(np.float32)
    for p in range(128):
        invf[p, 0] = inv_ret[p % 64]
    for p in range(64, 96):
        invf[p, 1] = inv_mla[(p - 64) % 16]
    C['invf'] = invf
    rmask = np.zeros((128, 2, 128), np.float64); qdec = np.zeros((128, 2, TT), np.float64)
    kdec = np.zeros((128, 2), np.float64); sdec = np.zeros((128, 2), np.float64)
    for hr in range(2):
        H = hh * 2 + hr
        g = 1.0 - 2.0 ** (-5.0 - H)
        for j in range(128):
            for i in range(128):
                cj, ci = j // 64, i // 64
                if cj == ci:
                    rmask[j, hr, i] = g ** abs(i - j)
                elif cj < ci:
                    rmask[j, hr, i] = g ** (i - j)
        qdec[:, hr, :] = (g ** ((np.arange(TT) % 128) + 1.0))[None, :]
        kdec[:, hr] = g ** (127.0 - np.arange(128))
        sdec[:, hr] = g ** 128.0
    C['rmask'] = rmask.astype(np.float32); C['qdec'] = qdec.astype(np.float32)
    C['kdec'] = kdec.astype(np.float32); C['sdec'] = sdec.astype(np.float32)
    sel = np.zeros((128, 64), np.float32); sel[64, :] = 1.0
    C['sel65'] = sel
    return C


def mixer_inputs(inp, l, b, hh, xT_b):
    w_in = inp['w_in'][l]
    m = dict(consts_for(hh))
    if xT_b is not None:
        m['xT'] = xT_b
    m['cc8'] = col8(inp['c'][b])
    m['b_ada8'] = col8(inp['b_ada'][l])
    m['gmix8'] = col8(inp['norm_mix_g'][l])
    wa = np.zeros((D, 1504), np.float32)
    wa[:, 0:640] = w_in[:, 0:640]
    wa[:, 704:736] = w_in[:, 640:672]
    for i, nm in enumerate(('cb', 'cc', 'cx')):
        wa[:, 736 + i * 256:736 + (i + 1) * 256] = w_in[:, OFF[nm] + hh * 256:OFF[nm] + (hh + 1) * 256]
    m['w_a'] = wa
    wb = np.empty((D, 1536), np.float32)
    wb[:, 0:256] = w_in[:, OFF['rq'] + hh * 256:OFF['rq'] + (hh + 1) * 256]
    wb[:, 256:512] = w_in[:, OFF['rk'] + hh * 256:OFF['rk'] + (hh + 1) * 256]
    wb[:, 512:1024] = w_in[:, OFF['rv'] + hh * 512:OFF['rv'] + (hh + 1) * 512]
    wb[:, 1024:1536] = w_in[:, OFF['rg'] + hh * 512:OFF['rg'] + (hh + 1) * 512]
    m['w_b'] = wb
    m['pos'] = np.ascontiguousarray(inp['positions'][b:b + 1].astype(np.int32))
    m['qg3'] = col8(inp['mla_q_norm_g'][l]); m['kvg2'] = col8(inp['mla_kv_norm_g'][l])
    m['wuq'] = np.ascontiguousarray(inp['w_uq'][l][:, hh * 384:(hh + 1) * 384])
    wukv = inp['w_ukv'][l].reshape(256, 8, 128)[:, hh * 4:(hh + 1) * 4, :]
    m['wuk'] = np.ascontiguousarray(wukv[:, :, 0:64].reshape(256, 256))
    m['wuv'] = np.ascontiguousarray(wukv[:, :, 64:128].reshape(256, 256))
    cwv = inp['conv_w'][l][:, hh * 256:(hh + 1) * 256]
    m['convw'] = np.ascontiguousarray(cwv.reshape(3, 2, 128).transpose(2, 1, 0))
    return m


SO = 4096; NTO = SO // TT
BIG = 1.0e30


def emit_ffn(k, X, A, modT, last, tok):
    if True:
        banks = X.banks
        rot = Rot(banks[0:7])
        pL = banks[7]
        gffn = k.sb('gffn', [128, 8], F32)
        k.dma('sp', gffn[:], A['gffn8'], writes=[gffn])
        gsc2 = k.sb('gsc2', [128, 8], F32)
        k.stt(gsc2[:], modT[:, 32:40], 1.0, gffn[:], ALU.add, ALU.mult, [modT, gffn], [gsc2])
        X1T = A['X1T_buf']; H2K = A['H2K_buf']; HSb = A['HS_buf']; YSb = A['YS_buf']; XO = A['xo_buf']
        NBLK = SO // 128; TS = 256; NTILE = 64
        rc = k.sb('rconst', [128, 128 + 128 + 64 + 16 + 32 + 2], F32)
        k.dma('sp', rc[:], A['rconst'], writes=[rc])
        tri = rc[:, 0:128]; ones_f = rc[:, 128:256]; iota64 = rc[:, 256:320]; thr16 = rc[:, 320:336]
        iota32 = rc[:, 336:368]; pbase2 = rc[:, 368:370]
        e1s = k.sb('e1s', [128, NBLK], F32); e2s = k.sb('e2s', [128, NBLK], F32)
        r1s = k.sb('r1s', [128, NBLK], F32); r2s = k.sb('r2s', [128, NBLK], F32)
        w1s = k.sb('w1s', [128, NBLK], F32); w2s = k.sb('w2s', [128, NBLK], F32)
        run_bc = k.sb('run_bc', [128, 32], F32)
        k.memset(run_bc[:], 0.0, [run_bc])
        pos1i = k.sb('pos1i', [128, NBLK], I32); pos2i = k.sb('pos2i', [128, NBLK], I32)
        widx = k.sb('widx', [128, NTILE, 2], I32)
        HTb = A['HT_buf']; ZTb = A['ZT_buf']; XSb = A['xs_buf']
        xTv = A['xT'].rearrange("(kc p) c -> p kc c", p=128)
        HTv = A['HT'].rearrange("(kc p) c -> p kc c", p=128)
        ZTv = A['ZT'].rearrange("(kc p) c -> p kc c", p=128)
        X1v = A['X1T'].rearrange("(kc p) c -> p kc c", p=128)
        XOv = A['xoT'].rearrange("(kc p) c -> p kc c", p=128)

        with k.scope():
            w_g = k.sb('w_g', [128, 8, 3072], BF16)
            wgv = A['w_g'].rearrange("(kc p) n -> p kc n", p=128)
            for kc in range(8):
                load_cast(k, w_g, lambda c, n: w_g[:, kc, c:c + n], lambda c, n: wgv[:, kc, c:c + n], 3072, step=1024)
            w_o = k.sb('w_o', [128, 16, 1024], BF16)
            wov = A['w_o'].rearrange("(kc p) n -> p kc n", p=128)
            for kc in range(16):
                k.dma('pool', w_o[:, kc, :], wov[:, kc, :], writes=[w_o], partial=True)
            w_m = k.sb('w_m', [128, 8, 1024], BF16)
            wmv = A['w_mix'].rearrange("(kc p) n -> p kc n", p=128)
            for kc in range(8):
                k.dma('pool', w_m[:, kc, :], wmv[:, kc, :], writes=[w_m], partial=True)
            w_r = k.sb('w_r', [128, 8, 36], F32)
            k.dma('sp', w_r[:], A['w_r'].rearrange("(kc p) n -> p kc n", p=128), writes=[w_r])
            b_r = k.sb('b_r', [128, 36], F32)
            k.dma('sp', b_r[:], A['b_r'].to_broadcast([128, 36]), writes=[b_r])
            hT = k.sb('hTc', [128, 8, TT], BF16)
            Zt = k.sb('Zt', [128, 16, TT], BF16)
            xt = k.sb('xtc', [128, 8, TT], F32)
            gt0 = k.sb('gt0', [128, 3, TT], BF16); gt = [gt0, gt0]
            tA = k.sb('tA', [128, TT], F32); tB = k.sb('tB', [128, TT], F32)
            mg = k.sb('mg', [128, 8, TT], BF16)
            rtmp = k.sb('rtmpc', [128, TT], F32); rstd = k.sb('rstdc', [128, TT], F32)
            h2f0 = k.sb('h2f0', [128, TT], F32); h2f = [h2f0, h2f0]
            h2b = k.sb('h2b', [128, 8, TT], BF16)
            lt = tA
            lg = k.sb('lg', [128, 36], F32); gmax = k.sb('gmax', [128, 1], F32); ngmax = k.sb('ngmax', [128, 1], F32)
            g1h = k.sb('g1h', [128, 4], F32); pen = k.sb('pen', [128, 4], F32)
            ej = k.sb('ej', [128, 4], F32); gsum = k.sb('gsum', [128, 1], F32); gval = k.sb('gval', [128, 1], F32)
            lem = k.sb('lem', [128, 32], F32); top8 = k.sb('top8', [128, 8], F32)
            m1 = k.sb('m1', [128, 32], F32); m2 = k.sb('m2', [128, 32], F32)
            dd = k.sb('dd', [128, 1], F32); ee = k.sb('ee', [128, 1], F32); den = k.sb('den', [128, 1], F32)
            oh = k.sb('oh', [128, 32], F32); rk = k.sb('rk', [128, 32], F32); jk = k.sb('jk', [128, 32], F32)
            cT = tB
            for t in range(NTO):
                cs = slice(t * TT, (t + 1) * TT)
                tsl = tok('sp', t * TT, TT)
                k.dma('sp', hT[:], HTv[:, :, tsl], reads=[HTb], writes=[hT])
                k.dma('sp', Zt[:], ZTv[:, :, tsl], reads=[ZTb], writes=[Zt])
                for kc in range(8):
                    k.dma('sp', xt[:, kc, :], xTv[:, kc, tsl], reads=[XSb], writes=[xt], partial=(kc > 0))
                for dc in range(8):
                    ds_ = slice(dc * 128, (dc + 1) * 128)
                    g = gt[dc % 2]
                    for br in range(3):
                        ps = rot.next()
                        for kc in range(8):
                            k.mm(ps[:, :], w_g[:, kc, br * 1024 + dc * 128: br * 1024 + (dc + 1) * 128], hT[:, kc, :],
                                 kc == 0, kc == 7, [w_g, hT], [ps])
                        k.act(g[:, br, :], ps[:, :], AF.Sigmoid, [ps], [g], partial=(br > 0))
                    pys = []
                    for (k0, k1) in ((0, 4), (4, 8), (8, 16)):
                        ps = rot.next()
                        for kc in range(k0, k1):
                            k.mm(ps[:, :], w_o[:, kc, ds_], Zt[:, kc, :], kc == k0, kc == k1 - 1, [w_o, Zt], [ps])
                        pys.append(ps)
                    k.tt(tA[:], pys[0][:, :], g[:, 0, :], ALU.mult, [pys[0], g], [tA])
                    k.tt(tB[:], pys[1][:, :], g[:, 1, :], ALU.mult, [pys[1], g], [tB])
                    k.tt(tA[:], tA[:], tB[:], ALU.add, [tA, tB], [tA], eng='pool')
                    k.tt(tB[:], pys[2][:, :], g[:, 2, :], ALU.mult, [pys[2], g], [tB])
                    k.tt(mg[:, dc, :], tA[:], tB[:], ALU.add, [tA, tB], [mg], eng='pool', partial=(dc > 0))
                for dc in range(8):
                    ps = rot.next()
                    for kc in range(8):
                        k.mm(ps[:, :], w_m[:, kc, dc * 128:(dc + 1) * 128], mg[:, kc, :], kc == 0, kc == 7, [w_m, mg], [ps])
                    k.stt(xt[:, dc, :], ps[:, :], modT[:, 16 + dc:17 + dc], xt[:, dc, :], ALU.mult, ALU.add,
                          [ps, modT, xt], [xt], partial=True)
                k.dma('pool', X1v[:, :, cs], xt[:], reads=[xt], writes=[X1T], partial=True)
                for kc in range(8):
                    k.act(mg[:, kc, :], xt[:, kc, :], AF.Square, [xt], [mg], partial=(kc > 0))
                pss = rot.next()
                for kc in range(8):
                    k.mm(pss[:, :], X.ones_bf[:, :], mg[:, kc, :], kc == 0, kc == 7, [X.ones_bf, mg], [pss])
                k.act(rtmp[:], pss[:, :], AF.Sqrt, [pss], [rtmp], bias=EPS, scale=1.0 / D)
                k.recip(rstd[:], rtmp[:], [rtmp], [rstd])
                for kc in range(8):
                    hf = h2f[kc % 2]
                    k.stt(hf[:], xt[:, kc, :], gsc2[:, kc:kc + 1], rstd[:], ALU.mult, ALU.mult, [xt, gsc2, rstd], [hf])
                    k.act(hf[:], hf[:], AF.Identity, [hf, modT], [hf], bias=modT[:, 24 + kc:25 + kc])
                    k.copy(h2b[:, kc, :], hf[:], [hf], [h2b], eng='pool', partial=(kc > 0))
                    k.mm(pL[0:36, :], w_r[:, kc, :], hf[:], kc == 0, kc == 7, [w_r, hf], [pL])
                for blk in range(4):
                    pH = rot.next()
                    for kc in range(8):
                        k.tr(pH.bf[:, kc * 128:(kc + 1) * 128], h2b[:, kc, blk * 128:(blk + 1) * 128], X.id_bf[:, :],
                             [h2b, X.id_bf], [pH], partial=(kc > 0))
                    hk = gt0
                    k.act(hk[:, 0:2, :], pH.bf[:, :].rearrange("p (a b) -> p a b", a=2), AF.Copy, [pH], [hk])
                    k.dma('pool', A['H2K'][(t * 4 + blk) * 128:(t * 4 + blk + 1) * 128, :].rearrange("p (a b) -> p a b", a=2),
                          hk[:, 0:2, :], reads=[hk], writes=[H2K], partial=True)
                k.act(lt[0:36, :], pL[0:36, :], AF.Copy, [pL], [lt])
                pT = rot.next()
                for blk in range(4):
                    k.tr(pT[:, blk * 36:(blk + 1) * 36], lt[0:36, blk * 128:(blk + 1) * 128], X.id_f[0:36, 0:36],
                         [lt, X.id_f], [pT], partial=(blk > 0))
                for blk in range(4):
                    gb = t * 4 + blk
                    w1 = w1s[:, gb:gb + 1]; w2 = w2s[:, gb:gb + 1]
                    k.tt(lg[:], pT[:, blk * 36:(blk + 1) * 36], b_r[:], ALU.add, [pT, b_r], [lg])
                    k.op('dve', lambda e: e.reduce_max(out=gmax[:], in_=lg[:, 0:4], axis=mybir.AxisListType.X),
                         reads=[lg], writes=[gmax])
                    k.ts(g1h[:], lg[:, 0:4], gmax[:, 0:1], ALU.is_equal, [lg, gmax], [g1h])
                    k.ts(ngmax[:], gmax[:], -1.0, ALU.mult, [gmax], [ngmax])
                    k.act(ej[:], lg[:, 0:4], AF.Exp, [lg, ngmax], [ej, gsum], bias=ngmax[:, 0:1], accum_out=gsum[:, 0:1])
                    k.recip(gval[:], gsum[:], [gsum], [gval])
                    k.ts(pen[:], g1h[:], -1.0, ALU.add, [g1h], [pen], s2=BIG, op1=ALU.mult)
                    for g_ in range(4):
                        k.ts(lem[:, g_ * 8:(g_ + 1) * 8], lg[:, 4 + g_ * 8:4 + (g_ + 1) * 8], pen[:, g_:g_ + 1], ALU.add,
                             [lg, pen], [lem], partial=(g_ > 0))
                    k.op('dve', lambda e: e.max(out=top8[:], in_=lem[:]), reads=[lem], writes=[top8])
                    k.ts(m1[:], lem[:], top8[:, 0:1], ALU.is_equal, [lem, top8], [m1])
                    k.ts(m2[:], lem[:], top8[:, 1:2], ALU.is_equal, [lem, top8], [m2])
                    k.tt(dd[:], top8[:, 1:2], top8[:, 0:1], ALU.subtract, [top8], [dd])
                    k.act(ee[:], dd[:], AF.Exp, [dd], [ee])
                    k.ts(den[:], ee[:], 1.0, ALU.add, [ee], [den])
                    k.recip(den[:], den[:], [den], [den])
                    k.tt(w1, den[:], gval[:], ALU.mult, [den, gval], [w1s], partial=True)
                    k.tt(w2, w1, ee[:], ALU.mult, [w1s, ee], [w2s], partial=True)
                    k.tt(oh[:], m1[:], m2[:], ALU.add, [m1, m2], [oh])
                    pP = rot.next()
                    k.mm(pP[:, 0:32], tri, oh[:], True, True, [rc, oh], [pP])
                    k.tt(rk[:], pP[:, 0:32], run_bc[:], ALU.add, [pP, run_bc], [rk])
                    k.op('dve', lambda e: e.scalar_tensor_tensor(out=jk[:], in0=rk[:], scalar=1.0, in1=m1[:], op0=ALU.mult, op1=ALU.mult,
                                                              accum_out=r1s[:, gb:gb + 1]), reads=[rk, m1], writes=[jk, r1s], partial=True)
                    k.op('dve', lambda e: e.scalar_tensor_tensor(out=jk[:], in0=rk[:], scalar=1.0, in1=m2[:], op0=ALU.mult, op1=ALU.mult,
                                                              accum_out=r2s[:, gb:gb + 1]), reads=[rk, m2], writes=[jk, r2s], partial=True)
                    k.op('dve', lambda e: e.scalar_tensor_tensor(out=jk[:], in0=iota32, scalar=1.0, in1=m1[:], op0=ALU.mult, op1=ALU.mult,
                                                              accum_out=e1s[:, gb:gb + 1]), reads=[rc, m1], writes=[jk, e1s], partial=True)
                    k.op('dve', lambda e: e.scalar_tensor_tensor(out=jk[:], in0=iota32, scalar=1.0, in1=m2[:], op0=ALU.mult, op1=ALU.mult,
                                                              accum_out=e2s[:, gb:gb + 1]), reads=[rc, m2], writes=[jk, e2s], partial=True)
                    pQ = rot.next()
                    k.mm(pQ[:, 0:32], ones_f, oh[:], True, True, [rc, oh], [pQ])
                    k.tt(run_bc[:], run_bc[:], pQ[:, 0:32], ALU.add, [run_bc, pQ], [run_bc])
            ptile = k.sb('ptile', [128, 32], F32); cmp16 = k.sb('cmp16', [128, 16], F32)
            start = k.sb('start', [128, 32], F32); endt = k.sb('endt', [128, 32], F32); sstart = k.sb('sstart', [128, 32], F32)
            et = k.sb('et', [128, NTILE], F32); wf = k.sb('wf', [128, NTILE, 2], F32)
            p1f = k.sb('p1f', [128, NBLK], F32); p2f = k.sb('p2f', [128, NBLK], F32)
            for e_ in range(32):
                k.op('dve', lambda e: e.tensor_scalar(out=cmp16[:], in0=thr16, scalar1=run_bc[:, e_:e_ + 1], scalar2=None,
                                                      op0=ALU.is_lt, op1=ALU.add, accum_out=ptile[:, e_:e_ + 1]),
                     reads=[rc, run_bc], writes=[cmp16, ptile], partial=True)
            k.memset(start[:, 0:1], 0.0, [start], partial=True)
            for e_ in range(1, 32):
                k.tt(start[:, e_:e_ + 1], start[:, e_ - 1:e_], ptile[:, e_ - 1:e_], ALU.add, [start, ptile], [start], partial=True)
            k.tt(endt[:], start[:], ptile[:], ALU.add, [start, ptile], [endt])
            k.ts(sstart[:], start[:], float(TS), ALU.mult, [start], [sstart])
            k.memset(et[:], 0.0, [et])
            for e_ in range(32):
                k.stt(et[:], iota64, endt[:, e_:e_ + 1], et[:], ALU.is_ge, ALU.add, [rc, endt, et], [et])
            k.ts(et[:], et[:], 31.0, ALU.min, [et], [et])
            same = k.sb('same', [128, NTILE], F32)
            k.memset(same[:, 0:1], 0.0, [same], partial=True)
            k.tt(same[:, 1:NTILE], et[:, 1:NTILE], et[:, 0:NTILE - 1], ALU.is_equal, [et], [same], partial=True)
            for j in range(2):
                k.ts(wf[:, :, j], et[:], 256.0, ALU.mult, [et, rc], [wf], s2=pbase2[:, j:j + 1], op1=ALU.add, partial=True)
                k.stt(wf[:, :, j], same[:], 1.0e7, wf[:, :, j], ALU.mult, ALU.add, [same, wf], [wf], partial=True)
            k.copy(widx[:], wf[:], [wf], [widx])
            for gb in range(NBLK):
                k.ts(m1[:], iota32, e1s[:, gb:gb + 1], ALU.is_equal, [rc, e1s], [m1])
                k.op('dve', lambda e: e.scalar_tensor_tensor(out=jk[:], in0=m1[:], scalar=1.0, in1=sstart[:], op0=ALU.mult, op1=ALU.mult,
                                                          accum_out=p1f[:, gb:gb + 1]), reads=[m1, sstart], writes=[jk, p1f], partial=True)
                k.ts(m2[:], iota32, e2s[:, gb:gb + 1], ALU.is_equal, [rc, e2s], [m2])
                k.op('dve', lambda e: e.scalar_tensor_tensor(out=jk[:], in0=m2[:], scalar=1.0, in1=sstart[:], op0=ALU.mult, op1=ALU.mult,
                                                          accum_out=p2f[:, gb:gb + 1]), reads=[m2, sstart], writes=[jk, p2f], partial=True)
            k.tt(p1f[:], p1f[:], r1s[:], ALU.add, [p1f, r1s], [p1f])
            k.tt(p2f[:], p2f[:], r2s[:], ALU.add, [p2f, r2s], [p2f])
            k.copy(pos1i[:], p1f[:], [p1f], [pos1i]); k.copy(pos2i[:], p2f[:], [p2f], [pos2i])

        if last:
            fin = k.sb('fin', [128, 8], F32)
            k.dma('sp', fin[:], A['fin8'], writes=[fin])
        with k.scope():
            hr_ = [k.sb('hr%d' % i, [128, 1024], BF16) for i in range(2)]
            for gb in range(NBLK):
                hb_ = hr_[gb % 2]
                k.dma('sp', hb_[:], A['H2K'][gb * 128:(gb + 1) * 128, :], reads=[H2K], writes=[hb_])
                k.idma(A['HS'][:, :], hb_[:, :], pos1i[:, gb:gb + 1], True, [hb_, pos1i], [HSb], hb_)
                k.idma(A['HS'][:, :], hb_[:, :], pos2i[:, gb:gb + 1], True, [hb_, pos2i], [HSb], hb_)
        with k.scope():
            wg0 = k.sb('wg0', [128, 4096], BF16); wu0 = k.sb('wu0', [128, 4096], BF16); wd0 = k.sb('wd0', [128, 4096], BF16)
            wg = [wg0, wg0]; wu = [wu0, wu0]; wd = [wd0, wd0]
            hst = [k.sb('hst%d' % i, [128, 2, 1024], BF16) for i in range(2)]
            hTs = [k.sb('hTs%d' % i, [128, 8, TS], BF16) for i in range(2)]
            sg = [k.sb('sg%d' % i, [128, TS], F32) for i in range(2)]
            hid = [k.sb('hid%d' % i, [128, 4, TS], BF16) for i in range(2)]
            ys = [k.sb('ys%d' % i, [128, 2, 1024], F32) for i in range(2)]
            HSv = A['HS'].rearrange("(i sb p) n -> i p sb n", sb=2, p=128)
            YSv = A['YS'].rearrange("(i sb p) n -> i p sb n", sb=2, p=128)

            def load_w(i, which):
                for (wt, src) in which:
                    for j in range(2):
                        k.idma(wt[:, j * 2048:(j + 1) * 2048], src[:, :], widx[:, i, j:j + 1], False, [widx], [wt], wt, partial=(j > 0), bound=8191)

            GU = ((wg0, A['w_eg']), (wu0, A['w_eu'])); DN = ((wd0, A['w_ed']),)
            load_w(0, GU); load_w(0, DN)
            for i in range(NTILE):
                bi = i % 2
                hs_ = hst[bi]; hT_ = hTs[bi]; hd = hid[bi]; y_ = ys[bi]
                if i == 0:
                    k.dma('sp', hs_[:], HSv[0], reads=[HSb], writes=[hs_])
                if i + 1 < NTILE:
                    k.dma('sp', hst[(i + 1) % 2][:], HSv[i + 1], reads=[HSb], writes=[hst[(i + 1) % 2]])
                for sb in range(2):
                    pH = rot.next()
                    for kc in range(8):
                        k.tr(pH.bf[:, kc * 128:(kc + 1) * 128], hs_[:, sb, kc * 128:(kc + 1) * 128], X.id_bf[:, :],
                             [hs_, X.id_bf], [pH], partial=(kc > 0))
                    k.act(hT_[:, :, sb * 128:(sb + 1) * 128], pH.bf[:, :].rearrange("p (kc s) -> p kc s", kc=8), AF.Copy,
                          [pH], [hT_], partial=(sb > 0))
                for fc in range(4):
                    pg = rot.next(); pu = rot.next()
                    for kc in range(8):
                        k.mm(pg[:, 0:TS], wg[bi][:, kc * 512 + fc * 128: kc * 512 + (fc + 1) * 128], hT_[:, kc, :], kc == 0, kc == 7, [wg[bi], hT_], [pg])
                    for kc in range(8):
                        k.mm(pu[:, 0:TS], wu[bi][:, kc * 512 + fc * 128: kc * 512 + (fc + 1) * 128], hT_[:, kc, :], kc == 0, kc == 7, [wu[bi], hT_], [pu])
                    s_ = sg[fc % 2]
                    k.act(s_[:], pg[:, 0:TS], AF.Silu, [pg], [s_])
                    k.tt(hd[:, fc, :], pu[:, 0:TS], s_[:], ALU.mult, [pu, s_], [hd], partial=(fc > 0))
                if i + 1 < NTILE:
                    load_w(i + 1, GU)
                for sb in range(2):
                    for dh in range(2):
                        pd = rot.next()
                        for fc in range(4):
                            k.mm(pd[:, :], hd[:, fc, sb * 128:(sb + 1) * 128], wd[bi][:, fc * 1024 + dh * 512: fc * 1024 + (dh + 1) * 512],
                                 fc == 0, fc == 3, [hd, wd[bi]], [pd])
                        if (sb + dh) % 2 == 0:
                            k.act(y_[:, sb, dh * 512:(dh + 1) * 512], pd[:, :], AF.Copy, [pd], [y_], partial=True)
                        else:
                            k.copy(y_[:, sb, dh * 512:(dh + 1) * 512], pd[:, :], [pd], [y_], partial=True)
                if i + 1 < NTILE:
                    load_w(i + 1, DN)
                k.dma('act', YSv[i], y_[:], reads=[y_], writes=[YSb], partial=True)
        with k.scope():
            y1 = [k.sb('y1_%d' % i, [128, 1024], F32) for i in range(2)]
            y2 = [k.sb('y2_%d' % i, [128, 1024], F32) for i in range(2)]
            mo = k.sb('mo', [128, 4, 1024], F32)
            x1 = [k.sb('x1_%d' % i, [128, 8, TT], F32) for i in range(2)]
            sqf = k.sb('sqf', [128, 8, TT], BF16)
            rt2 = k.sb('rt2', [128, TT], F32); rs2 = k.sb('rs2', [128, TT], F32)
            for t in range(NTO):
                gs = slice(t * TT, (t + 1) * TT)
                xb = x1[t % 2]
                k.dma('sp', xb[:], X1v[:, :, gs], reads=[X1T], writes=[xb])
                for blk in range(4):
                    gb = t * 4 + blk
                    a1 = y1[blk % 2]; a2 = y2[blk % 2]
                    k.idma(a1[:, :], A['YS'][:, :], pos1i[:, gb:gb + 1], False, [YSb, pos1i], [a1], a1, partial=False)
                    k.idma(a2[:, :], A['YS'][:, :], pos2i[:, gb:gb + 1], False, [YSb, pos2i], [a2], a2, partial=False)
                    k.ts(mo[:, blk, :], a1[:], w1s[:, gb:gb + 1], ALU.mult, [a1, w1s], [mo], partial=(blk > 0))
                    k.stt(mo[:, blk, :], a2[:], w2s[:, gb:gb + 1], mo[:, blk, :], ALU.mult, ALU.add, [a2, w2s, mo], [mo], partial=True)
                for dc in range(8):
                    pM = rot.next()
                    for blk in range(4):
                        k.tr(pM[:, blk * 128:(blk + 1) * 128], mo[:, blk, dc * 128:(dc + 1) * 128], X.id_f[:, :],
                             [mo, X.id_f], [pM], partial=(blk > 0))
                    k.stt(xb[:, dc, :], pM[:, :], modT[:, 40 + dc:41 + dc], xb[:, dc, :], ALU.mult, ALU.add,
                          [pM, modT, xb], [xb], partial=True)
                if last:
                    for kc in range(8):
                        k.act(sqf[:, kc, :], xb[:, kc, :], AF.Square, [xb], [sqf], partial=(kc > 0))
                    pss = rot.next()
                    for kc in range(8):
                        k.mm(pss[:, :], X.ones_bf[:, :], sqf[:, kc, :], kc == 0, kc == 7, [X.ones_bf, sqf], [pss])
                    k.act(rt2[:], pss[:, :], AF.Sqrt, [pss], [rt2], bias=EPS, scale=1.0 / D)
                    k.recip(rs2[:], rt2[:], [rt2], [rs2])
                    for kc in range(8):
                        k.stt(xb[:, kc, :], xb[:, kc, :], fin[:, kc:kc + 1], rs2[:], ALU.mult, ALU.mult,
                              [xb, fin, rs2], [xb], partial=True)
                k.dma('pool', XOv[:, :, gs], xb[:], reads=[xb], writes=[XO], partial=True)


FUSED_IN = {
    'xT': ([D, S], F32), 'cc8': ([128, 8], F32), 'w_ada': ([2, D, 6 * D], F32), 'b_ada8': ([2, 128, 48], F32),
    'gmix8': ([2, 128, 8], F32), 'gffn8': ([2, 128, 8], F32), 'fin8': ([128, 8], F32),
    'w_a': ([2, 2, D, 1504], F32), 'w_b': ([2, 2, D, 1536], F32), 'pos': ([1, S], I32),
    'qg3': ([2, 128, 3], F32), 'kvg2': ([2, 128, 2], F32), 'wuq': ([2, 2, 384, 384], F32),
    'wuk': ([2, 2, 256, 256], F32), 'wuv': ([2, 2, 256, 256], F32), 'convw': ([2, 2, 128, 2, 3], F32),
    'ident': ([128, 128], F32), 'prot': ([128, 2, 128], F32), 'invf': ([128, 2], F32),
    'rmask': ([2, 128, 2, 128], F32), 'qdec': ([2, 128, 2, TT], F32), 'kdec': ([2, 128, 2], F32),
    'sdec': ([2, 128, 2], F32), 'sel65': ([128, 64], F32),
    'w_g': ([2, D, 3072], F32), 'w_o': ([2, 2048, D], F32), 'w_mix': ([2, D, D], F32), 'w_r': ([2, D, 36], F32),
    'b_r': ([2, 1, 36], F32), 'w_eg0': ([8192, 2048], F32), 'w_eu0': ([8192, 2048], F32), 'w_ed0': ([8192, 2048], F32),
    'w_eg1': ([8192, 2048], F32), 'w_eu1': ([8192, 2048], F32), 'w_ed1': ([8192, 2048], F32), 'rconst': ([128, 370], F32),
}


def build_fused(nc, A):
    root = ExitStack()
    with root:
        k = K(nc, root)
        X = setup_common(k, A)
        V_sb = k.sb('V_sb', [128, 64, 4, 65], BF16)
        k.memset(V_sb[:, :, :, 64:65], 1.0, [V_sb], eng='pool')
        bufs = {n: Buf(n, A[n], dram=True) for n in ('HT', 'ZT', 'QTd', 'KTd', 'XO', 'X1T', 'H2K', 'HS', 'YS', 'xT', 'out', 'HTo', 'ZTo', 'XOo')}
        modT = [k.sb('modT%d' % l, [128, 48], F32) for l in range(2)]
        for l in range(2):
            emit_mod(k, X, {'cc8': A['cc8'], 'b_ada8': A['b_ada8'][l], 'w_ada': A['w_ada'][l]}, modT[l])
        pid = nc.sync.partition_id()
        for l in range(2):
            xsrc, xsb = (A['xT'], bufs['xT']) if l == 0 else (A['XO'], bufs['XO'])
            for hh in range(2):
                Am = dict(gmix8=A['gmix8'][l], invf=A['invf'], prot=A['prot'], HT=A['HT'], xT=xsrc,
                          HT_buf=bufs['HT'], ZT_buf=bufs['ZT'], QTd_buf=bufs['QTd'], KTd_buf=bufs['KTd'],
                          QTd=A['QTd'], KTd=A['KTd'], w_a=A['w_a'][l, hh], w_b=A['w_b'][l, hh],
                          qg3=A['qg3'][l], kvg2=A['kvg2'][l], wuq=A['wuq'][l, hh], wuk=A['wuk'][l, hh], wuv=A['wuv'][l, hh],
                          convw=A['convw'][l, hh], pos=A['pos'], rmask=A['rmask'][hh], qdec=A['qdec'][hh],
                          kdec=A['kdec'][hh], sdec=A['sdec'][hh], sel65=A['sel65'],
                          ZTm=A['ZT'][hh * 256:(hh + 1) * 256, :], ZTc=A['ZT'][512 + hh * 256:512 + (hh + 1) * 256, :],
                          ZTr=A['ZT'][1024 + hh * 512:1024 + (hh + 1) * 512, :])
                with k.scope():
                    emit_mixer(k, X, Am, modT[l], V_sb, write_ht=(hh == 0))
            last = (l == 1)
            if last:
                stg = Buf('stage')
                hoff = pid % 2 * SO
                for (dst, src) in (('HTo', 'HT'), ('ZTo', 'ZT'), ('XOo', 'XO')):
                    k.dma('sp', A[dst][:, :], A[src][:, bass.ds(hoff, SO)], reads=[bufs[src]], writes=[bufs[dst]], owner=stg)
            for th in ((None,) if last else (0, 1)):
                if last:
                    tok = lambda q, c0, n: slice(c0, c0 + n)
                    xo = A['out']; xob = bufs['out']
                    srcs = dict(xs_buf=bufs['XOo'], xT=A['XOo'], HT=A['HTo'], ZT=A['ZTo'], HT_buf=bufs['HTo'], ZT_buf=bufs['ZTo'])
                else:
                    tok = (lambda th_: (lambda q, c0, n: slice(th_ * SO + c0, th_ * SO + c0 + n)))(th)
                    xo = A['XO'][:, th * SO:(th + 1) * SO]; xob = bufs['XO']
                    srcs = dict(xs_buf=xsb, xT=xsrc, HT=A['HT'], ZT=A['ZT'], HT_buf=bufs['HT'], ZT_buf=bufs['ZT'])
                Af = dict(gffn8=A['gffn8'][l], X1T_buf=bufs['X1T'], H2K_buf=bufs['H2K'], HS_buf=bufs['HS'], YS_buf=bufs['YS'], xo_buf=xob,
                          rconst=A['rconst'], H2K=A['H2K'], HS=A['HS'], YS=A['YS'],
                          X1T=A['X1T'], xoT=xo, w_g=A['w_g'][l], w_o=A['w_o'][l],
                          w_mix=A['w_mix'][l], w_r=A['w_r'][l], b_r=A['b_r'][l], w_eg=A['w_eg%d' % l], w_eu=A['w_eu%d' % l],
                          w_ed=A['w_ed%d' % l], fin8=A['fin8'], **srcs)
                with k.scope():
                    emit_ffn(k, X, Af, modT[l], last, tok)
        k.barrier()
    return nc


def make_fused_nc():
    nc = bass.Bass("TRN2", target_bir_lowering=False)
    A = {}
    for name, (shape, dt) in FUSED_IN.items():
        A[name] = nc.dram_tensor(name, shape, dt, kind="ExternalInput").ap()
    A['out'] = nc.dram_tensor('out', [D, SO], F32, kind="ExternalOutput").ap()
    for name, shape, dt in (('HT', [D, S], BF16), ('ZT', [2048, S], BF16), ('QTd', [4, 96, S], BF16),
                            ('KTd', [4, 96, S], BF16), ('XO', [D, S], F32), ('X1T', [D, SO], F32),
                            ('H2K', [SO, D], BF16), ('HS', [16384, D], BF16), ('YS', [16384, D], F32), ('HTo', [D, SO], BF16),
                            ('ZTo', [2048, SO], BF16), ('XOo', [D, SO], F32)):
        A[name] = nc.dram_tensor(name, shape, dt, kind="Internal").ap()
    build_fused(nc, A)
    return nc


def fused_inputs(inp, b):
    L = range(2)
    m = {}
    c0 = consts_for(0); c1 = consts_for(1)
    for kk in ('ident', 'prot', 'invf', 'sel65'):
        m[kk] = c0[kk]
    for kk in ('rmask', 'qdec', 'kdec', 'sdec'):
        m[kk] = np.stack([c0[kk], c1[kk]], axis=0)
    mi = [[mixer_inputs(inp, l, b, hh, None) for hh in range(2)] for l in L]
    m['xT'] = np.ascontiguousarray(inp['x'][b].T)
    m['cc8'] = mi[0][0]['cc8']; m['pos'] = mi[0][0]['pos']
    m['w_ada'] = np.ascontiguousarray(inp['w_ada'])
    for kk in ('b_ada8', 'gmix8', 'qg3', 'kvg2'):
        m[kk] = np.stack([mi[l][0][kk] for l in L], axis=0)
    for kk in ('w_a', 'w_b', 'wuq', 'wuk', 'wuv', 'convw'):
        m[kk] = np.stack([np.stack([mi[l][hh][kk] for hh in range(2)], axis=0) for l in L], axis=0)
    m['gffn8'] = np.stack([col8(inp['norm_ffn_g'][l]) for l in L], axis=0)
    m['fin8'] = col8(inp['final_g'])
    m['w_g'] = np.ascontiguousarray(inp['w_in'][:, :, OFF['gl']:OFF['gl'] + 3072])
    m['w_o'] = np.concatenate([inp['w_o_mla'], inp['w_o_conv'], inp['w_o_ret']], axis=1)
    m['w_mix'] = np.ascontiguousarray(inp['w_mix_out'])
    m['w_r'] = np.concatenate([inp['w_route_group'], inp['w_route_expert']], axis=2)
    m['b_r'] = np.concatenate([inp['b_route_group'], inp['b_route_expert']], axis=1)[:, None, :].astype(np.float32)
    for l in L:
        m['w_eg%d' % l] = np.ascontiguousarray(inp['w_exp_gate'][l].reshape(32, 8, 128, 512).transpose(0, 2, 1, 3)).reshape(8192, 2048)
        m['w_eu%d' % l] = np.ascontiguousarray(inp['w_exp_up'][l].reshape(32, 8, 128, 512).transpose(0, 2, 1, 3)).reshape(8192, 2048)
        m['w_ed%d' % l] = np.ascontiguousarray(inp['w_exp_down'][l].reshape(32, 4, 128, 1024).transpose(0, 2, 1, 3)).reshape(8192, 2048)
    rcst = np.zeros((128, 370), np.float32)
    rcst[:, 0:128] = np.triu(np.ones((128, 128), np.float32), 1)
    rcst[:, 128:256] = 1.0
    rcst[:, 256:320] = np.arange(64, dtype=np.float32)[None, :]
    rcst[:, 320:336] = (np.arange(16, dtype=np.float32) * 256.0)[None, :]
    rcst[:, 336:368] = np.arange(32, dtype=np.float32)[None, :]
    rcst[:, 368] = 2.0 * np.arange(128); rcst[:, 369] = 2.0 * np.arange(128) + 1.0
    m['rconst'] = rcst
    return m


_NC_CACHE = {}


def kernel(**inp):
    inp = {k_: np.asarray(v) for k_, v in inp.items()}
    B = inp['x'].shape[0]
    cores = list(range(8))
    if 'fused' not in _NC_CACHE:
        _NC_CACHE['fused'] = make_fused_nc()
    per_b = [fused_inputs(inp, b) for b in range(B)]
    maps = [per_b[c // 2] for c in cores]
    res = run_bass_kernel_spmd(_NC_CACHE['fused'], maps, core_ids=cores).results
    out = np.empty((B, S, D), np.float32)
    for c in cores:
        b, th = c // 2, c % 2
        out[b, th * SO:(th + 1) * SO, :] = np.asarray(res[c]['out']).T
    return out
```

```python
import math
from contextlib import ExitStack, contextmanager
import numpy as np
import ml_dtypes
import concourse.bass as bass
import concourse.mybir as mybir
from concourse.bass_utils import run_bass_kernel_spmd

F32 = mybir.dt.float32; BF16 = mybir.dt.bfloat16; I32 = mybir.dt.int32
ALU = mybir.AluOpType; AF = mybir.ActivationFunctionType

D = 1024; S = 8192; TT = 512; NT = S // TT
EPS = 1e-6
TWO_PI = 2.0 * math.pi
CW1 = 6.28125
CW2 = TWO_PI - CW1
PI_LO = 3.1415925
MLA_SCALE = 96.0 ** -0.5
RET_KS = 128.0 ** -0.5


class Buf:
    def __init__(self, name, ap=None, dram=False):
        self.name = name; self.ap = ap; self.dram = dram
        self.writers = {}; self.readers = {}
        self.ds = {}
        self.psum = False

    def __getitem__(self, idx):
        return self.ap[idx]


class Rot:
    def __init__(self, bufs):
        self.bufs = bufs; self.i = 0

    def next(self):
        b = self.bufs[self.i % len(self.bufs)]; self.i += 1
        return b


class K:
    def __init__(self, nc, root):
        self.nc = nc; self.root = root; self.stacks = [root]
        self.eng = {'pe': nc.tensor, 'dve': nc.vector, 'act': nc.scalar, 'pool': nc.gpsimd, 'sp': nc.sync}
        self.sem = {}; self.cnt = {}
        for e in self.eng:
            self.sem[e] = root.enter_context(nc.semaphore('s_' + e)); self.cnt[e] = 0
        self.waited = {e: {} for e in self.eng}
        self.dma_latest = {}
        self.uid = 0
        self.free_dsems = {'sw': [], 'hw': []}
        self.bound_regs = {}
        self.scope_bufs = [[]]

    def sb(self, name, shape, dt):
        self.uid += 1
        t = self.stacks[-1].enter_context(self.nc.sbuf_tensor('%s_%d' % (name, self.uid), shape, dt))
        b = Buf(name, t)
        self.scope_bufs[-1].append(b)
        return b

    def ps(self, name, shape, dt=F32):
        t = self.root.enter_context(self.nc.psum_tensor(name, shape, dt))
        b = Buf(name, t); b.psum = True
        return b

    def dram(self, name, shape, dt, kind="Internal"):
        t = self.nc.dram_tensor(name, shape, dt, kind=kind).ap()
        return Buf(name, t, dram=True)

    @contextmanager
    def scope(self):
        st = ExitStack()
        self.stacks.append(st)
        self.scope_bufs.append([])
        try:
            yield
        finally:
            self.barrier()
            for b in self.scope_bufs.pop():
                for cls, rec in b.ds.items():
                    self.free_dsems[cls].append(tuple(rec))
                b.ds = {}
            self.stacks.pop()
            st.close()

    def _wait(self, e, sem, val, key):
        if self.waited[e].get(key, 0) >= val:
            return
        self.waited[e][key] = val
        self.eng[e].wait_ge(sem, val)

    def _deps(self, e, reads, writes, partial):
        for b in reads:
            for key, (sem, val) in b.writers.items():
                if e == 'pe' and key == 'pe':
                    continue
                self._wait(e, sem, val, key)
            if b.psum:
                for key, (sem, val) in b.readers.items():
                    if key != e:
                        self._wait(e, sem, val, key)
        for b in writes:
            if not partial:
                for key, (sem, val) in b.writers.items():
                    if e == 'pe' and key == 'pe':
                        continue
                    self._wait(e, sem, val, key)
            for key, (sem, val) in b.readers.items():
                if key == e:
                    continue
                self._wait(e, sem, val, key)

    def _mark(self, sem, val, key, reads, writes, partial):
        for b in reads:
            b.readers[key] = (sem, val)
        for b in writes:
            if partial:
                b.writers[key] = (sem, val)
            else:
                b.writers = {key: (sem, val)}

    def op(self, e, fn, reads=(), writes=(), partial=False):
        self._deps(e, reads, writes, partial)
        ins = fn(self.eng[e])
        self.cnt[e] += 1
        ins.then_inc(self.sem[e], 1)
        self._mark(self.sem[e], self.cnt[e], e, reads, writes, partial)
        return ins

    def _dsem(self, owner, q):
        cls = 'sw' if q == 'pool' else 'hw'
        if cls not in owner.ds:
            if self.free_dsems[cls]:
                owner.ds[cls] = list(self.free_dsems[cls].pop())
            else:
                self.uid += 1
                key = 'dsem%d' % self.uid
                owner.ds[cls] = [self.root.enter_context(self.nc.semaphore(key)), 0, key]
        return owner.ds[cls]

    def dma(self, q, out_ap, in_ap, reads=(), writes=(), owner=None, partial=False, **kw):
        if owner is None:
            owner = [b for b in list(writes) + list(reads) if not b.dram][0]
        rec = self._dsem(owner, q)
        self._deps(q, reads, writes, partial)
        if rec[1] > 0:
            self._wait(q, rec[0], rec[1], rec[2])
        ins = self.eng[q].dma_start(out=out_ap, in_=in_ap, **kw)
        rec[1] += 16
        ins.then_inc(rec[0], 16)
        self._mark(rec[0], rec[1], rec[2], reads, writes, partial)
        self.dma_latest[rec[2]] = (rec[0], rec[1])
        return ins

    def idma(self, out_ap, in_ap, idx_ap, scatter, reads, writes, owner, partial=True, bound=None):
        q = 'pool'
        rec = self._dsem(owner, q)
        self._deps(q, reads, writes, partial)
        if rec[1] > 0:
            self._wait(q, rec[0], rec[1], rec[2])
        off = bass.IndirectOffsetOnAxis(ap=idx_ap, axis=0)
        if scatter:
            ins = self.nc.gpsimd.indirect_dma_start(out=out_ap, out_offset=off, in_=in_ap, in_offset=None)
        else:
            if bound is None:
                ins = self.nc.gpsimd.indirect_dma_start(out=out_ap, out_offset=None, in_=in_ap, in_offset=off)
            else:
                if bound not in self.bound_regs:
                    self.bound_regs[bound] = self.nc.gpsimd.to_reg(bound)
                ins = self.nc.gpsimd.indirect_dma_start(out=out_ap, out_offset=None, in_=in_ap, in_offset=off,
                                                        bounds_check=self.bound_regs[bound], oob_is_err=False)
        rec[1] += 16
        ins.then_inc(rec[0], 16)
        self._mark(rec[0], rec[1], rec[2], reads, writes, partial)
        self.dma_latest[rec[2]] = (rec[0], rec[1])
        return ins

    def barrier(self):
        for e in self.eng:
            for e2 in self.eng:
                if e2 != e and self.cnt[e2] > 0:
                    self._wait(e, self.sem[e2], self.cnt[e2], e2)
            for key, (sem, val) in self.dma_latest.items():
                self._wait(e, sem, val, key)

    def mm(self, out, lhsT, rhs, start, stop, reads, writes):
        return self.op('pe', lambda e: e.matmul(out, lhsT, rhs, start=start, stop=stop),
                       reads=reads, writes=writes, partial=not start)

    def tr(self, out, in_, ident, reads, writes, partial=True):
        return self.op('pe', lambda e: e.transpose(out, in_, ident), reads=reads, writes=writes, partial=partial)

    def act(self, out, in_, func, reads, writes, bias=None, scale=None, accum_out=None, partial=False, eng='act'):
        kw = {}
        if bias is not None: kw['bias'] = bias
        if scale is not None: kw['scale'] = scale
        if accum_out is not None: kw['accum_out'] = accum_out
        return self.op(eng, lambda e: e.activation(out=out, in_=in_, func=func, **kw),
                       reads=reads, writes=writes, partial=partial)

    def tt(self, out, in0, in1, op, reads, writes, partial=False, eng='dve'):
        return self.op(eng, lambda e: e.tensor_tensor(out=out, in0=in0, in1=in1, op=op),
                       reads=reads, writes=writes, partial=partial)

    def ts(self, out, in0, s1, op0, reads, writes, s2=None, op1=None, partial=False, eng='dve'):
        if op1 is None:
            return self.op(eng, lambda e: e.tensor_scalar(out=out, in0=in0, scalar1=s1, scalar2=None, op0=op0),
                           reads=reads, writes=writes, partial=partial)
        return self.op(eng, lambda e: e.tensor_scalar(out=out, in0=in0, scalar1=s1, scalar2=s2, op0=op0, op1=op1),
                       reads=reads, writes=writes, partial=partial)

    def stt(self, out, in0, scalar, in1, op0, op1, reads, writes, partial=False):
        return self.op('dve', lambda e: e.scalar_tensor_tensor(out=out, in0=in0, scalar=scalar, in1=in1, op0=op0, op1=op1),
                       reads=reads, writes=writes, partial=partial)

    def copy(self, out, in_, reads, writes, partial=False, eng='dve'):
        return self.op(eng, lambda e: e.tensor_copy(out=out, in_=in_), reads=reads, writes=writes, partial=partial)

    def recip(self, out, in_, reads, writes, partial=False):
        return self.op('dve', lambda e: e.reciprocal(out=out, in_=in_), reads=reads, writes=writes, partial=partial)

    def memset(self, ap, val, writes, eng='dve', partial=False, reads=()):
        return self.op(eng, lambda e: e.memset(ap, val), reads=reads, writes=writes, partial=partial)


def load_cast(k, dst, dst_ap_fn, src_ap_fn, ncols, step=2048):
    c = 0
    while c < ncols:
        n = min(step, ncols - c)
        k.dma('pool', dst_ap_fn(c, n), src_ap_fn(c, n), writes=[dst], partial=True)
        c += n


class Ctx:
    pass


def setup_common(k, A):
    X = Ctx()
    X.banks = [k.ps('bank%d' % i, [128, 512], F32) for i in range(8)]
    for b in X.banks:
        b.bf = b.ap.bitcast(BF16)
    X.ones_bf = k.sb('ones_bf', [128, 128], BF16)
    k.memset(X.ones_bf[:], 1.0, [X.ones_bf])
    X.id_f = k.sb('id_f', [128, 128], F32)
    k.dma('sp', X.id_f[:], A['ident'][:, :], writes=[X.id_f])
    X.id_bf = k.sb('id_bf', [128, 128], BF16)
    k.copy(X.id_bf[:], X.id_f[:], [X.id_f], [X.id_bf])
    return X


def rope_tables(k, W, pos_ap, invcol, cosb, sinb):
    k.dma('sp', W.posi[:], pos_ap.to_broadcast([128, TT]), writes=[W.posi])
    k.copy(W.ang[:], W.posi[:], [W.posi], [W.ang])
    k.ts(W.ang[:], W.ang[:], invcol, ALU.mult, [W.ang], [W.ang])
    k.ts(W.kq[:], W.ang[:], 1.0 / TWO_PI, ALU.mult, [W.ang], [W.kq])
    k.copy(W.kf[:], W.kq[:], [W.kq], [W.kf])
    k.stt(W.r1[:], W.kf[:], -CW1, W.ang[:], ALU.mult, ALU.add, [W.kf, W.ang], [W.r1])
    k.stt(W.r1[:], W.kf[:], -CW2, W.r1[:], ALU.mult, ALU.add, [W.kf, W.r1], [W.r1])
    k.ts(W.r1[:], W.r1[:], PI_LO, ALU.min, [W.r1], [W.r1], s2=-PI_LO, op1=ALU.max)
    k.act(sinb[:], W.r1[:], AF.Sin, [W.r1], [sinb])
    k.stt(W.kf[:], W.r1[:], -1.0, W.r1[:], ALU.mult, ALU.max, [W.r1], [W.kf])
    k.act(cosb[:], W.kf[:], AF.Sin, [W.kf], [cosb], bias=W.halfpi[:, 0:1], scale=-1.0)


def rope_work(k):
    W = Ctx()
    W.posi = k.sb('posi', [128, TT], I32)
    W.ang = k.sb('ang', [128, TT], F32)
    W.kq = k.sb('kq', [128, TT], I32)
    W.kf = k.sb('kf', [128, TT], F32)
    W.r1 = k.sb('r1', [128, TT], F32)
    W.halfpi = k.sb('halfpi', [128, 1], F32)
    k.memset(W.halfpi[:], math.pi / 2.0, [W.halfpi])
    return W


def emit_mod(k, X, A, modT):
    with k.scope():
        cc = k.sb('cc', [128, 8], F32)
        k.dma('sp', cc[:], A['cc8'][:, :], writes=[cc])
        cact = k.sb('cact', [128, 8], F32)
        k.act(cact[:], cc[:], AF.Silu, [cc], [cact])
        bada = k.sb('bada', [128, 48], F32)
        k.dma('sp', bada[:], A['b_ada8'][:, :], writes=[bada])
        wv = A['w_ada'].rearrange("(kc p) n -> p kc n", p=128)
        wb = [k.sb('wada%d' % i, [128, 8, 768], F32) for i in range(2)]
        pm = X.banks[0]
        for blk in range(8):
            w = wb[blk % 2]
            for kc in range(8):
                k.dma('sp', w[:, kc, :], wv[:, kc, blk * 768:(blk + 1) * 768], writes=[w], partial=(kc > 0))
            for j in range(6):
                jj = blk * 6 + j
                for kc in range(8):
                    k.mm(pm[:, jj:jj + 1], w[:, kc, j * 128:(j + 1) * 128], cact[:, kc:kc + 1],
                         kc == 0, kc == 7, [w, cact], [pm])
        k.tt(modT[:], pm[:, 0:48], bada[:], ALU.add, [pm, bada], [modT])


def emit_mixer(k, X, A, modT, V_sb, write_ht, phases=('A', 'B', 'S2')):
    if True:
        banks = X.banks
        rot = Rot(banks)
        gmix = k.sb('gmix', [128, 8], F32)
        k.dma('sp', gmix[:], A['gmix8'], writes=[gmix])
        gsc = k.sb('gsc', [128, 8], F32)
        k.stt(gsc[:], modT[:, 8:16], 1.0, gmix[:], ALU.add, ALU.mult, [modT, gmix], [gsc])
        invf = k.sb('invf', [128, 2], F32)
        k.dma('sp', invf[:], A['invf'], writes=[invf])
        prot_f = k.sb('prot_f', [128, 2, 128], F32)
        k.dma('sp', prot_f[:], A['prot'], writes=[prot_f])
        prot = k.sb('prot', [128, 2, 128], BF16)
        k.copy(prot[:], prot_f[:], [prot_f], [prot])
        HT = A['HT_buf']; ZT = A['ZT_buf']; QTd = A['QTd_buf']; KTd = A['KTd_buf']
        HTv = A['HT'].rearrange("(kc p) c -> p kc c", p=128)
        xTv = A['xT'].rearrange("(kc p) c -> p kc c", p=128)

        if 'A' in phases:
          with k.scope():
            NA = 1504
            w_a = k.sb('w_a', [128, 8, NA], BF16)
            wav = A['w_a'].rearrange("(kc p) n -> p kc n", p=128)
            for kc in range(8):
                load_cast(k, w_a, lambda c, n: w_a[:, kc, c:c + n], lambda c, n: wav[:, kc, c:c + n], NA, step=752)
            st_f = k.sb('st_f', [128, 3, 384], F32)
            qg = k.sb('qg', [128, 3], F32); kvg = k.sb('kvg', [128, 2], F32)
            k.dma('sp', qg[:], A['qg3'], writes=[qg]); k.dma('sp', kvg[:], A['kvg2'], writes=[kvg])
            wuq = k.sb('wuq', [128, 3, 384], BF16)
            k.dma('sp', st_f[:], A['wuq'].rearrange("(rc p) n -> p rc n", p=128), writes=[st_f])
            for rc in range(3):
                k.ts(wuq[:, rc, :], st_f[:, rc, :], qg[:, rc:rc + 1], ALU.mult, [st_f, qg], [wuq], partial=True)
            wuk = k.sb('wuk', [128, 2, 256], BF16); wuv = k.sb('wuv', [128, 2, 256], BF16)
            st2 = k.sb('st2', [128, 2, 256], F32); st3 = k.sb('st3', [128, 2, 256], F32)
            k.dma('sp', st2[:], A['wuk'].rearrange("(rc p) n -> p rc n", p=128), writes=[st2])
            k.dma('sp', st3[:], A['wuv'].rearrange("(rc p) n -> p rc n", p=128), writes=[st3])
            for rc in range(2):
                k.ts(wuk[:, rc, :], st2[:, rc, :], kvg[:, rc:rc + 1], ALU.mult, [st2, kvg], [wuk], partial=True)
                k.ts(wuv[:, rc, :], st3[:, rc, :], kvg[:, rc:rc + 1], ALU.mult, [st3, kvg], [wuv], partial=True)
            cw = k.sb('cw', [128, 2, 3], F32)
            k.dma('sp', cw[:], A['convw'], writes=[cw])
            xts = [k.sb('xt%d' % i, [128, 8, TT], F32) for i in range(2)]
            sq = k.sb('sq', [128, 8, TT], BF16)
            hTa = [k.sb('hT%d' % i, [128, 8, TT], BF16) for i in range(2)]
            rtmp = k.sb('rtmp', [128, TT], F32)
            rstd = k.sb('rstd', [128, TT], F32)
            hx = [k.sb('hx%d' % i, [128, TT], F32) for i in range(2)]
            lat_f = k.sb('lat_f', [128, 5, TT], F32)
            sql = k.sb('sql', [128, 5, TT], BF16)
            rq_bc = k.sb('rq_bc', [128, TT], F32); rkv_bc = k.sb('rkv_bc', [128, TT], F32)
            qn = k.sb('qn', [128, 3, TT], BF16); kvn = k.sb('kvn', [128, 2, TT], BF16)
            QTs = [k.sb('QTs%d' % i, [128, TT], BF16) for i in range(4)]
            KTs = [k.sb('KTs%d' % i, [128, TT], BF16) for i in range(4)]
            qr_f = k.sb('qr_f', [128, TT], F32)
            kr_f = k.sb('kr_f', [128, TT], F32); kr_b = k.sb('kr_b', [128, TT], BF16)
            kpe = k.sb('kpe', [128, TT], BF16)
            t1 = k.sb('t1', [128, TT], F32); t2 = k.sb('t2', [128, TT], F32)
            cosm = k.sb('cosm', [128, TT], F32); sinm = k.sb('sinm', [128, TT], F32)
            W = rope_work(k)
            u = [k.sb('u%d' % i, [128, TT + 2], F32) for i in range(2)]
            cxs = k.sb('cxs', [128, TT], F32); yv = k.sb('yv', [128, TT], F32)
            zc = [k.sb('zc%d' % i, [128, TT], BF16) for i in range(2)]
            for ch in range(2):
                k.memset(u[ch][:], 0.0, [u[ch]])

            def load_x(t):
                xt = xts[t % 2]
                for kc in range(8):
                    k.dma('sp', xt[:, kc, :], xTv[:, kc, t * TT:(t + 1) * TT], writes=[xt], partial=(kc > 0))

            load_x(0)
            for t in range(NT):
                cs = slice(t * TT, (t + 1) * TT)
                if t + 1 < NT:
                    load_x(t + 1)
                xt = xts[t % 2]
                hT = hTa[t % 2]
                for kc in range(8):
                    k.act(sq[:, kc, :], xt[:, kc, :], AF.Square, [xt], [sq], partial=(kc > 0))
                pss = rot.next()
                for kc in range(8):
                    k.mm(pss[:, :], X.ones_bf[:, :], sq[:, kc, :], kc == 0, kc == 7, [X.ones_bf, sq], [pss])
                k.act(rtmp[:], pss[:, :], AF.Sqrt, [pss], [rtmp], bias=EPS, scale=1.0 / D)
                k.recip(rstd[:], rtmp[:], [rtmp], [rstd])
                for kc in range(8):
                    hb = hx[kc % 2]
                    k.stt(hb[:], xt[:, kc, :], gsc[:, kc:kc + 1], rstd[:], ALU.mult, ALU.mult, [xt, gsc, rstd], [hb])
                    k.act(hT[:, kc, :], hb[:], AF.Identity, [hb, modT], [hT], bias=modT[:, kc:kc + 1], partial=(kc > 0))
                if write_ht:
                    k.dma('pool', HTv[:, :, cs], hT[:], reads=[hT], writes=[HT], partial=True)
                rope_tables(k, W, A['pos'][0:1, cs], invf[:, 1:2], cosm, sinm)
                for j in range(5):
                    ps = rot.next()
                    for kc in range(8):
                        k.mm(ps[:, :], w_a[:, kc, j * 128:(j + 1) * 128], hT[:, kc, :], kc == 0, kc == 7, [w_a, hT], [ps])
                    k.act(lat_f[:, j, :], ps[:, :], AF.Copy, [ps], [lat_f], partial=(j > 0))
                    k.act(sql[:, j, :], ps[:, :], AF.Square, [ps], [sql], partial=(j > 0))
                for ch in range(2):
                    pb = rot.next(); pc = rot.next(); px = rot.next()
                    for (pp, base) in ((pb, 736), (pc, 992), (px, 1248)):
                        for kc in range(8):
                            k.mm(pp[:, :], w_a[:, kc, base + ch * 128: base + (ch + 1) * 128], hT[:, kc, :],
                                 kc == 0, kc == 7, [w_a, hT], [pp])
                    k.act(cxs[:], px[:, :], AF.Copy, [px], [cxs])
                    U = u[ch]
                    if t > 0:
                        k.copy(U[:, 0:2], U[:, TT:TT + 2], [U], [U])
                    k.tt(U[:, 2:TT + 2], pc[:, :], cxs[:], ALU.mult, [pc, cxs, U], [U])
                    k.ts(yv[:], U[:, 2:TT + 2], cw[:, ch, 2:3], ALU.mult, [U, cw], [yv])
                    k.stt(yv[:], U[:, 1:TT + 1], cw[:, ch, 1:2], yv[:], ALU.mult, ALU.add, [U, cw, yv], [yv])
                    k.stt(yv[:], U[:, 0:TT], cw[:, ch, 0:1], yv[:], ALU.mult, ALU.add, [U, cw, yv], [yv])
                    Z = zc[ch]
                    k.tt(Z[:], pb[:, :], yv[:], ALU.mult, [pb, yv], [Z])
                    k.dma('pool', A['ZTc'][ch * 128:(ch + 1) * 128, cs], Z[:], reads=[Z], writes=[ZT], partial=True)
                for (j0, j1, n, dst) in ((0, 3, 384.0, rq_bc), (3, 5, 256.0, rkv_bc)):
                    ps = rot.next()
                    for j in range(j0, j1):
                        k.mm(ps[:, :], X.ones_bf[:, :], sql[:, j, :], j == j0, j == j1 - 1, [X.ones_bf, sql], [ps])
                    k.act(rtmp[:], ps[:, :], AF.Sqrt, [ps], [rtmp], bias=EPS, scale=1.0 / n)
                    k.recip(dst[:], rtmp[:], [rtmp], [dst])
                for j in range(3):
                    k.tt(qn[:, j, :], lat_f[:, j, :], rq_bc[:], ALU.mult, [lat_f, rq_bc], [qn], partial=(j > 0))
                for j in range(2):
                    k.tt(kvn[:, j, :], lat_f[:, 3 + j, :], rkv_bc[:], ALU.mult, [lat_f, rkv_bc], [kvn], partial=(j > 0))
                ps = rot.next()
                for kc in range(8):
                    k.mm(ps[0:96, :], w_a[:, kc, 640:736], hT[:, kc, :], kc == 0, kc == 7, [w_a, hT], [ps])
                k.act(kr_f[64:96, :], ps[64:96, :], AF.Copy, [ps], [kr_f])
                k.act(kr_b[0:96, :], ps[0:96, :], AF.Copy, [ps], [kr_b])
                ps2 = rot.next()
                k.mm(ps2[0:96, :], prot[0:96, 1, 0:96], kr_b[0:96, :], True, True, [prot, kr_b], [ps2])
                k.tt(t1[64:96, :], kr_f[64:96, :], cosm[64:96, :], ALU.mult, [kr_f, cosm], [t1])
                k.tt(t2[64:96, :], ps2[64:96, :], sinm[64:96, :], ALU.mult, [ps2, sinm], [t2])
                k.tt(kpe[64:96, :], t1[64:96, :], t2[64:96, :], ALU.add, [t1, t2], [kpe])
                for h in range(4):
                    k.dma('pool', A['KTd'][h, 64:96, cs], kpe[64:96, :], reads=[kpe], writes=[KTd], partial=True)
                for h in range(4):
                    ps = rot.next()
                    for rc in range(3):
                        k.mm(ps[0:96, :], wuq[:, rc, h * 96:(h + 1) * 96], qn[:, rc, :], rc == 0, rc == 2, [wuq, qn], [ps])
                    Q = QTs[h]
                    k.act(Q[0:96, :], ps[0:96, :], AF.Copy, [ps], [Q], scale=MLA_SCALE)
                    k.act(qr_f[64:96, :], ps[64:96, :], AF.Copy, [ps], [qr_f], scale=MLA_SCALE)
                    ps2 = rot.next()
                    k.mm(ps2[0:96, :], prot[0:96, 1, 0:96], Q[0:96, :], True, True, [prot, Q], [ps2])
                    k.tt(t1[64:96, :], qr_f[64:96, :], cosm[64:96, :], ALU.mult, [qr_f, cosm], [t1])
                    k.tt(t2[64:96, :], ps2[64:96, :], sinm[64:96, :], ALU.mult, [ps2, sinm], [t2])
                    k.tt(Q[64:96, :], t1[64:96, :], t2[64:96, :], ALU.add, [t1, t2], [Q])
                    k.dma('pool', A['QTd'][h, :, cs], Q[0:96, :], reads=[Q], writes=[QTd], partial=True)
                for h in range(4):
                    ps = rot.next()
                    for rc in range(2):
                        k.mm(ps[0:64, :], wuk[:, rc, h * 64:(h + 1) * 64], kvn[:, rc, :], rc == 0, rc == 1, [wuk, kvn], [ps])
                    Kt = KTs[h]
                    k.act(Kt[0:64, :], ps[0:64, :], AF.Copy, [ps], [Kt])
                    k.dma('pool', A['KTd'][h, 0:64, cs], Kt[0:64, :], reads=[Kt], writes=[KTd], partial=True)
                for blk in range(4):
                    ps = rot.next()
                    for rc in range(2):
                        k.mm(ps[:, 0:256], kvn[:, rc, blk * 128:(blk + 1) * 128], wuv[:, rc, :], rc == 0, rc == 1, [kvn, wuv], [ps])
                    k.act(V_sb[:, t * 4 + blk, :, 0:64], ps[:, 0:256].rearrange("p (h v) -> p h v", h=4), AF.Copy,
                          [ps], [V_sb], partial=True)

        if 'B' in phases:
          with k.scope():
            NB = 1536
            w_b = k.sb('w_b', [128, 8, NB], BF16)
            wbv = A['w_b'].rearrange("(kc p) n -> p kc n", p=128)
            for kc in range(8):
                load_cast(k, w_b, lambda c, n: w_b[:, kc, c:c + n], lambda c, n: wbv[:, kc, c:c + n], NB, step=768)
            hTs = [k.sb('hTb%d' % i, [128, 8, TT], BF16) for i in range(2)]
            rmask = k.sb('rmask', [128, 2, 128], F32)
            k.dma('sp', rmask[:], A['rmask'], writes=[rmask])
            qdec = k.sb('qdec', [128, 2, TT], F32)
            k.dma('sp', qdec[:], A['qdec'], writes=[qdec])
            kdec = k.sb('kdec', [128, 2], F32)
            k.dma('sp', kdec[:], A['kdec'], writes=[kdec])
            sdec = k.sb('sdec', [128, 2], F32)
            k.dma('sp', sdec[:], A['sdec'], writes=[sdec])
            cosr = k.sb('cosr', [128, TT], F32); sinr = k.sb('sinr', [128, TT], F32)
            W = rope_work(k)
            qfs = [k.sb('qf%d' % i, [128, TT], F32) for i in range(4)]; qbs = [k.sb('qb%d' % i, [128, TT], BF16) for i in range(4)]
            t1s = [k.sb('t1b%d' % i, [128, TT], F32) for i in range(4)]; t2s = [k.sb('t2b%d' % i, [128, TT], F32) for i in range(4)]
            RQT = [k.sb('RQT%d' % i, [128, TT], BF16) for i in range(2)]
            RQd = [k.sb('RQd%d' % i, [128, TT], BF16) for i in range(2)]
            RKT = [k.sb('RKT%d' % i, [128, TT], BF16) for i in range(2)]
            RKd = [k.sb('RKd%d' % i, [128, 4, 128], BF16) for i in range(2)]
            RVb = [k.sb('RV%d' % i, [128, 512], BF16) for i in range(4)]; Gb = [k.sb('G%d' % i, [128, 512], BF16) for i in range(4)]
            AT = [k.sb('AT%d' % i, [128, 128], BF16) for i in range(2)]
            st_f = [k.sb('stf%d' % i, [128, 256], F32) for i in range(2)]
            st_b = [k.sb('stb%d' % i, [128, 256], BF16) for i in range(2)]
            junk = k.sb('junk', [128, 256], BF16)
            ssq = k.sb('ssq', [128, 1], F32); sd = k.sb('sd', [128, 1], F32); rinv = k.sb('rinv', [128, 1], F32)
            zr = [k.sb('zr%d' % i, [128, 256], BF16) for i in range(2)]
            zT = [k.sb('zT%d' % i, [128, 2, TT], BF16) for i in range(2)]
            for hr in range(2):
                k.memset(st_f[hr][:], 0.0, [st_f[hr]]); k.memset(st_b[hr][:], 0.0, [st_b[hr]])

            def load_h(t):
                hb = hTs[t % 2]
                k.dma('sp', hb[:], HTv[:, :, t * TT:(t + 1) * TT], reads=[HT], writes=[hb])

            load_h(0)
            for t in range(NT):
                cs = slice(t * TT, (t + 1) * TT)
                if t + 1 < NT:
                    load_h(t + 1)
                hT = hTs[t % 2]
                rope_tables(k, W, A['pos'][0:1, cs], invf[:, 0:1], cosr, sinr)
                pss_ = []
                for c in range(4):
                    hr, isk = c // 2, c % 2
                    ps = rot.next()
                    for kc in range(8):
                        k.mm(ps[:, :], w_b[:, kc, isk * 256 + hr * 128:isk * 256 + (hr + 1) * 128], hT[:, kc, :], kc == 0, kc == 7, [w_b, hT], [ps])
                    pss_.append(ps)
                for c in range(4):
                    sc_ = RET_KS if c % 2 else 1.0
                    k.act(qfs[c][:], pss_[c][:, :], AF.Copy, [pss_[c]], [qfs[c]], scale=sc_)
                    k.act(qbs[c][:], pss_[c][:, :], AF.Copy, [pss_[c]], [qbs[c]], scale=sc_)
                ps2s = []
                for c in range(4):
                    ps2 = rot.next()
                    k.mm(ps2[:, :], prot[:, 0, :], qbs[c][:], True, True, [prot, qbs[c]], [ps2])
                    ps2s.append(ps2)
                for c in range(4):
                    hr, isk = c // 2, c % 2
                    k.tt(t1s[c][:], qfs[c][:], cosr[:], ALU.mult, [qfs[c], cosr], [t1s[c]])
                    k.tt(t2s[c][:], ps2s[c][:, :], sinr[:], ALU.mult, [ps2s[c], sinr], [t2s[c]])
                    if isk:
                        k.tt(RKT[hr][:], t1s[c][:], t2s[c][:], ALU.add, [t1s[c], t2s[c]], [RKT[hr]], eng='pool')
                    else:
                        k.tt(t1s[c][:], t1s[c][:], t2s[c][:], ALU.add, [t1s[c], t2s[c]], [t1s[c]], eng='pool')
                        k.act(RQT[hr][:], t1s[c][:], AF.Copy, [t1s[c]], [RQT[hr]])
                        k.tt(RQd[hr][:], t1s[c][:], qdec[:, hr, :], ALU.mult, [t1s[c], qdec], [RQd[hr]])
                for hr in range(2):
                    pT = rot.next()
                    for blk in range(4):
                        k.tr(pT.bf[:, blk * 128:(blk + 1) * 128], RKT[hr][:, blk * 128:(blk + 1) * 128], X.id_bf[:, :],
                             [RKT[hr], X.id_bf], [pT], partial=(blk > 0))
                    for blk in range(4):
                        k.act(RKd[hr][:, blk, :], pT.bf[:, blk * 128:(blk + 1) * 128], AF.Copy, [pT, kdec], [RKd[hr]],
                              scale=kdec[:, hr:hr + 1], partial=(blk > 0))
                def proj_vg(blk):
                    ps = rot.next()
                    for kc in range(8):
                        k.mm(ps[:, :], hT[:, kc, blk * 128:(blk + 1) * 128], w_b[:, kc, 512:1024], kc == 0, kc == 7, [w_b, hT], [ps])
                    k.act(RVb[blk][:], ps[:, :], AF.Copy, [ps], [RVb[blk]])
                    ps = rot.next()
                    for kc in range(8):
                        k.mm(ps[:, :], hT[:, kc, blk * 128:(blk + 1) * 128], w_b[:, kc, 1024:1536], kc == 0, kc == 7, [w_b, hT], [ps])
                    k.act(Gb[blk][:], ps[:, :], AF.Silu, [ps], [Gb[blk]])

                proj_vg(0)
                for blk in range(4):
                    if blk + 1 < 4:
                        proj_vg(blk + 1)
                    RV = RVb[blk]; G = Gb[blk]
                    bs = slice(blk * 128, (blk + 1) * 128)
                    for hr in range(2):
                        vs = slice(hr * 256, (hr + 1) * 256)
                        pS = rot.next()
                        k.mm(pS[:, 0:128], RKT[hr][:, bs], RQT[hr][:, bs], True, True, [RKT[hr], RQT[hr]], [pS])
                        a = AT[hr]
                        k.tt(a[:], pS[:, 0:128], rmask[:, hr, :], ALU.mult, [pS, rmask], [a])
                        pO = rot.next()
                        k.mm(pO[:, 0:256], a[:], RV[:, vs], True, False, [a, RV], [pO])
                        k.mm(pO[:, 0:256], RQd[hr][:, bs], st_b[hr][:], False, True, [RQd[hr], st_b[hr]], [pO])
                        pN = rot.next()
                        k.mm(pN[:, 0:256], RKd[hr][:, blk, :], RV[:, vs], True, True, [RKd[hr], RV], [pN])
                        k.stt(st_f[hr][:], st_f[hr][:], sdec[:, hr:hr + 1], pN[:, 0:256], ALU.mult, ALU.add,
                              [st_f[hr], sdec, pN], [st_f[hr]])
                        k.act(st_b[hr][:], st_f[hr][:], AF.Copy, [st_f[hr]], [st_b[hr]])
                        k.act(junk[:], pO[:, 0:256], AF.Square, [pO], [junk, ssq], accum_out=ssq[:, 0:1])
                        k.act(sd[:], ssq[:], AF.Sqrt, [ssq], [sd], bias=EPS, scale=1.0 / 256.0)
                        k.recip(rinv[:], sd[:], [sd], [rinv])
                        z = zr[hr]
                        k.stt(z[:], pO[:, 0:256], rinv[:, 0:1], G[:, vs], ALU.mult, ALU.mult, [pO, rinv, G], [z])
                        pT = rot.next()
                        for vc in range(2):
                            k.tr(pT.bf[:, vc * 128:(vc + 1) * 128], z[:, vc * 128:(vc + 1) * 128], X.id_bf[:, :],
                                 [z, X.id_bf], [pT], partial=(vc > 0))
                        k.act(zT[hr][:, :, bs], pT.bf[:, 0:256].rearrange("p (v i) -> p v i", v=2), AF.Copy, [pT], [zT[hr]],
                              partial=True)
                for hr in range(2):
                    k.dma('pool', A['ZTr'][hr * 256:(hr + 1) * 256, cs].rearrange("(v p) c -> p v c", p=128),
                          zT[hr][:], reads=[zT[hr]], writes=[ZT], partial=True)

        if 'S2' in phases:
          with k.scope():
            kts = [k.sb('kt%d' % i, [128, S], BF16) for i in range(2)]
            qts = [k.sb('qt%d' % i, [128, TT], BF16) for i in range(3)]
            PTs = [k.sb('PT%d' % i, [128, TT], BF16) for i in range(3)]
            of = [k.sb('of%d' % i, [128, TT], F32) for i in range(2)]
            rb = k.sb('rb', [128, TT], F32)
            zm = [k.sb('zm%d' % i, [128, TT], BF16) for i in range(2)]
            sel = k.sb('sel', [128, 64], F32)
            for o_ in of:
                k.memset(o_[:], 0.0, [o_])
            k.dma('sp', sel[:], A['sel65'], writes=[sel])
            srot = Rot(banks[0:4]); orot = Rot(banks[4:6]); brot = Rot(banks[6:8])
            qi = 0
            for h in range(4):
                kt = kts[h % 2]
                for c4 in range(4):
                    k.dma('sp', kt[0:96, c4 * 2048:(c4 + 1) * 2048], A['KTd'][h, :, c4 * 2048:(c4 + 1) * 2048],
                          reads=[KTd], writes=[kt], partial=(c4 > 0))
                for qt in range(NT):
                    q = qts[qi % 3]; qi += 1
                    k.dma('sp', q[0:96, :], A['QTd'][h, :, qt * TT:(qt + 1) * TT], reads=[QTd], writes=[q])
                    nkb = 4 * qt + 4
                    O = orot.next()
                    pend = []

                    def emit_s(kb):
                        d = kb - 4 * qt
                        qlo = 0 if d < 0 else 128 * d
                        pS = srot.next()
                        k.mm(pS[:, qlo:TT], kt[0:96, kb * 128:(kb + 1) * 128], q[0:96, qlo:TT], True, True, [kt, q], [pS])
                        P = PTs[kb % 3]
                        k.act(P[:, qlo:TT], pS[:, qlo:TT], AF.Exp, [pS], [P])
                        if d >= 0:
                            k.memset(P[64:128, qlo:qlo + 64], 0.0, [P], eng='pool', partial=True, reads=[P])
                        return P, d, qlo

                    def emit_pv(kb, P, d, qlo):
                        k.mm(O[0:65, qlo:TT], V_sb[:, kb, h, :], P[:, qlo:TT], kb == 0, kb == nkb - 1, [V_sb, P], [O])

                    for kb in range(nkb):
                        pend.append((kb,) + emit_s(kb))
                        if len(pend) > 2:
                            a = pend.pop(0); emit_pv(*a)
                    while pend:
                        a = pend.pop(0); emit_pv(*a)
                    o = of[qt % 2]
                    k.act(o[0:65, :], O[0:65, :], AF.Copy, [O], [o], partial=True)
                    pB = brot.next()
                    k.mm(pB[0:64, :], sel[:, 0:64], o[:, :], True, True, [sel, o], [pB])
                    k.recip(rb[0:64, :], pB[0:64, :], [pB], [rb])
                    z = zm[qt % 2]
                    k.tt(z[0:64, :], o[0:64, :], rb[0:64, :], ALU.mult, [o, rb], [z])
                    k.dma('pool', A['ZTm'][h * 64:(h + 1) * 64, qt * TT:(qt + 1) * TT], z[0:64, :], reads=[z], writes=[ZT], partial=True)


OFF = {'q_lat': 0, 'kv_lat': 384, 'k_rope': 640, 'cb': 672, 'cc': 1184, 'cx': 1696, 'rq': 2208, 'rk': 2720,
       'rv': 3232, 'rg': 4256, 'gl': 5280}


def col8(v):
    return np.ascontiguousarray(v.reshape(-1, 128).T)


def consts_for(hh):
    C = {}
    C['ident'] = np.eye(128, dtype=np.float32)
    prot = np.zeros((128, 2, 128), np.float32)
    for r in range(64):
        prot[r + 64, 0, r] = -1.0
        prot[r, 0, r + 64] = 1.0
    for r in range(64, 80):
        prot[r + 16, 1, r] = -1.0
        prot[r, 1, r + 16] = 1.0
    C['prot'] = prot
    invf = np.zeros((128, 2), np.float32)
    inv_ret = (10000.0 ** (-np.arange(0, 128, 2, dtype=np.float32) / 128)).astype(np.float32)
    inv_mla = (10000.0 ** (-np.arange(0, 32, 2, dtype=np.float32) / 32)).astype(np.float32)
    for p in range(128):
        invf[p, 0] = inv_ret[p % 64]
    for p in range(64, 96):
        invf[p, 1] = inv_mla[(p - 64) % 16]
    C['invf'] = invf
    rmask = np.zeros((128, 2, 128), np.float64); qdec = np.zeros((128, 2, TT), np.float64)
    kdec = np.zeros((128, 2), np.float64); sdec = np.zeros((128, 2), np.float64)
    for hr in range(2):
        H = hh * 2 + hr
        g = 1.0 - 2.0 ** (-5.0 - H)
        for j in range(128):
            for i in range(128):
                cj, ci = j // 64, i // 64
                if cj == ci:
                    rmask[j, hr, i] = g ** abs(i - j)
                elif cj < ci:
                    rmask[j, hr, i] = g ** (i - j)
        qdec[:, hr, :] = (g ** ((np.arange(TT) % 128) + 1.0))[None, :]
        kdec[:, hr] = g ** (127.0 - np.arange(128))
        sdec[:, hr] = g ** 128.0
    C['rmask'] = rmask.astype(np.float32); C['qdec'] = qdec.astype(np.float32)
    C['kdec'] = kdec.astype(np.float32); C['sdec'] = sdec.astype(np.float32)
    sel = np.zeros((128, 64), np.float32); sel[64, :] = 1.0
    C['sel65'] = sel
    return C


def mixer_inputs(inp, l, b, hh, xT_b):
    w_in = inp['w_in'][l]
    m = dict(consts_for(hh))
    if xT_b is not None:
        m['xT'] = xT_b
    m['cc8'] = col8(inp['c'][b])
    m['b_ada8'] = col8(inp['b_ada'][l])
    m['gmix8'] = col8(inp['norm_mix_g'][l])
    wa = np.zeros((D, 1504), np.float32)
    wa[:, 0:640] = w_in[:, 0:640]
    wa[:, 704:736] = w_in[:, 640:672]
    for i, nm in enumerate(('cb', 'cc', 'cx')):
        wa[:, 736 + i * 256:736 + (i + 1) * 256] = w_in[:, OFF[nm] + hh * 256:OFF[nm] + (hh + 1) * 256]
    m['w_a'] = wa
    wb = np.empty((D, 1536), np.float32)
    wb[:, 0:256] = w_in[:, OFF['rq'] + hh * 256:OFF['rq'] + (hh + 1) * 256]
    wb[:, 256:512] = w_in[:, OFF['rk'] + hh * 256:OFF['rk'] + (hh + 1) * 256]
    wb[:, 512:1024] = w_in[:, OFF['rv'] + hh * 512:OFF['rv'] + (hh + 1) * 512]
    wb[:, 1024:1536] = w_in[:, OFF['rg'] + hh * 512:OFF['rg'] + (hh + 1) * 512]
    m['w_b'] = wb
    m['pos'] = np.ascontiguousarray(inp['positions'][b:b + 1].astype(np.int32))
    m['qg3'] = col8(inp['mla_q_norm_g'][l]); m['kvg2'] = col8(inp['mla_kv_norm_g'][l])
    m['wuq'] = np.ascontiguousarray(inp['w_uq'][l][:, hh * 384:(hh + 1) * 384])
    wukv = inp['w_ukv'][l].reshape(256, 8, 128)[:, hh * 4:(hh + 1) * 4, :]
    m['wuk'] = np.ascontiguousarray(wukv[:, :, 0:64].reshape(256, 256))
    m['wuv'] = np.ascontiguousarray(wukv[:, :, 64:128].reshape(256, 256))
    cwv = inp['conv_w'][l][:, hh * 256:(hh + 1) * 256]
    m['convw'] = np.ascontiguousarray(cwv.reshape(3, 2, 128).transpose(2, 1, 0))
    return m


SO = 4096; NTO = SO // TT
BIG = 1.0e30


def emit_ffn(k, X, A, modT, last, tok):
    if True:
        banks = X.banks
        rot = Rot(banks[0:7])
        pL = banks[7]
        gffn = k.sb('gffn', [128, 8], F32)
        k.dma('sp', gffn[:], A['gffn8'], writes=[gffn])
        gsc2 = k.sb('gsc2', [128, 8], F32)
        k.stt(gsc2[:], modT[:, 32:40], 1.0, gffn[:], ALU.add, ALU.mult, [modT, gffn], [gsc2])
        X1T = A['X1T_buf']; H2K = A['H2K_buf']; HSb = A['HS_buf']; YSb = A['YS_buf']; XO = A['xo_buf']
        NBLK = SO // 128; TS = 256; NTILE = 64
        rc = k.sb('rconst', [128, 128 + 128 + 64 + 16 + 32 + 2], F32)
        k.dma('sp', rc[:], A['rconst'], writes=[rc])
        tri = rc[:, 0:128]; ones_f = rc[:, 128:256]; iota64 = rc[:, 256:320]; thr16 = rc[:, 320:336]
        iota32 = rc[:, 336:368]; pbase2 = rc[:, 368:370]
        e1s = k.sb('e1s', [128, NBLK], F32); e2s = k.sb('e2s', [128, NBLK], F32)
        r1s = k.sb('r1s', [128, NBLK], F32); r2s = k.sb('r2s', [128, NBLK], F32)
        w1s = k.sb('w1s', [128, NBLK], F32); w2s = k.sb('w2s', [128, NBLK], F32)
        run_bc = k.sb('run_bc', [128, 32], F32)
        k.memset(run_bc[:], 0.0, [run_bc])
        pos1i = k.sb('pos1i', [128, NBLK], I32); pos2i = k.sb('pos2i', [128, NBLK], I32)
        widx = k.sb('widx', [128, NTILE, 2], I32)
        HTb = A['HT_buf']; ZTb = A['ZT_buf']; XSb = A['xs_buf']
        xTv = A['xT'].rearrange("(kc p) c -> p kc c", p=128)
        HTv = A['HT'].rearrange("(kc p) c -> p kc c", p=128)
        ZTv = A['ZT'].rearrange("(kc p) c -> p kc c", p=128)
        X1v = A['X1T'].rearrange("(kc p) c -> p kc c", p=128)
        XOv = A['xoT'].rearrange("(kc p) c -> p kc c", p=128)

        with k.scope():
            w_g = k.sb('w_g', [128, 8, 3072], BF16)
            wgv = A['w_g'].rearrange("(kc p) n -> p kc n", p=128)
            for kc in range(8):
                load_cast(k, w_g, lambda c, n: w_g[:, kc, c:c + n], lambda c, n: wgv[:, kc, c:c + n], 3072, step=1024)
            w_o = k.sb('w_o', [128, 16, 1024], BF16)
            wov = A['w_o'].rearrange("(kc p) n -> p kc n", p=128)
            for kc in range(16):
                k.dma('pool', w_o[:, kc, :], wov[:, kc, :], writes=[w_o], partial=True)
            w_m = k.sb('w_m', [128, 8, 1024], BF16)
            wmv = A['w_mix'].rearrange("(kc p) n -> p kc n", p=128)
            for kc in range(8):
                k.dma('pool', w_m[:, kc, :], wmv[:, kc, :], writes=[w_m], partial=True)
            w_r = k.sb('w_r', [128, 8, 36], F32)
            k.dma('sp', w_r[:], A['w_r'].rearrange("(kc p) n -> p kc n", p=128), writes=[w_r])
            b_r = k.sb('b_r', [128, 36], F32)
            k.dma('sp', b_r[:], A['b_r'].to_broadcast([128, 36]), writes=[b_r])
            hT = k.sb('hTc', [128, 8, TT], BF16)
            Zt = k.sb('Zt', [128, 16, TT], BF16)
            xt = k.sb('xtc', [128, 8, TT], F32)
            gt0 = k.sb('gt0', [128, 3, TT], BF16); gt = [gt0, gt0]
            tA = k.sb('tA', [128, TT], F32); tB = k.sb('tB', [128, TT], F32)
            mg = k.sb('mg', [128, 8, TT], BF16)
            rtmp = k.sb('rtmpc', [128, TT], F32); rstd = k.sb('rstdc', [128, TT], F32)
            h2f0 = k.sb('h2f0', [128, TT], F32); h2f = [h2f0, h2f0]
            h2b = k.sb('h2b', [128, 8, TT], BF16)
            lt = tA
            lg = k.sb('lg', [128, 36], F32); gmax = k.sb('gmax', [128, 1], F32); ngmax = k.sb('ngmax', [128, 1], F32)
            g1h = k.sb('g1h', [128, 4], F32); pen = k.sb('pen', [128, 4], F32)
            ej = k.sb('ej', [128, 4], F32); gsum = k.sb('gsum', [128, 1], F32); gval = k.sb('gval', [128, 1], F32)
            lem = k.sb('lem', [128, 32], F32); top8 = k.sb('top8', [128, 8], F32)
            m1 = k.sb('m1', [128, 32], F32); m2 = k.sb('m2', [128, 32], F32)
            dd = k.sb('dd', [128, 1], F32); ee = k.sb('ee', [128, 1], F32); den = k.sb('den', [128, 1], F32)
            oh = k.sb('oh', [128, 32], F32); rk = k.sb('rk', [128, 32], F32); jk = k.sb('jk', [128, 32], F32)
            cT = tB
            for t in range(NTO):
                cs = slice(t * TT, (t + 1) * TT)
                tsl = tok('sp', t * TT, TT)
                k.dma('sp', hT[:], HTv[:, :, tsl], reads=[HTb], writes=[hT])
                k.dma('sp', Zt[:], ZTv[:, :, tsl], reads=[ZTb], writes=[Zt])
                for kc in range(8):
                    k.dma('sp', xt[:, kc, :], xTv[:, kc, tsl], reads=[XSb], writes=[xt], partial=(kc > 0))
                for dc in range(8):
                    ds_ = slice(dc * 128, (dc + 1) * 128)
                    g = gt[dc % 2]
                    for br in range(3):
                        ps = rot.next()
                        for kc in range(8):
                            k.mm(ps[:, :], w_g[:, kc, br * 1024 + dc * 128: br * 1024 + (dc + 1) * 128], hT[:, kc, :],
                                 kc == 0, kc == 7, [w_g, hT], [ps])
                        k.act(g[:, br, :], ps[:, :], AF.Sigmoid, [ps], [g], partial=(br > 0))
                    pys = []
                    for (k0, k1) in ((0, 4), (4, 8), (8, 16)):
                        ps = rot.next()
                        for kc in range(k0, k1):
                            k.mm(ps[:, :], w_o[:, kc, ds_], Zt[:, kc, :], kc == k0, kc == k1 - 1, [w_o, Zt], [ps])
                        pys.append(ps)
                    k.tt(tA[:], pys[0][:, :], g[:, 0, :], ALU.mult, [pys[0], g], [tA])
                    k.tt(tB[:], pys[1][:, :], g[:, 1, :], ALU.mult, [pys[1], g], [tB])
                    k.tt(tA[:], tA[:], tB[:], ALU.add, [tA, tB], [tA], eng='pool')
                    k.tt(tB[:], pys[2][:, :], g[:, 2, :], ALU.mult, [pys[2], g], [tB])
                    k.tt(mg[:, dc, :], tA[:], tB[:], ALU.add, [tA, tB], [mg], eng='pool', partial=(dc > 0))
                for dc in range(8):
                    ps = rot.next()
                    for kc in range(8):
                        k.mm(ps[:, :], w_m[:, kc, dc * 128:(dc + 1) * 128], mg[:, kc, :], kc == 0, kc == 7, [w_m, mg], [ps])
                    k.stt(xt[:, dc, :], ps[:, :], modT[:, 16 + dc:17 + dc], xt[:, dc, :], ALU.mult, ALU.add,
                          [ps, modT, xt], [xt], partial=True)
                k.dma('pool', X1v[:, :, cs], xt[:], reads=[xt], writes=[X1T], partial=True)
                for kc in range(8):
                    k.act(mg[:, kc, :], xt[:, kc, :], AF.Square, [xt], [mg], partial=(kc > 0))
                pss = rot.next()
                for kc in range(8):
                    k.mm(pss[:, :], X.ones_bf[:, :], mg[:, kc, :], kc == 0, kc == 7, [X.ones_bf, mg], [pss])
                k.act(rtmp[:], pss[:, :], AF.Sqrt, [pss], [rtmp], bias=EPS, scale=1.0 / D)
                k.recip(rstd[:], rtmp[:], [rtmp], [rstd])
                for kc in range(8):
                    hf = h2f[kc % 2]
                    k.stt(hf[:], xt[:, kc, :], gsc2[:, kc:kc + 1], rstd[:], ALU.mult, ALU.mult, [xt, gsc2, rstd], [hf])
                    k.act(hf[:], hf[:], AF.Identity, [hf, modT], [hf], bias=modT[:, 24 + kc:25 + kc])
                    k.copy(h2b[:, kc, :], hf[:], [hf], [h2b], eng='pool', partial=(kc > 0))
                    k.mm(pL[0:36, :], w_r[:, kc, :], hf[:], kc == 0, kc == 7, [w_r, hf], [pL])
                for blk in range(4):
                    pH = rot.next()
                    for kc in range(8):
                        k.tr(pH.bf[:, kc * 128:(kc + 1) * 128], h2b[:, kc, blk * 128:(blk + 1) * 128], X.id_bf[:, :],
                             [h2b, X.id_bf], [pH], partial=(kc > 0))
                    hk = gt0
                    k.act(hk[:, 0:2, :], pH.bf[:, :].rearrange("p (a b) -> p a b", a=2), AF.Copy, [pH], [hk])
                    k.dma('pool', A['H2K'][(t * 4 + blk) * 128:(t * 4 + blk + 1) * 128, :].rearrange("p (a b) -> p a b", a=2),
                          hk[:, 0:2, :], reads=[hk], writes=[H2K], partial=True)
                k.act(lt[0:36, :], pL[0:36, :], AF.Copy, [pL], [lt])
                pT = rot.next()
                for blk in range(4):
                    k.tr(pT[:, blk * 36:(blk + 1) * 36], lt[0:36, blk * 128:(blk + 1) * 128], X.id_f[0:36, 0:36],
                         [lt, X.id_f], [pT], partial=(blk > 0))
                for blk in range(4):
                    gb = t * 4 + blk
                    w1 = w1s[:, gb:gb + 1]; w2 = w2s[:, gb:gb + 1]
                    k.tt(lg[:], pT[:, blk * 36:(blk + 1) * 36], b_r[:], ALU.add, [pT, b_r], [lg])
                    k.op('dve', lambda e: e.reduce_max(out=gmax[:], in_=lg[:, 0:4], axis=mybir.AxisListType.X),
                         reads=[lg], writes=[gmax])
                    k.ts(g1h[:], lg[:, 0:4], gmax[:, 0:1], ALU.is_equal, [lg, gmax], [g1h])
                    k.ts(ngmax[:], gmax[:], -1.0, ALU.mult, [gmax], [ngmax])
                    k.act(ej[:], lg[:, 0:4], AF.Exp, [lg, ngmax], [ej, gsum], bias=ngmax[:, 0:1], accum_out=gsum[:, 0:1])
                    k.recip(gval[:], gsum[:], [gsum], [gval])
                    k.ts(pen[:], g1h[:], -1.0, ALU.add, [g1h], [pen], s2=BIG, op1=ALU.mult)
                    for g_ in range(4):
                        k.ts(lem[:, g_ * 8:(g_ + 1) * 8], lg[:, 4 + g_ * 8:4 + (g_ + 1) * 8], pen[:, g_:g_ + 1], ALU.add,
                             [lg, pen], [lem], partial=(g_ > 0))
                    k.op('dve', lambda e: e.max(out=top8[:], in_=lem[:]), reads=[lem], writes=[top8])
                    k.ts(m1[:], lem[:], top8[:, 0:1], ALU.is_equal, [lem, top8], [m1])
                    k.ts(m2[:], lem[:], top8[:, 1:2], ALU.is_equal, [lem, top8], [m2])
                    k.tt(dd[:], top8[:, 1:2], top8[:, 0:1], ALU.subtract, [top8], [dd])
                    k.act(ee[:], dd[:], AF.Exp, [dd], [ee])
                    k.ts(den[:], ee[:], 1.0, ALU.add, [ee], [den])
                    k.recip(den[:], den[:], [den], [den])
                    k.tt(w1, den[:], gval[:], ALU.mult, [den, gval], [w1s], partial=True)
                    k.tt(w2, w1, ee[:], ALU.mult, [w1s, ee], [w2s], partial=True)
                    k.tt(oh[:], m1[:], m2[:], ALU.add, [m1, m2], [oh])
                    pP = rot.next()
                    k.mm(pP[:, 0:32], tri, oh[:], True, True, [rc, oh], [pP])
                    k.tt(rk[:], pP[:, 0:32], run_bc[:], ALU.add, [pP, run_bc], [rk])
                    k.op('dve', lambda e: e.scalar_tensor_tensor(out=jk[:], in0=rk[:], scalar=1.0, in1=m1[:], op0=ALU.mult, op1=ALU.mult,
                                                              accum_out=r1s[:, gb:gb + 1]), reads=[rk, m1], writes=[jk, r1s], partial=True)
                    k.op('dve', lambda e: e.scalar_tensor_tensor(out=jk[:], in0=rk[:], scalar=1.0, in1=m2[:], op0=ALU.mult, op1=ALU.mult,
                                                              accum_out=r2s[:, gb:gb + 1]), reads=[rk, m2], writes=[jk, r2s], partial=True)
                    k.op('dve', lambda e: e.scalar_tensor_tensor(out=jk[:], in0=iota32, scalar=1.0, in1=m1[:], op0=ALU.mult, op1=ALU.mult,
                                                              accum_out=e1s[:, gb:gb + 1]), reads=[rc, m1], writes=[jk, e1s], partial=True)
                    k.op('dve', lambda e: e.scalar_tensor_tensor(out=jk[:], in0=iota32, scalar=1.0, in1=m2[:], op0=ALU.mult, op1=ALU.mult,
                                                              accum_out=e2s[:, gb:gb + 1]), reads=[rc, m2], writes=[jk, e2s], partial=True)
                    pQ = rot.next()
                    k.mm(pQ[:, 0:32], ones_f, oh[:], True, True, [rc, oh], [pQ])
                    k.tt(run_bc[:], run_bc[:], pQ[:, 0:32], ALU.add, [run_bc, pQ], [run_bc])
            ptile = k.sb('ptile', [128, 32], F32); cmp16 = k.sb('cmp16', [128, 16], F32)
            start = k.sb('start', [128, 32], F32); endt = k.sb('endt', [128, 32], F32); sstart = k.sb('sstart', [128, 32], F32)
            et = k.sb('et', [128, NTILE], F32); wf = k.sb('wf', [128, NTILE, 2], F32)
            p1f = k.sb('p1f', [128, NBLK], F32); p2f = k.sb('p2f', [128, NBLK], F32)
            for e_ in range(32):
                k.op('dve', lambda e: e.tensor_scalar(out=cmp16[:], in0=thr16, scalar1=run_bc[:, e_:e_ + 1], scalar2=None,
                                                      op0=ALU.is_lt, op1=ALU.add, accum_out=ptile[:, e_:e_ + 1]),
                     reads=[rc, run_bc], writes=[cmp16, ptile], partial=True)
            k.memset(start[:, 0:1], 0.0, [start], partial=True)
            for e_ in range(1, 32):
                k.tt(start[:, e_:e_ + 1], start[:, e_ - 1:e_], ptile[:, e_ - 1:e_], ALU.add, [start, ptile], [start], partial=True)
            k.tt(endt[:], start[:], ptile[:], ALU.add, [start, ptile], [endt])
            k.ts(sstart[:], start[:], float(TS), ALU.mult, [start], [sstart])
            k.memset(et[:], 0.0, [et])
            for e_ in range(32):
                k.stt(et[:], iota64, endt[:, e_:e_ + 1], et[:], ALU.is_ge, ALU.add, [rc, endt, et], [et])
            k.ts(et[:], et[:], 31.0, ALU.min, [et], [et])
            same = k.sb('same', [128, NTILE], F32)
            k.memset(same[:, 0:1], 0.0, [same], partial=True)
            k.tt(same[:, 1:NTILE], et[:, 1:NTILE], et[:, 0:NTILE - 1], ALU.is_equal, [et], [same], partial=True)
            for j in range(2):
                k.ts(wf[:, :, j], et[:], 256.0, ALU.mult, [et, rc], [wf], s2=pbase2[:, j:j + 1], op1=ALU.add, partial=True)
                k.stt(wf[:, :, j], same[:], 1.0e7, wf[:, :, j], ALU.mult, ALU.add, [same, wf], [wf], partial=True)
            k.copy(widx[:], wf[:], [wf], [widx])
            for gb in range(NBLK):
                k.ts(m1[:], iota32, e1s[:, gb:gb + 1], ALU.is_equal, [rc, e1s], [m1])
                k.op('dve', lambda e: e.scalar_tensor_tensor(out=jk[:], in0=m1[:], scalar=1.0, in1=sstart[:], op0=ALU.mult, op1=ALU.mult,
                                                          accum_out=p1f[:, gb:gb + 1]), reads=[m1, sstart], writes=[jk, p1f], partial=True)
                k.ts(m2[:], iota32, e2s[:, gb:gb + 1], ALU.is_equal, [rc, e2s], [m2])
                k.op('dve', lambda e: e.scalar_tensor_tensor(out=jk[:], in0=m2[:], scalar=1.0, in1=sstart[:], op0=ALU.mult, op1=ALU.mult,
                                                          accum_out=p2f[:, gb:gb + 1]), reads=[m2, sstart], writes=[jk, p2f], partial=True)
            k.tt(p1f[:], p1f[:], r1s[:], ALU.add, [p1f, r1s], [p1f])
            k.tt(p2f[:], p2f[:], r2s[:], ALU.add, [p2f, r2s], [p2f])
            k.copy(pos1i[:], p1f[:], [p1f], [pos1i]); k.copy(pos2i[:], p2f[:], [p2f], [pos2i])

        if last:
            fin = k.sb('fin', [128, 8], F32)
            k.dma('sp', fin[:], A['fin8'], writes=[fin])
        with k.scope():
            hr_ = [k.sb('hr%d' % i, [128, 1024], BF16) for i in range(2)]
            for gb in range(NBLK):
                hb_ = hr_[gb % 2]
                k.dma('sp', hb_[:], A['H2K'][gb * 128:(gb + 1) * 128, :], reads=[H2K], writes=[hb_])
                k.idma(A['HS'][:, :], hb_[:, :], pos1i[:, gb:gb + 1], True, [hb_, pos1i], [HSb], hb_)
                k.idma(A['HS'][:, :], hb_[:, :], pos2i[:, gb:gb + 1], True, [hb_, pos2i], [HSb], hb_)
        with k.scope():
            wg0 = k.sb('wg0', [128, 4096], BF16); wu0 = k.sb('wu0', [128, 4096], BF16); wd0 = k.sb('wd0', [128, 4096], BF16)
            wg = [wg0, wg0]; wu = [wu0, wu0]; wd = [wd0, wd0]
            hst = [k.sb('hst%d' % i, [128, 2, 1024], BF16) for i in range(2)]
            hTs = [k.sb('hTs%d' % i, [128, 8, TS], BF16) for i in range(2)]
            sg = [k.sb('sg%d' % i, [128, TS], F32) for i in range(2)]
            hid = [k.sb('hid%d' % i, [128, 4, TS], BF16) for i in range(2)]
            ys = [k.sb('ys%d' % i, [128, 2, 1024], F32) for i in range(2)]
            HSv = A['HS'].rearrange("(i sb p) n -> i p sb n", sb=2, p=128)
            YSv = A['YS'].rearrange("(i sb p) n -> i p sb n", sb=2, p=128)

            def load_w(i, which):
                for (wt, src) in which:
                    for j in range(2):
                        k.idma(wt[:, j * 2048:(j + 1) * 2048], src[:, :], widx[:, i, j:j + 1], False, [widx], [wt], wt, partial=(j > 0), bound=8191)

            GU = ((wg0, A['w_eg']), (wu0, A['w_eu'])); DN = ((wd0, A['w_ed']),)
            load_w(0, GU); load_w(0, DN)
            for i in range(NTILE):
                bi = i % 2
                hs_ = hst[bi]; hT_ = hTs[bi]; hd = hid[bi]; y_ = ys[bi]
                if i == 0:
                    k.dma('sp', hs_[:], HSv[0], reads=[HSb], writes=[hs_])
                if i + 1 < NTILE:
                    k.dma('sp', hst[(i + 1) % 2][:], HSv[i + 1], reads=[HSb], writes=[hst[(i + 1) % 2]])
                for sb in range(2):
                    pH = rot.next()
                    for kc in range(8):
                        k.tr(pH.bf[:, kc * 128:(kc + 1) * 128], hs_[:, sb, kc * 128:(kc + 1) * 128], X.id_bf[:, :],
                             [hs_, X.id_bf], [pH], partial=(kc > 0))
                    k.act(hT_[:, :, sb * 128:(sb + 1) * 128], pH.bf[:, :].rearrange("p (kc s) -> p kc s", kc=8), AF.Copy,
                          [pH], [hT_], partial=(sb > 0))
                for fc in range(4):
                    pg = rot.next(); pu = rot.next()
                    for kc in range(8):
                        k.mm(pg[:, 0:TS], wg[bi][:, kc * 512 + fc * 128: kc * 512 + (fc + 1) * 128], hT_[:, kc, :], kc == 0, kc == 7, [wg[bi], hT_], [pg])
                    for kc in range(8):
                        k.mm(pu[:, 0:TS], wu[bi][:, kc * 512 + fc * 128: kc * 512 + (fc + 1) * 128], hT_[:, kc, :], kc == 0, kc == 7, [wu[bi], hT_], [pu])
                    s_ = sg[fc % 2]
                    k.act(s_[:], pg[:, 0:TS], AF.Silu, [pg], [s_])
                    k.tt(hd[:, fc, :], pu[:, 0:TS], s_[:], ALU.mult, [pu, s_], [hd], partial=(fc > 0))
                if i + 1 < NTILE:
                    load_w(i + 1, GU)
                for sb in range(2):
                    for dh in range(2):
                        pd = rot.next()
                        for fc in range(4):
                            k.mm(pd[:, :], hd[:, fc, sb * 128:(sb + 1) * 128], wd[bi][:, fc * 1024 + dh * 512: fc * 1024 + (dh + 1) * 512],
                                 fc == 0, fc == 3, [hd, wd[bi]], [pd])
                        if (sb + dh) % 2 == 0:
                            k.act(y_[:, sb, dh * 512:(dh + 1) * 512], pd[:, :], AF.Copy, [pd], [y_], partial=True)
                        else:
                            k.copy(y_[:, sb, dh * 512:(dh + 1) * 512], pd[:, :], [pd], [y_], partial=True)
                if i + 1 < NTILE:
                    load_w(i + 1, DN)
                k.dma('act', YSv[i], y_[:], reads=[y_], writes=[YSb], partial=True)
        with k.scope():
            y1 = [k.sb('y1_%d' % i, [128, 1024], F32) for i in range(2)]
            y2 = [k.sb('y2_%d' % i, [128, 1024], F32) for i in range(2)]
            mo = k.sb('mo', [128, 4, 1024], F32)
            x1 = [k.sb('x1_%d' % i, [128, 8, TT], F32) for i in range(2)]
            sqf = k.sb('sqf', [128, 8, TT], BF16)
            rt2 = k.sb('rt2', [128, TT], F32); rs2 = k.sb('rs2', [128, TT], F32)
            for t in range(NTO):
                gs = slice(t * TT, (t + 1) * TT)
                xb = x1[t % 2]
                k.dma('sp', xb[:], X1v[:, :, gs], reads=[X1T], writes=[xb])
                for blk in range(4):
                    gb = t * 4 + blk
                    a1 = y1[blk % 2]; a2 = y2[blk % 2]
                    k.idma(a1[:, :], A['YS'][:, :], pos1i[:, gb:gb + 1], False, [YSb, pos1i], [a1], a1, partial=False)
                    k.idma(a2[:, :], A['YS'][:, :], pos2i[:, gb:gb + 1], False, [YSb, pos2i], [a2], a2, partial=False)
                    k.ts(mo[:, blk, :], a1[:], w1s[:, gb:gb + 1], ALU.mult, [a1, w1s], [mo], partial=(blk > 0))
                    k.stt(mo[:, blk, :], a2[:], w2s[:, gb:gb + 1], mo[:, blk, :], ALU.mult, ALU.add, [a2, w2s, mo], [mo], partial=True)
                for dc in range(8):
                    pM = rot.next()
                    for blk in range(4):
                        k.tr(pM[:, blk * 128:(blk + 1) * 128], mo[:, blk, dc * 128:(dc + 1) * 128], X.id_f[:, :],
                             [mo, X.id_f], [pM], partial=(blk > 0))
                    k.stt(xb[:, dc, :], pM[:, :], modT[:, 40 + dc:41 + dc], xb[:, dc, :], ALU.mult, ALU.add,
                          [pM, modT, xb], [xb], partial=True)
                if last:
                    for kc in range(8):
                        k.act(sqf[:, kc, :], xb[:, kc, :], AF.Square, [xb], [sqf], partial=(kc > 0))
                    pss = rot.next()
                    for kc in range(8):
                        k.mm(pss[:, :], X.ones_bf[:, :], sqf[:, kc, :], kc == 0, kc == 7, [X.ones_bf, sqf], [pss])
                    k.act(rt2[:], pss[:, :], AF.Sqrt, [pss], [rt2], bias=EPS, scale=1.0 / D)
                    k.recip(rs2[:], rt2[:], [rt2], [rs2])
                    for kc in range(8):
                        k.stt(xb[:, kc, :], xb[:, kc, :], fin[:, kc:kc + 1], rs2[:], ALU.mult, ALU.mult,
                              [xb, fin, rs2], [xb], partial=True)
                k.dma('pool', XOv[:, :, gs], xb[:], reads=[xb], writes=[XO], partial=True)


FUSED_IN = {
    'xT': ([D, S], F32), 'cc8': ([128, 8], F32), 'w_ada': ([2, D, 6 * D], F32), 'b_ada8': ([2, 128, 48], F32),
    'gmix8': ([2, 128, 8], F32), 'gffn8': ([2, 128, 8], F32), 'fin8': ([128, 8], F32),
    'w_a': ([2, 2, D, 1504], F32), 'w_b': ([2, 2, D, 1536], F32), 'pos': ([1, S], I32),
    'qg3': ([2, 128, 3], F32), 'kvg2': ([2, 128, 2], F32), 'wuq': ([2, 2, 384, 384], F32),
    'wuk': ([2, 2, 256, 256], F32), 'wuv': ([2, 2, 256, 256], F32), 'convw': ([2, 2, 128, 2, 3], F32),
    'ident': ([128, 128], F32), 'prot': ([128, 2, 128], F32), 'invf': ([128, 2], F32),
    'rmask': ([2, 128, 2, 128], F32), 'qdec': ([2, 128, 2, TT], F32), 'kdec': ([2, 128, 2], F32),
    'sdec': ([2, 128, 2], F32), 'sel65': ([128, 64], F32),
    'w_g': ([2, D, 3072], F32), 'w_o': ([2, 2048, D], F32), 'w_mix': ([2, D, D], F32), 'w_r': ([2, D, 36], F32),
    'b_r': ([2, 1, 36], F32), 'w_eg0': ([8192, 2048], F32), 'w_eu0': ([8192, 2048], F32), 'w_ed0': ([8192, 2048], F32),
    'w_eg1': ([8192, 2048], F32), 'w_eu1': ([8192, 2048], F32), 'w_ed1': ([8192, 2048], F32), 'rconst': ([128, 370], F32),
}


def build_fused(nc, A):
    root = ExitStack()
    with root:
        k = K(nc, root)
        X = setup_common(k, A)
        V_sb = k.sb('V_sb', [128, 64, 4, 65], BF16)
        k.memset(V_sb[:, :, :, 64:65], 1.0, [V_sb], eng='pool')
        bufs = {n: Buf(n, A[n], dram=True) for n in ('HT', 'ZT', 'QTd', 'KTd', 'XO', 'X1T', 'H2K', 'HS', 'YS', 'xT', 'out', 'HTo', 'ZTo', 'XOo')}
        modT = [k.sb('modT%d' % l, [128, 48], F32) for l in range(2)]
        for l in range(2):
            emit_mod(k, X, {'cc8': A['cc8'], 'b_ada8': A['b_ada8'][l], 'w_ada': A['w_ada'][l]}, modT[l])
        pid = nc.sync.partition_id()
        for l in range(2):
            xsrc, xsb = (A['xT'], bufs['xT']) if l == 0 else (A['XO'], bufs['XO'])
            for hh in range(2):
                Am = dict(gmix8=A['gmix8'][l], invf=A['invf'], prot=A['prot'], HT=A['HT'], xT=xsrc,
                          HT_buf=bufs['HT'], ZT_buf=bufs['ZT'], QTd_buf=bufs['QTd'], KTd_buf=bufs['KTd'],
                          QTd=A['QTd'], KTd=A['KTd'], w_a=A['w_a'][l, hh], w_b=A['w_b'][l, hh],
                          qg3=A['qg3'][l], kvg2=A['kvg2'][l], wuq=A['wuq'][l, hh], wuk=A['wuk'][l, hh], wuv=A['wuv'][l, hh],
                          convw=A['convw'][l, hh], pos=A['pos'], rmask=A['rmask'][hh], qdec=A['qdec'][hh],
                          kdec=A['kdec'][hh], sdec=A['sdec'][hh], sel65=A['sel65'],
                          ZTm=A['ZT'][hh * 256:(hh + 1) * 256, :], ZTc=A['ZT'][512 + hh * 256:512 + (hh + 1) * 256, :],
                          ZTr=A['ZT'][1024 + hh * 512:1024 + (hh + 1) * 512, :])
                with k.scope():
                    emit_mixer(k, X, Am, modT[l], V_sb, write_ht=(hh == 0))
            last = (l == 1)
            if last:
                stg = Buf('stage')
                hoff = pid % 2 * SO
                for (dst, src) in (('HTo', 'HT'), ('ZTo', 'ZT'), ('XOo', 'XO')):
                    k.dma('sp', A[dst][:, :], A[src][:, bass.ds(hoff, SO)], reads=[bufs[src]], writes=[bufs[dst]], owner=stg)
            for th in ((None,) if last else (0, 1)):
                if last:
                    tok = lambda q, c0, n: slice(c0, c0 + n)
                    xo = A['out']; xob = bufs['out']
                    srcs = dict(xs_buf=bufs['XOo'], xT=A['XOo'], HT=A['HTo'], ZT=A['ZTo'], HT_buf=bufs['HTo'], ZT_buf=bufs['ZTo'])
                else:
                    tok = (lambda th_: (lambda q, c0, n: slice(th_ * SO + c0, th_ * SO + c0 + n)))(th)
                    xo = A['XO'][:, th * SO:(th + 1) * SO]; xob = bufs['XO']
                    srcs = dict(xs_buf=xsb, xT=xsrc, HT=A['HT'], ZT=A['ZT'], HT_buf=bufs['HT'], ZT_buf=bufs['ZT'])
                Af = dict(gffn8=A['gffn8'][l], X1T_buf=bufs['X1T'], H2K_buf=bufs['H2K'], HS_buf=bufs['HS'], YS_buf=bufs['YS'], xo_buf=xob,
                          rconst=A['rconst'], H2K=A['H2K'], HS=A['HS'], YS=A['YS'],
                          X1T=A['X1T'], xoT=xo, w_g=A['w_g'][l], w_o=A['w_o'][l],
                          w_mix=A['w_mix'][l], w_r=A['w_r'][l], b_r=A['b_r'][l], w_eg=A['w_eg%d' % l], w_eu=A['w_eu%d' % l],
                          w_ed=A['w_ed%d' % l], fin8=A['fin8'], **srcs)
                with k.scope():
                    emit_ffn(k, X, Af, modT[l], last, tok)
        k.barrier()
    return nc


def make_fused_nc():
    nc = bass.Bass("TRN2", target_bir_lowering=False)
    A = {}
    for name, (shape, dt) in FUSED_IN.items():
        A[name] = nc.dram_tensor(name, shape, dt, kind="ExternalInput").ap()
    A['out'] = nc.dram_tensor('out', [D, SO], F32, kind="ExternalOutput").ap()
    for name, shape, dt in (('HT', [D, S], BF16), ('ZT', [2048, S], BF16), ('QTd', [4, 96, S], BF16),
                            ('KTd', [4, 96, S], BF16), ('XO', [D, S], F32), ('X1T', [D, SO], F32),
                            ('H2K', [SO, D], BF16), ('HS', [16384, D], BF16), ('YS', [16384, D], F32), ('HTo', [D, SO], BF16),
                            ('ZTo', [2048, SO], BF16), ('XOo', [D, SO], F32)):
        A[name] = nc.dram_tensor(name, shape, dt, kind="Internal").ap()
    build_fused(nc, A)
    return nc


def fused_inputs(inp, b):
    L = range(2)
    m = {}
    c0 = consts_for(0); c1 = consts_for(1)
    for kk in ('ident', 'prot', 'invf', 'sel65'):
        m[kk] = c0[kk]
    for kk in ('rmask', 'qdec', 'kdec', 'sdec'):
        m[kk] = np.stack([c0[kk], c1[kk]], axis=0)
    mi = [[mixer_inputs(inp, l, b, hh, None) for hh in range(2)] for l in L]
    m['xT'] = np.ascontiguousarray(inp['x'][b].T)
    m['cc8'] = mi[0][0]['cc8']; m['pos'] = mi[0][0]['pos']
    m['w_ada'] = np.ascontiguousarray(inp['w_ada'])
    for kk in ('b_ada8', 'gmix8', 'qg3', 'kvg2'):
        m[kk] = np.stack([mi[l][0][kk] for l in L], axis=0)
    for kk in ('w_a', 'w_b', 'wuq', 'wuk', 'wuv', 'convw'):
        m[kk] = np.stack([np.stack([mi[l][hh][kk] for hh in range(2)], axis=0) for l in L], axis=0)
    m['gffn8'] = np.stack([col8(inp['norm_ffn_g'][l]) for l in L], axis=0)
    m['fin8'] = col8(inp['final_g'])
    m['w_g'] = np.ascontiguousarray(inp['w_in'][:, :, OFF['gl']:OFF['gl'] + 3072])
    m['w_o'] = np.concatenate([inp['w_o_mla'], inp['w_o_conv'], inp['w_o_ret']], axis=1)
    m['w_mix'] = np.ascontiguousarray(inp['w_mix_out'])
    m['w_r'] = np.concatenate([inp['w_route_group'], inp['w_route_expert']], axis=2)
    m['b_r'] = np.concatenate([inp['b_route_group'], inp['b_route_expert']], axis=1)[:, None, :].astype(np.float32)
    for l in L:
        m['w_eg%d' % l] = np.ascontiguousarray(inp['w_exp_gate'][l].reshape(32, 8, 128, 512).transpose(0, 2, 1, 3)).reshape(8192, 2048)
        m['w_eu%d' % l] = np.ascontiguousarray(inp['w_exp_up'][l].reshape(32, 8, 128, 512).transpose(0, 2, 1, 3)).reshape(8192, 2048)
        m['w_ed%d' % l] = np.ascontiguousarray(inp['w_exp_down'][l].reshape(32, 4, 128, 1024).transpose(0, 2, 1, 3)).reshape(8192, 2048)
    rcst = np.zeros((128, 370), np.float32)
    rcst[:, 0:128] = np.triu(np.ones((128, 128), np.float32), 1)
    rcst[:, 128:256] = 1.0
    rcst[:, 256:320] = np.arange(64, dtype=np.float32)[None, :]
    rcst[:, 320:336] = (np.arange(16, dtype=np.float32) * 256.0)[None, :]
    rcst[:, 336:368] = np.arange(32, dtype=np.float32)[None, :]
    rcst[:, 368] = 2.0 * np.arange(128); rcst[:, 369] = 2.0 * np.arange(128) + 1.0
    m['rconst'] = rcst
    return m


_NC_CACHE = {}


def kernel(**inp):
    inp = {k_: np.asarray(v) for k_, v in inp.items()}
    B = inp['x'].shape[0]
    cores = list(range(8))
    if 'fused' not in _NC_CACHE:
        _NC_CACHE['fused'] = make_fused_nc()
    per_b = [fused_inputs(inp, b) for b in range(B)]
    maps = [per_b[c // 2] for c in cores]
    res = run_bass_kernel_spmd(_NC_CACHE['fused'], maps, core_ids=cores).results
    out = np.empty((B, S, D), np.float32)
    for c in cores:
        b, th = c // 2, c % 2
        out[b, th * SO:(th + 1) * SO, :] = np.asarray(res[c]['out']).T
    return out
```
